# Optimizing a Trainium2 kernel written in Bass

```python
import math
import jax, jax.numpy as jnp
from jax import lax
import numpy as np

D_MODEL = 2048
BATCH = 16
SEQ = 2048
DEPTH = 2

F32 = jnp.float32

GROUP_WIDTH = D_MODEL // 4
MLA_HEADS = 4
MLA_NOPE = 128
MLA_ROPE = 64
MLA_V = GROUP_WIDTH // MLA_HEADS
MLA_Q_LORA = 384
MLA_KV_LORA = 256
ATTN_BLOCK = 128
RET_HEADS = 4
RET_DV = GROUP_WIDTH // RET_HEADS
RET_DK = RET_DV // 2
RET_CHUNK = 128
RET_DECAY_EXP_FWD = 5.0
RET_DECAY_EXP_BWD = 5.5
SSD_HEADDIM = 64
SSD_HEADS = GROUP_WIDTH // SSD_HEADDIM
SSD_GROUPS = 2
SSD_STATE = 128
SSD_CONV = 5
SSD_CHUNK = 128
SSD_CONV_CH = GROUP_WIDTH + 2 * SSD_GROUPS * SSD_STATE
HY_ORDER = 2
HY_WIDTH = GROUP_WIDTH
HY_SHORT = 3
HY_EMB = 33
HY_FILTER_HIDDEN = 64
HY_TARGET = 1e-2
HY_FAST_PCT = 0.3
HY_SLOW_PCT = 1.5
HY_MIN_DECAY = math.log(HY_TARGET) / HY_SLOW_PCT
HY_MAX_DECAY = math.log(HY_TARGET) / HY_FAST_PCT
D_FF = 5632
N_EXPERTS = 8
TOP_K = 2
D_FF_EXPERT = 7168
MOE_BLOCK = 256
ROPE_BASE = 10000.0
ALPHA = (2 * DEPTH) ** 0.25
BETA = (8 * DEPTH) ** -0.25
N_DENSE = (DEPTH + 1) // 2
N_MOE = DEPTH // 2

IN_SPLITS = (MLA_Q_LORA, MLA_KV_LORA, MLA_ROPE,
             RET_HEADS * RET_DK, RET_HEADS * RET_DK, GROUP_WIDTH, GROUP_WIDTH,
             GROUP_WIDTH, SSD_CONV_CH, 2 * SSD_HEADS,
             (HY_ORDER + 1) * HY_WIDTH)
IN_COLS = sum(IN_SPLITS)

kernel_name = 'hymba_style_mla_retnet_ssd_hyena_deepnorm_moe'


def rms_norm(x, w, eps=1e-6):
    xf = x.astype(F32)
    y = xf * lax.rsqrt(jnp.mean(xf * xf, axis=-1, keepdims=True) + eps)
    return (y * w.astype(F32)).astype(x.dtype)


def layer_norm(x, g, b, eps=1e-5):
    xf = x.astype(F32)
    mu = jnp.mean(xf, axis=-1, keepdims=True)
    var = jnp.mean(jnp.square(xf - mu), axis=-1, keepdims=True)
    return ((xf - mu) * lax.rsqrt(var + eps) * g.astype(F32) + b.astype(F32)).astype(x.dtype)


def rotary(x):
    s, d = x.shape[1], x.shape[-1]
    half = d // 2
    inv_freq = ROPE_BASE ** (-jnp.arange(half, dtype=F32) * 2.0 / d)
    ang = jnp.arange(s, dtype=F32)[:, None] * inv_freq[None, :]
    cos = jnp.cos(ang)[None, :, None, :]
    sin = jnp.sin(ang)[None, :, None, :]
    xf = x.astype(F32)
    x1, x2 = xf[..., :half], xf[..., half:]
    return jnp.concatenate([x1 * cos - x2 * sin, x2 * cos + x1 * sin], axis=-1).astype(x.dtype)


def depthwise_conv(x, w, b):
    k = w.shape[0]
    y = lax.conv_general_dilated(x, w[:, None, :].astype(x.dtype), window_strides=(1,),
                                 padding=[(k // 2, k // 2)],
                                 dimension_numbers=('NWC', 'WIO', 'NWC'),
                                 feature_group_count=x.shape[-1])
    return y + b.astype(x.dtype)


def mla_attention(q_nope, q_pe, k_nope, k_pe, v):
    bsz, s, h, _ = q_nope.shape
    nq = s // ATTN_BLOCK
    scale = (MLA_NOPE + MLA_ROPE) ** -0.5

    def to_blocks(t):
        return jnp.moveaxis(t.reshape(bsz, nq, ATTN_BLOCK, h, t.shape[-1]), 1, 0)

    def attend(blk):
        qn, qp = blk
        sc = (jnp.einsum('bqhd,bkhd->bhqk', qn, k_nope)
              + jnp.einsum('bqhr,bkr->bhqk', qp, k_pe))
        p = jax.nn.softmax(sc.astype(F32) * scale, axis=-1)
        return jnp.einsum('bhqk,bkhe->bqhe', p.astype(v.dtype), v)

    out = lax.map(attend, (to_blocks(q_nope), to_blocks(q_pe)))
    return jnp.moveaxis(out, 0, 1).reshape(bsz, s, h * v.shape[-1])


def retention_one_direction(q, k, v, log_gamma, include_diag):
    bsz, s, h, dk = q.shape
    dv = v.shape[-1]
    n = RET_CHUNK
    c = s // n
    qc = q.reshape(bsz, c, n, h, dk)
    kc = k.reshape(bsz, c, n, h, dk)
    vc = v.reshape(bsz, c, n, h, dv)
    idx = jnp.arange(n, dtype=F32)
    diff = idx[:, None] - idx[None, :]
    mask = (diff >= 0) if include_diag else (diff > 0)
    decay = jnp.where(mask[..., None], jnp.exp(jnp.where(mask, diff, 0.0)[..., None] * log_gamma), 0.0)
    scores = jnp.einsum('bcihd,bcjhd->bchij', qc, kc) * jnp.moveaxis(decay, -1, 0)
    inner = jnp.einsum('bchij,bcjhe->bcihe', scores, vc)
    k_decay = jnp.exp((n - 1.0 - idx)[:, None] * log_gamma)
    states = jnp.einsum('bcjhd,jh,bcjhe->bchde', kc, k_decay, vc)
    chunk_decay = jnp.exp(n * log_gamma)[:, None, None]

    def step(carry, st):
        return carry * chunk_decay + st, carry

    _, prev = lax.scan(step, jnp.zeros((bsz, h, dk, dv), F32), jnp.moveaxis(states, 1, 0))
    prev = jnp.moveaxis(prev, 0, 1)
    q_decay = jnp.exp((idx + 1.0)[:, None] * log_gamma)
    cross = jnp.einsum('bcihd,ih,bchde->bcihe', qc, q_decay, prev)
    return (inner + cross).reshape(bsz, s, h, dv)


def bidirectional_retention(q, k, v):
    q, k, v = (t.astype(F32) for t in (q, k, v))
    bsz, s, h, dv = v.shape
    heads = jnp.arange(RET_HEADS, dtype=F32)
    lg_fwd = jnp.log1p(-jnp.exp2(-RET_DECAY_EXP_FWD - heads))
    lg_bwd = jnp.log1p(-jnp.exp2(-RET_DECAY_EXP_BWD - heads))
    y = (retention_one_direction(q, k, v, lg_fwd, True)
         + jnp.flip(retention_one_direction(jnp.flip(q, 1), jnp.flip(k, 1), jnp.flip(v, 1), lg_bwd, False), 1))
    mu = jnp.mean(y, axis=-1, keepdims=True)
    var = jnp.mean(jnp.square(y - mu), axis=-1, keepdims=True)
    y = (y - mu) * lax.rsqrt(var + 1e-6)
    return y.reshape(bsz, s, h * dv)


def ssd_chunked(x, dt, a, bm, cm, include_diag):
    bsz, s, h, p = x.shape
    g, n = bm.shape[2], bm.shape[3]
    e = h // g
    q = SSD_CHUNK
    c = s // q
    xc = (x * dt[..., None]).reshape(bsz, c, q, g, e, p)
    la = (dt * a).reshape(bsz, c, q, g, e)
    bc = bm.reshape(bsz, c, q, g, n)
    cc = cm.reshape(bsz, c, q, g, n)
    cs = jnp.cumsum(la, axis=2)
    idx = jnp.arange(q)
    mask = (idx[:, None] >= idx[None, :]) if include_diag else (idx[:, None] > idx[None, :])
    mask6 = mask[:, :, None, None]
    seg = cs[:, :, :, None] - cs[:, :, None, :]
    lmat = jnp.where(mask6, jnp.exp(jnp.where(mask6, seg, 0.0)), 0.0)
    cb = jnp.einsum('bcign,bcjgn->bcijg', cc, bc)
    y_diag = jnp.einsum('bcijg,bcijge,bcjgep->bcigep', cb, lmat, xc)
    to_end = jnp.exp(cs[:, :, -1:] - cs)
    states = jnp.einsum('bcjgn,bcjge,bcjgep->bcgepn', bc, to_end, xc)
    chunk_decay = jnp.exp(cs[:, :, -1])

    def step(carry, inp):
        st, dec = inp
        return carry * dec[..., None, None] + st, carry

    init = jnp.zeros((bsz, g, e, p, n), F32)
    _, prev = lax.scan(step, init, (jnp.moveaxis(states, 1, 0), jnp.moveaxis(chunk_decay, 1, 0)))
    prev = jnp.moveaxis(prev, 0, 1)
    y_off = jnp.einsum('bcign,bcige,bcgepn->bcigep', cc, jnp.exp(cs), prev)
    return (y_diag + y_off).reshape(bsz, s, h, p)


def hyena_filter_spectrum(seq_len, w1, b1, w2, b2, w3, freq):
    t = jnp.linspace(0.0, 1.0, seq_len, dtype=F32)[:, None]
    bands = (HY_EMB - 1) // 2
    ang = 2.0 * math.pi * jnp.arange(seq_len, dtype=F32)[:, None] / seq_len
    f = jnp.linspace(1e-4, bands - 1, bands, dtype=F32)[None, :]
    z = jnp.concatenate([t, jnp.cos(f * ang), -jnp.sin(f * ang)], axis=-1)
    freq = freq.astype(F32)
    hid = jnp.sin(freq[0] * (z @ w1.astype(F32) + b1.astype(F32)))
    hid = jnp.sin(freq[1] * (hid @ w2.astype(F32) + b2.astype(F32)))
    filt = (hid @ w3.astype(F32)).reshape(seq_len, HY_ORDER, 2, HY_WIDTH)
    deltas = jnp.abs(jnp.linspace(HY_MIN_DECAY, HY_MAX_DECAY, HY_WIDTH, dtype=F32))
    filt = filt * jnp.exp(-t * deltas)[:, None, None, :]
    h_fwd, h_bwd = filt[:, :, 0], filt[:, :, 1]
    h_full = jnp.concatenate([h_fwd, jnp.zeros((1, HY_ORDER, HY_WIDTH), F32), jnp.flip(h_bwd[1:], 0)], axis=0)
    return jnp.fft.rfft(h_full, axis=0)


def fft_long_conv(u, h_freq, bias):
    seq_len = u.shape[1]
    spec = jnp.fft.rfft(u, n=2 * seq_len, axis=1) * h_freq[None]
    return jnp.fft.irfft(spec, n=2 * seq_len, axis=1)[:, :seq_len] + u * bias.astype(F32)


def hybrid_mixer(x, w_in, mla_q_norm, mla_w_uq, mla_kv_norm, mla_w_ukv, mla_out_norm,
                 ssd_conv_w, ssd_conv_b, ssd_dt_bias, ssd_a_log, ssd_d, ssd_norm,
                 hy_conv_w, hy_conv_b, hy_w1, hy_b1, hy_w2, hy_b2, hy_w3, hy_freq, hy_bias, hy_out_norm,
                 w_out):
    bsz, s, _ = x.shape
    dtype = x.dtype
    proj = jnp.einsum('bsd,de->bse', x, w_in)
    split_idx = np.cumsum(IN_SPLITS)[:-1].tolist()
    (q_c, kv_c, k_pe, r_q, r_k, r_v, r_g, m_z, m_xbc, m_dt, h_u) = jnp.split(proj, split_idx, axis=-1)

    q = jnp.einsum('bsr,re->bse', rms_norm(q_c, mla_q_norm), mla_w_uq).reshape(bsz, s, MLA_HEADS, MLA_NOPE + MLA_ROPE)
    q_nope, q_pe = q[..., :MLA_NOPE], rotary(q[..., MLA_NOPE:])
    kv = jnp.einsum('bsr,re->bse', rms_norm(kv_c, mla_kv_norm), mla_w_ukv).reshape(bsz, s, MLA_HEADS, MLA_NOPE + MLA_V)
    k_nope, v = kv[..., :MLA_NOPE], kv[..., MLA_NOPE:]
    k_pe = rotary(k_pe[:, :, None, :])[:, :, 0]
    out_a = rms_norm(mla_attention(q_nope, q_pe, k_nope, k_pe, v), mla_out_norm).astype(dtype)

    rq = rotary(r_q.reshape(bsz, s, RET_HEADS, RET_DK))
    rk = rotary(r_k.reshape(bsz, s, RET_HEADS, RET_DK)) * (RET_DK ** -0.5)
    rv = r_v.reshape(bsz, s, RET_HEADS, RET_DV)
    out_b = (bidirectional_retention(rq, rk, rv) * jax.nn.silu(r_g.astype(F32))).astype(dtype)

    gn = SSD_GROUPS * SSD_STATE
    xbc = jax.nn.silu(depthwise_conv(m_xbc, ssd_conv_w, ssd_conv_b)).astype(F32)
    xs = xbc[..., :GROUP_WIDTH].reshape(bsz, s, SSD_HEADS, SSD_HEADDIM)
    bm = xbc[..., GROUP_WIDTH:GROUP_WIDTH + gn].reshape(bsz, s, SSD_GROUPS, SSD_STATE)
    cm = xbc[..., GROUP_WIDTH + gn:].reshape(bsz, s, SSD_GROUPS, SSD_STATE)
    dt = jax.nn.softplus(m_dt.astype(F32).reshape(bsz, s, 2, SSD_HEADS) + ssd_dt_bias.astype(F32))
    a = -jnp.exp(ssd_a_log.astype(F32))
    y_f = ssd_chunked(xs, dt[:, :, 0], a[0], bm, cm, True)
    y_b = jnp.flip(ssd_chunked(jnp.flip(xs, 1), jnp.flip(dt[:, :, 1], 1), a[1],
                               jnp.flip(bm, 1), jnp.flip(cm, 1), False), 1)
    y_c = (y_f + y_b + xs * ssd_d.astype(F32)[:, None]).reshape(bsz, s, GROUP_WIDTH)
    out_c = rms_norm(y_c * jax.nn.silu(m_z.astype(F32)), ssd_norm).astype(dtype)

    u = depthwise_conv(h_u, hy_conv_w, hy_conv_b).astype(F32)
    hy_parts = jnp.split(u, HY_ORDER + 1, axis=-1)
    h_freq = hyena_filter_spectrum(s, hy_w1, hy_b1, hy_w2, hy_b2, hy_w3, hy_freq)
    z = hy_parts[0]
    for o in range(HY_ORDER):
        z = hy_parts[o + 1] * fft_long_conv(z, h_freq[:, o], hy_bias[o])
    out_d = rms_norm(z, hy_out_norm).astype(dtype)

    mixed = jnp.concatenate([out_a, out_b, out_c, out_d], axis=-1)
    return jnp.einsum('bse,ed->bsd', mixed, w_out)


def swiglu(x, w_gate, w_up, w_down):
    return (jax.nn.silu(x @ w_gate) * (x @ w_up)) @ w_down


def moe_swiglu(x, w_router, w_gate, w_up, w_down):
    bsz, s, d = x.shape
    xf = x.reshape(-1, d)
    n_tok = xf.shape[0]
    n_asg = n_tok * TOP_K
    logits = (xf @ w_router).astype(F32)
    top_val, top_idx = lax.top_k(logits, TOP_K)
    gates = jax.nn.softmax(top_val, axis=-1)
    e_flat = top_idx.reshape(-1)
    g_flat = gates.reshape(-1)
    tok_flat = jnp.repeat(jnp.arange(n_tok, dtype=jnp.int32), TOP_K)
    order = jnp.argsort(e_flat)
    e_sorted, tok_sorted, g_sorted = e_flat[order], tok_flat[order], g_flat[order]
    counts = jnp.bincount(e_flat, length=N_EXPERTS)
    padded = (counts + MOE_BLOCK - 1) // MOE_BLOCK * MOE_BLOCK
    start = jnp.cumsum(counts) - counts
    pend = jnp.cumsum(padded)
    pstart = pend - padded
    dest = pstart[e_sorted] + (jnp.arange(n_asg, dtype=jnp.int32) - start[e_sorted])
    cap = n_asg + N_EXPERTS * MOE_BLOCK
    n_blocks = cap // MOE_BLOCK
    slot_tok = jnp.full((cap,), n_tok, jnp.int32).at[dest].set(tok_sorted)
    slot_gate = jnp.zeros((cap,), F32).at[dest].set(g_sorted)
    block_expert = jnp.minimum(
        jnp.searchsorted(pend, jnp.arange(n_blocks, dtype=pend.dtype) * MOE_BLOCK, side='right'),
        N_EXPERTS - 1)
    x_pad = jnp.concatenate([xf, jnp.zeros((1, d), xf.dtype)], axis=0)
    x_slots = x_pad[slot_tok].reshape(n_blocks, MOE_BLOCK, d)

    def expert_block(args):
        xb, e = args
        return swiglu(xb, w_gate[e], w_up[e], w_down[e])

    y_slots = lax.map(expert_block, (x_slots, block_expert)).reshape(cap, d)
    y = jnp.zeros((n_tok + 1, d), y_slots.dtype).at[slot_tok].add(y_slots * slot_gate[:, None].astype(y_slots.dtype))
    return y[:n_tok].reshape(bsz, s, d).astype(x.dtype)


def setup_inputs(seed: int = 0) -> dict:
    key = jax.random.key(seed)
    ks = jax.random.split(key, 40)

    def nrm(i, shape, scale):
        return jax.random.normal(ks[i], shape, F32) * scale

    L = DEPTH
    D = D_MODEL
    W = GROUP_WIDTH
    HID = HY_FILTER_HIDDEN
    dt0 = jnp.exp(jax.random.uniform(ks[9], (L, 2, SSD_HEADS), F32, minval=math.log(1e-3), maxval=math.log(1e-1)))
    return {
        'x': nrm(0, (BATCH, SEQ, D), 1.0),
        'w_in': nrm(1, (L, D, IN_COLS), D ** -0.5),
        'mla_q_norm': 1.0 + nrm(2, (L, MLA_Q_LORA), 0.02),
        'mla_w_uq': nrm(3, (L, MLA_Q_LORA, MLA_HEADS * (MLA_NOPE + MLA_ROPE)), MLA_Q_LORA ** -0.5),
        'mla_kv_norm': 1.0 + nrm(4, (L, MLA_KV_LORA), 0.02),
        'mla_w_ukv': nrm(5, (L, MLA_KV_LORA, MLA_HEADS * (MLA_NOPE + MLA_V)), MLA_KV_LORA ** -0.5),
        'mla_out_norm': 1.0 + nrm(6, (L, W), 0.02),
        'ssd_conv_w': nrm(7, (L, SSD_CONV, SSD_CONV_CH), SSD_CONV ** -0.5),
        'ssd_conv_b': nrm(8, (L, SSD_CONV_CH), 0.02),
        'ssd_dt_bias': dt0 + jnp.log(-jnp.expm1(-dt0)),
        'ssd_a_log': jnp.log(jax.random.uniform(ks[10], (L, 2, SSD_HEADS), F32, minval=1.0, maxval=16.0)),
        'ssd_d': 1.0 + nrm(11, (L, SSD_HEADS), 0.02),
        'ssd_norm': 1.0 + nrm(12, (L, W), 0.02),
        'hy_conv_w': nrm(13, (L, HY_SHORT, (HY_ORDER + 1) * W), HY_SHORT ** -0.5),
        'hy_conv_b': nrm(14, (L, (HY_ORDER + 1) * W), 0.02),
        'hy_w1': nrm(15, (L, HY_EMB, HID), HY_EMB ** -0.5),
        'hy_b1': nrm(16, (L, HID), 0.1),
        'hy_w2': nrm(17, (L, HID, HID), HID ** -0.5),
        'hy_b2': nrm(18, (L, HID), 0.1),
        'hy_w3': nrm(19, (L, HID, HY_ORDER * 2 * W), HID ** -0.5),
        'hy_freq': 1.0 + nrm(20, (L, 2, HID), 0.02),
        'hy_bias': nrm(21, (L, HY_ORDER, W), 1.0),
        'hy_out_norm': 1.0 + nrm(22, (L, W), 0.02),
        'w_out': nrm(23, (L, D, D), BETA * D ** -0.5),
        'ln1_g': 1.0 + nrm(24, (L, D), 0.02),
        'ln1_b': nrm(25, (L, D), 0.02),
        'ln2_g': 1.0 + nrm(26, (L, D), 0.02),
        'ln2_b': nrm(27, (L, D), 0.02),
        'ffn_w_gate': nrm(28, (N_DENSE, D, D_FF), D ** -0.5),
        'ffn_w_up': nrm(29, (N_DENSE, D, D_FF), D ** -0.5),
        'ffn_w_down': nrm(30, (N_DENSE, D_FF, D), BETA * D_FF ** -0.5),
        'moe_router': nrm(31, (N_MOE, D, N_EXPERTS), D ** -0.5),
        'moe_w_gate': nrm(32, (N_MOE, N_EXPERTS, D, D_FF_EXPERT), D ** -0.5),
        'moe_w_up': nrm(33, (N_MOE, N_EXPERTS, D, D_FF_EXPERT), D ** -0.5),
        'moe_w_down': nrm(34, (N_MOE, N_EXPERTS, D_FF_EXPERT, D), BETA * D_FF_EXPERT ** -0.5),
    }


def reference(x, w_in, mla_q_norm, mla_w_uq, mla_kv_norm, mla_w_ukv, mla_out_norm,
              ssd_conv_w, ssd_conv_b, ssd_dt_bias, ssd_a_log, ssd_d, ssd_norm,
              hy_conv_w, hy_conv_b, hy_w1, hy_b1, hy_w2, hy_b2, hy_w3, hy_freq, hy_bias, hy_out_norm,
              w_out, ln1_g, ln1_b, ln2_g, ln2_b,
              ffn_w_gate, ffn_w_up, ffn_w_down,
              moe_router, moe_w_gate, moe_w_up, moe_w_down):
    for layer in range(DEPTH):
        h = hybrid_mixer(x, w_in[layer], mla_q_norm[layer], mla_w_uq[layer], mla_kv_norm[layer],
                         mla_w_ukv[layer], mla_out_norm[layer],
                         ssd_conv_w[layer], ssd_conv_b[layer], ssd_dt_bias[layer], ssd_a_log[layer],
                         ssd_d[layer], ssd_norm[layer],
                         hy_conv_w[layer], hy_conv_b[layer], hy_w1[layer], hy_b1[layer], hy_w2[layer],
                         hy_b2[layer], hy_w3[layer], hy_freq[layer], hy_bias[layer], hy_out_norm[layer],
                         w_out[layer])
        x = layer_norm(ALPHA * x + h, ln1_g[layer], ln1_b[layer])
        j = layer // 2
        if layer % 2 == 0:
            f = swiglu(x, ffn_w_gate[j], ffn_w_up[j], ffn_w_down[j])
        else:
            f = moe_swiglu(x, moe_router[j], moe_w_gate[j], moe_w_up[j], moe_w_down[j])
        x = layer_norm(ALPHA * x + f, ln2_g[layer], ln2_b[layer])
    return x
```

```python
import math
from contextlib import ExitStack
import numpy as np
import ml_dtypes
import concourse.bass as bass
import concourse.mybir as mybir
from concourse.bass_utils import run_bass_kernel_spmd

F32 = mybir.dt.float32
BF16 = mybir.dt.bfloat16
AF = mybir.ActivationFunctionType
ALU = mybir.AluOpType
SEM_LIMIT = 8000

L = 2
D = 2048
S = 2048
NSEQ = 2
T = NSEQ * S
INC = 5328
DFF = 5632
DFE = 7168
NE = 8
ALPHA = (2 * L) ** 0.25
NF = 17
NFP = NF * 128
C_QC, C_KVC, C_KPE, C_RQ, C_RK, C_RV, C_RG, C_MZ, C_XBC, C_DT, C_HU = 0, 384, 640, 704, 960, 1216, 1728, 2240, 2752, 3776, 3792
RET_LG_F = [math.log1p(-2.0 ** (-5.0 - h)) for h in range(4)]
RET_LG_B = [math.log1p(-2.0 ** (-5.5 - h)) for h in range(4)]


class Buf:
    __slots__ = ("name", "w", "r")

    def __init__(self, name=""):
        self.name = name
        self.w = {}
        self.r = {}


class Prog:
    def __init__(self, nc, stack):
        self.nc = nc
        self.stack = stack
        self.eng = {"pe": nc.tensor, "dve": nc.vector, "act": nc.scalar, "pool": nc.gpsimd, "sp": nc.sync}
        self.cur_sem, self.cnt, self.sems, self.nsem = {}, {}, {}, 0
        for e in self.eng:
            self._new_eng_sem(e)
        self.waited = {e: {} for e in self.eng}
        self.nslots = 8
        self.slots = {q: [[self._new_sem("d%s%d" % (q, i)), 0] for i in range(self.nslots)] for q in ("sp", "pool", "act")}
        self.slot_i = {q: 0 for q in self.slots}
        self.n_inst = 0

    def _new_sem(self, name):
        self.nsem += 1
        key = "%s_%d" % (name, self.nsem)
        self.sems[key] = self.stack.enter_context(self.nc.semaphore(key))
        return key

    def _new_eng_sem(self, e):
        self.cur_sem[e] = self._new_sem("c" + e)
        self.cnt[e] = 0

    def _wait(self, e, tok):
        if tok is None:
            return
        key, val, src = tok
        if src == e and e == "pe":
            return
        w = self.waited[e]
        if w.get(key, 0) >= val:
            return
        self.eng[e].wait_ge(self.sems[key], val)
        w[key] = val

    def _deps(self, e, reads, writes):
        for b in reads:
            for t in b.w.values():
                self._wait(e, t)
        for b in writes:
            for t in b.w.values():
                self._wait(e, t)
            for t in b.r.values():
                if t[2] != e:
                    self._wait(e, t)

    def _record(self, tok, reads, writes):
        for b in reads:
            b.r[tok[0]] = tok
        for b in writes:
            b.w[tok[0]] = tok
            b.r = {}

    def op(self, e, fn, reads=(), writes=()):
        self._deps(e, reads, writes)
        ins = fn()
        self.n_inst += 1
        if self.cnt[e] >= SEM_LIMIT:
            self._new_eng_sem(e)
        self.cnt[e] += 1
        ins.then_inc(self.sems[self.cur_sem[e]], 1)
        tok = (self.cur_sem[e], self.cnt[e], e)
        self._record(tok, reads, writes)
        return tok

    def dma(self, q, out, in_, reads=(), writes=(), **kw):
        self._deps(q, reads, writes)
        i = self.slot_i[q]
        self.slot_i[q] = (i + 1) % self.nslots
        sl = self.slots[q][i]
        if sl[1] > 0:
            self._wait(q, (sl[0], sl[1], "dma"))
        if sl[1] + 16 > SEM_LIMIT:
            sl[0] = self._new_sem("d%s%d" % (q, i))
            sl[1] = 0
        sl[1] += 16
        self.eng[q].dma_start(out=out, in_=in_, **kw).then_inc(self.sems[sl[0]], 16)
        tok = (sl[0], sl[1], "dma")
        self._record(tok, reads, writes)
        self.n_inst += 1
        return tok

    def dma_raw(self, q, emit, reads=(), writes=()):
        self._deps(q, reads, writes)
        i = self.slot_i[q]
        self.slot_i[q] = (i + 1) % self.nslots
        sl = self.slots[q][i]
        if sl[1] > 0:
            self._wait(q, (sl[0], sl[1], "dma"))
        if sl[1] + 16 > SEM_LIMIT:
            sl[0] = self._new_sem("d%s%d" % (q, i))
            sl[1] = 0
        sl[1] += 16
        emit().then_inc(self.sems[sl[0]], 16)
        tok = (sl[0], sl[1], "dma")
        self._record(tok, reads, writes)
        self.n_inst += 1
        return tok

    def barrier(self):
        toks = [(self.cur_sem[e], self.cnt[e], e) for e in self.eng if self.cnt[e] > 0]
        for q in self.slots:
            for key, val in self.slots[q]:
                if val:
                    toks.append((key, val, "dma"))
        for e in self.eng:
            for t in toks:
                if t[2] != e:
                    self._wait(e, t)

    def finish(self):
        for q in self.slots:
            for key, val in self.slots[q]:
                if val:
                    self._wait("sp", (key, val, "dma"))


class Phase(ExitStack):
    def __init__(self, kb):
        super().__init__()
        self.kb = kb

    def __exit__(self, *a):
        self.kb.P.barrier()
        return super().__exit__(*a)


class Tl:
    __slots__ = ("t", "b")

    def __init__(self, t, name=""):
        self.t = t
        self.b = Buf(name)


class KB:
    def __init__(self, nc, st):
        self.nc = nc
        self.st = st
        self.P = Prog(nc, st)
        self.uid = 0
        self.banks = [Tl(st.enter_context(nc.psum_tensor("bank%d" % i, [128, 512], F32)), "bank%d" % i) for i in range(8)]
        self.evac_i = 0
        self.consts = {}

    def sb(self, stack, shape, dt, name="t"):
        self.uid += 1
        nm = "%s_%d" % (name, self.uid)
        return Tl(stack.enter_context(self.nc.sbuf_tensor(nm, list(shape), dt)), nm)

    def dram(self, name, shape, dt):
        return Tl(self.nc.dram_tensor(name, list(shape), dt).ap(), name)

    def cst(self, val):
        if val not in self.consts:
            t = self.sb(self.st, [128, 1], F32, "cst")
            self.P.op("pool", lambda: self.nc.gpsimd.memset(t.t[:], float(val)), writes=[t.b])
            self.consts[val] = t
        return self.consts[val]

    @staticmethod
    def _b(xs):
        return [x.b for x in xs]

    def act(self, out, in_, func, reads, writes, bias=None, scale=None):
        kw = {}
        rd = list(reads)
        if bias is not None:
            if isinstance(bias, (int, float)):
                c = self.cst(bias)
                rd.append(c)
                bias = c.t[0:out.shape[0], 0:1]
            kw["bias"] = bias
        if scale is not None:
            kw["scale"] = scale
        return self.P.op("act", lambda: self.nc.scalar.activation(out=out, in_=in_, func=func, **kw), self._b(rd), self._b(writes))

    def tt(self, out, in0, in1, op, reads, writes, eng="dve"):
        e = self.nc.vector if eng == "dve" else self.nc.gpsimd
        return self.P.op(eng, lambda: e.tensor_tensor(out=out, in0=in0, in1=in1, op=op), self._b(reads), self._b(writes))

    def ts(self, out, in0, s1, s2, op0, op1, reads, writes, eng="dve"):
        e = self.nc.vector if eng == "dve" else self.nc.gpsimd
        if op1 is None:
            return self.P.op(eng, lambda: e.tensor_scalar(out=out, in0=in0, scalar1=s1, scalar2=None, op0=op0), self._b(reads), self._b(writes))
        return self.P.op(eng, lambda: e.tensor_scalar(out=out, in0=in0, scalar1=s1, scalar2=s2, op0=op0, op1=op1), self._b(reads), self._b(writes))

    def stt(self, out, in0, scalar, in1, op0, op1, reads, writes):
        return self.P.op("dve", lambda: self.nc.vector.scalar_tensor_tensor(out=out, in0=in0, scalar=scalar, in1=in1, op0=op0, op1=op1), self._b(reads), self._b(writes))

    def copy(self, out, in_, reads, writes, eng=None):
        if eng is None:
            self.evac_i += 1
            eng = "act" if self.evac_i % 2 else "dve"
        if eng == "act":
            return self.P.op("act", lambda: self.nc.scalar.copy(out=out, in_=in_), self._b(reads), self._b(writes))
        e = self.nc.vector if eng == "dve" else self.nc.gpsimd
        return self.P.op(eng, lambda: e.tensor_copy(out=out, in_=in_), self._b(reads), self._b(writes))

    def recip(self, out, in_, reads, writes):
        return self.P.op("dve", lambda: self.nc.vector.reciprocal(out=out, in_=in_), self._b(reads), self._b(writes))

    def memset(self, out, val, writes, eng="pool"):
        e = self.nc.vector if eng == "dve" else self.nc.gpsimd
        return self.P.op(eng, lambda: e.memset(out, float(val)), [], self._b(writes))

    def mm(self, bank, out, lhsT, rhs, start, stop, reads):
        return self.P.op("pe", lambda: self.nc.tensor.matmul(out, lhsT=lhsT, rhs=rhs, start=start, stop=stop), self._b(reads), [bank.b])

    def tr(self, bank, out, in_, reads):
        rd = list(reads) + [self.ident]
        return self.P.op("pe", lambda: self.nc.tensor.transpose(out=out, in_=in_, identity=self.ident.t[0:in_.shape[0], 0:in_.shape[0]]), self._b(rd), [bank.b])

    def dma(self, q, out, in_, reads, writes, **kw):
        return self.P.dma(q, out, in_, self._b(reads), self._b(writes), **kw)

    def setup_consts(self):
        nc = self.nc
        self.ident = self.sb(self.st, [128, 128], F32, "ident")
        self.memset(self.ident.t[:], 1.0, [self.ident])
        self.P.op("pool", lambda: nc.gpsimd.affine_select(out=self.ident.t[:], in_=self.ident.t[:], pattern=[[1, 128]], compare_op=ALU.is_equal, fill=0.0, base=0, channel_multiplier=-1), self._b([self.ident]), self._b([self.ident]))
        self.ones_bf = self.sb(self.st, [128, 128], BF16, "ones_bf")
        self.memset(self.ones_bf.t[:], 1.0, [self.ones_bf])
        self.ones_f = self.sb(self.st, [128, 128], F32, "ones_f")
        self.memset(self.ones_f.t[:], 1.0, [self.ones_f])
        for v in (1e-6, 1e-5, 1.0, 0.0):
            self.cst(v)
        self.sel = self.sb(self.st, [16, 16, 128], F32, "sel")
        self.memset(self.sel.t[:], 0.0, [self.sel])
        self.P.op("pool", lambda: nc.gpsimd.affine_select(out=self.sel.t[:], in_=self.sel.t[:], pattern=[[-1, 16], [0, 128]], compare_op=ALU.not_equal, fill=1.0, base=0, channel_multiplier=1), self._b([self.sel]), self._b([self.sel]))

    def bcast_row(self, stack, row_ap, n, name):
        out = self.sb(stack, [128, n], F32, name)
        with Phase(self) as p:
            rowt = self.sb(p, [1, n], F32, name + "_row")
            self.dma("sp", rowt.t[:], row_ap, [], [rowt])
            for c in range(0, n, 512):
                w = min(512, n - c)
                bk = self.banks[(c // 512) % 2]
                self.mm(bk, bk.t[:, 0:w], self.ones_f.t[0:1, :], rowt.t[0:1, c:c + w], True, True, [self.ones_f, rowt])
                self.copy(out.t[:, c:c + w], bk.t[:, 0:w], [bk], [out])
        return out

    def load_xT(self, src, tok0, xT, xtiles, ntt=4, x32=None, post=None):
        for ts_ in range(ntt):
            xt = xtiles[ts_ % len(xtiles)]
            self.dma("sp", xt.t[:], src.t[tok0 + ts_ * 128: tok0 + (ts_ + 1) * 128, :], [src], [xt])
            for j in range(4):
                bk = self.banks[4 + j]
                for k in range(4):
                    dc = 4 * j + k
                    self.tr(bk, bk.t[:, k * 128:(k + 1) * 128], xt.t[:, dc * 128:(dc + 1) * 128], [xt])
                bv = bk.t[:].rearrange("p (a b) -> p a b", a=4)
                ce = "act" if j % 2 else "dve"
                self.copy(xT.t[:, 4 * j:4 * j + 4, ts_ * 128:(ts_ + 1) * 128], bv, [bk], [xT], eng=ce)
                if x32 is not None:
                    self.copy(x32.t[:, 4 * j:4 * j + 4, :], bv, [bk], [x32], eng=ce)
            if post is not None:
                post(ts_)

    def cast_dram(self, dst, src_ap, rows, step=256):
        if src_ap.shape[-1] > 5632:
            step = 128
        for r0 in range(0, rows, step):
            r1 = min(rows, r0 + step)
            self.dma("pool", dst.t[r0:r1, :], src_ap[r0:r1, :], [], [dst])

    def build_rot(self, l, w_in_ap, wrot):
        with Phase(self) as ph:
            src = self.sb(ph, [128, 16, 576], F32, "rsrc")
            dst = self.sb(ph, [128, 16, 576], BF16, "rdst")
            self.dma("sp", src.t[:], w_in_ap[l, :, C_KPE:C_KPE + 576].rearrange("(dc p) c -> p dc c", p=128), [], [src])
            for dc in range(16):
                sv = src.t[:, dc, :].rearrange("p (g two h) -> p g two h", two=2, h=32)
                dv = dst.t[:, dc, :].rearrange("p (g two h) -> p g two h", two=2, h=32)
                self.ts(dv[:, :, 0, :], sv[:, :, 1, :], -1.0, None, ALU.mult, None, [src], [dst])
                self.copy(dv[:, :, 1, :], sv[:, :, 0, :], [src], [dst], eng="dve")
            self.dma("pool", wrot.t[l].rearrange("(dc p) c -> p dc c", p=128), dst.t[:], [dst], [wrot])

    def inproj(self, l, xres, winb, wrot, seg, cst, tok_range):
        with Phase(self) as ph:
            xT = self.sb(ph, [128, 16, 512], BF16, "xT")
            xtiles = [self.sb(ph, [128, 2048], F32, "xtile") for _ in range(2)]
            wbuf = [self.sb(ph, [128, 16, 576], BF16, "wbuf") for _ in range(2)]
            rbuf = self.sb(ph, [128, 16, 576], BF16, "rbuf")
            cos = self.sb(ph, [128, 2048], F32, "cos")
            sin = self.sb(ph, [128, 2048], F32, "sin")
            self.dma("sp", cos.t[:], cst["rope_cos"], [], [cos])
            self.dma("sp", sin.t[:], cst["rope_sin"], [], [sin])
            self.dma("sp", rbuf.t[:], wrot.t[l].rearrange("(dc p) c -> p dc c", p=128), [wrot], [rbuf])
            stage = [self.sb(ph, [128, 512], BF16, "stg") for _ in range(4)]
            st32 = [self.sb(ph, [128, 512], F32, "st32") for _ in range(3)]
            stdt = self.sb(ph, [16, 512], F32, "stdt")
            sti = [0]
            bki = [0]
            groups = [(0, 384, "fm", [("qc", 0, 0, 128), ("qc", 128, 128, 128), ("qc", 256, 256, 128)]),
                      (384, 256, "fm", [("kvc", 0, 0, 128), ("kvc", 128, 128, 128)]),
                      (640, 576, "rope", [("kpe", 0, 0, 64, 1.0), ("rq", 0, 64, 128, 1.0), ("rq", 128, 192, 128, 1.0),
                                          ("rk", 0, 320, 128, 0.125), ("rk", 128, 448, 128, 0.125)]),
                      (1216, 512, "tm", None),
                      (1728, 512, "fm", [("rg", i * 128, i * 128, 128) for i in range(4)]),
                      (2240, 512, "fm", [("mz", i * 128, i * 128, 128) for i in range(4)]),
                      (2752, 512, "fm", [("xbc", i * 128, i * 128, 128) for i in range(4)]),
                      (3264, 512, "fm", [("xbc", 512 + i * 128, i * 128, 128) for i in range(4)]),
                      (3776, 16, "dt", None),
                      (3792, 512, "fm", [("hu", i * 128, i * 128, 128) for i in range(4)]),
                      (4304, 512, "fm", [("hu", 512 + i * 128, i * 128, 128) for i in range(4)]),
                      (4816, 512, "fm", [("hu", 1024 + i * 128, i * 128, 128) for i in range(4)])]

            def loadw(gi):
                c0, cw = groups[gi][0], groups[gi][1]
                wt = wbuf[gi % 2]
                self.dma("sp", wt.t[:, :, 0:cw], winb.t[l, :, c0:c0 + cw].rearrange("(dc p) c -> p dc c", p=128), [winb], [wt])

            for tok0 in range(tok_range[0], tok_range[1], 512):
                tpos = tok0 % S
                self.load_xT(xres, tok0, xT, xtiles)
                loadw(0)
                for gi, (c0, cw, kind, chunks) in enumerate(groups):
                    if gi + 1 < len(groups):
                        loadw(gi + 1)
                    wt = wbuf[gi % 2]
                    if kind == "fm":
                        for (sname, row0, lc, n) in chunks:
                            bk = self.banks[bki[0] % 4]
                            bki[0] += 1
                            for dc in range(16):
                                self.mm(bk, bk.t[0:n, :], wt.t[:, dc, lc:lc + n], xT.t[:, dc, :], dc == 0, dc == 15, [wt, xT])
                            sg = stage[sti[0] % 4]
                            sti[0] += 1
                            self.copy(sg.t[0:n, :], bk.t[0:n, :], [bk], [sg])
                            self.dma("pool", seg[sname].t[row0:row0 + n, tok0:tok0 + 512], sg.t[0:n, :], [sg], [seg[sname]])
                    elif kind == "dt":
                        bk = self.banks[bki[0] % 4]
                        bki[0] += 1
                        for dc in range(16):
                            self.mm(bk, bk.t[0:16, :], wt.t[:, dc, 0:16], xT.t[:, dc, :], dc == 0, dc == 15, [wt, xT])
                        self.copy(stdt.t[:], bk.t[0:16, :], [bk], [stdt])
                        self.dma("pool", seg["dt"].t[:, tok0:tok0 + 512], stdt.t[:], [stdt], [seg["dt"]])
                    elif kind == "tm":
                        for ts_ in range(4):
                            bk = self.banks[bki[0] % 4]
                            bki[0] += 1
                            for dc in range(16):
                                self.mm(bk, bk.t[:, :], xT.t[:, dc, ts_ * 128:(ts_ + 1) * 128], wt.t[:, dc, 0:512], dc == 0, dc == 15, [wt, xT])
                            sg = stage[sti[0] % 4]
                            sti[0] += 1
                            self.copy(sg.t[:, :], bk.t[:, :], [bk], [sg])
                            self.dma("pool", seg["rv"].t[tok0 + ts_ * 128: tok0 + (ts_ + 1) * 128, :], sg.t[:, :], [sg], [seg["rv"]])
                    else:
                        for (sname, row0, lc, n, scl) in chunks:
                            bA = self.banks[bki[0] % 4]
                            bB = self.banks[(bki[0] + 1) % 4]
                            bki[0] += 2
                            for dc in range(16):
                                self.mm(bA, bA.t[0:n, :], wt.t[:, dc, lc:lc + n], xT.t[:, dc, :], dc == 0, dc == 15, [wt, xT])
                            for dc in range(16):
                                self.mm(bB, bB.t[0:n, :], rbuf.t[:, dc, lc:lc + n], xT.t[:, dc, :], dc == 0, dc == 15, [rbuf, xT])
                            self.tt(st32[0].t[0:n, :], bA.t[0:n, :], cos.t[0:n, tpos:tpos + 512], ALU.mult, [bA, cos], [st32[0]])
                            self.tt(st32[1].t[0:n, :], bB.t[0:n, :], sin.t[0:n, tpos:tpos + 512], ALU.mult, [bB, sin], [st32[1]])
                            self.tt(st32[2].t[0:n, :], st32[0].t[0:n, :], st32[1].t[0:n, :], ALU.add, [st32[0], st32[1]], [st32[2]])
                            sg = stage[sti[0] % 4]
                            sti[0] += 1
                            self.act(sg.t[0:n, :], st32[2].t[0:n, :], AF.Copy, [st32[2]], [sg], scale=scl)
                            self.dma("pool", seg[sname].t[row0:row0 + n, tok0:tok0 + 512], sg.t[0:n, :], [sg], [seg[sname]])

    def rms_rstd(self, ph, src, nch, rows, cols, nfeat, eps, bank, tmp_sq, out_rstd):
        c0, c1 = cols
        for c in range(nch):
            self.act(tmp_sq.t[0:rows, c, :], src.t[0:rows, c, c0:c1], AF.Square, [src], [tmp_sq])
        for c in range(nch):
            self.mm(bank, bank.t[:, :], self.ones_bf.t[0:rows, :], tmp_sq.t[0:rows, c, :], c == 0, c == nch - 1, [self.ones_bf, tmp_sq])
        self.act(out_rstd.t[:, :], bank.t[:, :], AF.Sqrt, [bank], [out_rstd], bias=eps, scale=1.0 / nfeat)
        self.recip(out_rstd.t[:, :], out_rstd.t[:, :], [out_rstd], [out_rstd])

    def group_ret(self, s, seg, mixedT):
        nc = self.nc
        t0 = s * S
        with Phase(self) as ph:
            rq = self.sb(ph, [128, 2, S], BF16, "rq")
            rk = self.sb(ph, [128, 2, S], BF16, "rk")
            rg = self.sb(ph, [128, 4, S], BF16, "rg")
            V = self.sb(ph, [128, 16, 512], BF16, "rv")
            self.dma("sp", rq.t[:], seg["rq"].t[:, t0:t0 + S].rearrange("(c p) t -> p c t", p=128), [seg["rq"]], [rq])
            self.dma("sp", rk.t[:], seg["rk"].t[:, t0:t0 + S].rearrange("(c p) t -> p c t", p=128), [seg["rk"]], [rk])
            self.dma("sp", rg.t[:], seg["rg"].t[:, t0:t0 + S].rearrange("(c p) t -> p c t", p=128), [seg["rg"]], [rg])
            self.dma("sp", V.t[:], seg["rv"].t[t0:t0 + S, :].rearrange("(c p) e -> p c e", p=128), [seg["rv"]], [V])
            W = 3968
            strip = self.sb(ph, [128, 4, W], BF16, "strip")
            dl = self.sb(ph, [128, W], F32, "dl")
            tA = self.sb(ph, [128, W], F32, "tA")
            tB = self.sb(ph, [128, W], F32, "tB")
            self.P.op("pool", lambda: nc.gpsimd.iota(dl.t[:], pattern=[[1, W]], base=-1920, channel_multiplier=-1, allow_small_or_imprecise_dtypes=True), [], [dl.b])
            for h in range(4):
                self.ts(tA.t[:], dl.t[:], 0.0, RET_LG_F[h], ALU.max, ALU.mult, [dl], [tA])
                self.ts(tB.t[:], dl.t[:], 0.0, -RET_LG_B[h], ALU.min, ALU.mult, [dl], [tB])
                self.tt(tA.t[:], tA.t[:], tB.t[:], ALU.add, [tA, tB], [tA])
                self.act(strip.t[:, h, :], tA.t[:], AF.Exp, [tA], [strip])
            Pb = [self.sb(ph, [128, 512], BF16, "P") for _ in range(3)]
            ysb = self.sb(ph, [128, 512], F32, "ysb")
            ysq = self.sb(ph, [128, 512], F32, "ysq")
            mean = self.sb(ph, [128, 512], F32, "mean")
            var = self.sb(ph, [128, 512], F32, "var")
            gate = self.sb(ph, [128, 512], F32, "gate")
            ob = [self.sb(ph, [128, 512], BF16, "ob") for _ in range(2)]
            pi = 0
            for h in range(4):
                c, base = h // 2, 64 * (h % 2)
                for ib in range(4):
                    i0 = ib * 512
                    yb = self.banks[4 + (h * 4 + ib) % 2]
                    for jc in range(16):
                        j0 = jc * 128
                        sbk = self.banks[jc % 3]
                        self.mm(sbk, sbk.t[:, :], rk.t[base:base + 64, c, j0:j0 + 128], rq.t[base:base + 64, c, i0:i0 + 512], True, True, [rk, rq])
                        pb = Pb[pi % 3]
                        pi += 1
                        x0 = i0 - j0 + 1920
                        self.tt(pb.t[:, :], sbk.t[:, :], strip.t[:, h, x0:x0 + 512], ALU.mult, [sbk, strip], [pb])
                        self.mm(yb, yb.t[:, :], V.t[:, jc, h * 128:(h + 1) * 128], pb.t[:, :], jc == 0, jc == 15, [V, pb])
                    self.copy(ysb.t[:, :], yb.t[:, :], [yb], [ysb], eng="act")
                    self.act(ysq.t[:, :], yb.t[:, :], AF.Square, [yb], [ysq])
                    mb, vb = self.banks[6], self.banks[7]
                    self.mm(mb, mb.t[:, :], self.ones_f.t[:, :], ysb.t[:, :], True, True, [self.ones_f, ysb])
                    self.mm(vb, vb.t[:, :], self.ones_f.t[:, :], ysq.t[:, :], True, True, [self.ones_f, ysq])
                    self.act(mean.t[:, :], mb.t[:, :], AF.Copy, [mb], [mean], scale=1.0 / 128)
                    self.tt(var.t[:, :], mean.t[:, :], mean.t[:, :], ALU.mult, [mean], [var])
                    self.stt(var.t[:, :], vb.t[:, :], 1.0 / 128, var.t[:, :], ALU.mult, ALU.subtract, [vb, var], [var])
                    self.act(var.t[:, :], var.t[:, :], AF.Sqrt, [var], [var], bias=1e-6, scale=1.0)
                    self.recip(var.t[:, :], var.t[:, :], [var], [var])
                    self.tt(ysb.t[:, :], ysb.t[:, :], mean.t[:, :], ALU.subtract, [ysb, mean], [ysb])
                    self.tt(ysb.t[:, :], ysb.t[:, :], var.t[:, :], ALU.mult, [ysb, var], [ysb])
                    self.act(gate.t[:, :], rg.t[:, h, i0:i0 + 512], AF.Silu, [rg], [gate])
                    o = ob[(h * 4 + ib) % 2]
                    self.tt(o.t[:, :], ysb.t[:, :], gate.t[:, :], ALU.mult, [ysb, gate], [o])
                    self.dma("pool", mixedT.t[512 + h * 128: 512 + (h + 1) * 128, t0 + i0: t0 + i0 + 512], o.t[:, :], [o], [mixedT])

    def group_mla(self, l, s, seg, mixedT, prm, cst):
        t0 = s * S
        SC = (128 + 64) ** -0.5
        with Phase(self) as ph:
            qc = self.sb(ph, [128, 3, S], BF16, "qc")
            kvc = self.sb(ph, [128, 2, S], BF16, "kvc")
            kpe = self.sb(ph, [64, S], BF16, "kpe")
            self.dma("sp", qc.t[:], seg["qc"].t[:, t0:t0 + S].rearrange("(c p) t -> p c t", p=128), [seg["qc"]], [qc])
            self.dma("sp", kvc.t[:], seg["kvc"].t[:, t0:t0 + S].rearrange("(c p) t -> p c t", p=128), [seg["kvc"]], [kvc])
            self.dma("sp", kpe.t[:], seg["kpe"].t[:, t0:t0 + S], [seg["kpe"]], [kpe])
            cos = self.sb(ph, [64, S], F32, "cos")
            sin = self.sb(ph, [64, S], F32, "sin")
            self.dma("sp", cos.t[:], cst["rope_cos"][0:64, :], [], [cos])
            self.dma("sp", sin.t[:], cst["rope_sin"][0:64, :], [], [sin])
            wq32 = self.sb(ph, [128, 3, 768], F32, "wq32")
            wkv32 = self.sb(ph, [128, 2, 1024], F32, "wkv32")
            self.dma("sp", wq32.t[:], prm["mla_w_uq"][l].rearrange("(c p) e -> p c e", p=128), [], [wq32])
            self.dma("sp", wkv32.t[:], prm["mla_w_ukv"][l].rearrange("(c p) e -> p c e", p=128), [], [wkv32])
            wq = self.sb(ph, [128, 3, 768], BF16, "wq")
            wkv = self.sb(ph, [128, 2, 1024], BF16, "wkv")
            wqr = self.sb(ph, [128, 3, 4, 64], BF16, "wqr")
            self.copy(wq.t[:], wq32.t[:], [wq32], [wq], eng="dve")
            self.copy(wkv.t[:], wkv32.t[:], [wkv32], [wkv], eng="dve")
            for c in range(3):
                for h in range(4):
                    b0 = 192 * h + 128
                    self.ts(wqr.t[:, c, h, 0:32], wq32.t[:, c, b0 + 32:b0 + 64], -1.0, None, ALU.mult, None, [wq32], [wqr])
                    self.copy(wqr.t[:, c, h, 32:64], wq32.t[:, c, b0:b0 + 32], [wq32], [wqr], eng="dve")
            qnw = self.sb(ph, [128, 3], F32, "qnw")
            kvnw = self.sb(ph, [128, 2], F32, "kvnw")
            onw = self.sb(ph, [128, 4], F32, "onw")
            self.dma("sp", qnw.t[:], prm["mla_q_norm_pp"][:, l * 3:(l + 1) * 3], [], [qnw])
            self.dma("sp", kvnw.t[:], prm["mla_kv_norm_pp"][:, l * 2:(l + 1) * 2], [], [kvnw])
            self.dma("sp", onw.t[:], prm["mla_out_norm_pp"][:, l * 4:(l + 1) * 4], [], [onw])
            qn = self.sb(ph, [128, 3, S], BF16, "qn")
            kvn = self.sb(ph, [128, 2, S], BF16, "kvn")
            sq = self.sb(ph, [128, 4, 512], BF16, "sq")
            rstd = self.sb(ph, [128, 512], F32, "rstd")
            for tb in range(4):
                c0 = tb * 512
                self.rms_rstd(ph, qc, 3, 128, (c0, c0 + 512), 384.0, 1e-6, self.banks[0], sq, rstd)
                for c in range(3):
                    self.stt(qn.t[:, c, c0:c0 + 512], qc.t[:, c, c0:c0 + 512], qnw.t[:, c:c + 1], rstd.t[:, :], ALU.mult, ALU.mult, [qc, qnw, rstd], [qn])
                self.rms_rstd(ph, kvc, 2, 128, (c0, c0 + 512), 256.0, 1e-6, self.banks[1], sq, rstd)
                for c in range(2):
                    self.stt(kvn.t[:, c, c0:c0 + 512], kvc.t[:, c, c0:c0 + 512], kvnw.t[:, c:c + 1], rstd.t[:, :], ALU.mult, ALU.mult, [kvc, kvnw, rstd], [kvn])
            qhn = self.sb(ph, [128, 4, S], BF16, "qhn")
            qhp = self.sb(ph, [64, 4, S], BF16, "qhp")
            khn = self.sb(ph, [128, 4, S], BF16, "khn")
            Vt = self.sb(ph, [128, 16, 512], BF16, "Vt")
            t1 = self.sb(ph, [64, 512], F32, "t1")
            t2 = self.sb(ph, [64, 512], F32, "t2")
            bi = 0
            for tb in range(4):
                c0 = tb * 512
                for h in range(4):
                    bk = self.banks[bi % 4]; bi += 1
                    for c in range(3):
                        self.mm(bk, bk.t[:, :], wq.t[:, c, 192 * h:192 * h + 128], qn.t[:, c, c0:c0 + 512], c == 0, c == 2, [wq, qn])
                    self.copy(qhn.t[:, h, c0:c0 + 512], bk.t[:, :], [bk], [qhn])
                    bA = self.banks[bi % 4]; bi += 1
                    bB = self.banks[bi % 4]; bi += 1
                    for c in range(3):
                        self.mm(bA, bA.t[0:64, :], wq.t[:, c, 192 * h + 128:192 * h + 192], qn.t[:, c, c0:c0 + 512], c == 0, c == 2, [wq, qn])
                    for c in range(3):
                        self.mm(bB, bB.t[0:64, :], wqr.t[:, c, h, :], qn.t[:, c, c0:c0 + 512], c == 0, c == 2, [wqr, qn])
                    self.tt(t1.t[:, :], bA.t[0:64, :], cos.t[:, c0:c0 + 512], ALU.mult, [bA, cos], [t1])
                    self.tt(t2.t[:, :], bB.t[0:64, :], sin.t[:, c0:c0 + 512], ALU.mult, [bB, sin], [t2])
                    self.tt(qhp.t[:, h, c0:c0 + 512], t1.t[:, :], t2.t[:, :], ALU.add, [t1, t2], [qhp])
                    bk = self.banks[bi % 4]; bi += 1
                    for c in range(2):
                        self.mm(bk, bk.t[:, :], wkv.t[:, c, 256 * h:256 * h + 128], kvn.t[:, c, c0:c0 + 512], c == 0, c == 1, [wkv, kvn])
                    self.copy(khn.t[:, h, c0:c0 + 512], bk.t[:, :], [bk], [khn])
            for tc in range(16):
                bk = self.banks[bi % 4]; bi += 1
                for h in range(4):
                    for c in range(2):
                        self.mm(bk, bk.t[:, h * 128:(h + 1) * 128], kvn.t[:, c, tc * 128:(tc + 1) * 128], wkv.t[:, c, 256 * h + 128:256 * h + 256], c == 0, c == 1, [wkv, kvn])
                self.copy(Vt.t[:, tc, :], bk.t[:, :], [bk], [Vt])
            Pb = [self.sb(ph, [128, 512], BF16, "P") for _ in range(3)]
            oblk = self.sb(ph, [128, 4, 512], F32, "oblk")
            den = self.sb(ph, [128, 512], F32, "den")
            ob = [self.sb(ph, [128, 512], BF16, "ob") for _ in range(2)]
            pi = 0
            for qb in range(4):
                q0 = qb * 512
                for h in range(4):
                    ob_k, dn_k = self.banks[4 + 2 * (h % 2)], self.banks[5 + 2 * (h % 2)]
                    for kc in range(16):
                        k0 = kc * 128
                        sbk = self.banks[kc % 3]
                        self.mm(sbk, sbk.t[:, :], khn.t[:, h, k0:k0 + 128], qhn.t[:, h, q0:q0 + 512], True, False, [khn, qhn])
                        self.mm(sbk, sbk.t[:, :], kpe.t[0:64, k0:k0 + 128], qhp.t[0:64, h, q0:q0 + 512], False, True, [kpe, qhp])
                        pb = Pb[pi % 3]; pi += 1
                        self.act(pb.t[:, :], sbk.t[:, :], AF.Exp, [sbk], [pb], scale=SC)
                        self.mm(ob_k, ob_k.t[:, :], Vt.t[:, kc, h * 128:(h + 1) * 128], pb.t[:, :], kc == 0, kc == 15, [Vt, pb])
                        self.mm(dn_k, dn_k.t[:, :], self.ones_bf.t[:, :], pb.t[:, :], kc == 0, kc == 15, [self.ones_bf, pb])
                    self.recip(den.t[:, :], dn_k.t[:, :], [dn_k], [den])
                    self.tt(oblk.t[:, h, :], ob_k.t[:, :], den.t[:, :], ALU.mult, [ob_k, den], [oblk])
                for h in range(4):
                    self.act(sq.t[:, h, :], oblk.t[:, h, :], AF.Square, [oblk], [sq])
                nb = self.banks[3]
                for h in range(4):
                    self.mm(nb, nb.t[:, :], self.ones_bf.t[:, :], sq.t[:, h, :], h == 0, h == 3, [self.ones_bf, sq])
                self.act(rstd.t[:, :], nb.t[:, :], AF.Sqrt, [nb], [rstd], bias=1e-6, scale=1.0 / 512)
                self.recip(rstd.t[:, :], rstd.t[:, :], [rstd], [rstd])
                for h in range(4):
                    o = ob[h % 2]
                    self.stt(o.t[:, :], oblk.t[:, h, :], onw.t[:, h:h + 1], rstd.t[:, :], ALU.mult, ALU.mult, [oblk, onw, rstd], [o])
                    self.dma("pool", mixedT.t[h * 128:(h + 1) * 128, t0 + q0:t0 + q0 + 512], o.t[:, :], [o], [mixedT])


def _pp(v, nl):
    v = np.asarray(v, np.float32)
    n = v.shape[-1]
    return np.ascontiguousarray(v.reshape(nl, n // 128, 128).transpose(2, 0, 1).reshape(128, nl * (n // 128)))


def host_consts():
    c = {}
    half = 32
    inv = (10000.0 ** (-np.arange(half, dtype=np.float32) * 2.0 / 64)).astype(np.float32)
    ang = np.arange(S, dtype=np.float32)[None, :] * inv[:, None]
    c["rope_cos"] = np.ascontiguousarray(np.tile(np.cos(ang), (4, 1)).astype(np.float32))
    c["rope_sin"] = np.ascontiguousarray(np.tile(np.sin(ang), (4, 1)).astype(np.float32))
    t = np.linspace(0.0, 1.0, S, dtype=np.float32)[:, None]
    bands = 16
    angp = (2.0 * math.pi * np.arange(S, dtype=np.float32)[:, None] / S).astype(np.float32)
    f = np.linspace(1e-4, bands - 1, bands, dtype=np.float32)[None, :]
    z = np.concatenate([t, np.cos(f * angp), -np.sin(f * angp)], axis=-1).astype(np.float32)
    c["hy_zT"] = np.ascontiguousarray(z.T)
    c["hy_ntlin_pp"] = np.ascontiguousarray((-t[:, 0]).reshape(16, 128).T.astype(np.float32))
    mn, mx = math.log(1e-2) / 1.5, math.log(1e-2) / 0.3
    dl = np.abs(np.linspace(mn, mx, 512, dtype=np.float32))
    c["hy_delta_b"] = np.ascontiguousarray(np.tile(dl[None, :], (128, 1)).astype(np.float32))
    idx = np.arange(NFP, dtype=np.int64)
    ph_ = (np.outer(idx, idx) % 4096).astype(np.float64) * (2.0 * math.pi / 4096.0)
    valid = (idx <= 2048).astype(np.float64)
    Cm = np.cos(ph_) * valid[:, None] * valid[None, :]
    Sm = -np.sin(ph_) * valid[:, None] * valid[None, :]
    bf = ml_dtypes.bfloat16
    c["dft_Cnat"] = np.ascontiguousarray(Cm[:, :S].astype(np.float32).astype(bf))
    c["dft_Snat"] = np.ascontiguousarray(Sm[:, :S].astype(np.float32).astype(bf))
    c["dft_Cblk"] = np.ascontiguousarray(Cm[:S, :].reshape(16, 128, NF, 128).transpose(2, 1, 0, 3).astype(np.float32).astype(bf))
    c["dft_Sblk"] = np.ascontiguousarray(Sm[:S, :].reshape(16, 128, NF, 128).transpose(2, 1, 0, 3).astype(np.float32).astype(bf))
    wfv = np.where(idx <= 2048, 2.0, 0.0)
    wfv[0] = 1.0
    wfv[2048] = 1.0
    c["dft_wf_pp"] = np.ascontiguousarray((wfv / 4096.0).reshape(NF, 128).T.astype(np.float32))
    return c


def host_params(inp):
    p = {}
    p["mla_w_uq"] = np.ascontiguousarray(inp["mla_w_uq"], dtype=np.float32)
    p["mla_w_ukv"] = np.ascontiguousarray(inp["mla_w_ukv"], dtype=np.float32)
    p["mla_q_norm_pp"] = _pp(inp["mla_q_norm"], L)
    p["mla_kv_norm_pp"] = _pp(inp["mla_kv_norm"], L)
    p["mla_out_norm_pp"] = _pp(inp["mla_out_norm"], L)
    cwv = np.asarray(inp["ssd_conv_w"], np.float32)
    p["ssd_conv_w_pp"] = np.ascontiguousarray(cwv.reshape(L, 5, 8, 128).transpose(3, 0, 2, 1).reshape(128, L * 40))
    p["ssd_conv_b_pp"] = _pp(inp["ssd_conv_b"], L)
    p["ssd_dtb"] = np.ascontiguousarray(np.asarray(inp["ssd_dt_bias"], np.float32).reshape(L, 16).T)
    p["ssd_alog"] = np.ascontiguousarray(np.asarray(inp["ssd_a_log"], np.float32).reshape(L, 16).T)
    dd = np.asarray(inp["ssd_d"], np.float32)
    p["ssd_d_pp"] = np.ascontiguousarray(np.repeat(dd, 64, axis=1).reshape(L, 4, 128).transpose(2, 0, 1).reshape(128, L * 4))
    p["ssd_norm_pp"] = _pp(inp["ssd_norm"], L)
    hw = np.asarray(inp["hy_conv_w"], np.float32)
    p["hy_conv_w_pp"] = np.ascontiguousarray(hw.reshape(L, 3, 12, 128).transpose(3, 0, 2, 1).reshape(128, L * 36))
    p["hy_conv_b_pp"] = _pp(inp["hy_conv_b"], L)
    p["hy_w1"] = np.ascontiguousarray(inp["hy_w1"], dtype=np.float32)
    p["hy_w2"] = np.ascontiguousarray(inp["hy_w2"], dtype=np.float32)
    p["hy_w3"] = np.ascontiguousarray(inp["hy_w3"], dtype=np.float32)
    b12 = np.stack([np.asarray(inp["hy_b1"], np.float32), np.asarray(inp["hy_b2"], np.float32)], 1)
    p["hy_b_pp"] = np.ascontiguousarray(b12.transpose(2, 0, 1).reshape(64, L * 2))
    p["hy_freq_pp"] = np.ascontiguousarray(np.asarray(inp["hy_freq"], np.float32).transpose(2, 0, 1).reshape(64, L * 2))
    hbv = np.asarray(inp["hy_bias"], np.float32)
    p["hy_bias_pp"] = np.ascontiguousarray(hbv.reshape(L, 2, 4, 128).transpose(3, 0, 1, 2).reshape(128, L * 8))
    p["hy_out_norm_pp"] = _pp(inp["hy_out_norm"], L)
    p["dirsign"] = np.concatenate([-np.ones((8, 1), np.float32), np.ones((8, 1), np.float32)], 0)
    p["ndirmask"] = np.concatenate([np.zeros((8, 1), np.float32), -np.ones((8, 1), np.float32)], 0)
    return p


def build(cfg, shapes):
    nc = bass.Bass("TRN2", target_bir_lowering=False)
    ext = {}
    for name, (shape, dt) in shapes.items():
        ext[name] = nc.dram_tensor(name, list(shape), dt, kind="ExternalInput").ap()
    with ExitStack() as st:
        kb = KB(nc, st)
        kb.setup_consts()
        winb = kb.dram("winb", [L, D, INC], BF16)
        wrot = kb.dram("wrot", [L, D, 576], BF16)
        seg = {"qc": kb.dram("s_qc", [384, T], BF16), "kvc": kb.dram("s_kvc", [256, T], BF16),
               "kpe": kb.dram("s_kpe", [64, T], BF16), "rq": kb.dram("s_rq", [256, T], BF16),
               "rk": kb.dram("s_rk", [256, T], BF16), "rg": kb.dram("s_rg", [512, T], BF16),
               "mz": kb.dram("s_mz", [512, T], BF16), "xbc": kb.dram("s_xbc", [1024, T], BF16),
               "dt": kb.dram("s_dt", [16, T], F32), "hu": kb.dram("s_hu", [1536, T], BF16),
               "rv": kb.dram("s_rv", [T, 512], BF16)}
        xin = Tl(ext["x"], "x")
        if cfg.get("dbg"):
            kb.dbg = {"BC": Tl(nc.dram_tensor("d_BC", [16, S], F32, kind="ExternalOutput").ap()),
                      "dt_tok": Tl(nc.dram_tensor("d_dt_tok", [128, 256], F32, kind="ExternalOutput").ap()),
                      "bias_tok": Tl(nc.dram_tensor("d_bias_tok", [128, 256], F32, kind="ExternalOutput").ap()),
                      "xsT": Tl(nc.dram_tensor("d_xsT", [128, S], F32, kind="ExternalOutput").ap()),
                      "xdt": Tl(nc.dram_tensor("d_xdt", [128, 1024], BF16, kind="ExternalOutput").ap())}
        nseq = cfg.get("nseq", NSEQ)
        if cfg["mode"] == "mixtest":
            l = cfg["layer"]
            mixedT = Tl(nc.dram_tensor("mixedT", [D, T], BF16, kind="ExternalOutput").ap(), "mixedT")
            kb.cast_dram(Tl(winb.t[l], "x").__class__(winb.t[l]) if False else _sub(winb, winb.t[l]), ext["w_in"][l], D)
            kb.build_rot(l, ext["w_in"], wrot)
            kb.inproj(l, xin, winb, wrot, seg, ext, (0, nseq * S))
            for s in range(nseq):
                if "A" in cfg["groups"]:
                    kb.group_mla(l, s, seg, mixedT, ext, ext)
                if "B" in cfg["groups"]:
                    kb.group_ret(s, seg, mixedT)
                if "C" in cfg["groups"]:
                    kb.group_ssd(l, s, seg, mixedT, ext)
            if "D" in cfg["groups"]:
                Hs = kb.dram("Hs", [2, 2, NFP, 512], F32)
                hyu = kb.dram("hyu", [3, NSEQ * 512, S], F32)
                z1s = kb.dram("z1s", [NSEQ * 512, S], F32)
                kb.hyena_filter(l, ext, ext, Hs)
                kb.group_hyena(l, nseq, seg, mixedT, ext, ext, Hs, hyu, z1s)
        kb.P.finish()
        print("instructions:", kb.P.n_inst, "sems:", kb.P.nsem)
    return nc


def _sub(parent, ap):
    t = Tl(ap, parent.b.name)
    t.b = parent.b
    return t


def _group_ssd(self, l, s, seg, mixedT, prm):
    nc = self.nc
    t0 = s * S
    with Phase(self) as ph:
        xsT = self.sb(ph, [128, 4, S], F32, "xsT")
        BT = self.sb(ph, [128, 2, S], BF16, "BT")
        CT = self.sb(ph, [128, 2, S], BF16, "CT")
        mz = self.sb(ph, [128, 4, S], BF16, "mz")
        self.dma("sp", mz.t[:], seg["mz"].t[:, t0:t0 + S].rearrange("(c p) t -> p c t", p=128), [seg["mz"]], [mz])
        cw = self.sb(ph, [128, 40], F32, "cw")
        cb = self.sb(ph, [128, 8], F32, "cb")
        dpp = self.sb(ph, [128, 4], F32, "dpp")
        nw = self.sb(ph, [128, 4], F32, "nw")
        self.dma("sp", cw.t[:], prm["ssd_conv_w_pp"][:, l * 40:(l + 1) * 40], [], [cw])
        self.dma("sp", cb.t[:], prm["ssd_conv_b_pp"][:, l * 8:(l + 1) * 8], [], [cb])
        self.dma("sp", dpp.t[:], prm["ssd_d_pp"][:, l * 4:(l + 1) * 4], [], [dpp])
        self.dma("sp", nw.t[:], prm["ssd_norm_pp"][:, l * 4:(l + 1) * 4], [], [nw])
        dt_tok = self.sb(ph, [128, 16, 16], F32, "dt_tok")
        bias_tok = self.sb(ph, [128, 16, 16], F32, "bias_tok")
        BC = self.sb(ph, [16, S], F32, "BC")
        xdt = [self.sb(ph, [128, 16, 8, 128], BF16, "xdt%d" % d) for d in range(2)]
        for d in range(2):
            self.memset(xdt[d].t[:], 0.0, [xdt[d]])
        with Phase(self) as p2:
            raw = self.sb(p2, [128, 8, S], BF16, "raw")
            acc = self.sb(p2, [128, S], F32, "acc")
            self.dma("sp", raw.t[:], seg["xbc"].t[:, t0:t0 + S].rearrange("(c p) t -> p c t", p=128), [seg["xbc"]], [raw])
            for c in range(8):
                self.ts(acc.t[:, :], raw.t[:, c, :], cw.t[:, c * 5 + 2:c * 5 + 3], None, ALU.mult, None, [raw, cw], [acc])
                for k in (0, 1, 3, 4):
                    sh = k - 2
                    a0, a1 = max(0, -sh), S - max(0, sh)
                    self.stt(acc.t[:, a0:a1], raw.t[:, c, a0 + sh:a1 + sh], cw.t[:, c * 5 + k:c * 5 + k + 1], acc.t[:, a0:a1], ALU.mult, ALU.add, [raw, cw, acc], [acc])
                if c < 4:
                    dst, dtl = xsT.t[:, c, :], xsT
                elif c < 6:
                    dst, dtl = BT.t[:, c - 4, :], BT
                else:
                    dst, dtl = CT.t[:, c - 6, :], CT
                self.act(dst, acc.t[:, :], AF.Silu, [acc, cb], [dtl], bias=cb.t[:, c:c + 1])
        with Phase(self) as p2:
            dtr = self.sb(p2, [16, S], F32, "dtr")
            ax = self.sb(p2, [16, S], F32, "ax")
            dtv = self.sb(p2, [16, S], F32, "dtv")
            la = self.sb(p2, [16, S], F32, "la")
            cs = self.sb(p2, [16, S], F32, "cs")
            one16 = self.sb(p2, [16, S], F32, "one16")
            sm = self.sb(p2, [16, 8], F32, "sm")
            self.dma("sp", dtr.t[:], seg["dt"].t[:, t0:t0 + S], [seg["dt"]], [dtr])
            self.dma("sp", sm.t[:, 0:1], prm["ssd_dtb"][:, l:l + 1], [], [sm], allow_slow_non_contiguous=True)
            self.dma("sp", sm.t[:, 1:2], prm["ssd_alog"][:, l:l + 1], [], [sm], allow_slow_non_contiguous=True)
            self.dma("sp", sm.t[:, 2:3], prm["dirsign"][:, 0:1], [], [sm], allow_slow_non_contiguous=True)
            self.dma("sp", sm.t[:, 3:4], prm["ndirmask"][:, 0:1], [], [sm], allow_slow_non_contiguous=True)
            self.ts(dtr.t[:], dtr.t[:], sm.t[:, 0:1], None, ALU.add, None, [dtr, sm], [dtr])
            self.stt(ax.t[:], dtr.t[:], -1.0, dtr.t[:], ALU.mult, ALU.max, [dtr], [ax])
            self.act(ax.t[:], ax.t[:], AF.Exp, [ax], [ax], scale=-1.0)
            self.act(ax.t[:], ax.t[:], AF.Ln, [ax], [ax], bias=1.0)
            self.stt(dtv.t[:], dtr.t[:], 0.0, ax.t[:], ALU.max, ALU.add, [dtr, ax], [dtv])
            self.act(sm.t[:, 4:5], sm.t[:, 1:2], AF.Exp, [sm], [sm])
            self.ts(sm.t[:, 5:6], sm.t[:, 4:5], -1.0, None, ALU.mult, None, [sm], [sm])
            self.ts(la.t[:], dtv.t[:], sm.t[:, 5:6], None, ALU.mult, None, [dtv, sm], [la])
            self.memset(one16.t[:], 1.0, [one16])
            self.P.op("dve", lambda: nc.vector.tensor_tensor_scan(out=cs.t[:], data0=one16.t[:], data1=la.t[:], initial=0.0, op0=ALU.mult, op1=ALU.add), self._b([one16, la]), self._b([cs]))
            self.stt(BC.t[:], la.t[:], sm.t[:, 3:4], cs.t[:], ALU.mult, ALU.add, [la, sm, cs], [BC])
            self.ts(cs.t[:], BC.t[:], sm.t[:, 2:3], None, ALU.mult, None, [BC, sm], [cs])
            b6, b7 = self.banks[6], self.banks[7]
            for tc in range(16):
                self.tr(b6, b6.t[:, tc * 16:(tc + 1) * 16], dtv.t[0:16, tc * 128:(tc + 1) * 128], [dtv])
                self.tr(b7, b7.t[:, tc * 16:(tc + 1) * 16], cs.t[0:16, tc * 128:(tc + 1) * 128], [cs])
            self.copy(dt_tok.t[:], b6.t[:, 0:256].rearrange("p (a b) -> p a b", a=16), [b6], [dt_tok])
            self.copy(bias_tok.t[:], b7.t[:, 0:256].rearrange("p (a b) -> p a b", a=16), [b7], [bias_tok])
            for tc in range(16):
                bk = self.banks[4 + tc % 2]
                for c in range(4):
                    self.tr(bk, bk.t[:, c * 128:(c + 1) * 128], xsT.t[:, c, tc * 128:(tc + 1) * 128], [xsT])
                for d in range(2):
                    for h in range(8):
                        self.ts(xdt[d].t[:, tc, h, 64 * (h % 2):64 * (h % 2) + 64], bk.t[:, h * 64:(h + 1) * 64], dt_tok.t[:, tc, d * 8 + h:d * 8 + h + 1], None, ALU.mult, None, [bk, dt_tok], [xdt[d]])
        if getattr(self, "dbg", None) is not None:
            self.dma("pool", self.dbg["BC"].t[:, :], BC.t[:, :], [BC], [self.dbg["BC"]])
            self.dma("pool", self.dbg["dt_tok"].t[:, :], dt_tok.t[:].rearrange("p a b -> p (a b)"), [dt_tok], [self.dbg["dt_tok"]])
            self.dma("pool", self.dbg["bias_tok"].t[:, :], bias_tok.t[:].rearrange("p a b -> p (a b)"), [bias_tok], [self.dbg["bias_tok"]])
            self.dma("pool", self.dbg["xsT"].t[:, :], xsT.t[:, 0, :], [xsT], [self.dbg["xsT"]])
            self.dma("pool", self.dbg["xdt"].t[:, :], xdt[0].t[:, 0, :, :].rearrange("p a b -> p (a b)"), [xdt[0]], [self.dbg["xdt"]])
        Mf = self.sb(ph, [128, 896], BF16, "Mf")
        Mb = self.sb(ph, [128, 896], BF16, "Mb")
        self.memset(Mf.t[:], 1.0, [Mf])
        self.memset(Mb.t[:], 1.0, [Mb])
        self.P.op("pool", lambda: nc.gpsimd.affine_select(out=Mf.t[:], in_=Mf.t[:], pattern=[[1, 896]], compare_op=ALU.is_ge, fill=0.0, base=-384, channel_multiplier=-1), self._b([Mf]), self._b([Mf]))
        self.P.op("pool", lambda: nc.gpsimd.affine_select(out=Mb.t[:], in_=Mb.t[:], pattern=[[-1, 896]], compare_op=ALU.is_gt, fill=0.0, base=384, channel_multiplier=1), self._b([Mb]), self._b([Mb]))
        bcs = [[self.sb(ph, [128, 512], F32, "bcs") for _ in range(2)] for _ in range(2)]
        Lb = [self.sb(ph, [128, 512], F32, "L") for _ in range(3)]
        Pb = [self.sb(ph, [128, 512], BF16, "P") for _ in range(3)]
        ybuf = self.sb(ph, [128, 4, 512], F32, "ybuf")
        yv = self.sb(ph, [128, 512], F32, "yv")
        gate = self.sb(ph, [128, 512], F32, "gate")
        sq = self.sb(ph, [128, 4, 512], BF16, "sq")
        rstd = self.sb(ph, [128, 512], F32, "rstd")
        ob = [self.sb(ph, [128, 512], BF16, "ob") for _ in range(2)]
        li = 0
        for ib in range(4):
            i0 = ib * 512
            for pair in range(4):
                g = pair // 2
                yb = self.banks[4 + pair % 2]
                for d in range(2):
                    for hh in range(2):
                        h = pair * 2 + hh
                        bb = self.banks[3]
                        self.mm(bb, bb.t[:, :], self.sel.t[:, d * 8 + h, :], BC.t[0:16, i0:i0 + 512], True, True, [self.sel, BC])
                        self.copy(bcs[d][hh].t[:, :], bb.t[:, :], [bb], [bcs[d][hh]])
                items = []
                for jc in range(16):
                    for d in range(2):
                        valid = (jc <= 4 * ib + 3) if d == 0 else (jc >= 4 * ib)
                        if valid:
                            for hh in range(2):
                                items.append((jc, d, hh))
                last_jc = -1
                for n, (jc, d, hh) in enumerate(items):
                    j0 = jc * 128
                    h = pair * 2 + hh
                    if jc != last_jc:
                        sbk = self.banks[jc % 3]
                        self.mm(sbk, sbk.t[:, :], BT.t[:, g, j0:j0 + 128], CT.t[:, g, i0:i0 + 512], True, True, [BT, CT])
                        last_jc = jc
                    diag = 4 * ib <= jc <= 4 * ib + 3
                    Lt = Lb[li % 3]
                    pb = Pb[li % 3]
                    li += 1
                    sgn = 1.0 if d == 0 else -1.0
                    bia = bias_tok.t[:, jc, d * 8 + h:d * 8 + h + 1]
                    if diag:
                        self.ts(Lt.t[:, :], bcs[d][hh].t[:, :], sgn, bia, ALU.mult, ALU.add, [bcs[d][hh], bias_tok], [Lt])
                        self.ts(Lt.t[:, :], Lt.t[:, :], 0.0, None, ALU.min, None, [Lt], [Lt])
                        self.act(Lt.t[:, :], Lt.t[:, :], AF.Exp, [Lt], [Lt])
                    else:
                        self.act(Lt.t[:, :], bcs[d][hh].t[:, :], AF.Exp, [bcs[d][hh], bias_tok], [Lt], bias=bia, scale=sgn)
                    self.tt(pb.t[:, :], sbk.t[:, :], Lt.t[:, :], ALU.mult, [sbk, Lt], [pb])
                    if diag:
                        m = jc - 4 * ib
                        M = Mf if d == 0 else Mb
                        self.tt(pb.t[:, :], pb.t[:, :], M.t[:, 384 - 128 * m:384 - 128 * m + 512], ALU.mult, [pb, M], [pb])
                    self.mm(yb, yb.t[:, :], xdt[d].t[:, jc, h, :], pb.t[:, :], n == 0, n == len(items) - 1, [xdt[d], pb])
                self.stt(yv.t[:, :], xsT.t[:, pair, i0:i0 + 512], dpp.t[:, pair:pair + 1], yb.t[:, :], ALU.mult, ALU.add, [xsT, dpp, yb], [yv])
                self.act(gate.t[:, :], mz.t[:, pair, i0:i0 + 512], AF.Silu, [mz], [gate])
                self.tt(ybuf.t[:, pair, :], yv.t[:, :], gate.t[:, :], ALU.mult, [yv, gate], [ybuf])
            for c in range(4):
                self.act(sq.t[:, c, :], ybuf.t[:, c, :], AF.Square, [ybuf], [sq])
            nb = self.banks[6]
            for c in range(4):
                self.mm(nb, nb.t[:, :], self.ones_bf.t[:, :], sq.t[:, c, :], c == 0, c == 3, [self.ones_bf, sq])
            self.act(rstd.t[:, :], nb.t[:, :], AF.Sqrt, [nb], [rstd], bias=1e-6, scale=1.0 / 512)
            self.recip(rstd.t[:, :], rstd.t[:, :], [rstd], [rstd])
            for c in range(4):
                o = ob[c % 2]
                self.stt(o.t[:, :], ybuf.t[:, c, :], nw.t[:, c:c + 1], rstd.t[:, :], ALU.mult, ALU.mult, [ybuf, nw, rstd], [o])
                self.dma("pool", mixedT.t[1024 + c * 128:1024 + (c + 1) * 128, t0 + i0:t0 + i0 + 512], o.t[:, :], [o], [mixedT])


KB.group_ssd = _group_ssd


TWO_PI = 2.0 * math.pi
MAGIC = 12582912.0


def _sin_rr(self, out, x, tmp, reads_x, writes_out, x_tl, tmp_tl):
    self.ts(tmp, x, 1.0 / TWO_PI, MAGIC, ALU.mult, ALU.add, [x_tl], [tmp_tl])
    self.ts(tmp, tmp, MAGIC, -TWO_PI, ALU.subtract, ALU.mult, [tmp_tl], [tmp_tl])
    self.tt(x, x, tmp, ALU.add, [x_tl, tmp_tl], [x_tl])
    self.ts(x, x, 3.1415925, -3.1415925, ALU.min, ALU.max, [x_tl], [x_tl])
    self.act(out, x, AF.Sin, [x_tl], writes_out)


def _hyena_filter(self, l, prm, cst, Hs):
    with Phase(self) as ph:
        zT = self.sb(ph, [33, S], F32, "zT")
        w1 = self.sb(ph, [33, 64], F32, "w1")
        w2 = self.sb(ph, [64, 64], F32, "w2")
        w3 = self.sb(ph, [64, 2048], F32, "w3")
        sm = self.sb(ph, [64, 4], F32, "sm")
        self.dma("sp", zT.t[:], cst["hy_zT"], [], [zT])
        self.dma("sp", w1.t[:], prm["hy_w1"][l], [], [w1])
        self.dma("sp", w2.t[:], prm["hy_w2"][l], [], [w2])
        self.dma("sp", w3.t[:], prm["hy_w3"][l], [], [w3])
        self.dma("sp", sm.t[:, 0:2], prm["hy_b_pp"][:, l * 2:l * 2 + 2], [], [sm])
        self.dma("sp", sm.t[:, 2:4], prm["hy_freq_pp"][:, l * 2:l * 2 + 2], [], [sm])
        ntl = self.sb(ph, [128, 16], F32, "ntl")
        dlb = self.sb(ph, [128, 512], F32, "dlb")
        wf = self.sb(ph, [128, NF], F32, "wf")
        self.dma("sp", ntl.t[:], cst["hy_ntlin_pp"], [], [ntl])
        self.dma("sp", dlb.t[:], cst["hy_delta_b"], [], [dlb])
        self.dma("sp", wf.t[:], cst["dft_wf_pp"], [], [wf])
        hid1 = self.sb(ph, [64, S], F32, "hid1")
        hid2 = self.sb(ph, [64, S], F32, "hid2")
        xa = self.sb(ph, [64, 512], F32, "xa")
        xb = self.sb(ph, [64, 512], F32, "xb")
        for tb in range(4):
            bk = self.banks[tb % 2]
            self.mm(bk, bk.t[0:64, :], w1.t[0:33, :], zT.t[0:33, tb * 512:(tb + 1) * 512], True, True, [w1, zT])
            self.ts(xa.t[:, :], bk.t[0:64, :], sm.t[:, 0:1], sm.t[:, 2:3], ALU.add, ALU.mult, [bk, sm], [xa])
            _sin_rr(self, hid1.t[:, tb * 512:(tb + 1) * 512], xa.t[:, :], xb.t[:, :], None, [hid1], xa, xb)
        for tb in range(4):
            bk = self.banks[tb % 2]
            self.mm(bk, bk.t[0:64, :], w2.t[0:64, :], hid1.t[0:64, tb * 512:(tb + 1) * 512], True, True, [w2, hid1])
            self.ts(xa.t[:, :], bk.t[0:64, :], sm.t[:, 1:2], sm.t[:, 3:4], ALU.add, ALU.mult, [bk, sm], [xa])
            _sin_rr(self, hid2.t[:, tb * 512:(tb + 1) * 512], xa.t[:, :], xb.t[:, :], None, [hid2], xa, xb)
        hsum = [self.sb(ph, [128, 16, 512], BF16, "hsum") for _ in range(2)]
        hdif = [self.sb(ph, [128, 16, 512], BF16, "hdif") for _ in range(2)]
        dec = self.sb(ph, [128, 512], F32, "dec")
        hb = self.sb(ph, [128, 512], F32, "hb")
        t1 = self.sb(ph, [128, 512], F32, "t1")
        for tc in range(16):
            self.act(dec.t[:, :], dlb.t[:, :], AF.Exp, [dlb, ntl], [dec], scale=ntl.t[:, tc:tc + 1])
            for o in range(2):
                bf_, bb_ = self.banks[2 * o], self.banks[2 * o + 1]
                self.mm(bf_, bf_.t[:, :], hid2.t[0:64, tc * 128:(tc + 1) * 128], w3.t[0:64, (2 * o) * 512:(2 * o + 1) * 512], True, True, [hid2, w3])
                self.mm(bb_, bb_.t[:, :], hid2.t[0:64, tc * 128:(tc + 1) * 128], w3.t[0:64, (2 * o + 1) * 512:(2 * o + 2) * 512], True, True, [hid2, w3])
                self.copy(hb.t[:, :], bb_.t[:, :], [bb_], [hb], eng="act")
                if tc == 0:
                    self.memset(hb.t[0:1, :], 0.0, [hb])
                self.tt(t1.t[:, :], bf_.t[:, :], hb.t[:, :], ALU.add, [bf_, hb], [t1])
                self.tt(hsum[o].t[:, tc, :], t1.t[:, :], dec.t[:, :], ALU.mult, [t1, dec], [hsum[o]])
                self.tt(t1.t[:, :], bf_.t[:, :], hb.t[:, :], ALU.subtract, [bf_, hb], [t1])
                self.tt(hdif[o].t[:, tc, :], t1.t[:, :], dec.t[:, :], ALU.mult, [t1, dec], [hdif[o]])
        Cb = [self.sb(ph, [128, 16, 128], BF16, "Cb") for _ in range(2)]
        Sb = [self.sb(ph, [128, 16, 128], BF16, "Sb") for _ in range(2)]
        ho = [self.sb(ph, [128, 512], F32, "ho") for _ in range(2)]
        n = 0
        for fc in range(NF):
            cb_, sb_ = Cb[fc % 2], Sb[fc % 2]
            self.dma("sp", cb_.t[:], cst["dft_Cblk"][fc], [], [cb_])
            self.dma("sp", sb_.t[:], cst["dft_Sblk"][fc], [], [sb_])
            for o in range(2):
                for ri, (tab, src) in enumerate(((cb_, hsum[o]), (sb_, hdif[o]))):
                    bk = self.banks[4 + n % 4]
                    for tc in range(16):
                        self.mm(bk, bk.t[:, :], tab.t[:, tc, :], src.t[:, tc, :], tc == 0, tc == 15, [tab, src])
                    h_ = ho[n % 2]
                    n += 1
                    self.ts(h_.t[:, :], bk.t[:, :], wf.t[:, fc:fc + 1], None, ALU.mult, None, [bk, wf], [h_])
                    self.dma("pool", Hs.t[o, ri, fc * 128:(fc + 1) * 128, :], h_.t[:, :], [h_], [Hs])


def _group_hyena(self, l, nseq, seg, mixedT, prm, cst, Hs, hyu, z1s):
    NCOL = nseq * 512
    with Phase(self) as ph:
        U = self.sb(ph, [128, 16, NCOL], BF16, "U")
        Yre = self.sb(ph, [128, NF, NCOL], BF16, "Yre")
        Yim = self.sb(ph, [128, NF, NCOL], BF16, "Yim")
        cw = self.sb(ph, [128, 36], F32, "cw")
        cb = self.sb(ph, [128, 12], F32, "cb")
        hbias = self.sb(ph, [128, 8], F32, "hbias")
        nw = self.sb(ph, [128, 4], F32, "nw")
        self.dma("sp", cw.t[:], prm["hy_conv_w_pp"][:, l * 36:(l + 1) * 36], [], [cw])
        self.dma("sp", cb.t[:], prm["hy_conv_b_pp"][:, l * 12:(l + 1) * 12], [], [cb])
        self.dma("sp", hbias.t[:], prm["hy_bias_pp"][:, l * 8:(l + 1) * 8], [], [hbias])
        self.dma("sp", nw.t[:], prm["hy_out_norm_pp"][:, l * 4:(l + 1) * 4], [], [nw])
        with Phase(self) as p2:
            raw = [self.sb(p2, [128, S], BF16, "raw") for _ in range(2)]
            acc = [self.sb(p2, [128, S], F32, "acc") for _ in range(2)]
            n = 0
            for j in range(3):
                for s in range(nseq):
                    for cc in range(4):
                        c = j * 4 + cc
                        r_, a_ = raw[n % 2], acc[n % 2]
                        n += 1
                        self.dma("sp", r_.t[:, :], seg["hu"].t[c * 128:(c + 1) * 128, s * S:(s + 1) * S], [seg["hu"]], [r_])
                        self.ts(a_.t[:, :], r_.t[:, :], cw.t[:, c * 3 + 1:c * 3 + 2], cb.t[:, c:c + 1], ALU.mult, ALU.add, [r_, cw, cb], [a_])
                        self.stt(a_.t[:, 1:S], r_.t[:, 0:S - 1], cw.t[:, c * 3:c * 3 + 1], a_.t[:, 1:S], ALU.mult, ALU.add, [r_, cw, a_], [a_])
                        self.stt(a_.t[:, 0:S - 1], r_.t[:, 1:S], cw.t[:, c * 3 + 2:c * 3 + 3], a_.t[:, 0:S - 1], ALU.mult, ALU.add, [r_, cw, a_], [a_])
                        self.dma("pool", hyu.t[j, (s * 4 + cc) * 128:(s * 4 + cc + 1) * 128, :], a_.t[:, :], [a_], [hyu])
                        if j == 0:
                            for tcg in range(4):
                                bk = self.banks[6 + tcg % 2]
                                for k in range(4):
                                    tc = tcg * 4 + k
                                    self.tr(bk, bk.t[:, k * 128:(k + 1) * 128], a_.t[:, tc * 128:(tc + 1) * 128], [a_])
                                self.copy(U.t[:, tcg * 4:tcg * 4 + 4, (s * 4 + cc) * 128:(s * 4 + cc + 1) * 128], bk.t[:].rearrange("p (a b) -> p a b", a=4), [bk], [U])
        Cb = [self.sb(ph, [128, 16, 128], BF16, "Cb") for _ in range(2)]
        Sb = [self.sb(ph, [128, 16, 128], BF16, "Sb") for _ in range(2)]
        Hre = [self.sb(ph, [128, 512], F32, "Hre") for _ in range(2)]
        Him = [self.sb(ph, [128, 512], F32, "Him") for _ in range(2)]
        Cn = self.sb(ph, [128, NF, 512], BF16, "Cn")
        Sn = self.sb(ph, [128, NF, 512], BF16, "Sn")
        ta = self.sb(ph, [128, 512], F32, "ta")
        tb_ = self.sb(ph, [128, 512], F32, "tb")
        zp = [self.sb(ph, [128, 512], F32, "zp") for _ in range(2)]
        xg = [self.sb(ph, [128, 512], F32, "xg") for _ in range(2)]
        zn = [self.sb(ph, [128, 512], F32, "zn") for _ in range(2)]
        zfin = self.sb(ph, [128, nseq * 4, 512], F32, "zfin")
        sq = self.sb(ph, [128, 4, 512], BF16, "sq")
        rstd = self.sb(ph, [128, 512], F32, "rstd")
        ob = [self.sb(ph, [128, 512], BF16, "ob") for _ in range(2)]
        for o in range(2):
            for fc in range(NF):
                cb_, sb_ = Cb[fc % 2], Sb[fc % 2]
                hr, hi = Hre[fc % 2], Him[fc % 2]
                self.dma("sp", cb_.t[:], cst["dft_Cblk"][fc], [], [cb_])
                self.dma("sp", sb_.t[:], cst["dft_Sblk"][fc], [], [sb_])
                self.dma("sp", hr.t[:, :], Hs.t[o, 0, fc * 128:(fc + 1) * 128, :], [Hs], [hr])
                self.dma("sp", hi.t[:, :], Hs.t[o, 1, fc * 128:(fc + 1) * 128, :], [Hs], [hi])
                for s in range(nseq):
                    br, bi = self.banks[2 * (s % 2)], self.banks[2 * (s % 2) + 1]
                    for tc in range(16):
                        self.mm(br, br.t[:, :], cb_.t[:, tc, :], U.t[:, tc, s * 512:(s + 1) * 512], tc == 0, tc == 15, [cb_, U])
                    for tc in range(16):
                        self.mm(bi, bi.t[:, :], sb_.t[:, tc, :], U.t[:, tc, s * 512:(s + 1) * 512], tc == 0, tc == 15, [sb_, U])
                    self.tt(ta.t[:, :], br.t[:, :], hr.t[:, :], ALU.mult, [br, hr], [ta])
                    self.tt(tb_.t[:, :], bi.t[:, :], hi.t[:, :], ALU.mult, [bi, hi], [tb_])
                    self.tt(Yre.t[:, fc, s * 512:(s + 1) * 512], ta.t[:, :], tb_.t[:, :], ALU.subtract, [ta, tb_], [Yre])
                    self.tt(ta.t[:, :], br.t[:, :], hi.t[:, :], ALU.mult, [br, hi], [ta])
                    self.tt(tb_.t[:, :], bi.t[:, :], hr.t[:, :], ALU.mult, [bi, hr], [tb_])
                    self.tt(Yim.t[:, fc, s * 512:(s + 1) * 512], ta.t[:, :], tb_.t[:, :], ALU.add, [ta, tb_], [Yim])
            for tb in range(4):
                c0 = tb * 512
                self.dma("sp", Cn.t[:], cst["dft_Cnat"][:, c0:c0 + 512].rearrange("(f p) t -> p f t", p=128), [], [Cn])
                self.dma("sp", Sn.t[:], cst["dft_Snat"][:, c0:c0 + 512].rearrange("(f p) t -> p f t", p=128), [], [Sn])
                for sc in range(nseq * 4):
                    s, cc = sc // 4, sc % 4
                    bk = self.banks[4 + sc % 2]
                    for fc in range(NF):
                        self.mm(bk, bk.t[:, :], Yre.t[:, fc, sc * 128:(sc + 1) * 128], Cn.t[:, fc, :], fc == 0, False, [Yre, Cn])
                        self.mm(bk, bk.t[:, :], Yim.t[:, fc, sc * 128:(sc + 1) * 128], Sn.t[:, fc, :], False, fc == NF - 1, [Yim, Sn])
                    z_, x_, n_ = zp[sc % 2], xg[sc % 2], zn[sc % 2]
                    zsrc = hyu.t[0] if o == 0 else z1s.t
                    zsrc_tl = hyu if o == 0 else z1s
                    self.dma("sp", z_.t[:, :], zsrc[sc * 128:(sc + 1) * 128, c0:c0 + 512], [zsrc_tl], [z_])
                    self.dma("sp", x_.t[:, :], hyu.t[o + 1, sc * 128:(sc + 1) * 128, c0:c0 + 512], [hyu], [x_])
                    self.stt(n_.t[:, :], z_.t[:, :], hbias.t[:, o * 4 + cc:o * 4 + cc + 1], bk.t[:, :], ALU.mult, ALU.add, [z_, hbias, bk], [n_])
                    if o == 0:
                        self.tt(n_.t[:, :], n_.t[:, :], x_.t[:, :], ALU.mult, [n_, x_], [n_])
                        self.dma("pool", z1s.t[sc * 128:(sc + 1) * 128, c0:c0 + 512], n_.t[:, :], [n_], [z1s])
                        b6 = self.banks[6 + sc % 2]
                        for k in range(4):
                            self.tr(b6, b6.t[:, k * 128:(k + 1) * 128], n_.t[:, k * 128:(k + 1) * 128], [n_])
                        self.copy(U.t[:, tb * 4:tb * 4 + 4, sc * 128:(sc + 1) * 128], b6.t[:].rearrange("p (a b) -> p a b", a=4), [b6], [U])
                    else:
                        self.tt(zfin.t[:, sc, :], n_.t[:, :], x_.t[:, :], ALU.mult, [n_, x_], [zfin])
                if o == 1:
                    for s in range(nseq):
                        for cc in range(4):
                            self.act(sq.t[:, cc, :], zfin.t[:, s * 4 + cc, :], AF.Square, [zfin], [sq])
                        nb = self.banks[6]
                        for cc in range(4):
                            self.mm(nb, nb.t[:, :], self.ones_bf.t[:, :], sq.t[:, cc, :], cc == 0, cc == 3, [self.ones_bf, sq])
                        self.act(rstd.t[:, :], nb.t[:, :], AF.Sqrt, [nb], [rstd], bias=1e-6, scale=1.0 / 512)
                        self.recip(rstd.t[:, :], rstd.t[:, :], [rstd], [rstd])
                        for cc in range(4):
                            o_ = ob[cc % 2]
                            self.stt(o_.t[:, :], zfin.t[:, s * 4 + cc, :], nw.t[:, cc:cc + 1], rstd.t[:, :], ALU.mult, ALU.mult, [zfin, nw, rstd], [o_])
                            self.dma("pool", mixedT.t[1536 + cc * 128:1536 + (cc + 1) * 128, s * S + c0:s * S + c0 + 512], o_.t[:, :], [o_], [mixedT])


KB.hyena_filter = _hyena_filter
KB.group_hyena = _group_hyena


def _ln_rows(self, y, gB, bB, st6, mv, eps):
    nc = self.nc
    for q in range(4):
        self.P.op("dve", lambda q=q: nc.vector.bn_stats(out=st6.t[:, q, :], in_=y.t[:, q * 512:(q + 1) * 512]), self._b([y]), self._b([st6]))
    self.P.op("dve", lambda: nc.vector.bn_aggr(out=mv.t[:, 0:2], in_=st6.t[:].rearrange("p a b -> p (a b)")), self._b([st6]), self._b([mv]))
    self.act(mv.t[:, 2:3], mv.t[:, 1:2], AF.Sqrt, [mv], [mv], bias=eps, scale=1.0)
    self.recip(mv.t[:, 3:4], mv.t[:, 2:3], [mv], [mv])
    self.ts(y.t[:, :], y.t[:, :], mv.t[:, 0:1], mv.t[:, 3:4], ALU.subtract, ALU.mult, [y, mv], [y])
    self.tt(y.t[:, :], y.t[:, :], gB.t[:, :], ALU.mult, [y, gB], [y])
    self.tt(y.t[:, :], y.t[:, :], bB.t[:, :], ALU.add, [y, bB], [y])


def _outproj_ln(self, l, mixedT, woutb, xin, xout, prm, tok_range):
    with Phase(self) as ph:
        W = self.sb(ph, [128, 16, D], BF16, "Wout")
        self.dma("sp", W.t[:], woutb.t[l].rearrange("(ec p) d -> p ec d", p=128), [woutb], [W])
        gB = self.bcast_row(ph, prm["ln1_g"][l:l + 1, :], D, "gB")
        bB = self.bcast_row(ph, prm["ln1_b"][l:l + 1, :], D, "bB")
        mT = [self.sb(ph, [128, 16, 128], BF16, "mT") for _ in range(2)]
        xt = [self.sb(ph, [128, D], F32, "xt") for _ in range(2)]
        y = [self.sb(ph, [128, D], F32, "y") for _ in range(2)]
        st6 = self.sb(ph, [128, 4, 6], F32, "st6")
        mv = self.sb(ph, [128, 4], F32, "mv")
        for i, tok0 in enumerate(range(tok_range[0], tok_range[1], 128)):
            m_, x_, y_ = mT[i % 2], xt[i % 2], y[i % 2]
            self.dma("sp", m_.t[:], mixedT.t[:, tok0:tok0 + 128].rearrange("(ec p) t -> p ec t", p=128), [mixedT], [m_])
            self.dma("sp", x_.t[:], xin.t[tok0:tok0 + 128, :], [xin], [x_])
            for q in range(4):
                bk = self.banks[(i * 4 + q) % 8]
                for ec in range(16):
                    self.mm(bk, bk.t[:, :], m_.t[:, ec, :], W.t[:, ec, q * 512:(q + 1) * 512], ec == 0, ec == 15, [m_, W])
                self.stt(y_.t[:, q * 512:(q + 1) * 512], x_.t[:, q * 512:(q + 1) * 512], ALPHA, bk.t[:, :], ALU.mult, ALU.add, [x_, bk], [y_])
            _ln_rows(self, y_, gB, bB, st6, mv, 1e-5)
            self.dma("pool", xout.t[tok0:tok0 + 128, :], y_.t[:, :], [y_], [xout])


def _wsl(W, r0, r1, c0, c1, pat):
    if isinstance(W, tuple) and W[0] == "dynflat":
        _, tens, v, kind = W
        if kind == "gu":
            return tens[c0 // 256][bass.ds(v, MSEG)].rearrange("(dc p f) -> p dc f", p=128, f=256)
        return tens[(c0 // 512) * 7 + r0 // 1024][bass.ds(v, MSEG)].rearrange("(a p f) -> p a f", p=128, f=512)
    if isinstance(W, tuple):
        _, t3, reg = W
        return t3[bass.ds(reg, 1), r0:r1, c0:c1].rearrange("o " + pat, p=128, o=1).rearrange("p o a b -> p (o a) b") if False else \
            t3[bass.ds(reg, 1), r0:r1, c0:c1].rearrange("1 " + pat, p=128)
    return W[r0:r1, c0:c1].rearrange(pat, p=128)


def _ffn(self, l, xin, xout, experts, F_, prm, tok_range, wr_ap=None, slot_mode=False):
    nc = self.nc
    nfc = F_ // 128
    nftiles = 7 if slot_mode else (8 if nfc % 8 == 0 else 4)
    nft = nfc // nftiles
    import os
    if os.environ.get("MOE_DBG", "") == "dense":
        wr_ap = None
    gated = wr_ap is not None
    with Phase(self) as ph:
        xT = self.sb(ph, [128, 16, 512], BF16, "xT")
        xtiles = [self.sb(ph, [128, D], F32, "xtile") for _ in range(2)]
        hT = self.sb(ph, [128, nfc, 512], BF16, "hT")
        acc = self.sb(ph, [128, 4, D], F32, "acc")
        wg = [self.sb(ph, [128, 16, 256], BF16, "wg") for _ in range(2)]
        wu = [self.sb(ph, [128, 16, 256], BF16, "wu") for _ in range(2)]
        wd = [self.sb(ph, [128, nft, 512], BF16, "wd") for _ in range(2)]
        sg = [self.sb(ph, [128, 512], F32, "sg") for _ in range(2)]
        if not slot_mode:
            gB = self.bcast_row(ph, prm["ln2_g"][l:l + 1, :], D, "gB2")
            bB = self.bcast_row(ph, prm["ln2_b"][l:l + 1, :], D, "bB2")
        st6 = self.sb(ph, [128, 4, 6], F32, "st6")
        mv = self.sb(ph, [128, 4], F32, "mv")
        G = self.sb(ph, [128, 4, 8], F32, "G")
        if gated:
            x32 = self.sb(ph, [128, 16, 128], F32, "x32")
            x32_keep = x32
            wr = self.sb(ph, [128, 16, 8], F32, "wr")
            if not os.environ.get("NOWR"):
                self.dma("sp", wr.t[:], wr_ap.rearrange("(dc p) e -> p dc e", p=128), [], [wr])
            lg = self.sb(ph, [128, 8], F32, "lg")
            srt = self.sb(ph, [128, 8], F32, "srt")
            gg = self.sb(ph, [128, 4], F32, "gg")
            g2t = self.sb(ph, [128, 8], F32, "g2t")
        else:
            x32 = None

        import os
        dbgm = os.environ.get("MOE_DBG", "")

        def router(ts_):
            if dbgm == "norouter":
                self.memset(G.t[:, ts_, :], 0.125, [G])
                return
            bk = self.banks[0]
            for dc in range(16):
                self.mm(bk, bk.t[:, 0:8], x32.t[:, dc, :], wr.t[:, dc, :], dc == 0, dc == 15, [x32, wr])
            self.copy(lg.t[:, :], bk.t[:, 0:8], [bk], [lg], eng="dve")
            self.P.op("dve", lambda: nc.vector.max(out=srt.t[:, :], in_=lg.t[:, :]), self._b([lg]), self._b([srt]))
            self.tt(gg.t[:, 0:1], srt.t[:, 1:2], srt.t[:, 0:1], ALU.subtract, [srt], [gg])
            self.act(gg.t[:, 1:2], gg.t[:, 0:1], AF.Sigmoid, [gg], [gg])
            self.ts(gg.t[:, 2:3], gg.t[:, 1:2], -1.0, 1.0, ALU.mult, ALU.add, [gg], [gg])
            self.ts(G.t[:, ts_, :], lg.t[:, :], srt.t[:, 0:1], gg.t[:, 2:3], ALU.is_equal, ALU.mult, [lg, srt, gg], [G])
            self.ts(g2t.t[:, :], lg.t[:, :], srt.t[:, 1:2], gg.t[:, 1:2], ALU.is_equal, ALU.mult, [lg, srt, gg], [g2t])
            self.tt(G.t[:, ts_, :], G.t[:, ts_, :], g2t.t[:, :], ALU.add, [G, g2t], [G])

        n1 = 0
        n2 = 0
        for tok0 in range(tok_range[0], tok_range[1], 512):
            self.load_xT(xin, tok0, xT, xtiles, x32=None if os.environ.get("NOX32") else x32, post=router if gated else None)
            exl = experts(tok0) if callable(experts) else experts
            for e, (Wg, Wu, Wd) in enumerate(exl):
                for fg in range(0 if os.environ.get("FFN_SKIP1") else nfc // 2):
                    g_, u_ = wg[n1 % 2], wu[n1 % 2]
                    n1 += 1
                    self.dma("sp", g_.t[:], _wsl(Wg, 0, D, fg * 256, (fg + 1) * 256, "(dc p) f -> p dc f"), [self.wsrc], [g_])
                    self.dma("sp", u_.t[:], _wsl(Wu, 0, D, fg * 256, (fg + 1) * 256, "(dc p) f -> p dc f"), [self.wsrc], [u_])
                    for j in range(2):
                        fc = fg * 2 + j
                        bg, bu = self.banks[fc % 2], self.banks[2 + fc % 2]
                        for dc in range(16):
                            self.mm(bg, bg.t[:, :], g_.t[:, dc, j * 128:(j + 1) * 128], xT.t[:, dc, :], dc == 0, dc == 15, [g_, xT])
                        for dc in range(16):
                            self.mm(bu, bu.t[:, :], u_.t[:, dc, j * 128:(j + 1) * 128], xT.t[:, dc, :], dc == 0, dc == 15, [u_, xT])
                        s_ = sg[fc % 2]
                        self.act(s_.t[:, :], bg.t[:, :], AF.Silu, [bg], [s_])
                        self.tt(hT.t[:, fc, :], s_.t[:, :], bu.t[:, :], ALU.mult, [s_, bu], [hT])
                for q in range(4):
                    if os.environ.get("FFN_SKIP2"):
                        for ts_ in range(4):
                            self.memset(acc.t[:, ts_, q * 512:(q + 1) * 512], 0.0, [acc])
                        continue
                    for ft in range(nftiles):
                        d_ = wd[n2 % 2]
                        n2 += 1
                        self.dma("sp", d_.t[:], _wsl(Wd, ft * nft * 128, (ft + 1) * nft * 128, q * 512, (q + 1) * 512, "(a p) d -> p a d"), [self.wsrc], [d_])
                        for a in range(nft):
                            fc = ft * nft + a
                            for ts_ in range(4):
                                bk = self.banks[4 + ts_]
                                self.mm(bk, bk.t[:, :], hT.t[:, fc, ts_ * 128:(ts_ + 1) * 128], d_.t[:, a, :], fc == 0, fc == nfc - 1, [hT, d_])
                    for ts_ in range(4):
                        bk = self.banks[4 + ts_]
                        dst = acc.t[:, ts_, q * 512:(q + 1) * 512]
                        if not gated:
                            self.copy(dst, bk.t[:, :], [bk], [acc])
                        elif e == 0:
                            self.ts(dst, bk.t[:, :], G.t[:, ts_, e:e + 1], None, ALU.mult, None, [bk, G], [acc])
                        else:
                            self.stt(dst, bk.t[:, :], G.t[:, ts_, e:e + 1], dst, ALU.mult, ALU.add, [bk, G, acc], [acc])
            if slot_mode:
                for ts_ in range(4):
                    self.dma("pool", xout.t[tok0 + ts_ * 128:tok0 + (ts_ + 1) * 128, :], acc.t[:, ts_, :], [acc], [xout])
                continue
            for ts_ in range(4):
                x_ = xtiles[ts_ % 2]
                self.dma("sp", x_.t[:], xin.t[tok0 + ts_ * 128:tok0 + (ts_ + 1) * 128, :], [xin], [x_])
                self.stt(x_.t[:, :], x_.t[:, :], ALPHA, acc.t[:, ts_, :], ALU.mult, ALU.add, [x_, acc], [x_])
                _ln_rows(self, x_, gB, bB, st6, mv, 1e-5)
                self.dma("pool", xout.t[tok0 + ts_ * 128:tok0 + (ts_ + 1) * 128, :], x_.t[:, :], [x_], [xout])


KB.outproj_ln = _outproj_ln
KB.ffn = _ffn


def build_full(shapes, nseq=NSEQ, layers=(0, 1), ne=NE, skip_mixer=False):
    nc = bass.Bass("TRN2", target_bir_lowering=False)
    ext = {}
    for name, (shape, dt) in shapes.items():
        ext[name] = nc.dram_tensor(name, list(shape), dt, kind="ExternalInput").ap()
    out = Tl(nc.dram_tensor("out", [T, D], F32, kind="ExternalOutput").ap(), "out")
    with ExitStack() as st:
        kb = KB(nc, st)
        kb.setup_consts()
        kb.wsrc = Tl(None, "wsrc")
        winb = kb.dram("winb", [L, D, INC], BF16)
        wrot = kb.dram("wrot", [L, D, 576], BF16)
        woutb = kb.dram("woutb", [L, D, D], BF16)
        fg = kb.dram("fgb", [D, DFF], BF16)
        fu = kb.dram("fub", [D, DFF], BF16)
        fd = kb.dram("fdb", [DFF, D], BF16)
        seg = {"qc": kb.dram("s_qc", [384, T], BF16), "kvc": kb.dram("s_kvc", [256, T], BF16),
               "kpe": kb.dram("s_kpe", [64, T], BF16), "rq": kb.dram("s_rq", [256, T], BF16),
               "rk": kb.dram("s_rk", [256, T], BF16), "rg": kb.dram("s_rg", [512, T], BF16),
               "mz": kb.dram("s_mz", [512, T], BF16), "xbc": kb.dram("s_xbc", [1024, T], BF16),
               "dt": kb.dram("s_dt", [16, T], F32), "hu": kb.dram("s_hu", [1536, T], BF16),
               "rv": kb.dram("s_rv", [T, 512], BF16)}
        mixedT = kb.dram("mixedT", [D, T], BF16)
        Hs = kb.dram("Hs", [2, 2, NFP, 512], F32)
        hyu = kb.dram("hyu", [3, NSEQ * 512, S], F32)
        z1s = kb.dram("z1s", [NSEQ * 512, S], F32)
        xa = kb.dram("xa", [T, D], F32)
        xb = kb.dram("xb", [T, D], F32)
        xin = Tl(ext["x"], "x")
        rng = (0, nseq * S)
        import os
        if os.environ.get("RNG"):
            rng = (0, int(os.environ["RNG"]))
        for l in layers:
            kb.cast_dram(_sub(winb, winb.t[l]), ext["w_in"][l], D)
            kb.cast_dram(_sub(woutb, woutb.t[l]), ext["w_out"][l], D)
        if 0 in layers:
            for dst, nm, rows in ((fg, "ffn_w_gate", D), (fu, "ffn_w_up", D), (fd, "ffn_w_down", DFF)):
                t_ = _sub(dst, dst.t)
                t_.b = kb.wsrc.b
                kb.cast_dram(t_, ext[nm][0], rows)
        if 1 in layers:
            mg = [nc.dram_tensor("mgb%d" % i, [NE * MSEG], BF16).ap() for i in range(28)]
            mu = [nc.dram_tensor("mub%d" % i, [NE * MSEG], BF16).ap() for i in range(28)]
            md = [nc.dram_tensor("mdb%d" % i, [NE * MSEG], BF16).ap() for i in range(28)]
            wdst = Tl(None, "wsrc")
            wdst.b = kb.wsrc.b
            for e in range(ne):
                for fg_ in range(28):
                    for tens, nm in ((mg, "moe_w_gate"), (mu, "moe_w_up")):
                        kb.dma("pool", tens[fg_][e * MSEG:(e + 1) * MSEG].rearrange("(r c) -> r c", c=256),
                               ext[nm][0, e][:, fg_ * 256:(fg_ + 1) * 256], [], [wdst])
                for q in range(4):
                    for ft in range(7):
                        kb.dma("pool", md[q * 7 + ft][e * MSEG:(e + 1) * MSEG].rearrange("(r c) -> r c", c=512),
                               ext["moe_w_down"][0, e][ft * 1024:(ft + 1) * 1024, q * 512:(q + 1) * 512], [], [wdst])
        cur = xin
        import os
        stages = os.environ.get("STAGES", "mix,op,ffn").split(",")
        for l in layers:
            if "mix" in stages:
                kb.build_rot(l, ext["w_in"], wrot)
                kb.inproj(l, cur, winb, wrot, seg, ext, rng)
                for s in range(nseq):
                    kb.group_mla(l, s, seg, mixedT, ext, ext)
                    kb.group_ret(s, seg, mixedT)
                    kb.group_ssd(l, s, seg, mixedT, ext)
                kb.hyena_filter(l, ext, ext, Hs)
                kb.group_hyena(l, nseq, seg, mixedT, ext, ext, Hs, hyu, z1s)
            if "op" in stages:
                kb.outproj_ln(l, mixedT, woutb, cur, xa, ext, rng)
            nxt = out if l == layers[-1] else xb
            if "ffn" not in stages:
                continue
            if l == 0:
                kb.ffn(l, xa, nxt, [(fg.t, fu.t, fd.t)], DFF, ext, rng)
            else:
                if True:
                    kb.moe_routed(l, xa, nxt, mg, mu, md, ext, rng[1], ext["moe_router"][0])
            cur = nxt
        kb.P.finish()
        print("instructions:", kb.P.n_inst, "sems:", kb.P.nsem)
    return nc


BIG = ("w_in", "w_out", "ffn_w_gate", "ffn_w_up", "ffn_w_down", "moe_router", "moe_w_gate", "moe_w_up", "moe_w_down",
       "ln1_g", "ln1_b", "ln2_g", "ln2_b")


def kernel(**inputs):
    common = {}
    for k in BIG:
        common[k] = np.ascontiguousarray(np.asarray(inputs[k], dtype=np.float32))
    common.update(host_consts())
    common.update(host_params(inputs))
    x = np.asarray(inputs["x"], dtype=np.float32)
    ncores = 8
    shapes = {k: (v.shape, BF16 if v.dtype == ml_dtypes.bfloat16 else F32) for k, v in common.items()}
    shapes["x"] = ((T, D), F32)
    nc = build_full(shapes)
    in_maps = []
    for c in range(ncores):
        m = dict(common)
        m["x"] = np.ascontiguousarray(x[c * NSEQ:(c + 1) * NSEQ].reshape(T, D))
        in_maps.append(m)
    res = run_bass_kernel_spmd(nc, in_maps, core_ids=list(range(ncores)))
    outs = [np.asarray(r["out"], dtype=np.float32).reshape(NSEQ, S, D) for r in res.results]
    return np.concatenate(outs, axis=0)


MSEG = 2048 * 256
NBLK = 24
I32 = mybir.dt.int32


def _moe_routed(self, l, xin, xout, mg, mu, md, prm, ntok, wr_ap):
    nc = self.nc
    NT = ntok // 128
    xslots = self.dram("xslots", [NBLK * 512, D], F32)
    yslots = self.dram("yslots", [NBLK * 512, D], F32)
    with Phase(self) as pr:
        M1 = self.sb(pr, [128, NT, 8], F32, "M1")
        M2 = self.sb(pr, [128, NT, 8], F32, "M2")
        LOC = self.sb(pr, [128, NT, 8], F32, "LOC")
        TMP = self.sb(pr, [128, NT, 8], F32, "TMPr")
        G12 = self.sb(pr, [128, NT, 2], F32, "G12")
        d1f = self.sb(pr, [128, NT], F32, "d1f")
        d2f = self.sb(pr, [128, NT], F32, "d2f")
        d1i = self.sb(pr, [128, NT], I32, "d1i")
        d2i = self.sb(pr, [128, NT], I32, "d2i")
        bei = self.sb(pr, [128, NBLK], I32, "bei")
        with Phase(self) as ph:
            xt = [self.sb(ph, [128, D], F32, "xt") for _ in range(2)]
            x32 = self.sb(ph, [128, 16, 128], F32, "x32")
            wr = self.sb(ph, [128, 16, 8], F32, "wr")
            self.dma("sp", wr.t[:], wr_ap.rearrange("(dc p) e -> p dc e", p=128), [], [wr])
            ltri = self.sb(ph, [128, 128], F32, "ltri")
            self.memset(ltri.t[:], 1.0, [ltri])
            self.P.op("pool", lambda: nc.gpsimd.affine_select(out=ltri.t[:], in_=ltri.t[:], pattern=[[1, 128]], compare_op=ALU.is_ge, fill=0.0, base=-1, channel_multiplier=-1), self._b([ltri]), self._b([ltri]))
            base = self.sb(ph, [128, 8], F32, "base")
            self.memset(base.t[:], 0.0, [base])
            lg = self.sb(ph, [128, 8], F32, "lg")
            srt = self.sb(ph, [128, 8], F32, "srt")
            gg = self.sb(ph, [128, 4], F32, "gg")
            ms = self.sb(ph, [128, 8], F32, "ms")
            for i in range(NT):
                x_ = xt[i % 2]
                self.dma("sp", x_.t[:], xin.t[i * 128:(i + 1) * 128, :], [xin], [x_])
                for j in range(4):
                    bk = self.banks[4 + j]
                    for k in range(4):
                        dc = 4 * j + k
                        self.tr(bk, bk.t[:, k * 128:(k + 1) * 128], x_.t[:, dc * 128:(dc + 1) * 128], [x_])
                    self.copy(x32.t[:, 4 * j:4 * j + 4, :], bk.t[:].rearrange("p (a b) -> p a b", a=4), [bk], [x32])
                bk = self.banks[0]
                for dc in range(16):
                    self.mm(bk, bk.t[:, 0:8], x32.t[:, dc, :], wr.t[:, dc, :], dc == 0, dc == 15, [x32, wr])
                self.copy(lg.t[:, :], bk.t[:, 0:8], [bk], [lg], eng="dve")
                self.P.op("dve", lambda: nc.vector.max(out=srt.t[:, :], in_=lg.t[:, :]), self._b([lg]), self._b([srt]))
                self.tt(gg.t[:, 0:1], srt.t[:, 1:2], srt.t[:, 0:1], ALU.subtract, [srt], [gg])
                self.act(G12.t[:, i, 1:2], gg.t[:, 0:1], AF.Sigmoid, [gg], [G12])
                self.ts(G12.t[:, i, 0:1], G12.t[:, i, 1:2], -1.0, 1.0, ALU.mult, ALU.add, [G12], [G12])
                self.ts(M1.t[:, i, :], lg.t[:, :], srt.t[:, 0:1], None, ALU.is_equal, None, [lg, srt], [M1])
                self.ts(M2.t[:, i, :], lg.t[:, :], srt.t[:, 1:2], None, ALU.is_equal, None, [lg, srt], [M2])
                self.tt(ms.t[:, :], M1.t[:, i, :], M2.t[:, i, :], ALU.add, [M1, M2], [ms])
                b1, b2 = self.banks[1], self.banks[2]
                self.mm(b1, b1.t[:, 0:8], ltri.t[:, :], ms.t[:, :], True, True, [ltri, ms])
                self.mm(b2, b2.t[:, 0:8], self.ones_f.t[:, :], ms.t[:, :], True, True, [self.ones_f, ms])
                self.tt(LOC.t[:, i, :], b1.t[:, 0:8], base.t[:, :], ALU.add, [b1, base], [LOC])
                self.tt(base.t[:, :], b2.t[:, 0:8], base.t[:, :], ALU.add, [b2, base], [base])
            pad = self.sb(ph, [128, 8], F32, "pad")
            pend = self.sb(ph, [128, 8], F32, "pend")
            pst = self.sb(ph, [128, 8], F32, "pst")
            one8 = self.sb(ph, [128, 8], F32, "one8")
            self.memset(one8.t[:], 1.0, [one8])
            self.ts(pad.t[:, :], base.t[:, :], 1.0 / 512, 0.4990234375, ALU.mult, ALU.add, [base], [pad])
            self.ts(pad.t[:, :], pad.t[:, :], MAGIC, None, ALU.add, None, [pad], [pad])
            self.ts(pad.t[:, :], pad.t[:, :], MAGIC, 512.0, ALU.subtract, ALU.mult, [pad], [pad])
            self.P.op("dve", lambda: nc.vector.tensor_tensor_scan(out=pend.t[:, :], data0=one8.t[:, :], data1=pad.t[:, :], initial=0.0, op0=ALU.mult, op1=ALU.add), self._b([one8, pad]), self._b([pend]))
            self.tt(pst.t[:, :], pend.t[:, :], pad.t[:, :], ALU.subtract, [pend, pad], [pst])
            for i in range(NT):
                self.tt(LOC.t[:, i, :], LOC.t[:, i, :], pst.t[:, :], ALU.add, [LOC, pst], [LOC])
            for Mx, df, di in ((M1, d1f, d1i), (M2, d2f, d2i)):
                self.tt(TMP.t[:], Mx.t[:], LOC.t[:], ALU.mult, [Mx, LOC], [TMP])
                self.P.op("dve", lambda df=df: nc.vector.tensor_reduce(out=df.t[:, :], in_=TMP.t[:], axis=mybir.AxisListType.X, op=ALU.add), self._b([TMP]), self._b([df]))
                self.copy(di.t[:, :], df.t[:, :], [df], [di], eng="dve")
            thr = self.sb(ph, [128, NBLK], F32, "thr")
            bef = self.sb(ph, [128, NBLK], F32, "bef")
            cmpt = self.sb(ph, [128, NBLK], F32, "cmpt")
            self.P.op("pool", lambda: nc.gpsimd.iota(thr.t[:], pattern=[[512, NBLK]], base=0, channel_multiplier=0, allow_small_or_imprecise_dtypes=True), [], [thr.b])
            self.memset(bef.t[:], 0.0, [bef])
            for e in range(8):
                self.ts(cmpt.t[:, :], thr.t[:, :], pend.t[:, e:e + 1], None, ALU.is_ge, None, [thr, pend], [cmpt])
                self.tt(bef.t[:, :], bef.t[:, :], cmpt.t[:, :], ALU.add, [bef, cmpt], [bef])
            self.ts(bef.t[:, :], bef.t[:, :], 7.0, float(MSEG), ALU.min, ALU.mult, [bef], [bef])
            self.copy(bei.t[:, :], bef.t[:, :], [bef], [bei], eng="dve")
            for i in range(NT):
                x_ = xt[i % 2]
                self.dma("sp", x_.t[:], xin.t[i * 128:(i + 1) * 128, :], [xin], [x_])
                for di in (d1i, d2i):
                    self.P.dma_raw("pool", lambda di=di, x_=x_, i=i: nc.gpsimd.indirect_dma_start(
                        out=xslots.t[:, :], out_offset=bass.IndirectOffsetOnAxis(ap=di.t[:, i:i + 1], axis=0), in_=x_.t[:, :], in_offset=None),
                        self._b([x_, di]), self._b([xslots]))
        self.P._deps("sp", self._b([bei]), [])
        cur_reg = [None]

        def experts(tok0):
            b = tok0 // 512
            if cur_reg[0] is not None:
                nc.sync.free_register(cur_reg[0])
            reg = nc.sync.alloc_register()
            nc.sync.reg_load(reg, bei.t[0:1, b:b + 1])
            r = nc.sync.snap(reg, donate=True, min_val=0, max_val=7 * MSEG)
            cur_reg[0] = reg
            return [(("dynflat", mg, r, "gu"), ("dynflat", mu, r, "gu"), ("dynflat", md, r, "d"))]

        self.ffn(l, xslots, yslots, experts, DFE, prm, (0, NBLK * 512), slot_mode=True)
        if cur_reg[0] is not None:
            nc.sync.free_register(cur_reg[0])
        with Phase(self) as ph:
            gB = self.bcast_row(ph, prm["ln2_g"][l:l + 1, :], D, "gB2")
            bB = self.bcast_row(ph, prm["ln2_b"][l:l + 1, :], D, "bB2")
            st6 = self.sb(ph, [128, 4, 6], F32, "st6")
            mv = self.sb(ph, [128, 4], F32, "mv")
            xt = [self.sb(ph, [128, D], F32, "xt") for _ in range(2)]
            ya = [self.sb(ph, [128, D], F32, "ya") for _ in range(2)]
            yb = [self.sb(ph, [128, D], F32, "yb") for _ in range(2)]
            for i in range(NT):
                x_, a_, b_ = xt[i % 2], ya[i % 2], yb[i % 2]
                self.dma("sp", x_.t[:], xin.t[i * 128:(i + 1) * 128, :], [xin], [x_])
                for di, y_ in ((d1i, a_), (d2i, b_)):
                    self.P.dma_raw("pool", lambda di=di, y_=y_, i=i: nc.gpsimd.indirect_dma_start(
                        out=y_.t[:, :], out_offset=None, in_=yslots.t[:, :], in_offset=bass.IndirectOffsetOnAxis(ap=di.t[:, i:i + 1], axis=0)),
                        self._b([yslots, di]), self._b([y_]))
                self.ts(a_.t[:, :], a_.t[:, :], G12.t[:, i, 0:1], None, ALU.mult, None, [a_, G12], [a_])
                self.stt(a_.t[:, :], b_.t[:, :], G12.t[:, i, 1:2], a_.t[:, :], ALU.mult, ALU.add, [b_, G12, a_], [a_])
                self.stt(x_.t[:, :], x_.t[:, :], ALPHA, a_.t[:, :], ALU.mult, ALU.add, [x_, a_], [x_])
                _ln_rows(self, x_, gB, bB, st6, mv, 1e-5)
                self.dma("pool", xout.t[i * 128:(i + 1) * 128, :], x_.t[:, :], [x_], [xout])


KB.moe_routed = _moe_routed
```

```python
import math
from contextlib import ExitStack
import numpy as np
import ml_dtypes
import concourse.bass as bass
import concourse.mybir as mybir
from concourse.bass_utils import run_bass_kernel_spmd

F32 = mybir.dt.float32
BF16 = mybir.dt.bfloat16
AF = mybir.ActivationFunctionType
ALU = mybir.AluOpType
SEM_LIMIT = 8000

L = 2
D = 2048
S = 2048
NSEQ = 2
T = NSEQ * S
INC = 5328
DFF = 5632
DFE = 7168
NE = 8
ALPHA = (2 * L) ** 0.25
NF = 17
NFP = NF * 128
C_QC, C_KVC, C_KPE, C_RQ, C_RK, C_RV, C_RG, C_MZ, C_XBC, C_DT, C_HU = 0, 384, 640, 704, 960, 1216, 1728, 2240, 2752, 3776, 3792
RET_LG_F = [math.log1p(-2.0 ** (-5.0 - h)) for h in range(4)]
RET_LG_B = [math.log1p(-2.0 ** (-5.5 - h)) for h in range(4)]


class Buf:
    __slots__ = ("name", "w", "r")

    def __init__(self, name=""):
        self.name = name
        self.w = {}
        self.r = {}


class Prog:
    def __init__(self, nc, stack):
        self.nc = nc
        self.stack = stack
        self.eng = {"pe": nc.tensor, "dve": nc.vector, "act": nc.scalar, "pool": nc.gpsimd, "sp": nc.sync}
        self.cur_sem, self.cnt, self.sems, self.nsem = {}, {}, {}, 0
        for e in self.eng:
            self._new_eng_sem(e)
        self.waited = {e: {} for e in self.eng}
        self.nslots = 8
        self.slots = {q: [[self._new_sem("d%s%d" % (q, i)), 0] for i in range(self.nslots)] for q in ("sp", "pool", "act")}
        self.slot_i = {q: 0 for q in self.slots}
        self.n_inst = 0

    def _new_sem(self, name):
        self.nsem += 1
        key = "%s_%d" % (name, self.nsem)
        self.sems[key] = self.stack.enter_context(self.nc.semaphore(key))
        return key

    def _new_eng_sem(self, e):
        self.cur_sem[e] = self._new_sem("c" + e)
        self.cnt[e] = 0

    def _wait(self, e, tok):
        if tok is None:
            return
        key, val, src = tok
        if src == e and e == "pe":
            return
        w = self.waited[e]
        if w.get(key, 0) >= val:
            return
        self.eng[e].wait_ge(self.sems[key], val)
        w[key] = val

    def _deps(self, e, reads, writes):
        for b in reads:
            for t in b.w.values():
                self._wait(e, t)
        for b in writes:
            for t in b.w.values():
                self._wait(e, t)
            for t in b.r.values():
                if t[2] != e:
                    self._wait(e, t)

    def _record(self, tok, reads, writes):
        for b in reads:
            b.r[tok[0]] = tok
        for b in writes:
            b.w[tok[0]] = tok
            b.r = {}

    def op(self, e, fn, reads=(), writes=()):
        self._deps(e, reads, writes)
        ins = fn()
        self.n_inst += 1
        if self.cnt[e] >= SEM_LIMIT:
            self._new_eng_sem(e)
        self.cnt[e] += 1
        ins.then_inc(self.sems[self.cur_sem[e]], 1)
        tok = (self.cur_sem[e], self.cnt[e], e)
        self._record(tok, reads, writes)
        return tok

    def dma(self, q, out, in_, reads=(), writes=(), **kw):
        self._deps(q, reads, writes)
        i = self.slot_i[q]
        self.slot_i[q] = (i + 1) % self.nslots
        sl = self.slots[q][i]
        if sl[1] > 0:
            self._wait(q, (sl[0], sl[1], "dma"))
        if sl[1] + 16 > SEM_LIMIT:
            sl[0] = self._new_sem("d%s%d" % (q, i))
            sl[1] = 0
        sl[1] += 16
        self.eng[q].dma_start(out=out, in_=in_, **kw).then_inc(self.sems[sl[0]], 16)
        tok = (sl[0], sl[1], "dma")
        self._record(tok, reads, writes)
        self.n_inst += 1
        return tok

    def dma_raw(self, q, emit, reads=(), writes=()):
        self._deps(q, reads, writes)
        i = self.slot_i[q]
        self.slot_i[q] = (i + 1) % self.nslots
        sl = self.slots[q][i]
        if sl[1] > 0:
            self._wait(q, (sl[0], sl[1], "dma"))
        if sl[1] + 16 > SEM_LIMIT:
            sl[0] = self._new_sem("d%s%d" % (q, i))
            sl[1] = 0
        sl[1] += 16
        emit().then_inc(self.sems[sl[0]], 16)
        tok = (sl[0], sl[1], "dma")
        self._record(tok, reads, writes)
        self.n_inst += 1
        return tok

    def barrier(self):
        toks = [(self.cur_sem[e], self.cnt[e], e) for e in self.eng if self.cnt[e] > 0]
        for q in self.slots:
            for key, val in self.slots[q]:
                if val:
                    toks.append((key, val, "dma"))
        for e in self.eng:
            for t in toks:
                if t[2] != e:
                    self._wait(e, t)

    def finish(self):
        for q in self.slots:
            for key, val in self.slots[q]:
                if val:
                    self._wait("sp", (key, val, "dma"))


class Phase(ExitStack):
    def __init__(self, kb):
        super().__init__()
        self.kb = kb

    def __exit__(self, *a):
        self.kb.P.barrier()
        return super().__exit__(*a)


class Tl:
    __slots__ = ("t", "b")

    def __init__(self, t, name=""):
        self.t = t
        self.b = Buf(name)


class KB:
    def __init__(self, nc, st):
        self.nc = nc
        self.st = st
        self.P = Prog(nc, st)
        self.uid = 0
        self.banks = [Tl(st.enter_context(nc.psum_tensor("bank%d" % i, [128, 512], F32)), "bank%d" % i) for i in range(8)]
        self.evac_i = 0
        self.consts = {}

    def defer(self, tag, fn):
        if not hasattr(self, "pending"):
            self.pending = []
        self.pending.append((tag, fn))

    def pump(self, n=1):
        p = getattr(self, "pending", None)
        while p and n > 0:
            p.pop(0)[1]()
            n -= 1

    def flush_tag(self, tag):
        p = getattr(self, "pending", None)
        if not p:
            return
        last = -1
        for i, (t, _) in enumerate(p):
            if t == tag:
                last = i
        for _ in range(last + 1):
            p.pop(0)[1]()

    def sb(self, stack, shape, dt, name="t"):
        self.uid += 1
        nm = "%s_%d" % (name, self.uid)
        return Tl(stack.enter_context(self.nc.sbuf_tensor(nm, list(shape), dt)), nm)

    def dram(self, name, shape, dt):
        return Tl(self.nc.dram_tensor(name, list(shape), dt).ap(), name)

    def cst(self, val):
        if val not in self.consts:
            t = self.sb(self.st, [128, 1], F32, "cst")
            self.P.op("pool", lambda: self.nc.gpsimd.memset(t.t[:], float(val)), writes=[t.b])
            self.consts[val] = t
        return self.consts[val]

    @staticmethod
    def _b(xs):
        return [x.b for x in xs]

    def act(self, out, in_, func, reads, writes, bias=None, scale=None):
        kw = {}
        rd = list(reads)
        if bias is not None:
            if isinstance(bias, (int, float)):
                c = self.cst(bias)
                rd.append(c)
                bias = c.t[0:out.shape[0], 0:1]
            kw["bias"] = bias
        if scale is not None:
            kw["scale"] = scale
        return self.P.op("act", lambda: self.nc.scalar.activation(out=out, in_=in_, func=func, **kw), self._b(rd), self._b(writes))

    def tt(self, out, in0, in1, op, reads, writes, eng="dve"):
        e = self.nc.vector if eng == "dve" else self.nc.gpsimd
        return self.P.op(eng, lambda: e.tensor_tensor(out=out, in0=in0, in1=in1, op=op), self._b(reads), self._b(writes))

    def ts(self, out, in0, s1, s2, op0, op1, reads, writes, eng="dve"):
        e = self.nc.vector if eng == "dve" else self.nc.gpsimd
        if op1 is None:
            return self.P.op(eng, lambda: e.tensor_scalar(out=out, in0=in0, scalar1=s1, scalar2=None, op0=op0), self._b(reads), self._b(writes))
        return self.P.op(eng, lambda: e.tensor_scalar(out=out, in0=in0, scalar1=s1, scalar2=s2, op0=op0, op1=op1), self._b(reads), self._b(writes))

    def stt(self, out, in0, scalar, in1, op0, op1, reads, writes):
        return self.P.op("dve", lambda: self.nc.vector.scalar_tensor_tensor(out=out, in0=in0, scalar=scalar, in1=in1, op0=op0, op1=op1), self._b(reads), self._b(writes))

    def copy(self, out, in_, reads, writes, eng=None):
        if eng is None:
            self.evac_i += 1
            eng = "act" if self.evac_i % 2 else "dve"
        if eng == "act":
            return self.P.op("act", lambda: self.nc.scalar.copy(out=out, in_=in_), self._b(reads), self._b(writes))
        e = self.nc.vector if eng == "dve" else self.nc.gpsimd
        return self.P.op(eng, lambda: e.tensor_copy(out=out, in_=in_), self._b(reads), self._b(writes))

    def recip(self, out, in_, reads, writes):
        return self.P.op("dve", lambda: self.nc.vector.reciprocal(out=out, in_=in_), self._b(reads), self._b(writes))

    def memset(self, out, val, writes, eng="pool"):
        e = self.nc.vector if eng == "dve" else self.nc.gpsimd
        return self.P.op(eng, lambda: e.memset(out, float(val)), [], self._b(writes))

    def mm(self, bank, out, lhsT, rhs, start, stop, reads):
        return self.P.op("pe", lambda: self.nc.tensor.matmul(out, lhsT=lhsT, rhs=rhs, start=start, stop=stop), self._b(reads), [bank.b])

    def tr(self, bank, out, in_, reads):
        rd = list(reads) + [self.ident]
        return self.P.op("pe", lambda: self.nc.tensor.transpose(out=out, in_=in_, identity=self.ident.t[0:in_.shape[0], 0:in_.shape[0]]), self._b(rd), [bank.b])

    def dma(self, q, out, in_, reads, writes, **kw):
        return self.P.dma(q, out, in_, self._b(reads), self._b(writes), **kw)

    def setup_consts(self):
        nc = self.nc
        self.ident = self.sb(self.st, [128, 128], F32, "ident")
        self.memset(self.ident.t[:], 1.0, [self.ident])
        self.P.op("pool", lambda: nc.gpsimd.affine_select(out=self.ident.t[:], in_=self.ident.t[:], pattern=[[1, 128]], compare_op=ALU.is_equal, fill=0.0, base=0, channel_multiplier=-1), self._b([self.ident]), self._b([self.ident]))
        self.ones_bf = self.sb(self.st, [128, 128], BF16, "ones_bf")
        self.memset(self.ones_bf.t[:], 1.0, [self.ones_bf])
        self.ones_f = self.sb(self.st, [128, 128], F32, "ones_f")
        self.memset(self.ones_f.t[:], 1.0, [self.ones_f])
        for v in (1e-6, 1e-5, 1.0, 0.0):
            self.cst(v)
        self.sel = self.sb(self.st, [16, 16, 128], F32, "sel")
        self.memset(self.sel.t[:], 0.0, [self.sel])
        self.P.op("pool", lambda: nc.gpsimd.affine_select(out=self.sel.t[:], in_=self.sel.t[:], pattern=[[-1, 16], [0, 128]], compare_op=ALU.not_equal, fill=1.0, base=0, channel_multiplier=1), self._b([self.sel]), self._b([self.sel]))

    def bcast_row(self, stack, row_ap, n, name):
        out = self.sb(stack, [128, n], F32, name)
        with Phase(self) as p:
            rowt = self.sb(p, [1, n], F32, name + "_row")
            self.dma("sp", rowt.t[:], row_ap, [], [rowt])
            for c in range(0, n, 512):
                w = min(512, n - c)
                bk = self.banks[(c // 512) % 2]
                self.mm(bk, bk.t[:, 0:w], self.ones_f.t[0:1, :], rowt.t[0:1, c:c + w], True, True, [self.ones_f, rowt])
                self.copy(out.t[:, c:c + w], bk.t[:, 0:w], [bk], [out])
        return out

    def load_xT(self, src, tok0, xT, xtiles, ntt=4, x32=None, post=None):
        for ts_ in range(ntt):
            xt = xtiles[ts_ % len(xtiles)]
            self.dma("sp", xt.t[:], src.t[tok0 + ts_ * 128: tok0 + (ts_ + 1) * 128, :], [src], [xt])
            for j in range(4):
                bk = self.banks[4 + j]
                for k in range(4):
                    dc = 4 * j + k
                    self.tr(bk, bk.t[:, k * 128:(k + 1) * 128], xt.t[:, dc * 128:(dc + 1) * 128], [xt])
                bv = bk.t[:].rearrange("p (a b) -> p a b", a=4)
                ce = "act" if j % 2 else "dve"
                self.copy(xT.t[:, 4 * j:4 * j + 4, ts_ * 128:(ts_ + 1) * 128], bv, [bk], [xT], eng=ce)
                if x32 is not None:
                    self.copy(x32.t[:, 4 * j:4 * j + 4, :], bv, [bk], [x32], eng=ce)
            if post is not None:
                post(ts_)

    def cast_dram(self, dst, src_ap, rows, step=256, tag=None):
        if src_ap.shape[-1] > 5632:
            step = 128
        for r0 in range(0, rows, step):
            r1 = min(rows, r0 + step)
            if tag is None:
                self.dma("pool", dst.t[r0:r1, :], src_ap[r0:r1, :], [], [dst])
            else:
                self.defer(tag, lambda r0=r0, r1=r1: self.dma("pool", dst.t[r0:r1, :], src_ap[r0:r1, :], [], [dst]))

    def build_rot(self, l, w_in_ap, wrot):
        with Phase(self) as ph:
            src = self.sb(ph, [128, 16, 576], F32, "rsrc")
            dst = self.sb(ph, [128, 16, 576], BF16, "rdst")
            self.dma("sp", src.t[:], w_in_ap[l, :, C_KPE:C_KPE + 576].rearrange("(dc p) c -> p dc c", p=128), [], [src])
            for dc in range(16):
                sv = src.t[:, dc, :].rearrange("p (g two h) -> p g two h", two=2, h=32)
                dv = dst.t[:, dc, :].rearrange("p (g two h) -> p g two h", two=2, h=32)
                self.ts(dv[:, :, 0, :], sv[:, :, 1, :], -1.0, None, ALU.mult, None, [src], [dst])
                self.copy(dv[:, :, 1, :], sv[:, :, 0, :], [src], [dst], eng="dve")
            self.dma("pool", wrot.t[l].rearrange("(dc p) c -> p dc c", p=128), dst.t[:], [dst], [wrot])

    def inproj(self, l, xres, winb, wrot, seg, cst, tok_range, wdep=None):
        with Phase(self) as ph:
            xT = self.sb(ph, [128, 16, 512], BF16, "xT")
            xtiles = [self.sb(ph, [128, 2048], F32, "xtile") for _ in range(2)]
            wbuf = [self.sb(ph, [128, 16, 576], BF16, "wbuf") for _ in range(2)]
            rbuf = self.sb(ph, [128, 16, 576], BF16, "rbuf")
            cos = self.sb(ph, [128, 2048], F32, "cos")
            sin = self.sb(ph, [128, 2048], F32, "sin")
            self.dma("sp", cos.t[:], cst["rope_cos"], [], [cos])
            self.dma("sp", sin.t[:], cst["rope_sin"], [], [sin])
            self.dma("sp", rbuf.t[:], wrot.t[l].rearrange("(dc p) c -> p dc c", p=128), [wrot], [rbuf])
            stage = [self.sb(ph, [128, 512], BF16, "stg") for _ in range(4)]
            st32 = [self.sb(ph, [128, 512], F32, "st32") for _ in range(3)]
            stdt = self.sb(ph, [16, 512], F32, "stdt")
            sti = [0]
            bki = [0]
            groups = [(0, 384, "fm", [("qc", 0, 0, 128), ("qc", 128, 128, 128), ("qc", 256, 256, 128)]),
                      (384, 256, "fm", [("kvc", 0, 0, 128), ("kvc", 128, 128, 128)]),
                      (640, 576, "rope", [("kpe", 0, 0, 64, 1.0), ("rq", 0, 64, 128, 1.0), ("rq", 128, 192, 128, 1.0),
                                          ("rk", 0, 320, 128, 0.125), ("rk", 128, 448, 128, 0.125)]),
                      (1216, 512, "tm", None),
                      (1728, 512, "fm", [("rg", i * 128, i * 128, 128) for i in range(4)]),
                      (2240, 512, "fm", [("mz", i * 128, i * 128, 128) for i in range(4)]),
                      (2752, 512, "fm", [("xbc", i * 128, i * 128, 128) for i in range(4)]),
                      (3264, 512, "fm", [("xbc", 512 + i * 128, i * 128, 128) for i in range(4)]),
                      (3776, 16, "dt", None),
                      (3792, 512, "fm", [("hu", i * 128, i * 128, 128) for i in range(4)]),
                      (4304, 512, "fm", [("hu", 512 + i * 128, i * 128, 128) for i in range(4)]),
                      (4816, 512, "fm", [("hu", 1024 + i * 128, i * 128, 128) for i in range(4)])]

            def loadw(gi):
                c0, cw = groups[gi][0], groups[gi][1]
                wt = wbuf[gi % 2]
                self.dma("sp", wt.t[:, :, 0:cw], winb.t[l, :, c0:c0 + cw].rearrange("(dc p) c -> p dc c", p=128), [wdep or winb], [wt])

            for tok0 in range(tok_range[0], tok_range[1], 512):
                tpos = tok0 % S
                self.load_xT(xres, tok0, xT, xtiles)
                loadw(0)
                for gi, (c0, cw, kind, chunks) in enumerate(groups):
                    self.pump(2)
                    if gi + 1 < len(groups):
                        loadw(gi + 1)
                    wt = wbuf[gi % 2]
                    if kind == "fm":
                        for (sname, row0, lc, n) in chunks:
                            bk = self.banks[bki[0] % 4]
                            bki[0] += 1
                            for dc in range(16):
                                self.mm(bk, bk.t[0:n, :], wt.t[:, dc, lc:lc + n], xT.t[:, dc, :], dc == 0, dc == 15, [wt, xT])
                            sg = stage[sti[0] % 4]
                            sti[0] += 1
                            self.copy(sg.t[0:n, :], bk.t[0:n, :], [bk], [sg])
                            self.dma("pool", seg[sname].t[row0:row0 + n, tok0:tok0 + 512], sg.t[0:n, :], [sg], [seg[sname]])
                    elif kind == "dt":
                        bk = self.banks[bki[0] % 4]
                        bki[0] += 1
                        for dc in range(16):
                            self.mm(bk, bk.t[0:16, :], wt.t[:, dc, 0:16], xT.t[:, dc, :], dc == 0, dc == 15, [wt, xT])
                        self.copy(stdt.t[:], bk.t[0:16, :], [bk], [stdt])
                        self.dma("pool", seg["dt"].t[:, tok0:tok0 + 512], stdt.t[:], [stdt], [seg["dt"]])
                    elif kind == "tm":
                        for ts_ in range(4):
                            bk = self.banks[bki[0] % 4]
                            bki[0] += 1
                            for dc in range(16):
                                self.mm(bk, bk.t[:, :], xT.t[:, dc, ts_ * 128:(ts_ + 1) * 128], wt.t[:, dc, 0:512], dc == 0, dc == 15, [wt, xT])
                            sg = stage[sti[0] % 4]
                            sti[0] += 1
                            self.copy(sg.t[:, :], bk.t[:, :], [bk], [sg])
                            self.dma("pool", seg["rv"].t[tok0 + ts_ * 128: tok0 + (ts_ + 1) * 128, :], sg.t[:, :], [sg], [seg["rv"]])
                    else:
                        for (sname, row0, lc, n, scl) in chunks:
                            bA = self.banks[bki[0] % 4]
                            bB = self.banks[(bki[0] + 1) % 4]
                            bki[0] += 2
                            for dc in range(16):
                                self.mm(bA, bA.t[0:n, :], wt.t[:, dc, lc:lc + n], xT.t[:, dc, :], dc == 0, dc == 15, [wt, xT])
                            for dc in range(16):
                                self.mm(bB, bB.t[0:n, :], rbuf.t[:, dc, lc:lc + n], xT.t[:, dc, :], dc == 0, dc == 15, [rbuf, xT])
                            self.tt(st32[0].t[0:n, :], bA.t[0:n, :], cos.t[0:n, tpos:tpos + 512], ALU.mult, [bA, cos], [st32[0]])
                            self.tt(st32[1].t[0:n, :], bB.t[0:n, :], sin.t[0:n, tpos:tpos + 512], ALU.mult, [bB, sin], [st32[1]])
                            self.tt(st32[2].t[0:n, :], st32[0].t[0:n, :], st32[1].t[0:n, :], ALU.add, [st32[0], st32[1]], [st32[2]])
                            sg = stage[sti[0] % 4]
                            sti[0] += 1
                            self.act(sg.t[0:n, :], st32[2].t[0:n, :], AF.Copy, [st32[2]], [sg], scale=scl)
                            self.dma("pool", seg[sname].t[row0:row0 + n, tok0:tok0 + 512], sg.t[0:n, :], [sg], [seg[sname]])

    def rms_rstd(self, ph, src, nch, rows, cols, nfeat, eps, bank, tmp_sq, out_rstd):
        c0, c1 = cols
        for c in range(nch):
            self.act(tmp_sq.t[0:rows, c, :], src.t[0:rows, c, c0:c1], AF.Square, [src], [tmp_sq])
        for c in range(nch):
            self.mm(bank, bank.t[:, :], self.ones_bf.t[0:rows, :], tmp_sq.t[0:rows, c, :], c == 0, c == nch - 1, [self.ones_bf, tmp_sq])
        self.act(out_rstd.t[:, :], bank.t[:, :], AF.Sqrt, [bank], [out_rstd], bias=eps, scale=1.0 / nfeat)
        self.recip(out_rstd.t[:, :], out_rstd.t[:, :], [out_rstd], [out_rstd])

    def group_ret(self, s, seg, mixedT):
        nc = self.nc
        t0 = s * S
        with Phase(self) as ph:
            rq = self.sb(ph, [128, 2, S], BF16, "rq")
            rk = self.sb(ph, [128, 2, S], BF16, "rk")
            rg = self.sb(ph, [128, 4, S], BF16, "rg")
            V = self.sb(ph, [128, 16, 512], BF16, "rv")
            self.dma("sp", rq.t[:], seg["rq"].t[:, t0:t0 + S].rearrange("(c p) t -> p c t", p=128), [seg["rq"]], [rq])
            self.dma("sp", rk.t[:], seg["rk"].t[:, t0:t0 + S].rearrange("(c p) t -> p c t", p=128), [seg["rk"]], [rk])
            self.dma("sp", rg.t[:], seg["rg"].t[:, t0:t0 + S].rearrange("(c p) t -> p c t", p=128), [seg["rg"]], [rg])
            self.dma("sp", V.t[:], seg["rv"].t[t0:t0 + S, :].rearrange("(c p) e -> p c e", p=128), [seg["rv"]], [V])
            W = 3968
            strip = self.sb(ph, [128, 4, W], BF16, "strip")
            dl = self.sb(ph, [128, W], F32, "dl")
            tA = self.sb(ph, [128, W], F32, "tA")
            tB = self.sb(ph, [128, W], F32, "tB")
            self.P.op("pool", lambda: nc.gpsimd.iota(dl.t[:], pattern=[[1, W]], base=-1920, channel_multiplier=-1, allow_small_or_imprecise_dtypes=True), [], [dl.b])
            for h in range(4):
                self.ts(tA.t[:], dl.t[:], 0.0, RET_LG_F[h], ALU.max, ALU.mult, [dl], [tA])
                self.ts(tB.t[:], dl.t[:], 0.0, -RET_LG_B[h], ALU.min, ALU.mult, [dl], [tB])
                self.tt(tA.t[:], tA.t[:], tB.t[:], ALU.add, [tA, tB], [tA])
                self.act(strip.t[:, h, :], tA.t[:], AF.Exp, [tA], [strip])
            Pb = [self.sb(ph, [128, 512], BF16, "P") for _ in range(3)]
            ysb = self.sb(ph, [128, 512], F32, "ysb")
            ysq = self.sb(ph, [128, 512], F32, "ysq")
            mean = self.sb(ph, [128, 512], F32, "mean")
            var = self.sb(ph, [128, 512], F32, "var")
            gate = self.sb(ph, [128, 512], F32, "gate")
            ob = [self.sb(ph, [128, 512], BF16, "ob") for _ in range(2)]
            pi = 0
            for h in range(4):
                c, base = h // 2, 64 * (h % 2)
                for ib in range(4):
                    self.pump(3)
                    i0 = ib * 512
                    yb = self.banks[4 + (h * 4 + ib) % 2]
                    for jc in range(16):
                        j0 = jc * 128
                        sbk = self.banks[jc % 3]
                        self.mm(sbk, sbk.t[:, :], rk.t[base:base + 64, c, j0:j0 + 128], rq.t[base:base + 64, c, i0:i0 + 512], True, True, [rk, rq])
                        pb = Pb[pi % 3]
                        pi += 1
                        x0 = i0 - j0 + 1920
                        self.tt(pb.t[:, :], sbk.t[:, :], strip.t[:, h, x0:x0 + 512], ALU.mult, [sbk, strip], [pb])
                        self.mm(yb, yb.t[:, :], V.t[:, jc, h * 128:(h + 1) * 128], pb.t[:, :], jc == 0, jc == 15, [V, pb])
                    self.copy(ysb.t[:, :], yb.t[:, :], [yb], [ysb], eng="act")
                    self.act(ysq.t[:, :], yb.t[:, :], AF.Square, [yb], [ysq])
                    mb, vb = self.banks[6], self.banks[7]
                    self.mm(mb, mb.t[:, :], self.ones_f.t[:, :], ysb.t[:, :], True, True, [self.ones_f, ysb])
                    self.mm(vb, vb.t[:, :], self.ones_f.t[:, :], ysq.t[:, :], True, True, [self.ones_f, ysq])
                    self.act(mean.t[:, :], mb.t[:, :], AF.Copy, [mb], [mean], scale=1.0 / 128)
                    self.tt(var.t[:, :], mean.t[:, :], mean.t[:, :], ALU.mult, [mean], [var])
                    self.stt(var.t[:, :], vb.t[:, :], 1.0 / 128, var.t[:, :], ALU.mult, ALU.subtract, [vb, var], [var])
                    self.act(var.t[:, :], var.t[:, :], AF.Sqrt, [var], [var], bias=1e-6, scale=1.0)
                    self.recip(var.t[:, :], var.t[:, :], [var], [var])
                    self.tt(ysb.t[:, :], ysb.t[:, :], mean.t[:, :], ALU.subtract, [ysb, mean], [ysb])
                    self.tt(ysb.t[:, :], ysb.t[:, :], var.t[:, :], ALU.mult, [ysb, var], [ysb])
                    self.act(gate.t[:, :], rg.t[:, h, i0:i0 + 512], AF.Silu, [rg], [gate])
                    o = ob[(h * 4 + ib) % 2]
                    self.tt(o.t[:, :], ysb.t[:, :], gate.t[:, :], ALU.mult, [ysb, gate], [o])
                    self.dma("pool", mixedT.t[512 + h * 128: 512 + (h + 1) * 128, t0 + i0: t0 + i0 + 512], o.t[:, :], [o], [mixedT])

    def group_mla(self, l, s, seg, mixedT, prm, cst):
        t0 = s * S
        SC = (128 + 64) ** -0.5
        with Phase(self) as ph:
            qc = self.sb(ph, [128, 3, S], BF16, "qc")
            kvc = self.sb(ph, [128, 2, S], BF16, "kvc")
            kpe = self.sb(ph, [64, S], BF16, "kpe")
            self.dma("sp", qc.t[:], seg["qc"].t[:, t0:t0 + S].rearrange("(c p) t -> p c t", p=128), [seg["qc"]], [qc])
            self.dma("sp", kvc.t[:], seg["kvc"].t[:, t0:t0 + S].rearrange("(c p) t -> p c t", p=128), [seg["kvc"]], [kvc])
            self.dma("sp", kpe.t[:], seg["kpe"].t[:, t0:t0 + S], [seg["kpe"]], [kpe])
            cos = self.sb(ph, [64, S], F32, "cos")
            sin = self.sb(ph, [64, S], F32, "sin")
            self.dma("sp", cos.t[:], cst["rope_cos"][0:64, :], [], [cos])
            self.dma("sp", sin.t[:], cst["rope_sin"][0:64, :], [], [sin])
            wq32 = self.sb(ph, [128, 3, 768], F32, "wq32")
            wkv32 = self.sb(ph, [128, 2, 1024], F32, "wkv32")
            self.dma("sp", wq32.t[:], prm["mla_w_uq"][l].rearrange("(c p) e -> p c e", p=128), [], [wq32])
            self.dma("sp", wkv32.t[:], prm["mla_w_ukv"][l].rearrange("(c p) e -> p c e", p=128), [], [wkv32])
            wq = self.sb(ph, [128, 3, 768], BF16, "wq")
            wkv = self.sb(ph, [128, 2, 1024], BF16, "wkv")
            wqr = self.sb(ph, [128, 3, 4, 64], BF16, "wqr")
            self.copy(wq.t[:], wq32.t[:], [wq32], [wq], eng="dve")
            self.copy(wkv.t[:], wkv32.t[:], [wkv32], [wkv], eng="dve")
            for c in range(3):
                for h in range(4):
                    b0 = 192 * h + 128
                    self.ts(wqr.t[:, c, h, 0:32], wq32.t[:, c, b0 + 32:b0 + 64], -1.0, None, ALU.mult, None, [wq32], [wqr])
                    self.copy(wqr.t[:, c, h, 32:64], wq32.t[:, c, b0:b0 + 32], [wq32], [wqr], eng="dve")
            qnw = self.sb(ph, [128, 3], F32, "qnw")
            kvnw = self.sb(ph, [128, 2], F32, "kvnw")
            onw = self.sb(ph, [128, 4], F32, "onw")
            self.dma("sp", qnw.t[:], prm["mla_q_norm_pp"][:, l * 3:(l + 1) * 3], [], [qnw])
            self.dma("sp", kvnw.t[:], prm["mla_kv_norm_pp"][:, l * 2:(l + 1) * 2], [], [kvnw])
            self.dma("sp", onw.t[:], prm["mla_out_norm_pp"][:, l * 4:(l + 1) * 4], [], [onw])
            qn = self.sb(ph, [128, 3, S], BF16, "qn")
            kvn = self.sb(ph, [128, 2, S], BF16, "kvn")
            sq = self.sb(ph, [128, 4, 512], BF16, "sq")
            rstd = self.sb(ph, [128, 512], F32, "rstd")
            for tb in range(4):
                c0 = tb * 512
                self.rms_rstd(ph, qc, 3, 128, (c0, c0 + 512), 384.0, 1e-6, self.banks[0], sq, rstd)
                for c in range(3):
                    self.stt(qn.t[:, c, c0:c0 + 512], qc.t[:, c, c0:c0 + 512], qnw.t[:, c:c + 1], rstd.t[:, :], ALU.mult, ALU.mult, [qc, qnw, rstd], [qn])
                self.rms_rstd(ph, kvc, 2, 128, (c0, c0 + 512), 256.0, 1e-6, self.banks[1], sq, rstd)
                for c in range(2):
                    self.stt(kvn.t[:, c, c0:c0 + 512], kvc.t[:, c, c0:c0 + 512], kvnw.t[:, c:c + 1], rstd.t[:, :], ALU.mult, ALU.mult, [kvc, kvnw, rstd], [kvn])
            qhn = self.sb(ph, [128, 4, S], BF16, "qhn")
            qhp = self.sb(ph, [64, 4, S], BF16, "qhp")
            khn = self.sb(ph, [128, 4, S], BF16, "khn")
            Vt = self.sb(ph, [128, 16, 512], BF16, "Vt")
            t1 = self.sb(ph, [64, 512], F32, "t1")
            t2 = self.sb(ph, [64, 512], F32, "t2")
            bi = 0
            for tb in range(4):
                c0 = tb * 512
                for h in range(4):
                    bk = self.banks[bi % 4]; bi += 1
                    for c in range(3):
                        self.mm(bk, bk.t[:, :], wq.t[:, c, 192 * h:192 * h + 128], qn.t[:, c, c0:c0 + 512], c == 0, c == 2, [wq, qn])
                    self.copy(qhn.t[:, h, c0:c0 + 512], bk.t[:, :], [bk], [qhn])
                    bA = self.banks[bi % 4]; bi += 1
                    bB = self.banks[bi % 4]; bi += 1
                    for c in range(3):
                        self.mm(bA, bA.t[0:64, :], wq.t[:, c, 192 * h + 128:192 * h + 192], qn.t[:, c, c0:c0 + 512], c == 0, c == 2, [wq, qn])
                    for c in range(3):
                        self.mm(bB, bB.t[0:64, :], wqr.t[:, c, h, :], qn.t[:, c, c0:c0 + 512], c == 0, c == 2, [wqr, qn])
                    self.tt(t1.t[:, :], bA.t[0:64, :], cos.t[:, c0:c0 + 512], ALU.mult, [bA, cos], [t1])
                    self.tt(t2.t[:, :], bB.t[0:64, :], sin.t[:, c0:c0 + 512], ALU.mult, [bB, sin], [t2])
                    self.tt(qhp.t[:, h, c0:c0 + 512], t1.t[:, :], t2.t[:, :], ALU.add, [t1, t2], [qhp])
                    bk = self.banks[bi % 4]; bi += 1
                    for c in range(2):
                        self.mm(bk, bk.t[:, :], wkv.t[:, c, 256 * h:256 * h + 128], kvn.t[:, c, c0:c0 + 512], c == 0, c == 1, [wkv, kvn])
                    self.copy(khn.t[:, h, c0:c0 + 512], bk.t[:, :], [bk], [khn])
            for tc in range(16):
                bk = self.banks[bi % 4]; bi += 1
                for h in range(4):
                    for c in range(2):
                        self.mm(bk, bk.t[:, h * 128:(h + 1) * 128], kvn.t[:, c, tc * 128:(tc + 1) * 128], wkv.t[:, c, 256 * h + 128:256 * h + 256], c == 0, c == 1, [wkv, kvn])
                self.copy(Vt.t[:, tc, :], bk.t[:, :], [bk], [Vt])
            Pb = [self.sb(ph, [128, 512], BF16, "P") for _ in range(3)]
            oblk = self.sb(ph, [128, 4, 512], F32, "oblk")
            den = self.sb(ph, [128, 512], F32, "den")
            ob = [self.sb(ph, [128, 512], BF16, "ob") for _ in range(2)]
            pi = 0
            for qb in range(4):
                q0 = qb * 512
                for h in range(4):
                    self.pump(3)
                    ob_k, dn_k = self.banks[4 + 2 * (h % 2)], self.banks[5 + 2 * (h % 2)]
                    for kc in range(16):
                        k0 = kc * 128
                        sbk = self.banks[kc % 3]
                        self.mm(sbk, sbk.t[:, :], khn.t[:, h, k0:k0 + 128], qhn.t[:, h, q0:q0 + 512], True, False, [khn, qhn])
                        self.mm(sbk, sbk.t[:, :], kpe.t[0:64, k0:k0 + 128], qhp.t[0:64, h, q0:q0 + 512], False, True, [kpe, qhp])
                        pb = Pb[pi % 3]; pi += 1
                        self.act(pb.t[:, :], sbk.t[:, :], AF.Exp, [sbk], [pb], scale=SC)
                        self.mm(ob_k, ob_k.t[:, :], Vt.t[:, kc, h * 128:(h + 1) * 128], pb.t[:, :], kc == 0, kc == 15, [Vt, pb])
                        self.mm(dn_k, dn_k.t[:, :], self.ones_bf.t[:, :], pb.t[:, :], kc == 0, kc == 15, [self.ones_bf, pb])
                    self.recip(den.t[:, :], dn_k.t[:, :], [dn_k], [den])
                    self.tt(oblk.t[:, h, :], ob_k.t[:, :], den.t[:, :], ALU.mult, [ob_k, den], [oblk])
                for h in range(4):
                    self.act(sq.t[:, h, :], oblk.t[:, h, :], AF.Square, [oblk], [sq])
                nb = self.banks[3]
                for h in range(4):
                    self.mm(nb, nb.t[:, :], self.ones_bf.t[:, :], sq.t[:, h, :], h == 0, h == 3, [self.ones_bf, sq])
                self.act(rstd.t[:, :], nb.t[:, :], AF.Sqrt, [nb], [rstd], bias=1e-6, scale=1.0 / 512)
                self.recip(rstd.t[:, :], rstd.t[:, :], [rstd], [rstd])
                for h in range(4):
                    o = ob[h % 2]
                    self.stt(o.t[:, :], oblk.t[:, h, :], onw.t[:, h:h + 1], rstd.t[:, :], ALU.mult, ALU.mult, [oblk, onw, rstd], [o])
                    self.dma("pool", mixedT.t[h * 128:(h + 1) * 128, t0 + q0:t0 + q0 + 512], o.t[:, :], [o], [mixedT])


def _pp(v, nl):
    v = np.asarray(v, np.float32)
    n = v.shape[-1]
    return np.ascontiguousarray(v.reshape(nl, n // 128, 128).transpose(2, 0, 1).reshape(128, nl * (n // 128)))


def host_consts():
    c = {}
    half = 32
    inv = (10000.0 ** (-np.arange(half, dtype=np.float32) * 2.0 / 64)).astype(np.float32)
    ang = np.arange(S, dtype=np.float32)[None, :] * inv[:, None]
    c["rope_cos"] = np.ascontiguousarray(np.tile(np.cos(ang), (4, 1)).astype(np.float32))
    c["rope_sin"] = np.ascontiguousarray(np.tile(np.sin(ang), (4, 1)).astype(np.float32))
    t = np.linspace(0.0, 1.0, S, dtype=np.float32)[:, None]
    bands = 16
    angp = (2.0 * math.pi * np.arange(S, dtype=np.float32)[:, None] / S).astype(np.float32)
    f = np.linspace(1e-4, bands - 1, bands, dtype=np.float32)[None, :]
    z = np.concatenate([t, np.cos(f * angp), -np.sin(f * angp)], axis=-1).astype(np.float32)
    c["hy_zT"] = np.ascontiguousarray(z.T)
    c["hy_ntlin_pp"] = np.ascontiguousarray((-t[:, 0]).reshape(16, 128).T.astype(np.float32))
    mn, mx = math.log(1e-2) / 1.5, math.log(1e-2) / 0.3
    dl = np.abs(np.linspace(mn, mx, 512, dtype=np.float32))
    c["hy_delta_b"] = np.ascontiguousarray(np.tile(dl[None, :], (128, 1)).astype(np.float32))
    idx = np.arange(NFP, dtype=np.int64)
    ph_ = (np.outer(idx, idx) % 4096).astype(np.float64) * (2.0 * math.pi / 4096.0)
    valid = (idx <= 2048).astype(np.float64)
    Cm = np.cos(ph_) * valid[:, None] * valid[None, :]
    Sm = -np.sin(ph_) * valid[:, None] * valid[None, :]
    bf = ml_dtypes.bfloat16
    c["dft_Cnat"] = np.ascontiguousarray(Cm[:, :S].astype(np.float32).astype(bf))
    c["dft_Snat"] = np.ascontiguousarray(Sm[:, :S].astype(np.float32).astype(bf))
    c["dft_Cblk"] = np.ascontiguousarray(Cm[:S, :].reshape(16, 128, NF, 128).transpose(2, 1, 0, 3).astype(np.float32).astype(bf))
    c["dft_Sblk"] = np.ascontiguousarray(Sm[:S, :].reshape(16, 128, NF, 128).transpose(2, 1, 0, 3).astype(np.float32).astype(bf))
    wfv = np.where(idx <= 2048, 2.0, 0.0)
    wfv[0] = 1.0
    wfv[2048] = 1.0
    c["dft_wf_pp"] = np.ascontiguousarray((wfv / 4096.0).reshape(NF, 128).T.astype(np.float32))
    return c


def host_params(inp):
    p = {}
    p["mla_w_uq"] = np.ascontiguousarray(inp["mla_w_uq"], dtype=np.float32)
    p["mla_w_ukv"] = np.ascontiguousarray(inp["mla_w_ukv"], dtype=np.float32)
    p["mla_q_norm_pp"] = _pp(inp["mla_q_norm"], L)
    p["mla_kv_norm_pp"] = _pp(inp["mla_kv_norm"], L)
    p["mla_out_norm_pp"] = _pp(inp["mla_out_norm"], L)
    cwv = np.asarray(inp["ssd_conv_w"], np.float32)
    p["ssd_conv_w_pp"] = np.ascontiguousarray(cwv.reshape(L, 5, 8, 128).transpose(3, 0, 2, 1).reshape(128, L * 40))
    p["ssd_conv_b_pp"] = _pp(inp["ssd_conv_b"], L)
    p["ssd_dtb"] = np.ascontiguousarray(np.asarray(inp["ssd_dt_bias"], np.float32).reshape(L, 16).T)
    p["ssd_alog"] = np.ascontiguousarray(np.asarray(inp["ssd_a_log"], np.float32).reshape(L, 16).T)
    dd = np.asarray(inp["ssd_d"], np.float32)
    p["ssd_d_pp"] = np.ascontiguousarray(np.repeat(dd, 64, axis=1).reshape(L, 4, 128).transpose(2, 0, 1).reshape(128, L * 4))
    p["ssd_norm_pp"] = _pp(inp["ssd_norm"], L)
    hw = np.asarray(inp["hy_conv_w"], np.float32)
    p["hy_conv_w_pp"] = np.ascontiguousarray(hw.reshape(L, 3, 12, 128).transpose(3, 0, 2, 1).reshape(128, L * 36))
    p["hy_conv_b_pp"] = _pp(inp["hy_conv_b"], L)
    p["hy_w1"] = np.ascontiguousarray(inp["hy_w1"], dtype=np.float32)
    p["hy_w2"] = np.ascontiguousarray(inp["hy_w2"], dtype=np.float32)
    p["hy_w3"] = np.ascontiguousarray(inp["hy_w3"], dtype=np.float32)
    b12 = np.stack([np.asarray(inp["hy_b1"], np.float32), np.asarray(inp["hy_b2"], np.float32)], 1)
    p["hy_b_pp"] = np.ascontiguousarray(b12.transpose(2, 0, 1).reshape(64, L * 2))
    p["hy_freq_pp"] = np.ascontiguousarray(np.asarray(inp["hy_freq"], np.float32).transpose(2, 0, 1).reshape(64, L * 2))
    hbv = np.asarray(inp["hy_bias"], np.float32)
    p["hy_bias_pp"] = np.ascontiguousarray(hbv.reshape(L, 2, 4, 128).transpose(3, 0, 1, 2).reshape(128, L * 8))
    p["hy_out_norm_pp"] = _pp(inp["hy_out_norm"], L)
    p["dirsign"] = np.concatenate([-np.ones((8, 1), np.float32), np.ones((8, 1), np.float32)], 0)
    p["ndirmask"] = np.concatenate([np.zeros((8, 1), np.float32), -np.ones((8, 1), np.float32)], 0)
    return p


def build(cfg, shapes):
    nc = bass.Bass("TRN2", target_bir_lowering=False)
    ext = {}
    for name, (shape, dt) in shapes.items():
        ext[name] = nc.dram_tensor(name, list(shape), dt, kind="ExternalInput").ap()
    with ExitStack() as st:
        kb = KB(nc, st)
        kb.setup_consts()
        winb = kb.dram("winb", [L, D, INC], BF16)
        wrot = kb.dram("wrot", [L, D, 576], BF16)
        seg = {"qc": kb.dram("s_qc", [384, T], BF16), "kvc": kb.dram("s_kvc", [256, T], BF16),
               "kpe": kb.dram("s_kpe", [64, T], BF16), "rq": kb.dram("s_rq", [256, T], BF16),
               "rk": kb.dram("s_rk", [256, T], BF16), "rg": kb.dram("s_rg", [512, T], BF16),
               "mz": kb.dram("s_mz", [512, T], BF16), "xbc": kb.dram("s_xbc", [1024, T], BF16),
               "dt": kb.dram("s_dt", [16, T], F32), "hu": kb.dram("s_hu", [1536, T], BF16),
               "rv": kb.dram("s_rv", [T, 512], BF16)}
        xin = Tl(ext["x"], "x")
        if cfg.get("dbg"):
            kb.dbg = {"BC": Tl(nc.dram_tensor("d_BC", [16, S], F32, kind="ExternalOutput").ap()),
                      "dt_tok": Tl(nc.dram_tensor("d_dt_tok", [128, 256], F32, kind="ExternalOutput").ap()),
                      "bias_tok": Tl(nc.dram_tensor("d_bias_tok", [128, 256], F32, kind="ExternalOutput").ap()),
                      "xsT": Tl(nc.dram_tensor("d_xsT", [128, S], F32, kind="ExternalOutput").ap()),
                      "xdt": Tl(nc.dram_tensor("d_xdt", [128, 1024], BF16, kind="ExternalOutput").ap())}
        nseq = cfg.get("nseq", NSEQ)
        if cfg["mode"] == "mixtest":
            l = cfg["layer"]
            mixedT = Tl(nc.dram_tensor("mixedT", [D, T], BF16, kind="ExternalOutput").ap(), "mixedT")
            kb.cast_dram(Tl(winb.t[l], "x").__class__(winb.t[l]) if False else _sub(winb, winb.t[l]), ext["w_in"][l], D)
            kb.build_rot(l, ext["w_in"], wrot)
            kb.inproj(l, xin, winb, wrot, seg, ext, (0, nseq * S))
            for s in range(nseq):
                if "A" in cfg["groups"]:
                    kb.group_mla(l, s, seg, mixedT, ext, ext)
                if "B" in cfg["groups"]:
                    kb.group_ret(s, seg, mixedT)
                if "C" in cfg["groups"]:
                    kb.group_ssd(l, s, seg, mixedT, ext)
            if "D" in cfg["groups"]:
                Hs = kb.dram("Hs", [2, 2, NFP, 512], F32)
                hyu = kb.dram("hyu", [3, NSEQ * 512, S], F32)
                z1s = kb.dram("z1s", [NSEQ * 512, S], F32)
                kb.hyena_filter(l, ext, ext, Hs)
                kb.group_hyena(l, nseq, seg, mixedT, ext, ext, Hs, hyu, z1s)
        kb.P.finish()
        print("instructions:", kb.P.n_inst, "sems:", kb.P.nsem)
    return nc


def _sub(parent, ap):
    t = Tl(ap, parent.b.name)
    t.b = parent.b
    return t


def _group_ssd(self, l, s, seg, mixedT, prm):
    nc = self.nc
    t0 = s * S
    with Phase(self) as ph:
        xsT = self.sb(ph, [128, 4, S], F32, "xsT")
        BT = self.sb(ph, [128, 2, S], BF16, "BT")
        CT = self.sb(ph, [128, 2, S], BF16, "CT")
        mz = self.sb(ph, [128, 4, S], BF16, "mz")
        self.dma("sp", mz.t[:], seg["mz"].t[:, t0:t0 + S].rearrange("(c p) t -> p c t", p=128), [seg["mz"]], [mz])
        cw = self.sb(ph, [128, 40], F32, "cw")
        cb = self.sb(ph, [128, 8], F32, "cb")
        dpp = self.sb(ph, [128, 4], F32, "dpp")
        nw = self.sb(ph, [128, 4], F32, "nw")
        self.dma("sp", cw.t[:], prm["ssd_conv_w_pp"][:, l * 40:(l + 1) * 40], [], [cw])
        self.dma("sp", cb.t[:], prm["ssd_conv_b_pp"][:, l * 8:(l + 1) * 8], [], [cb])
        self.dma("sp", dpp.t[:], prm["ssd_d_pp"][:, l * 4:(l + 1) * 4], [], [dpp])
        self.dma("sp", nw.t[:], prm["ssd_norm_pp"][:, l * 4:(l + 1) * 4], [], [nw])
        dt_tok = self.sb(ph, [128, 16, 16], F32, "dt_tok")
        bias_tok = self.sb(ph, [128, 16, 16], F32, "bias_tok")
        BC = self.sb(ph, [16, S], F32, "BC")
        xdt = [self.sb(ph, [128, 16, 8, 128], BF16, "xdt%d" % d) for d in range(2)]
        for d in range(2):
            self.memset(xdt[d].t[:], 0.0, [xdt[d]])
        with Phase(self) as p2:
            raw = self.sb(p2, [128, 8, S], BF16, "raw")
            acc = self.sb(p2, [128, S], F32, "acc")
            self.dma("sp", raw.t[:], seg["xbc"].t[:, t0:t0 + S].rearrange("(c p) t -> p c t", p=128), [seg["xbc"]], [raw])
            for c in range(8):
                self.ts(acc.t[:, :], raw.t[:, c, :], cw.t[:, c * 5 + 2:c * 5 + 3], None, ALU.mult, None, [raw, cw], [acc])
                for k in (0, 1, 3, 4):
                    sh = k - 2
                    a0, a1 = max(0, -sh), S - max(0, sh)
                    self.stt(acc.t[:, a0:a1], raw.t[:, c, a0 + sh:a1 + sh], cw.t[:, c * 5 + k:c * 5 + k + 1], acc.t[:, a0:a1], ALU.mult, ALU.add, [raw, cw, acc], [acc])
                if c < 4:
                    dst, dtl = xsT.t[:, c, :], xsT
                elif c < 6:
                    dst, dtl = BT.t[:, c - 4, :], BT
                else:
                    dst, dtl = CT.t[:, c - 6, :], CT
                self.act(dst, acc.t[:, :], AF.Silu, [acc, cb], [dtl], bias=cb.t[:, c:c + 1])
        with Phase(self) as p2:
            dtr = self.sb(p2, [16, S], F32, "dtr")
            ax = self.sb(p2, [16, S], F32, "ax")
            dtv = self.sb(p2, [16, S], F32, "dtv")
            la = self.sb(p2, [16, S], F32, "la")
            cs = self.sb(p2, [16, S], F32, "cs")
            one16 = self.sb(p2, [16, S], F32, "one16")
            sm = self.sb(p2, [16, 8], F32, "sm")
            self.dma("sp", dtr.t[:], seg["dt"].t[:, t0:t0 + S], [seg["dt"]], [dtr])
            self.dma("sp", sm.t[:, 0:1], prm["ssd_dtb"][:, l:l + 1], [], [sm], allow_slow_non_contiguous=True)
            self.dma("sp", sm.t[:, 1:2], prm["ssd_alog"][:, l:l + 1], [], [sm], allow_slow_non_contiguous=True)
            self.dma("sp", sm.t[:, 2:3], prm["dirsign"][:, 0:1], [], [sm], allow_slow_non_contiguous=True)
            self.dma("sp", sm.t[:, 3:4], prm["ndirmask"][:, 0:1], [], [sm], allow_slow_non_contiguous=True)
            self.ts(dtr.t[:], dtr.t[:], sm.t[:, 0:1], None, ALU.add, None, [dtr, sm], [dtr])
            self.stt(ax.t[:], dtr.t[:], -1.0, dtr.t[:], ALU.mult, ALU.max, [dtr], [ax])
            self.act(ax.t[:], ax.t[:], AF.Exp, [ax], [ax], scale=-1.0)
            self.act(ax.t[:], ax.t[:], AF.Ln, [ax], [ax], bias=1.0)
            self.stt(dtv.t[:], dtr.t[:], 0.0, ax.t[:], ALU.max, ALU.add, [dtr, ax], [dtv])
            self.act(sm.t[:, 4:5], sm.t[:, 1:2], AF.Exp, [sm], [sm])
            self.ts(sm.t[:, 5:6], sm.t[:, 4:5], -1.0, None, ALU.mult, None, [sm], [sm])
            self.ts(la.t[:], dtv.t[:], sm.t[:, 5:6], None, ALU.mult, None, [dtv, sm], [la])
            self.memset(one16.t[:], 1.0, [one16])
            self.P.op("dve", lambda: nc.vector.tensor_tensor_scan(out=cs.t[:], data0=one16.t[:], data1=la.t[:], initial=0.0, op0=ALU.mult, op1=ALU.add), self._b([one16, la]), self._b([cs]))
            self.stt(BC.t[:], la.t[:], sm.t[:, 3:4], cs.t[:], ALU.mult, ALU.add, [la, sm, cs], [BC])
            self.ts(cs.t[:], BC.t[:], sm.t[:, 2:3], None, ALU.mult, None, [BC, sm], [cs])
            b6, b7 = self.banks[6], self.banks[7]
            for tc in range(16):
                self.tr(b6, b6.t[:, tc * 16:(tc + 1) * 16], dtv.t[0:16, tc * 128:(tc + 1) * 128], [dtv])
                self.tr(b7, b7.t[:, tc * 16:(tc + 1) * 16], cs.t[0:16, tc * 128:(tc + 1) * 128], [cs])
            self.copy(dt_tok.t[:], b6.t[:, 0:256].rearrange("p (a b) -> p a b", a=16), [b6], [dt_tok])
            self.copy(bias_tok.t[:], b7.t[:, 0:256].rearrange("p (a b) -> p a b", a=16), [b7], [bias_tok])
            for tc in range(16):
                bk = self.banks[4 + tc % 2]
                for c in range(4):
                    self.tr(bk, bk.t[:, c * 128:(c + 1) * 128], xsT.t[:, c, tc * 128:(tc + 1) * 128], [xsT])
                for d in range(2):
                    for h in range(8):
                        self.ts(xdt[d].t[:, tc, h, 64 * (h % 2):64 * (h % 2) + 64], bk.t[:, h * 64:(h + 1) * 64], dt_tok.t[:, tc, d * 8 + h:d * 8 + h + 1], None, ALU.mult, None, [bk, dt_tok], [xdt[d]])
        if getattr(self, "dbg", None) is not None:
            self.dma("pool", self.dbg["BC"].t[:, :], BC.t[:, :], [BC], [self.dbg["BC"]])
            self.dma("pool", self.dbg["dt_tok"].t[:, :], dt_tok.t[:].rearrange("p a b -> p (a b)"), [dt_tok], [self.dbg["dt_tok"]])
            self.dma("pool", self.dbg["bias_tok"].t[:, :], bias_tok.t[:].rearrange("p a b -> p (a b)"), [bias_tok], [self.dbg["bias_tok"]])
            self.dma("pool", self.dbg["xsT"].t[:, :], xsT.t[:, 0, :], [xsT], [self.dbg["xsT"]])
            self.dma("pool", self.dbg["xdt"].t[:, :], xdt[0].t[:, 0, :, :].rearrange("p a b -> p (a b)"), [xdt[0]], [self.dbg["xdt"]])
        Mf = self.sb(ph, [128, 896], BF16, "Mf")
        Mb = self.sb(ph, [128, 896], BF16, "Mb")
        self.memset(Mf.t[:], 1.0, [Mf])
        self.memset(Mb.t[:], 1.0, [Mb])
        self.P.op("pool", lambda: nc.gpsimd.affine_select(out=Mf.t[:], in_=Mf.t[:], pattern=[[1, 896]], compare_op=ALU.is_ge, fill=0.0, base=-384, channel_multiplier=-1), self._b([Mf]), self._b([Mf]))
        self.P.op("pool", lambda: nc.gpsimd.affine_select(out=Mb.t[:], in_=Mb.t[:], pattern=[[-1, 896]], compare_op=ALU.is_gt, fill=0.0, base=384, channel_multiplier=1), self._b([Mb]), self._b([Mb]))
        bcs = [[self.sb(ph, [128, 512], F32, "bcs") for _ in range(2)] for _ in range(2)]
        Lb = [self.sb(ph, [128, 512], F32, "L") for _ in range(3)]
        Pb = [self.sb(ph, [128, 512], BF16, "P") for _ in range(3)]
        ybuf = self.sb(ph, [128, 4, 512], F32, "ybuf")
        yv = self.sb(ph, [128, 512], F32, "yv")
        gate = self.sb(ph, [128, 512], F32, "gate")
        sq = self.sb(ph, [128, 4, 512], BF16, "sq")
        rstd = self.sb(ph, [128, 512], F32, "rstd")
        ob = [self.sb(ph, [128, 512], BF16, "ob") for _ in range(2)]
        li = 0
        for ib in range(4):
            i0 = ib * 512
            for pair in range(4):
                self.pump(3)
                g = pair // 2
                yb = self.banks[4 + pair % 2]
                for d in range(2):
                    for hh in range(2):
                        h = pair * 2 + hh
                        bb = self.banks[3]
                        self.mm(bb, bb.t[:, :], self.sel.t[:, d * 8 + h, :], BC.t[0:16, i0:i0 + 512], True, True, [self.sel, BC])
                        self.copy(bcs[d][hh].t[:, :], bb.t[:, :], [bb], [bcs[d][hh]])
                items = []
                for jc in range(16):
                    for d in range(2):
                        valid = (jc <= 4 * ib + 3) if d == 0 else (jc >= 4 * ib)
                        if valid:
                            for hh in range(2):
                                items.append((jc, d, hh))
                last_jc = -1
                for n, (jc, d, hh) in enumerate(items):
                    j0 = jc * 128
                    h = pair * 2 + hh
                    if jc != last_jc:
                        sbk = self.banks[jc % 3]
                        self.mm(sbk, sbk.t[:, :], BT.t[:, g, j0:j0 + 128], CT.t[:, g, i0:i0 + 512], True, True, [BT, CT])
                        last_jc = jc
                    diag = 4 * ib <= jc <= 4 * ib + 3
                    Lt = Lb[li % 3]
                    pb = Pb[li % 3]
                    li += 1
                    sgn = 1.0 if d == 0 else -1.0
                    bia = bias_tok.t[:, jc, d * 8 + h:d * 8 + h + 1]
                    if diag:
                        self.ts(Lt.t[:, :], bcs[d][hh].t[:, :], sgn, bia, ALU.mult, ALU.add, [bcs[d][hh], bias_tok], [Lt])
                        self.ts(Lt.t[:, :], Lt.t[:, :], 0.0, None, ALU.min, None, [Lt], [Lt])
                        self.act(Lt.t[:, :], Lt.t[:, :], AF.Exp, [Lt], [Lt])
                    else:
                        self.act(Lt.t[:, :], bcs[d][hh].t[:, :], AF.Exp, [bcs[d][hh], bias_tok], [Lt], bias=bia, scale=sgn)
                    self.tt(pb.t[:, :], sbk.t[:, :], Lt.t[:, :], ALU.mult, [sbk, Lt], [pb])
                    if diag:
                        m = jc - 4 * ib
                        M = Mf if d == 0 else Mb
                        self.tt(pb.t[:, :], pb.t[:, :], M.t[:, 384 - 128 * m:384 - 128 * m + 512], ALU.mult, [pb, M], [pb])
                    self.mm(yb, yb.t[:, :], xdt[d].t[:, jc, h, :], pb.t[:, :], n == 0, n == len(items) - 1, [xdt[d], pb])
                self.stt(yv.t[:, :], xsT.t[:, pair, i0:i0 + 512], dpp.t[:, pair:pair + 1], yb.t[:, :], ALU.mult, ALU.add, [xsT, dpp, yb], [yv])
                self.act(gate.t[:, :], mz.t[:, pair, i0:i0 + 512], AF.Silu, [mz], [gate])
                self.tt(ybuf.t[:, pair, :], yv.t[:, :], gate.t[:, :], ALU.mult, [yv, gate], [ybuf])
            for c in range(4):
                self.act(sq.t[:, c, :], ybuf.t[:, c, :], AF.Square, [ybuf], [sq])
            nb = self.banks[6]
            for c in range(4):
                self.mm(nb, nb.t[:, :], self.ones_bf.t[:, :], sq.t[:, c, :], c == 0, c == 3, [self.ones_bf, sq])
            self.act(rstd.t[:, :], nb.t[:, :], AF.Sqrt, [nb], [rstd], bias=1e-6, scale=1.0 / 512)
            self.recip(rstd.t[:, :], rstd.t[:, :], [rstd], [rstd])
            for c in range(4):
                o = ob[c % 2]
                self.stt(o.t[:, :], ybuf.t[:, c, :], nw.t[:, c:c + 1], rstd.t[:, :], ALU.mult, ALU.mult, [ybuf, nw, rstd], [o])
                self.dma("pool", mixedT.t[1024 + c * 128:1024 + (c + 1) * 128, t0 + i0:t0 + i0 + 512], o.t[:, :], [o], [mixedT])


KB.group_ssd = _group_ssd


TWO_PI = 2.0 * math.pi
MAGIC = 12582912.0


def _sin_rr(self, out, x, tmp, reads_x, writes_out, x_tl, tmp_tl):
    self.ts(tmp, x, 1.0 / TWO_PI, MAGIC, ALU.mult, ALU.add, [x_tl], [tmp_tl])
    self.ts(tmp, tmp, MAGIC, -TWO_PI, ALU.subtract, ALU.mult, [tmp_tl], [tmp_tl])
    self.tt(x, x, tmp, ALU.add, [x_tl, tmp_tl], [x_tl])
    self.ts(x, x, 3.1415925, -3.1415925, ALU.min, ALU.max, [x_tl], [x_tl])
    self.act(out, x, AF.Sin, [x_tl], writes_out)


def _hyena_filter(self, l, prm, cst, Hs):
    with Phase(self) as ph:
        zT = self.sb(ph, [33, S], F32, "zT")
        w1 = self.sb(ph, [33, 64], F32, "w1")
        w2 = self.sb(ph, [64, 64], F32, "w2")
        w3 = self.sb(ph, [64, 2048], F32, "w3")
        sm = self.sb(ph, [64, 4], F32, "sm")
        self.dma("sp", zT.t[:], cst["hy_zT"], [], [zT])
        self.dma("sp", w1.t[:], prm["hy_w1"][l], [], [w1])
        self.dma("sp", w2.t[:], prm["hy_w2"][l], [], [w2])
        self.dma("sp", w3.t[:], prm["hy_w3"][l], [], [w3])
        self.dma("sp", sm.t[:, 0:2], prm["hy_b_pp"][:, l * 2:l * 2 + 2], [], [sm])
        self.dma("sp", sm.t[:, 2:4], prm["hy_freq_pp"][:, l * 2:l * 2 + 2], [], [sm])
        ntl = self.sb(ph, [128, 16], F32, "ntl")
        dlb = self.sb(ph, [128, 512], F32, "dlb")
        wf = self.sb(ph, [128, NF], F32, "wf")
        self.dma("sp", ntl.t[:], cst["hy_ntlin_pp"], [], [ntl])
        self.dma("sp", dlb.t[:], cst["hy_delta_b"], [], [dlb])
        self.dma("sp", wf.t[:], cst["dft_wf_pp"], [], [wf])
        hid1 = self.sb(ph, [64, S], F32, "hid1")
        hid2 = self.sb(ph, [64, S], F32, "hid2")
        xa = self.sb(ph, [64, 512], F32, "xa")
        xb = self.sb(ph, [64, 512], F32, "xb")
        for tb in range(4):
            bk = self.banks[tb % 2]
            self.mm(bk, bk.t[0:64, :], w1.t[0:33, :], zT.t[0:33, tb * 512:(tb + 1) * 512], True, True, [w1, zT])
            self.ts(xa.t[:, :], bk.t[0:64, :], sm.t[:, 0:1], sm.t[:, 2:3], ALU.add, ALU.mult, [bk, sm], [xa])
            _sin_rr(self, hid1.t[:, tb * 512:(tb + 1) * 512], xa.t[:, :], xb.t[:, :], None, [hid1], xa, xb)
        for tb in range(4):
            bk = self.banks[tb % 2]
            self.mm(bk, bk.t[0:64, :], w2.t[0:64, :], hid1.t[0:64, tb * 512:(tb + 1) * 512], True, True, [w2, hid1])
            self.ts(xa.t[:, :], bk.t[0:64, :], sm.t[:, 1:2], sm.t[:, 3:4], ALU.add, ALU.mult, [bk, sm], [xa])
            _sin_rr(self, hid2.t[:, tb * 512:(tb + 1) * 512], xa.t[:, :], xb.t[:, :], None, [hid2], xa, xb)
        hsum = [self.sb(ph, [128, 16, 512], BF16, "hsum") for _ in range(2)]
        hdif = [self.sb(ph, [128, 16, 512], BF16, "hdif") for _ in range(2)]
        dec = self.sb(ph, [128, 512], F32, "dec")
        hb = self.sb(ph, [128, 512], F32, "hb")
        t1 = self.sb(ph, [128, 512], F32, "t1")
        for tc in range(16):
            self.act(dec.t[:, :], dlb.t[:, :], AF.Exp, [dlb, ntl], [dec], scale=ntl.t[:, tc:tc + 1])
            for o in range(2):
                bf_, bb_ = self.banks[2 * o], self.banks[2 * o + 1]
                self.mm(bf_, bf_.t[:, :], hid2.t[0:64, tc * 128:(tc + 1) * 128], w3.t[0:64, (2 * o) * 512:(2 * o + 1) * 512], True, True, [hid2, w3])
                self.mm(bb_, bb_.t[:, :], hid2.t[0:64, tc * 128:(tc + 1) * 128], w3.t[0:64, (2 * o + 1) * 512:(2 * o + 2) * 512], True, True, [hid2, w3])
                self.copy(hb.t[:, :], bb_.t[:, :], [bb_], [hb], eng="act")
                if tc == 0:
                    self.memset(hb.t[0:1, :], 0.0, [hb])
                self.tt(t1.t[:, :], bf_.t[:, :], hb.t[:, :], ALU.add, [bf_, hb], [t1])
                self.tt(hsum[o].t[:, tc, :], t1.t[:, :], dec.t[:, :], ALU.mult, [t1, dec], [hsum[o]])
                self.tt(t1.t[:, :], bf_.t[:, :], hb.t[:, :], ALU.subtract, [bf_, hb], [t1])
                self.tt(hdif[o].t[:, tc, :], t1.t[:, :], dec.t[:, :], ALU.mult, [t1, dec], [hdif[o]])
        Cb = [self.sb(ph, [128, 16, 128], BF16, "Cb") for _ in range(2)]
        Sb = [self.sb(ph, [128, 16, 128], BF16, "Sb") for _ in range(2)]
        ho = [self.sb(ph, [128, 512], F32, "ho") for _ in range(2)]
        n = 0
        for fc in range(NF):
            cb_, sb_ = Cb[fc % 2], Sb[fc % 2]
            self.dma("sp", cb_.t[:], cst["dft_Cblk"][fc], [], [cb_])
            self.dma("sp", sb_.t[:], cst["dft_Sblk"][fc], [], [sb_])
            for o in range(2):
                for ri, (tab, src) in enumerate(((cb_, hsum[o]), (sb_, hdif[o]))):
                    bk = self.banks[4 + n % 4]
                    for tc in range(16):
                        self.mm(bk, bk.t[:, :], tab.t[:, tc, :], src.t[:, tc, :], tc == 0, tc == 15, [tab, src])
                    h_ = ho[n % 2]
                    n += 1
                    self.ts(h_.t[:, :], bk.t[:, :], wf.t[:, fc:fc + 1], None, ALU.mult, None, [bk, wf], [h_])
                    self.dma("pool", Hs.t[o, ri, fc * 128:(fc + 1) * 128, :], h_.t[:, :], [h_], [Hs])


def _group_hyena(self, l, nseq, seg, mixedT, prm, cst, Hs, hyu, z1s):
    NCOL = nseq * 512
    with Phase(self) as ph:
        U = self.sb(ph, [128, 16, NCOL], BF16, "U")
        Yre = self.sb(ph, [128, NF, NCOL], BF16, "Yre")
        Yim = self.sb(ph, [128, NF, NCOL], BF16, "Yim")
        cw = self.sb(ph, [128, 36], F32, "cw")
        cb = self.sb(ph, [128, 12], F32, "cb")
        hbias = self.sb(ph, [128, 8], F32, "hbias")
        nw = self.sb(ph, [128, 4], F32, "nw")
        self.dma("sp", cw.t[:], prm["hy_conv_w_pp"][:, l * 36:(l + 1) * 36], [], [cw])
        self.dma("sp", cb.t[:], prm["hy_conv_b_pp"][:, l * 12:(l + 1) * 12], [], [cb])
        self.dma("sp", hbias.t[:], prm["hy_bias_pp"][:, l * 8:(l + 1) * 8], [], [hbias])
        self.dma("sp", nw.t[:], prm["hy_out_norm_pp"][:, l * 4:(l + 1) * 4], [], [nw])
        with Phase(self) as p2:
            raw = [self.sb(p2, [128, S], BF16, "raw") for _ in range(2)]
            acc = [self.sb(p2, [128, S], F32, "acc") for _ in range(2)]
            n = 0
            for j in range(3):
                for s in range(nseq):
                    for cc in range(4):
                        c = j * 4 + cc
                        r_, a_ = raw[n % 2], acc[n % 2]
                        n += 1
                        self.dma("sp", r_.t[:, :], seg["hu"].t[c * 128:(c + 1) * 128, s * S:(s + 1) * S], [seg["hu"]], [r_])
                        self.ts(a_.t[:, :], r_.t[:, :], cw.t[:, c * 3 + 1:c * 3 + 2], cb.t[:, c:c + 1], ALU.mult, ALU.add, [r_, cw, cb], [a_])
                        self.stt(a_.t[:, 1:S], r_.t[:, 0:S - 1], cw.t[:, c * 3:c * 3 + 1], a_.t[:, 1:S], ALU.mult, ALU.add, [r_, cw, a_], [a_])
                        self.stt(a_.t[:, 0:S - 1], r_.t[:, 1:S], cw.t[:, c * 3 + 2:c * 3 + 3], a_.t[:, 0:S - 1], ALU.mult, ALU.add, [r_, cw, a_], [a_])
                        self.dma("pool", hyu.t[j, (s * 4 + cc) * 128:(s * 4 + cc + 1) * 128, :], a_.t[:, :], [a_], [hyu])
                        if j == 0:
                            for tcg in range(4):
                                bk = self.banks[6 + tcg % 2]
                                for k in range(4):
                                    tc = tcg * 4 + k
                                    self.tr(bk, bk.t[:, k * 128:(k + 1) * 128], a_.t[:, tc * 128:(tc + 1) * 128], [a_])
                                self.copy(U.t[:, tcg * 4:tcg * 4 + 4, (s * 4 + cc) * 128:(s * 4 + cc + 1) * 128], bk.t[:].rearrange("p (a b) -> p a b", a=4), [bk], [U])
        Cb = [self.sb(ph, [128, 16, 128], BF16, "Cb") for _ in range(2)]
        Sb = [self.sb(ph, [128, 16, 128], BF16, "Sb") for _ in range(2)]
        Hre = [self.sb(ph, [128, 512], F32, "Hre") for _ in range(2)]
        Him = [self.sb(ph, [128, 512], F32, "Him") for _ in range(2)]
        Cn = self.sb(ph, [128, NF, 512], BF16, "Cn")
        Sn = self.sb(ph, [128, NF, 512], BF16, "Sn")
        ta = self.sb(ph, [128, 512], F32, "ta")
        tb_ = self.sb(ph, [128, 512], F32, "tb")
        zp = [self.sb(ph, [128, 512], F32, "zp") for _ in range(2)]
        xg = [self.sb(ph, [128, 512], F32, "xg") for _ in range(2)]
        zn = [self.sb(ph, [128, 512], F32, "zn") for _ in range(2)]
        zfin = self.sb(ph, [128, nseq * 4, 512], F32, "zfin")
        sq = self.sb(ph, [128, 4, 512], BF16, "sq")
        rstd = self.sb(ph, [128, 512], F32, "rstd")
        ob = [self.sb(ph, [128, 512], BF16, "ob") for _ in range(2)]
        for o in range(2):
            for fc in range(NF):
                self.pump(2)
                cb_, sb_ = Cb[fc % 2], Sb[fc % 2]
                hr, hi = Hre[fc % 2], Him[fc % 2]
                self.dma("sp", cb_.t[:], cst["dft_Cblk"][fc], [], [cb_])
                self.dma("sp", sb_.t[:], cst["dft_Sblk"][fc], [], [sb_])
                self.dma("sp", hr.t[:, :], Hs.t[o, 0, fc * 128:(fc + 1) * 128, :], [Hs], [hr])
                self.dma("sp", hi.t[:, :], Hs.t[o, 1, fc * 128:(fc + 1) * 128, :], [Hs], [hi])
                for s in range(nseq):
                    br, bi = self.banks[2 * (s % 2)], self.banks[2 * (s % 2) + 1]
                    for tc in range(16):
                        self.mm(br, br.t[:, :], cb_.t[:, tc, :], U.t[:, tc, s * 512:(s + 1) * 512], tc == 0, tc == 15, [cb_, U])
                    for tc in range(16):
                        self.mm(bi, bi.t[:, :], sb_.t[:, tc, :], U.t[:, tc, s * 512:(s + 1) * 512], tc == 0, tc == 15, [sb_, U])
                    self.tt(ta.t[:, :], br.t[:, :], hr.t[:, :], ALU.mult, [br, hr], [ta])
                    self.tt(tb_.t[:, :], bi.t[:, :], hi.t[:, :], ALU.mult, [bi, hi], [tb_])
                    self.tt(Yre.t[:, fc, s * 512:(s + 1) * 512], ta.t[:, :], tb_.t[:, :], ALU.subtract, [ta, tb_], [Yre])
                    self.tt(ta.t[:, :], br.t[:, :], hi.t[:, :], ALU.mult, [br, hi], [ta])
                    self.tt(tb_.t[:, :], bi.t[:, :], hr.t[:, :], ALU.mult, [bi, hr], [tb_])
                    self.tt(Yim.t[:, fc, s * 512:(s + 1) * 512], ta.t[:, :], tb_.t[:, :], ALU.add, [ta, tb_], [Yim])
            for tb in range(4):
                c0 = tb * 512
                self.dma("sp", Cn.t[:], cst["dft_Cnat"][:, c0:c0 + 512].rearrange("(f p) t -> p f t", p=128), [], [Cn])
                self.dma("sp", Sn.t[:], cst["dft_Snat"][:, c0:c0 + 512].rearrange("(f p) t -> p f t", p=128), [], [Sn])
                for sc in range(nseq * 4):
                    s, cc = sc // 4, sc % 4
                    bk = self.banks[4 + sc % 2]
                    for fc in range(NF):
                        self.mm(bk, bk.t[:, :], Yre.t[:, fc, sc * 128:(sc + 1) * 128], Cn.t[:, fc, :], fc == 0, False, [Yre, Cn])
                        self.mm(bk, bk.t[:, :], Yim.t[:, fc, sc * 128:(sc + 1) * 128], Sn.t[:, fc, :], False, fc == NF - 1, [Yim, Sn])
                    z_, x_, n_ = zp[sc % 2], xg[sc % 2], zn[sc % 2]
                    zsrc = hyu.t[0] if o == 0 else z1s.t
                    zsrc_tl = hyu if o == 0 else z1s
                    self.dma("sp", z_.t[:, :], zsrc[sc * 128:(sc + 1) * 128, c0:c0 + 512], [zsrc_tl], [z_])
                    self.dma("sp", x_.t[:, :], hyu.t[o + 1, sc * 128:(sc + 1) * 128, c0:c0 + 512], [hyu], [x_])
                    self.stt(n_.t[:, :], z_.t[:, :], hbias.t[:, o * 4 + cc:o * 4 + cc + 1], bk.t[:, :], ALU.mult, ALU.add, [z_, hbias, bk], [n_])
                    if o == 0:
                        self.tt(n_.t[:, :], n_.t[:, :], x_.t[:, :], ALU.mult, [n_, x_], [n_])
                        self.dma("pool", z1s.t[sc * 128:(sc + 1) * 128, c0:c0 + 512], n_.t[:, :], [n_], [z1s])
                        b6 = self.banks[6 + sc % 2]
                        for k in range(4):
                            self.tr(b6, b6.t[:, k * 128:(k + 1) * 128], n_.t[:, k * 128:(k + 1) * 128], [n_])
                        self.copy(U.t[:, tb * 4:tb * 4 + 4, sc * 128:(sc + 1) * 128], b6.t[:].rearrange("p (a b) -> p a b", a=4), [b6], [U])
                    else:
                        self.tt(zfin.t[:, sc, :], n_.t[:, :], x_.t[:, :], ALU.mult, [n_, x_], [zfin])
                if o == 1:
                    for s in range(nseq):
                        for cc in range(4):
                            self.act(sq.t[:, cc, :], zfin.t[:, s * 4 + cc, :], AF.Square, [zfin], [sq])
                        nb = self.banks[6]
                        for cc in range(4):
                            self.mm(nb, nb.t[:, :], self.ones_bf.t[:, :], sq.t[:, cc, :], cc == 0, cc == 3, [self.ones_bf, sq])
                        self.act(rstd.t[:, :], nb.t[:, :], AF.Sqrt, [nb], [rstd], bias=1e-6, scale=1.0 / 512)
                        self.recip(rstd.t[:, :], rstd.t[:, :], [rstd], [rstd])
                        for cc in range(4):
                            o_ = ob[cc % 2]
                            self.stt(o_.t[:, :], zfin.t[:, s * 4 + cc, :], nw.t[:, cc:cc + 1], rstd.t[:, :], ALU.mult, ALU.mult, [zfin, nw, rstd], [o_])
                            self.dma("pool", mixedT.t[1536 + cc * 128:1536 + (cc + 1) * 128, s * S + c0:s * S + c0 + 512], o_.t[:, :], [o_], [mixedT])


KB.hyena_filter = _hyena_filter
KB.group_hyena = _group_hyena


def _ln_rows(self, y, gB, bB, st6, mv, eps):
    nc = self.nc
    for q in range(4):
        self.P.op("dve", lambda q=q: nc.vector.bn_stats(out=st6.t[:, q, :], in_=y.t[:, q * 512:(q + 1) * 512]), self._b([y]), self._b([st6]))
    self.P.op("dve", lambda: nc.vector.bn_aggr(out=mv.t[:, 0:2], in_=st6.t[:].rearrange("p a b -> p (a b)")), self._b([st6]), self._b([mv]))
    self.act(mv.t[:, 2:3], mv.t[:, 1:2], AF.Sqrt, [mv], [mv], bias=eps, scale=1.0)
    self.recip(mv.t[:, 3:4], mv.t[:, 2:3], [mv], [mv])
    self.ts(y.t[:, :], y.t[:, :], mv.t[:, 0:1], mv.t[:, 3:4], ALU.subtract, ALU.mult, [y, mv], [y])
    self.tt(y.t[:, :], y.t[:, :], gB.t[:, :], ALU.mult, [y, gB], [y])
    self.tt(y.t[:, :], y.t[:, :], bB.t[:, :], ALU.add, [y, bB], [y])


def _outproj_ln(self, l, mixedT, woutb, xin, xout, prm, tok_range, wdep=None):
    with Phase(self) as ph:
        W = self.sb(ph, [128, 16, D], BF16, "Wout")
        self.dma("sp", W.t[:], woutb.t[l].rearrange("(ec p) d -> p ec d", p=128), [wdep or woutb], [W])
        gB = self.bcast_row(ph, prm["ln1_g"][l:l + 1, :], D, "gB")
        bB = self.bcast_row(ph, prm["ln1_b"][l:l + 1, :], D, "bB")
        mT = [self.sb(ph, [128, 16, 128], BF16, "mT") for _ in range(2)]
        xt = [self.sb(ph, [128, D], F32, "xt") for _ in range(2)]
        y = [self.sb(ph, [128, D], F32, "y") for _ in range(2)]
        st6 = self.sb(ph, [128, 4, 6], F32, "st6")
        mv = self.sb(ph, [128, 4], F32, "mv")
        for i, tok0 in enumerate(range(tok_range[0], tok_range[1], 128)):
            self.pump(2)
            m_, x_, y_ = mT[i % 2], xt[i % 2], y[i % 2]
            self.dma("sp", m_.t[:], mixedT.t[:, tok0:tok0 + 128].rearrange("(ec p) t -> p ec t", p=128), [mixedT], [m_])
            self.dma("sp", x_.t[:], xin.t[tok0:tok0 + 128, :], [xin], [x_])
            for q in range(4):
                bk = self.banks[(i * 4 + q) % 8]
                for ec in range(16):
                    self.mm(bk, bk.t[:, :], m_.t[:, ec, :], W.t[:, ec, q * 512:(q + 1) * 512], ec == 0, ec == 15, [m_, W])
                self.stt(y_.t[:, q * 512:(q + 1) * 512], x_.t[:, q * 512:(q + 1) * 512], ALPHA, bk.t[:, :], ALU.mult, ALU.add, [x_, bk], [y_])
            _ln_rows(self, y_, gB, bB, st6, mv, 1e-5)
            self.dma("pool", xout.t[tok0:tok0 + 128, :], y_.t[:, :], [y_], [xout])


def _wsl(W, r0, r1, c0, c1, pat):
    if isinstance(W, tuple) and W[0] == "dynflat":
        _, tens, v, kind = W
        if kind == "gu":
            return tens[c0 // 256][bass.ds(v, MSEG)].rearrange("(dc p f) -> p dc f", p=128, f=256)
        return tens[(c0 // 512) * 7 + r0 // 1024][bass.ds(v, MSEG)].rearrange("(a p f) -> p a f", p=128, f=512)
    if isinstance(W, tuple):
        _, t3, reg = W
        return t3[bass.ds(reg, 1), r0:r1, c0:c1].rearrange("o " + pat, p=128, o=1).rearrange("p o a b -> p (o a) b") if False else \
            t3[bass.ds(reg, 1), r0:r1, c0:c1].rearrange("1 " + pat, p=128)
    return W[r0:r1, c0:c1].rearrange(pat, p=128)


def _ffn(self, l, xin, xout, experts, F_, prm, tok_range, wr_ap=None, slot_mode=False, wdep=None):
    nc = self.nc
    wdep = wdep or self.wsrc
    nfc = F_ // 128
    nftiles = 7 if slot_mode else (8 if nfc % 8 == 0 else 4)
    nft = nfc // nftiles
    import os
    if os.environ.get("MOE_DBG", "") == "dense":
        wr_ap = None
    gated = wr_ap is not None
    with Phase(self) as ph:
        xT = self.sb(ph, [128, 16, 512], BF16, "xT")
        xtiles = [self.sb(ph, [128, D], F32, "xtile") for _ in range(2)]
        hT = self.sb(ph, [128, nfc, 512], BF16, "hT")
        acc = self.sb(ph, [128, 4, D], F32, "acc")
        wg = [self.sb(ph, [128, 16, 256], BF16, "wg") for _ in range(2)]
        wu = [self.sb(ph, [128, 16, 256], BF16, "wu") for _ in range(2)]
        wd = [self.sb(ph, [128, nft, 512], BF16, "wd") for _ in range(2)]
        sg = [self.sb(ph, [128, 512], F32, "sg") for _ in range(2)]
        if not slot_mode:
            gB = self.bcast_row(ph, prm["ln2_g"][l:l + 1, :], D, "gB2")
            bB = self.bcast_row(ph, prm["ln2_b"][l:l + 1, :], D, "bB2")
        st6 = self.sb(ph, [128, 4, 6], F32, "st6")
        mv = self.sb(ph, [128, 4], F32, "mv")
        G = self.sb(ph, [128, 4, 8], F32, "G")
        if gated:
            x32 = self.sb(ph, [128, 16, 128], F32, "x32")
            x32_keep = x32
            wr = self.sb(ph, [128, 16, 8], F32, "wr")
            if not os.environ.get("NOWR"):
                self.dma("sp", wr.t[:], wr_ap.rearrange("(dc p) e -> p dc e", p=128), [], [wr])
            lg = self.sb(ph, [128, 8], F32, "lg")
            srt = self.sb(ph, [128, 8], F32, "srt")
            gg = self.sb(ph, [128, 4], F32, "gg")
            g2t = self.sb(ph, [128, 8], F32, "g2t")
        else:
            x32 = None

        import os
        dbgm = os.environ.get("MOE_DBG", "")

        def router(ts_):
            if dbgm == "norouter":
                self.memset(G.t[:, ts_, :], 0.125, [G])
                return
            bk = self.banks[0]
            for dc in range(16):
                self.mm(bk, bk.t[:, 0:8], x32.t[:, dc, :], wr.t[:, dc, :], dc == 0, dc == 15, [x32, wr])
            self.copy(lg.t[:, :], bk.t[:, 0:8], [bk], [lg], eng="dve")
            self.P.op("dve", lambda: nc.vector.max(out=srt.t[:, :], in_=lg.t[:, :]), self._b([lg]), self._b([srt]))
            self.tt(gg.t[:, 0:1], srt.t[:, 1:2], srt.t[:, 0:1], ALU.subtract, [srt], [gg])
            self.act(gg.t[:, 1:2], gg.t[:, 0:1], AF.Sigmoid, [gg], [gg])
            self.ts(gg.t[:, 2:3], gg.t[:, 1:2], -1.0, 1.0, ALU.mult, ALU.add, [gg], [gg])
            self.ts(G.t[:, ts_, :], lg.t[:, :], srt.t[:, 0:1], gg.t[:, 2:3], ALU.is_equal, ALU.mult, [lg, srt, gg], [G])
            self.ts(g2t.t[:, :], lg.t[:, :], srt.t[:, 1:2], gg.t[:, 1:2], ALU.is_equal, ALU.mult, [lg, srt, gg], [g2t])
            self.tt(G.t[:, ts_, :], G.t[:, ts_, :], g2t.t[:, :], ALU.add, [G, g2t], [G])

        n1 = 0
        n2 = 0
        for tok0 in range(tok_range[0], tok_range[1], 512):
            self.load_xT(xin, tok0, xT, xtiles, x32=None if os.environ.get("NOX32") else x32, post=router if gated else None)
            exl = experts(tok0) if callable(experts) else experts
            for e, (Wg, Wu, Wd) in enumerate(exl):
                for fg in range(0 if os.environ.get("FFN_SKIP1") else nfc // 2):
                    self.pump(2)
                    g_, u_ = wg[n1 % 2], wu[n1 % 2]
                    n1 += 1
                    self.dma("sp", g_.t[:], _wsl(Wg, 0, D, fg * 256, (fg + 1) * 256, "(dc p) f -> p dc f"), [wdep], [g_])
                    self.dma("sp", u_.t[:], _wsl(Wu, 0, D, fg * 256, (fg + 1) * 256, "(dc p) f -> p dc f"), [wdep], [u_])
                    for j in range(2):
                        fc = fg * 2 + j
                        bg, bu = self.banks[fc % 2], self.banks[2 + fc % 2]
                        for dc in range(16):
                            self.mm(bg, bg.t[:, :], g_.t[:, dc, j * 128:(j + 1) * 128], xT.t[:, dc, :], dc == 0, dc == 15, [g_, xT])
                        for dc in range(16):
                            self.mm(bu, bu.t[:, :], u_.t[:, dc, j * 128:(j + 1) * 128], xT.t[:, dc, :], dc == 0, dc == 15, [u_, xT])
                        s_ = sg[fc % 2]
                        self.act(s_.t[:, :], bg.t[:, :], AF.Silu, [bg], [s_])
                        self.tt(hT.t[:, fc, :], s_.t[:, :], bu.t[:, :], ALU.mult, [s_, bu], [hT])
                for q in range(4):
                    if os.environ.get("FFN_SKIP2"):
                        for ts_ in range(4):
                            self.memset(acc.t[:, ts_, q * 512:(q + 1) * 512], 0.0, [acc])
                        continue
                    for ft in range(nftiles):
                        d_ = wd[n2 % 2]
                        n2 += 1
                        self.dma("sp", d_.t[:], _wsl(Wd, ft * nft * 128, (ft + 1) * nft * 128, q * 512, (q + 1) * 512, "(a p) d -> p a d"), [wdep], [d_])
                        for a in range(nft):
                            fc = ft * nft + a
                            for ts_ in range(4):
                                bk = self.banks[4 + ts_]
                                self.mm(bk, bk.t[:, :], hT.t[:, fc, ts_ * 128:(ts_ + 1) * 128], d_.t[:, a, :], fc == 0, fc == nfc - 1, [hT, d_])
                    for ts_ in range(4):
                        bk = self.banks[4 + ts_]
                        dst = acc.t[:, ts_, q * 512:(q + 1) * 512]
                        if not gated:
                            self.copy(dst, bk.t[:, :], [bk], [acc])
                        elif e == 0:
                            self.ts(dst, bk.t[:, :], G.t[:, ts_, e:e + 1], None, ALU.mult, None, [bk, G], [acc])
                        else:
                            self.stt(dst, bk.t[:, :], G.t[:, ts_, e:e + 1], dst, ALU.mult, ALU.add, [bk, G, acc], [acc])
            if slot_mode:
                for ts_ in range(4):
                    self.dma("pool", xout.t[tok0 + ts_ * 128:tok0 + (ts_ + 1) * 128, :], acc.t[:, ts_, :], [acc], [xout])
                continue
            for ts_ in range(4):
                x_ = xtiles[ts_ % 2]
                self.dma("sp", x_.t[:], xin.t[tok0 + ts_ * 128:tok0 + (ts_ + 1) * 128, :], [xin], [x_])
                self.stt(x_.t[:, :], x_.t[:, :], ALPHA, acc.t[:, ts_, :], ALU.mult, ALU.add, [x_, acc], [x_])
                _ln_rows(self, x_, gB, bB, st6, mv, 1e-5)
                self.dma("pool", xout.t[tok0 + ts_ * 128:tok0 + (ts_ + 1) * 128, :], x_.t[:, :], [x_], [xout])


KB.outproj_ln = _outproj_ln
KB.ffn = _ffn


def build_full(shapes, nseq=NSEQ, layers=(0, 1), ne=NE, skip_mixer=False):
    nc = bass.Bass("TRN2", target_bir_lowering=False)
    ext = {}
    for name, (shape, dt) in shapes.items():
        ext[name] = nc.dram_tensor(name, list(shape), dt, kind="ExternalInput").ap()
    out = Tl(nc.dram_tensor("out", [T, D], F32, kind="ExternalOutput").ap(), "out")
    with ExitStack() as st:
        kb = KB(nc, st)
        kb.setup_consts()
        kb.wsrc = Tl(None, "wsrc")
        winb = kb.dram("winb", [L, D, INC], BF16)
        wrot = kb.dram("wrot", [L, D, 576], BF16)
        woutb = kb.dram("woutb", [L, D, D], BF16)
        fg = kb.dram("fgb", [D, DFF], BF16)
        fu = kb.dram("fub", [D, DFF], BF16)
        fd = kb.dram("fdb", [DFF, D], BF16)
        seg = {"qc": kb.dram("s_qc", [384, T], BF16), "kvc": kb.dram("s_kvc", [256, T], BF16),
               "kpe": kb.dram("s_kpe", [64, T], BF16), "rq": kb.dram("s_rq", [256, T], BF16),
               "rk": kb.dram("s_rk", [256, T], BF16), "rg": kb.dram("s_rg", [512, T], BF16),
               "mz": kb.dram("s_mz", [512, T], BF16), "xbc": kb.dram("s_xbc", [1024, T], BF16),
               "dt": kb.dram("s_dt", [16, T], F32), "hu": kb.dram("s_hu", [1536, T], BF16),
               "rv": kb.dram("s_rv", [T, 512], BF16)}
        mixedT = kb.dram("mixedT", [D, T], BF16)
        Hs = kb.dram("Hs", [2, 2, NFP, 512], F32)
        hyu = kb.dram("hyu", [3, NSEQ * 512, S], F32)
        z1s = kb.dram("z1s", [NSEQ * 512, S], F32)
        xa = kb.dram("xa", [T, D], F32)
        xb = kb.dram("xb", [T, D], F32)
        xin = Tl(ext["x"], "x")
        rng = (0, nseq * S)
        import os
        if os.environ.get("RNG"):
            rng = (0, int(os.environ["RNG"]))
        kb.wmoe = Tl(None, "wmoe")
        woutL = [Tl(woutb.t[l], "wout%d" % l) for l in range(L)]
        winL = [Tl(winb.t[l], "win%d" % l) for l in range(L)]
        for l in layers:
            kb.cast_dram(winL[l], ext["w_in"][l], D, tag=None if l == layers[0] else "win%d" % l)
            kb.cast_dram(woutL[l], ext["w_out"][l], D, tag="wout%d" % l)
            if l == 0:
                for dst, nm, rows in ((fg, "ffn_w_gate", D), (fu, "ffn_w_up", D), (fd, "ffn_w_down", DFF)):
                    t_ = _sub(dst, dst.t)
                    t_.b = kb.wsrc.b
                    kb.cast_dram(t_, ext[nm][0], rows, tag="ffn")
        if 1 in layers:
            mg = [nc.dram_tensor("mgb%d" % i, [NE * MSEG], BF16).ap() for i in range(28)]
            mu = [nc.dram_tensor("mub%d" % i, [NE * MSEG], BF16).ap() for i in range(28)]
            md = [nc.dram_tensor("mdb%d" % i, [NE * MSEG], BF16).ap() for i in range(28)]
            for e in range(ne):
                for fg_ in range(28):
                    for tens, nm in ((mg, "moe_w_gate"), (mu, "moe_w_up")):
                        kb.defer("moe", lambda tens=tens, nm=nm, e=e, fg_=fg_: kb.dma(
                            "pool", tens[fg_][e * MSEG:(e + 1) * MSEG].rearrange("(r c) -> r c", c=256),
                            ext[nm][0, e][:, fg_ * 256:(fg_ + 1) * 256], [], [kb.wmoe]))
                for q in range(4):
                    for ft in range(7):
                        kb.defer("moe", lambda e=e, q=q, ft=ft: kb.dma(
                            "pool", md[q * 7 + ft][e * MSEG:(e + 1) * MSEG].rearrange("(r c) -> r c", c=512),
                            ext["moe_w_down"][0, e][ft * 1024:(ft + 1) * 1024, q * 512:(q + 1) * 512], [], [kb.wmoe]))
        cur = xin
        import os
        stages = os.environ.get("STAGES", "mix,op,ffn").split(",")
        for l in layers:
            if "mix" in stages:
                kb.flush_tag("win%d" % l)
                kb.build_rot(l, ext["w_in"], wrot)
                kb.inproj(l, cur, winb, wrot, seg, ext, rng, wdep=winL[l])
                for s in range(nseq):
                    kb.group_mla(l, s, seg, mixedT, ext, ext)
                    kb.group_ret(s, seg, mixedT)
                    kb.group_ssd(l, s, seg, mixedT, ext)
                kb.hyena_filter(l, ext, ext, Hs)
                kb.group_hyena(l, nseq, seg, mixedT, ext, ext, Hs, hyu, z1s)
            if "op" in stages:
                kb.flush_tag("wout%d" % l)
                kb.outproj_ln(l, mixedT, woutb, cur, xa, ext, rng, wdep=woutL[l])
            nxt = out if l == layers[-1] else xb
            if "ffn" not in stages:
                continue
            if l == 0:
                kb.flush_tag("ffn")
                kb.ffn(l, xa, nxt, [(fg.t, fu.t, fd.t)], DFF, ext, rng)
            else:
                if True:
                    kb.flush_tag("moe")
                    kb.moe_routed(l, xa, nxt, mg, mu, md, ext, rng[1], ext["moe_router"][0])
            cur = nxt
        kb.P.finish()
        print("instructions:", kb.P.n_inst, "sems:", kb.P.nsem)
    return nc


BIG = ("w_in", "w_out", "ffn_w_gate", "ffn_w_up", "ffn_w_down", "moe_router", "moe_w_gate", "moe_w_up", "moe_w_down",
       "ln1_g", "ln1_b", "ln2_g", "ln2_b")


def kernel(**inputs):
    common = {}
    for k in BIG:
        common[k] = np.ascontiguousarray(np.asarray(inputs[k], dtype=np.float32))
    common.update(host_consts())
    common.update(host_params(inputs))
    x = np.asarray(inputs["x"], dtype=np.float32)
    ncores = 8
    shapes = {k: (v.shape, BF16 if v.dtype == ml_dtypes.bfloat16 else F32) for k, v in common.items()}
    shapes["x"] = ((T, D), F32)
    nc = build_full(shapes)
    in_maps = []
    for c in range(ncores):
        m = dict(common)
        m["x"] = np.ascontiguousarray(x[c * NSEQ:(c + 1) * NSEQ].reshape(T, D))
        in_maps.append(m)
    res = run_bass_kernel_spmd(nc, in_maps, core_ids=list(range(ncores)))
    outs = [np.asarray(r["out"], dtype=np.float32).reshape(NSEQ, S, D) for r in res.results]
    return np.concatenate(outs, axis=0)


MSEG = 2048 * 256
NBLK = 24
I32 = mybir.dt.int32


def _moe_routed(self, l, xin, xout, mg, mu, md, prm, ntok, wr_ap):
    nc = self.nc
    NT = ntok // 128
    xslots = self.dram("xslots", [NBLK * 512, D], F32)
    yslots = self.dram("yslots", [NBLK * 512, D], F32)
    with Phase(self) as pr:
        M1 = self.sb(pr, [128, NT, 8], F32, "M1")
        M2 = self.sb(pr, [128, NT, 8], F32, "M2")
        LOC = self.sb(pr, [128, NT, 8], F32, "LOC")
        TMP = self.sb(pr, [128, NT, 8], F32, "TMPr")
        G12 = self.sb(pr, [128, NT, 2], F32, "G12")
        d1f = self.sb(pr, [128, NT], F32, "d1f")
        d2f = self.sb(pr, [128, NT], F32, "d2f")
        d1i = self.sb(pr, [128, NT], I32, "d1i")
        d2i = self.sb(pr, [128, NT], I32, "d2i")
        bei = self.sb(pr, [128, NBLK], I32, "bei")
        with Phase(self) as ph:
            xt = [self.sb(ph, [128, D], F32, "xt") for _ in range(2)]
            x32 = self.sb(ph, [128, 16, 128], F32, "x32")
            wr = self.sb(ph, [128, 16, 8], F32, "wr")
            self.dma("sp", wr.t[:], wr_ap.rearrange("(dc p) e -> p dc e", p=128), [], [wr])
            ltri = self.sb(ph, [128, 128], F32, "ltri")
            self.memset(ltri.t[:], 1.0, [ltri])
            self.P.op("pool", lambda: nc.gpsimd.affine_select(out=ltri.t[:], in_=ltri.t[:], pattern=[[1, 128]], compare_op=ALU.is_ge, fill=0.0, base=-1, channel_multiplier=-1), self._b([ltri]), self._b([ltri]))
            base = self.sb(ph, [128, 8], F32, "base")
            self.memset(base.t[:], 0.0, [base])
            lg = self.sb(ph, [128, 8], F32, "lg")
            srt = self.sb(ph, [128, 8], F32, "srt")
            gg = self.sb(ph, [128, 4], F32, "gg")
            ms = self.sb(ph, [128, 8], F32, "ms")
            for i in range(NT):
                x_ = xt[i % 2]
                self.dma("sp", x_.t[:], xin.t[i * 128:(i + 1) * 128, :], [xin], [x_])
                for j in range(4):
                    bk = self.banks[4 + j]
                    for k in range(4):
                        dc = 4 * j + k
                        self.tr(bk, bk.t[:, k * 128:(k + 1) * 128], x_.t[:, dc * 128:(dc + 1) * 128], [x_])
                    self.copy(x32.t[:, 4 * j:4 * j + 4, :], bk.t[:].rearrange("p (a b) -> p a b", a=4), [bk], [x32])
                bk = self.banks[0]
                for dc in range(16):
                    self.mm(bk, bk.t[:, 0:8], x32.t[:, dc, :], wr.t[:, dc, :], dc == 0, dc == 15, [x32, wr])
                self.copy(lg.t[:, :], bk.t[:, 0:8], [bk], [lg], eng="dve")
                self.P.op("dve", lambda: nc.vector.max(out=srt.t[:, :], in_=lg.t[:, :]), self._b([lg]), self._b([srt]))
                self.tt(gg.t[:, 0:1], srt.t[:, 1:2], srt.t[:, 0:1], ALU.subtract, [srt], [gg])
                self.act(G12.t[:, i, 1:2], gg.t[:, 0:1], AF.Sigmoid, [gg], [G12])
                self.ts(G12.t[:, i, 0:1], G12.t[:, i, 1:2], -1.0, 1.0, ALU.mult, ALU.add, [G12], [G12])
                self.ts(M1.t[:, i, :], lg.t[:, :], srt.t[:, 0:1], None, ALU.is_equal, None, [lg, srt], [M1])
                self.ts(M2.t[:, i, :], lg.t[:, :], srt.t[:, 1:2], None, ALU.is_equal, None, [lg, srt], [M2])
                self.tt(ms.t[:, :], M1.t[:, i, :], M2.t[:, i, :], ALU.add, [M1, M2], [ms])
                b1, b2 = self.banks[1], self.banks[2]
                self.mm(b1, b1.t[:, 0:8], ltri.t[:, :], ms.t[:, :], True, True, [ltri, ms])
                self.mm(b2, b2.t[:, 0:8], self.ones_f.t[:, :], ms.t[:, :], True, True, [self.ones_f, ms])
                self.tt(LOC.t[:, i, :], b1.t[:, 0:8], base.t[:, :], ALU.add, [b1, base], [LOC])
                self.tt(base.t[:, :], b2.t[:, 0:8], base.t[:, :], ALU.add, [b2, base], [base])
            pad = self.sb(ph, [128, 8], F32, "pad")
            pend = self.sb(ph, [128, 8], F32, "pend")
            pst = self.sb(ph, [128, 8], F32, "pst")
            one8 = self.sb(ph, [128, 8], F32, "one8")
            self.memset(one8.t[:], 1.0, [one8])
            self.ts(pad.t[:, :], base.t[:, :], 1.0 / 512, 0.4990234375, ALU.mult, ALU.add, [base], [pad])
            self.ts(pad.t[:, :], pad.t[:, :], MAGIC, None, ALU.add, None, [pad], [pad])
            self.ts(pad.t[:, :], pad.t[:, :], MAGIC, 512.0, ALU.subtract, ALU.mult, [pad], [pad])
            self.P.op("dve", lambda: nc.vector.tensor_tensor_scan(out=pend.t[:, :], data0=one8.t[:, :], data1=pad.t[:, :], initial=0.0, op0=ALU.mult, op1=ALU.add), self._b([one8, pad]), self._b([pend]))
            self.tt(pst.t[:, :], pend.t[:, :], pad.t[:, :], ALU.subtract, [pend, pad], [pst])
            for i in range(NT):
                self.tt(LOC.t[:, i, :], LOC.t[:, i, :], pst.t[:, :], ALU.add, [LOC, pst], [LOC])
            for Mx, df, di in ((M1, d1f, d1i), (M2, d2f, d2i)):
                self.tt(TMP.t[:], Mx.t[:], LOC.t[:], ALU.mult, [Mx, LOC], [TMP])
                self.P.op("dve", lambda df=df: nc.vector.tensor_reduce(out=df.t[:, :], in_=TMP.t[:], axis=mybir.AxisListType.X, op=ALU.add), self._b([TMP]), self._b([df]))
                self.copy(di.t[:, :], df.t[:, :], [df], [di], eng="dve")
            thr = self.sb(ph, [128, NBLK], F32, "thr")
            bef = self.sb(ph, [128, NBLK], F32, "bef")
            cmpt = self.sb(ph, [128, NBLK], F32, "cmpt")
            self.P.op("pool", lambda: nc.gpsimd.iota(thr.t[:], pattern=[[512, NBLK]], base=0, channel_multiplier=0, allow_small_or_imprecise_dtypes=True), [], [thr.b])
            self.memset(bef.t[:], 0.0, [bef])
            for e in range(8):
                self.ts(cmpt.t[:, :], thr.t[:, :], pend.t[:, e:e + 1], None, ALU.is_ge, None, [thr, pend], [cmpt])
                self.tt(bef.t[:, :], bef.t[:, :], cmpt.t[:, :], ALU.add, [bef, cmpt], [bef])
            self.ts(bef.t[:, :], bef.t[:, :], 7.0, float(MSEG), ALU.min, ALU.mult, [bef], [bef])
            self.copy(bei.t[:, :], bef.t[:, :], [bef], [bei], eng="dve")
            for i in range(NT):
                x_ = xt[i % 2]
                self.dma("sp", x_.t[:], xin.t[i * 128:(i + 1) * 128, :], [xin], [x_])
                for di in (d1i, d2i):
                    self.P.dma_raw("pool", lambda di=di, x_=x_, i=i: nc.gpsimd.indirect_dma_start(
                        out=xslots.t[:, :], out_offset=bass.IndirectOffsetOnAxis(ap=di.t[:, i:i + 1], axis=0), in_=x_.t[:, :], in_offset=None),
                        self._b([x_, di]), self._b([xslots]))
        self.P._deps("sp", self._b([bei]), [])
        cur_reg = [None]

        def experts(tok0):
            b = tok0 // 512
            if cur_reg[0] is not None:
                nc.sync.free_register(cur_reg[0])
            reg = nc.sync.alloc_register()
            nc.sync.reg_load(reg, bei.t[0:1, b:b + 1])
            r = nc.sync.snap(reg, donate=True, min_val=0, max_val=7 * MSEG)
            cur_reg[0] = reg
            return [(("dynflat", mg, r, "gu"), ("dynflat", mu, r, "gu"), ("dynflat", md, r, "d"))]

        self.ffn(l, xslots, yslots, experts, DFE, prm, (0, NBLK * 512), slot_mode=True, wdep=self.wmoe)
        if cur_reg[0] is not None:
            nc.sync.free_register(cur_reg[0])
        with Phase(self) as ph:
            gB = self.bcast_row(ph, prm["ln2_g"][l:l + 1, :], D, "gB2")
            bB = self.bcast_row(ph, prm["ln2_b"][l:l + 1, :], D, "bB2")
            st6 = self.sb(ph, [128, 4, 6], F32, "st6")
            mv = self.sb(ph, [128, 4], F32, "mv")
            xt = [self.sb(ph, [128, D], F32, "xt") for _ in range(2)]
            ya = [self.sb(ph, [128, D], F32, "ya") for _ in range(2)]
            yb = [self.sb(ph, [128, D], F32, "yb") for _ in range(2)]
            for i in range(NT):
                x_, a_, b_ = xt[i % 2], ya[i % 2], yb[i % 2]
                self.dma("sp", x_.t[:], xin.t[i * 128:(i + 1) * 128, :], [xin], [x_])
                for di, y_ in ((d1i, a_), (d2i, b_)):
                    self.P.dma_raw("pool", lambda di=di, y_=y_, i=i: nc.gpsimd.indirect_dma_start(
                        out=y_.t[:, :], out_offset=None, in_=yslots.t[:, :], in_offset=bass.IndirectOffsetOnAxis(ap=di.t[:, i:i + 1], axis=0)),
                        self._b([yslots, di]), self._b([y_]))
                self.ts(a_.t[:, :], a_.t[:, :], G12.t[:, i, 0:1], None, ALU.mult, None, [a_, G12], [a_])
                self.stt(a_.t[:, :], b_.t[:, :], G12.t[:, i, 1:2], a_.t[:, :], ALU.mult, ALU.add, [b_, G12, a_], [a_])
                self.stt(x_.t[:, :], x_.t[:, :], ALPHA, a_.t[:, :], ALU.mult, ALU.add, [x_, a_], [x_])
                _ln_rows(self, x_, gB, bB, st6, mv, 1e-5)
                self.dma("pool", xout.t[i * 128:(i + 1) * 128, :], x_.t[:, :], [x_], [xout])


KB.moe_routed = _moe_routed
```

```python
import math
from contextlib import ExitStack
import numpy as np
import ml_dtypes
import concourse.bass as bass
import concourse.mybir as mybir
from concourse.bass_utils import run_bass_kernel_spmd

F32 = mybir.dt.float32
BF16 = mybir.dt.bfloat16
AF = mybir.ActivationFunctionType
ALU = mybir.AluOpType
SEM_LIMIT = 8000

L = 2
D = 2048
S = 2048
NSEQ = 2
T = NSEQ * S
INC = 5328
DFF = 5632
DFE = 7168
NE = 8
ALPHA = (2 * L) ** 0.25
NF = 17
NFP = NF * 128
C_QC, C_KVC, C_KPE, C_RQ, C_RK, C_RV, C_RG, C_MZ, C_XBC, C_DT, C_HU = 0, 384, 640, 704, 960, 1216, 1728, 2240, 2752, 3776, 3792
RET_LG_F = [math.log1p(-2.0 ** (-5.0 - h)) for h in range(4)]
RET_LG_B = [math.log1p(-2.0 ** (-5.5 - h)) for h in range(4)]


class Buf:
    __slots__ = ("name", "w", "r")

    def __init__(self, name=""):
        self.name = name
        self.w = {}
        self.r = {}


class Prog:
    def __init__(self, nc, stack):
        self.nc = nc
        self.stack = stack
        self.eng = {"pe": nc.tensor, "dve": nc.vector, "act": nc.scalar, "pool": nc.gpsimd, "sp": nc.sync}
        self.cur_sem, self.cnt, self.sems, self.nsem = {}, {}, {}, 0
        for e in self.eng:
            self._new_eng_sem(e)
        self.waited = {e: {} for e in self.eng}
        self.nslots = 8
        self.slots = {q: [[self._new_sem("d%s%d" % (q, i)), 0] for i in range(self.nslots)] for q in ("sp", "pool", "act")}
        self.slot_i = {q: 0 for q in self.slots}
        self.n_inst = 0

    def _new_sem(self, name):
        self.nsem += 1
        key = "%s_%d" % (name, self.nsem)
        self.sems[key] = self.stack.enter_context(self.nc.semaphore(key))
        return key

    def _new_eng_sem(self, e):
        self.cur_sem[e] = self._new_sem("c" + e)
        self.cnt[e] = 0

    def _wait(self, e, tok):
        if tok is None:
            return
        key, val, src = tok
        if src == e and e == "pe":
            return
        w = self.waited[e]
        if w.get(key, 0) >= val:
            return
        self.eng[e].wait_ge(self.sems[key], val)
        w[key] = val

    def _deps(self, e, reads, writes):
        for b in reads:
            for t in b.w.values():
                self._wait(e, t)
        for b in writes:
            for t in b.w.values():
                self._wait(e, t)
            for t in b.r.values():
                if t[2] != e:
                    self._wait(e, t)

    def _record(self, tok, reads, writes):
        for b in reads:
            b.r[tok[0]] = tok
        for b in writes:
            b.w[tok[0]] = tok
            b.r = {}

    def op(self, e, fn, reads=(), writes=()):
        self._deps(e, reads, writes)
        ins = fn()
        self.n_inst += 1
        if self.cnt[e] >= SEM_LIMIT:
            self._new_eng_sem(e)
        self.cnt[e] += 1
        ins.then_inc(self.sems[self.cur_sem[e]], 1)
        tok = (self.cur_sem[e], self.cnt[e], e)
        self._record(tok, reads, writes)
        return tok

    def dma(self, q, out, in_, reads=(), writes=(), **kw):
        self._deps(q, reads, writes)
        i = self.slot_i[q]
        self.slot_i[q] = (i + 1) % self.nslots
        sl = self.slots[q][i]
        if sl[1] > 0:
            self._wait(q, (sl[0], sl[1], "dma"))
        if sl[1] + 16 > SEM_LIMIT:
            sl[0] = self._new_sem("d%s%d" % (q, i))
            sl[1] = 0
        sl[1] += 16
        self.eng[q].dma_start(out=out, in_=in_, **kw).then_inc(self.sems[sl[0]], 16)
        tok = (sl[0], sl[1], "dma")
        self._record(tok, reads, writes)
        self.n_inst += 1
        return tok

    def dma_raw(self, q, emit, reads=(), writes=()):
        self._deps(q, reads, writes)
        i = self.slot_i[q]
        self.slot_i[q] = (i + 1) % self.nslots
        sl = self.slots[q][i]
        if sl[1] > 0:
            self._wait(q, (sl[0], sl[1], "dma"))
        if sl[1] + 16 > SEM_LIMIT:
            sl[0] = self._new_sem("d%s%d" % (q, i))
            sl[1] = 0
        sl[1] += 16
        emit().then_inc(self.sems[sl[0]], 16)
        tok = (sl[0], sl[1], "dma")
        self._record(tok, reads, writes)
        self.n_inst += 1
        return tok

    def barrier(self):
        toks = [(self.cur_sem[e], self.cnt[e], e) for e in self.eng if self.cnt[e] > 0]
        for q in self.slots:
            for key, val in self.slots[q]:
                if val:
                    toks.append((key, val, "dma"))
        for e in self.eng:
            for t in toks:
                if t[2] != e:
                    self._wait(e, t)

    def finish(self):
        for q in self.slots:
            for key, val in self.slots[q]:
                if val:
                    self._wait("sp", (key, val, "dma"))


class Phase(ExitStack):
    def __init__(self, kb):
        super().__init__()
        self.kb = kb

    def __exit__(self, *a):
        self.kb.P.barrier()
        return super().__exit__(*a)


class Tl:
    __slots__ = ("t", "b")

    def __init__(self, t, name=""):
        self.t = t
        self.b = Buf(name)


class KB:
    def __init__(self, nc, st):
        self.nc = nc
        self.st = st
        self.P = Prog(nc, st)
        self.uid = 0
        self.banks = [Tl(st.enter_context(nc.psum_tensor("bank%d" % i, [128, 512], F32)), "bank%d" % i) for i in range(8)]
        self.evac_i = 0
        self.consts = {}

    def defer(self, tag, fn):
        if not hasattr(self, "pending"):
            self.pending = []
        self.pending.append((tag, fn))

    def pump(self, n=1):
        p = getattr(self, "pending", None)
        while p and n > 0:
            p.pop(0)[1]()
            n -= 1

    def flush_tag(self, tag):
        p = getattr(self, "pending", None)
        if not p:
            return
        last = -1
        for i, (t, _) in enumerate(p):
            if t == tag:
                last = i
        for _ in range(last + 1):
            p.pop(0)[1]()

    def sb(self, stack, shape, dt, name="t"):
        self.uid += 1
        nm = "%s_%d" % (name, self.uid)
        return Tl(stack.enter_context(self.nc.sbuf_tensor(nm, list(shape), dt)), nm)

    def dram(self, name, shape, dt):
        return Tl(self.nc.dram_tensor(name, list(shape), dt).ap(), name)

    def cst(self, val):
        if val not in self.consts:
            t = self.sb(self.st, [128, 1], F32, "cst")
            self.P.op("pool", lambda: self.nc.gpsimd.memset(t.t[:], float(val)), writes=[t.b])
            self.consts[val] = t
        return self.consts[val]

    @staticmethod
    def _b(xs):
        return [x.b for x in xs]

    def act(self, out, in_, func, reads, writes, bias=None, scale=None):
        kw = {}
        rd = list(reads)
        if bias is not None:
            if isinstance(bias, (int, float)):
                c = self.cst(bias)
                rd.append(c)
                bias = c.t[0:out.shape[0], 0:1]
            kw["bias"] = bias
        if scale is not None:
            kw["scale"] = scale
        return self.P.op("act", lambda: self.nc.scalar.activation(out=out, in_=in_, func=func, **kw), self._b(rd), self._b(writes))

    def tt(self, out, in0, in1, op, reads, writes, eng="dve"):
        e = self.nc.vector if eng == "dve" else self.nc.gpsimd
        return self.P.op(eng, lambda: e.tensor_tensor(out=out, in0=in0, in1=in1, op=op), self._b(reads), self._b(writes))

    def ts(self, out, in0, s1, s2, op0, op1, reads, writes, eng="dve"):
        e = self.nc.vector if eng == "dve" else self.nc.gpsimd
        if op1 is None:
            return self.P.op(eng, lambda: e.tensor_scalar(out=out, in0=in0, scalar1=s1, scalar2=None, op0=op0), self._b(reads), self._b(writes))
        return self.P.op(eng, lambda: e.tensor_scalar(out=out, in0=in0, scalar1=s1, scalar2=s2, op0=op0, op1=op1), self._b(reads), self._b(writes))

    def stt(self, out, in0, scalar, in1, op0, op1, reads, writes):
        return self.P.op("dve", lambda: self.nc.vector.scalar_tensor_tensor(out=out, in0=in0, scalar=scalar, in1=in1, op0=op0, op1=op1), self._b(reads), self._b(writes))

    def copy(self, out, in_, reads, writes, eng=None):
        if eng is None:
            self.evac_i += 1
            eng = "act" if self.evac_i % 2 else "dve"
        if eng == "act":
            return self.P.op("act", lambda: self.nc.scalar.copy(out=out, in_=in_), self._b(reads), self._b(writes))
        e = self.nc.vector if eng == "dve" else self.nc.gpsimd
        return self.P.op(eng, lambda: e.tensor_copy(out=out, in_=in_), self._b(reads), self._b(writes))

    def recip(self, out, in_, reads, writes):
        return self.P.op("dve", lambda: self.nc.vector.reciprocal(out=out, in_=in_), self._b(reads), self._b(writes))

    def memset(self, out, val, writes, eng="pool"):
        e = self.nc.vector if eng == "dve" else self.nc.gpsimd
        return self.P.op(eng, lambda: e.memset(out, float(val)), [], self._b(writes))

    def mm(self, bank, out, lhsT, rhs, start, stop, reads):
        return self.P.op("pe", lambda: self.nc.tensor.matmul(out, lhsT=lhsT, rhs=rhs, start=start, stop=stop), self._b(reads), [bank.b])

    def tr(self, bank, out, in_, reads):
        rd = list(reads) + [self.ident]
        return self.P.op("pe", lambda: self.nc.tensor.transpose(out=out, in_=in_, identity=self.ident.t[0:in_.shape[0], 0:in_.shape[0]]), self._b(rd), [bank.b])

    def dma(self, q, out, in_, reads, writes, **kw):
        return self.P.dma(q, out, in_, self._b(reads), self._b(writes), **kw)

    def setup_consts(self):
        nc = self.nc
        self.ident = self.sb(self.st, [128, 128], F32, "ident")
        self.memset(self.ident.t[:], 1.0, [self.ident])
        self.P.op("pool", lambda: nc.gpsimd.affine_select(out=self.ident.t[:], in_=self.ident.t[:], pattern=[[1, 128]], compare_op=ALU.is_equal, fill=0.0, base=0, channel_multiplier=-1), self._b([self.ident]), self._b([self.ident]))
        self.ones_bf = self.sb(self.st, [128, 128], BF16, "ones_bf")
        self.memset(self.ones_bf.t[:], 1.0, [self.ones_bf])
        self.ones_f = self.sb(self.st, [128, 128], F32, "ones_f")
        self.memset(self.ones_f.t[:], 1.0, [self.ones_f])
        for v in (1e-6, 1e-5, 1.0, 0.0):
            self.cst(v)
        self.sel = self.sb(self.st, [16, 16, 128], F32, "sel")
        self.memset(self.sel.t[:], 0.0, [self.sel])
        self.P.op("pool", lambda: nc.gpsimd.affine_select(out=self.sel.t[:], in_=self.sel.t[:], pattern=[[-1, 16], [0, 128]], compare_op=ALU.not_equal, fill=1.0, base=0, channel_multiplier=1), self._b([self.sel]), self._b([self.sel]))

    def bcast_row(self, stack, row_ap, n, name):
        out = self.sb(stack, [128, n], F32, name)
        with Phase(self) as p:
            rowt = self.sb(p, [1, n], F32, name + "_row")
            self.dma("sp", rowt.t[:], row_ap, [], [rowt])
            for c in range(0, n, 512):
                w = min(512, n - c)
                bk = self.banks[(c // 512) % 2]
                self.mm(bk, bk.t[:, 0:w], self.ones_f.t[0:1, :], rowt.t[0:1, c:c + w], True, True, [self.ones_f, rowt])
                self.copy(out.t[:, c:c + w], bk.t[:, 0:w], [bk], [out])
        return out

    def load_xT(self, src, tok0, xT, xtiles, ntt=4, x32=None, post=None):
        for ts_ in range(ntt):
            xt = xtiles[ts_ % len(xtiles)]
            self.dma("sp", xt.t[:], src.t[tok0 + ts_ * 128: tok0 + (ts_ + 1) * 128, :], [src], [xt])
            for j in range(4):
                bk = self.banks[4 + j]
                for k in range(4):
                    dc = 4 * j + k
                    self.tr(bk, bk.t[:, k * 128:(k + 1) * 128], xt.t[:, dc * 128:(dc + 1) * 128], [xt])
                bv = bk.t[:].rearrange("p (a b) -> p a b", a=4)
                ce = "act" if j % 2 else "dve"
                self.copy(xT.t[:, 4 * j:4 * j + 4, ts_ * 128:(ts_ + 1) * 128], bv, [bk], [xT], eng=ce)
                if x32 is not None:
                    self.copy(x32.t[:, 4 * j:4 * j + 4, :], bv, [bk], [x32], eng=ce)
            if post is not None:
                post(ts_)

    def cast_dram(self, dst, src_ap, rows, step=256, tag=None):
        if src_ap.shape[-1] > 5632:
            step = 128
        for r0 in range(0, rows, step):
            r1 = min(rows, r0 + step)
            if tag is None:
                self.dma("pool", dst.t[r0:r1, :], src_ap[r0:r1, :], [], [dst])
            else:
                self.defer(tag, lambda r0=r0, r1=r1: self.dma("pool", dst.t[r0:r1, :], src_ap[r0:r1, :], [], [dst]))

    def build_rot(self, l, w_in_ap, wrot):
        with Phase(self) as ph:
            src = self.sb(ph, [128, 16, 576], F32, "rsrc")
            dst = self.sb(ph, [128, 16, 576], BF16, "rdst")
            self.dma("sp", src.t[:], w_in_ap[l, :, C_KPE:C_KPE + 576].rearrange("(dc p) c -> p dc c", p=128), [], [src])
            for dc in range(16):
                sv = src.t[:, dc, :].rearrange("p (g two h) -> p g two h", two=2, h=32)
                dv = dst.t[:, dc, :].rearrange("p (g two h) -> p g two h", two=2, h=32)
                self.ts(dv[:, :, 0, :], sv[:, :, 1, :], -1.0, None, ALU.mult, None, [src], [dst])
                self.copy(dv[:, :, 1, :], sv[:, :, 0, :], [src], [dst], eng="dve")
            self.dma("pool", wrot.t[l].rearrange("(dc p) c -> p dc c", p=128), dst.t[:], [dst], [wrot])

    def inproj(self, l, xres, winb, wrot, seg, cst, tok_range, wdep=None):
        with Phase(self) as ph:
            xT = self.sb(ph, [128, 16, 512], BF16, "xT")
            xtiles = [self.sb(ph, [128, 2048], F32, "xtile") for _ in range(2)]
            wbuf = [self.sb(ph, [128, 16, 576], BF16, "wbuf") for _ in range(3)]
            rbuf = self.sb(ph, [128, 16, 576], BF16, "rbuf")
            cos = self.sb(ph, [128, 2048], F32, "cos")
            sin = self.sb(ph, [128, 2048], F32, "sin")
            self.dma("sp", cos.t[:], cst["rope_cos"], [], [cos])
            self.dma("sp", sin.t[:], cst["rope_sin"], [], [sin])
            self.dma("sp", rbuf.t[:], wrot.t[l].rearrange("(dc p) c -> p dc c", p=128), [wrot], [rbuf])
            stage = [self.sb(ph, [128, 512], BF16, "stg") for _ in range(4)]
            st32 = [self.sb(ph, [128, 512], F32, "st32") for _ in range(3)]
            stdt = self.sb(ph, [16, 512], F32, "stdt")
            sti = [0]
            bki = [0]
            groups = [(0, 384, "fm", [("qc", 0, 0, 128), ("qc", 128, 128, 128), ("qc", 256, 256, 128)]),
                      (384, 256, "fm", [("kvc", 0, 0, 128), ("kvc", 128, 128, 128)]),
                      (640, 576, "rope", [("kpe", 0, 0, 64, 1.0), ("rq", 0, 64, 128, 1.0), ("rq", 128, 192, 128, 1.0),
                                          ("rk", 0, 320, 128, 0.125), ("rk", 128, 448, 128, 0.125)]),
                      (1216, 512, "tm", None),
                      (1728, 512, "fm", [("rg", i * 128, i * 128, 128) for i in range(4)]),
                      (2240, 512, "fm", [("mz", i * 128, i * 128, 128) for i in range(4)]),
                      (2752, 512, "fm", [("xbc", i * 128, i * 128, 128) for i in range(4)]),
                      (3264, 512, "fm", [("xbc", 512 + i * 128, i * 128, 128) for i in range(4)]),
                      (3776, 16, "dt", None),
                      (3792, 512, "fm", [("hu", i * 128, i * 128, 128) for i in range(4)]),
                      (4304, 512, "fm", [("hu", 512 + i * 128, i * 128, 128) for i in range(4)]),
                      (4816, 512, "fm", [("hu", 1024 + i * 128, i * 128, 128) for i in range(4)])]

            def loadw(gi):
                c0, cw = groups[gi][0], groups[gi][1]
                wt = wbuf[gi % 3]
                self.dma("sp", wt.t[:, :, 0:cw], winb.t[l, :, c0:c0 + cw].rearrange("(dc p) c -> p dc c", p=128), [wdep or winb], [wt])

            for tok0 in range(tok_range[0], tok_range[1], 512):
                tpos = tok0 % S
                self.load_xT(xres, tok0, xT, xtiles)
                loadw(0)
                loadw(1)
                for gi, (c0, cw, kind, chunks) in enumerate(groups):
                    self.pump(2)
                    if gi + 2 < len(groups):
                        loadw(gi + 2)
                    wt = wbuf[gi % 3]
                    if kind == "fm":
                        for (sname, row0, lc, n) in chunks:
                            bk = self.banks[bki[0] % 4]
                            bki[0] += 1
                            for dc in range(16):
                                self.mm(bk, bk.t[0:n, :], wt.t[:, dc, lc:lc + n], xT.t[:, dc, :], dc == 0, dc == 15, [wt, xT])
                            sg = stage[sti[0] % 4]
                            sti[0] += 1
                            self.copy(sg.t[0:n, :], bk.t[0:n, :], [bk], [sg])
                            self.dma("pool", seg[sname].t[row0:row0 + n, tok0:tok0 + 512], sg.t[0:n, :], [sg], [seg[sname]])
                    elif kind == "dt":
                        bk = self.banks[bki[0] % 4]
                        bki[0] += 1
                        for dc in range(16):
                            self.mm(bk, bk.t[0:16, :], wt.t[:, dc, 0:16], xT.t[:, dc, :], dc == 0, dc == 15, [wt, xT])
                        self.copy(stdt.t[:], bk.t[0:16, :], [bk], [stdt])
                        self.dma("pool", seg["dt"].t[:, tok0:tok0 + 512], stdt.t[:], [stdt], [seg["dt"]])
                    elif kind == "tm":
                        for ts_ in range(4):
                            bk = self.banks[bki[0] % 4]
                            bki[0] += 1
                            for dc in range(16):
                                self.mm(bk, bk.t[:, :], xT.t[:, dc, ts_ * 128:(ts_ + 1) * 128], wt.t[:, dc, 0:512], dc == 0, dc == 15, [wt, xT])
                            sg = stage[sti[0] % 4]
                            sti[0] += 1
                            self.copy(sg.t[:, :], bk.t[:, :], [bk], [sg])
                            self.dma("pool", seg["rv"].t[tok0 + ts_ * 128: tok0 + (ts_ + 1) * 128, :], sg.t[:, :], [sg], [seg["rv"]])
                    else:
                        for (sname, row0, lc, n, scl) in chunks:
                            bA = self.banks[bki[0] % 4]
                            bB = self.banks[(bki[0] + 1) % 4]
                            bki[0] += 2
                            for dc in range(16):
                                self.mm(bA, bA.t[0:n, :], wt.t[:, dc, lc:lc + n], xT.t[:, dc, :], dc == 0, dc == 15, [wt, xT])
                            for dc in range(16):
                                self.mm(bB, bB.t[0:n, :], rbuf.t[:, dc, lc:lc + n], xT.t[:, dc, :], dc == 0, dc == 15, [rbuf, xT])
                            self.tt(st32[0].t[0:n, :], bA.t[0:n, :], cos.t[0:n, tpos:tpos + 512], ALU.mult, [bA, cos], [st32[0]])
                            self.tt(st32[1].t[0:n, :], bB.t[0:n, :], sin.t[0:n, tpos:tpos + 512], ALU.mult, [bB, sin], [st32[1]])
                            self.tt(st32[2].t[0:n, :], st32[0].t[0:n, :], st32[1].t[0:n, :], ALU.add, [st32[0], st32[1]], [st32[2]])
                            sg = stage[sti[0] % 4]
                            sti[0] += 1
                            self.act(sg.t[0:n, :], st32[2].t[0:n, :], AF.Copy, [st32[2]], [sg], scale=scl)
                            self.dma("pool", seg[sname].t[row0:row0 + n, tok0:tok0 + 512], sg.t[0:n, :], [sg], [seg[sname]])

    def rms_rstd(self, ph, src, nch, rows, cols, nfeat, eps, bank, tmp_sq, out_rstd):
        c0, c1 = cols
        for c in range(nch):
            self.act(tmp_sq.t[0:rows, c, :], src.t[0:rows, c, c0:c1], AF.Square, [src], [tmp_sq])
        for c in range(nch):
            self.mm(bank, bank.t[:, :], self.ones_bf.t[0:rows, :], tmp_sq.t[0:rows, c, :], c == 0, c == nch - 1, [self.ones_bf, tmp_sq])
        self.act(out_rstd.t[:, :], bank.t[:, :], AF.Sqrt, [bank], [out_rstd], bias=eps, scale=1.0 / nfeat)
        self.recip(out_rstd.t[:, :], out_rstd.t[:, :], [out_rstd], [out_rstd])

    def group_ret(self, s, seg, mixedT):
        nc = self.nc
        t0 = s * S
        with Phase(self) as ph:
            rq = self.sb(ph, [128, 2, S], BF16, "rq")
            rk = self.sb(ph, [128, 2, S], BF16, "rk")
            rg = self.sb(ph, [128, 4, S], BF16, "rg")
            V = self.sb(ph, [128, 16, 512], BF16, "rv")
            self.dma("sp", rq.t[:], seg["rq"].t[:, t0:t0 + S].rearrange("(c p) t -> p c t", p=128), [seg["rq"]], [rq])
            self.dma("sp", rk.t[:], seg["rk"].t[:, t0:t0 + S].rearrange("(c p) t -> p c t", p=128), [seg["rk"]], [rk])
            self.dma("sp", rg.t[:], seg["rg"].t[:, t0:t0 + S].rearrange("(c p) t -> p c t", p=128), [seg["rg"]], [rg])
            self.dma("sp", V.t[:], seg["rv"].t[t0:t0 + S, :].rearrange("(c p) e -> p c e", p=128), [seg["rv"]], [V])
            W = 3968
            strip = self.sb(ph, [128, 4, W], BF16, "strip")
            dl = self.sb(ph, [128, W], F32, "dl")
            tA = self.sb(ph, [128, W], F32, "tA")
            tB = self.sb(ph, [128, W], F32, "tB")
            self.P.op("pool", lambda: nc.gpsimd.iota(dl.t[:], pattern=[[1, W]], base=-1920, channel_multiplier=-1, allow_small_or_imprecise_dtypes=True), [], [dl.b])
            for h in range(4):
                self.ts(tA.t[:], dl.t[:], 0.0, RET_LG_F[h], ALU.max, ALU.mult, [dl], [tA])
                self.ts(tB.t[:], dl.t[:], 0.0, -RET_LG_B[h], ALU.min, ALU.mult, [dl], [tB])
                self.tt(tA.t[:], tA.t[:], tB.t[:], ALU.add, [tA, tB], [tA])
                self.act(strip.t[:, h, :], tA.t[:], AF.Exp, [tA], [strip])
            Pb = [self.sb(ph, [128, 512], BF16, "P") for _ in range(6)]
            ysb = self.sb(ph, [128, 512], F32, "ysb")
            ysq = self.sb(ph, [128, 512], F32, "ysq")
            mean = self.sb(ph, [128, 512], F32, "mean")
            var = self.sb(ph, [128, 512], F32, "var")
            gate = self.sb(ph, [128, 512], F32, "gate")
            ob = [self.sb(ph, [128, 512], BF16, "ob") for _ in range(2)]
            pi = 0
            for h in range(4):
                c, base = h // 2, 64 * (h % 2)
                for ib in range(4):
                    self.pump(3)
                    i0 = ib * 512
                    yb = self.banks[4 + (h * 4 + ib) % 2]
                    for jc in range(16):
                        j0 = jc * 128
                        sbk = self.banks[jc % 4]
                        self.mm(sbk, sbk.t[:, :], rk.t[base:base + 64, c, j0:j0 + 128], rq.t[base:base + 64, c, i0:i0 + 512], True, True, [rk, rq])
                        pb = Pb[pi % 6]
                        pi += 1
                        x0 = i0 - j0 + 1920
                        self.tt(pb.t[:, :], sbk.t[:, :], strip.t[:, h, x0:x0 + 512], ALU.mult, [sbk, strip], [pb])
                        self.mm(yb, yb.t[:, :], V.t[:, jc, h * 128:(h + 1) * 128], pb.t[:, :], jc == 0, jc == 15, [V, pb])
                    self.copy(ysb.t[:, :], yb.t[:, :], [yb], [ysb], eng="act")
                    self.act(ysq.t[:, :], yb.t[:, :], AF.Square, [yb], [ysq])
                    mb, vb = self.banks[6], self.banks[7]
                    self.mm(mb, mb.t[:, :], self.ones_f.t[:, :], ysb.t[:, :], True, True, [self.ones_f, ysb])
                    self.mm(vb, vb.t[:, :], self.ones_f.t[:, :], ysq.t[:, :], True, True, [self.ones_f, ysq])
                    self.act(mean.t[:, :], mb.t[:, :], AF.Copy, [mb], [mean], scale=1.0 / 128)
                    self.tt(var.t[:, :], mean.t[:, :], mean.t[:, :], ALU.mult, [mean], [var])
                    self.stt(var.t[:, :], vb.t[:, :], 1.0 / 128, var.t[:, :], ALU.mult, ALU.subtract, [vb, var], [var])
                    self.act(var.t[:, :], var.t[:, :], AF.Sqrt, [var], [var], bias=1e-6, scale=1.0)
                    self.recip(var.t[:, :], var.t[:, :], [var], [var])
                    self.tt(ysb.t[:, :], ysb.t[:, :], mean.t[:, :], ALU.subtract, [ysb, mean], [ysb])
                    self.tt(ysb.t[:, :], ysb.t[:, :], var.t[:, :], ALU.mult, [ysb, var], [ysb])
                    self.act(gate.t[:, :], rg.t[:, h, i0:i0 + 512], AF.Silu, [rg], [gate])
                    o = ob[(h * 4 + ib) % 2]
                    self.tt(o.t[:, :], ysb.t[:, :], gate.t[:, :], ALU.mult, [ysb, gate], [o])
                    self.dma("pool", mixedT.t[512 + h * 128: 512 + (h + 1) * 128, t0 + i0: t0 + i0 + 512], o.t[:, :], [o], [mixedT])

    def group_mla(self, l, s, seg, mixedT, prm, cst):
        t0 = s * S
        SC = (128 + 64) ** -0.5
        with Phase(self) as ph:
            qc = self.sb(ph, [128, 3, S], BF16, "qc")
            kvc = self.sb(ph, [128, 2, S], BF16, "kvc")
            kpe = self.sb(ph, [64, S], BF16, "kpe")
            self.dma("sp", qc.t[:], seg["qc"].t[:, t0:t0 + S].rearrange("(c p) t -> p c t", p=128), [seg["qc"]], [qc])
            self.dma("sp", kvc.t[:], seg["kvc"].t[:, t0:t0 + S].rearrange("(c p) t -> p c t", p=128), [seg["kvc"]], [kvc])
            self.dma("sp", kpe.t[:], seg["kpe"].t[:, t0:t0 + S], [seg["kpe"]], [kpe])
            cos = self.sb(ph, [64, S], F32, "cos")
            sin = self.sb(ph, [64, S], F32, "sin")
            self.dma("sp", cos.t[:], cst["rope_cos"][0:64, :], [], [cos])
            self.dma("sp", sin.t[:], cst["rope_sin"][0:64, :], [], [sin])
            wq32 = self.sb(ph, [128, 3, 768], F32, "wq32")
            wkv32 = self.sb(ph, [128, 2, 1024], F32, "wkv32")
            self.dma("sp", wq32.t[:], prm["mla_w_uq"][l].rearrange("(c p) e -> p c e", p=128), [], [wq32])
            self.dma("sp", wkv32.t[:], prm["mla_w_ukv"][l].rearrange("(c p) e -> p c e", p=128), [], [wkv32])
            wq = self.sb(ph, [128, 3, 768], BF16, "wq")
            wkv = self.sb(ph, [128, 2, 1024], BF16, "wkv")
            wqr = self.sb(ph, [128, 3, 4, 64], BF16, "wqr")
            self.copy(wq.t[:], wq32.t[:], [wq32], [wq], eng="dve")
            self.copy(wkv.t[:], wkv32.t[:], [wkv32], [wkv], eng="dve")
            for c in range(3):
                for h in range(4):
                    b0 = 192 * h + 128
                    self.ts(wqr.t[:, c, h, 0:32], wq32.t[:, c, b0 + 32:b0 + 64], -1.0, None, ALU.mult, None, [wq32], [wqr])
                    self.copy(wqr.t[:, c, h, 32:64], wq32.t[:, c, b0:b0 + 32], [wq32], [wqr], eng="dve")
            qnw = self.sb(ph, [128, 3], F32, "qnw")
            kvnw = self.sb(ph, [128, 2], F32, "kvnw")
            onw = self.sb(ph, [128, 4], F32, "onw")
            self.dma("sp", qnw.t[:], prm["mla_q_norm_pp"][:, l * 3:(l + 1) * 3], [], [qnw])
            self.dma("sp", kvnw.t[:], prm["mla_kv_norm_pp"][:, l * 2:(l + 1) * 2], [], [kvnw])
            self.dma("sp", onw.t[:], prm["mla_out_norm_pp"][:, l * 4:(l + 1) * 4], [], [onw])
            qn = self.sb(ph, [128, 3, S], BF16, "qn")
            kvn = self.sb(ph, [128, 2, S], BF16, "kvn")
            sq = self.sb(ph, [128, 4, 512], BF16, "sq")
            rstd = self.sb(ph, [128, 512], F32, "rstd")
            for tb in range(4):
                c0 = tb * 512
                self.rms_rstd(ph, qc, 3, 128, (c0, c0 + 512), 384.0, 1e-6, self.banks[0], sq, rstd)
                for c in range(3):
                    self.stt(qn.t[:, c, c0:c0 + 512], qc.t[:, c, c0:c0 + 512], qnw.t[:, c:c + 1], rstd.t[:, :], ALU.mult, ALU.mult, [qc, qnw, rstd], [qn])
                self.rms_rstd(ph, kvc, 2, 128, (c0, c0 + 512), 256.0, 1e-6, self.banks[1], sq, rstd)
                for c in range(2):
                    self.stt(kvn.t[:, c, c0:c0 + 512], kvc.t[:, c, c0:c0 + 512], kvnw.t[:, c:c + 1], rstd.t[:, :], ALU.mult, ALU.mult, [kvc, kvnw, rstd], [kvn])
            qhn = self.sb(ph, [128, 4, S], BF16, "qhn")
            qhp = self.sb(ph, [64, 4, S], BF16, "qhp")
            khn = self.sb(ph, [128, 4, S], BF16, "khn")
            Vt = self.sb(ph, [128, 16, 512], BF16, "Vt")
            t1 = self.sb(ph, [64, 512], F32, "t1")
            t2 = self.sb(ph, [64, 512], F32, "t2")
            bi = 0
            for tb in range(4):
                c0 = tb * 512
                for h in range(4):
                    bk = self.banks[bi % 4]; bi += 1
                    for c in range(3):
                        self.mm(bk, bk.t[:, :], wq.t[:, c, 192 * h:192 * h + 128], qn.t[:, c, c0:c0 + 512], c == 0, c == 2, [wq, qn])
                    self.copy(qhn.t[:, h, c0:c0 + 512], bk.t[:, :], [bk], [qhn])
                    bA = self.banks[bi % 4]; bi += 1
                    bB = self.banks[bi % 4]; bi += 1
                    for c in range(3):
                        self.mm(bA, bA.t[0:64, :], wq.t[:, c, 192 * h + 128:192 * h + 192], qn.t[:, c, c0:c0 + 512], c == 0, c == 2, [wq, qn])
                    for c in range(3):
                        self.mm(bB, bB.t[0:64, :], wqr.t[:, c, h, :], qn.t[:, c, c0:c0 + 512], c == 0, c == 2, [wqr, qn])
                    self.tt(t1.t[:, :], bA.t[0:64, :], cos.t[:, c0:c0 + 512], ALU.mult, [bA, cos], [t1])
                    self.tt(t2.t[:, :], bB.t[0:64, :], sin.t[:, c0:c0 + 512], ALU.mult, [bB, sin], [t2])
                    self.tt(qhp.t[:, h, c0:c0 + 512], t1.t[:, :], t2.t[:, :], ALU.add, [t1, t2], [qhp])
                    bk = self.banks[bi % 4]; bi += 1
                    for c in range(2):
                        self.mm(bk, bk.t[:, :], wkv.t[:, c, 256 * h:256 * h + 128], kvn.t[:, c, c0:c0 + 512], c == 0, c == 1, [wkv, kvn])
                    self.copy(khn.t[:, h, c0:c0 + 512], bk.t[:, :], [bk], [khn])
            for tc in range(16):
                bk = self.banks[bi % 4]; bi += 1
                for h in range(4):
                    for c in range(2):
                        self.mm(bk, bk.t[:, h * 128:(h + 1) * 128], kvn.t[:, c, tc * 128:(tc + 1) * 128], wkv.t[:, c, 256 * h + 128:256 * h + 256], c == 0, c == 1, [wkv, kvn])
                self.copy(Vt.t[:, tc, :], bk.t[:, :], [bk], [Vt])
            Pb = [self.sb(ph, [128, 512], BF16, "P") for _ in range(6)]
            oblk = self.sb(ph, [128, 4, 512], F32, "oblk")
            den = self.sb(ph, [128, 512], F32, "den")
            ob = [self.sb(ph, [128, 512], BF16, "ob") for _ in range(2)]
            pi = 0
            for qb in range(4):
                q0 = qb * 512
                for h in range(4):
                    self.pump(3)
                    ob_k, dn_k = self.banks[4 + 2 * (h % 2)], self.banks[5 + 2 * (h % 2)]
                    for kc in range(16):
                        k0 = kc * 128
                        sbk = self.banks[kc % 3]
                        self.mm(sbk, sbk.t[:, :], khn.t[:, h, k0:k0 + 128], qhn.t[:, h, q0:q0 + 512], True, False, [khn, qhn])
                        self.mm(sbk, sbk.t[:, :], kpe.t[0:64, k0:k0 + 128], qhp.t[0:64, h, q0:q0 + 512], False, True, [kpe, qhp])
                        pb = Pb[pi % 6]; pi += 1
                        self.act(pb.t[:, :], sbk.t[:, :], AF.Exp, [sbk], [pb], scale=SC)
                        self.mm(ob_k, ob_k.t[:, :], Vt.t[:, kc, h * 128:(h + 1) * 128], pb.t[:, :], kc == 0, kc == 15, [Vt, pb])
                        self.mm(dn_k, dn_k.t[:, :], self.ones_bf.t[:, :], pb.t[:, :], kc == 0, kc == 15, [self.ones_bf, pb])
                    self.recip(den.t[:, :], dn_k.t[:, :], [dn_k], [den])
                    self.tt(oblk.t[:, h, :], ob_k.t[:, :], den.t[:, :], ALU.mult, [ob_k, den], [oblk])
                for h in range(4):
                    self.act(sq.t[:, h, :], oblk.t[:, h, :], AF.Square, [oblk], [sq])
                nb = self.banks[3]
                for h in range(4):
                    self.mm(nb, nb.t[:, :], self.ones_bf.t[:, :], sq.t[:, h, :], h == 0, h == 3, [self.ones_bf, sq])
                self.act(rstd.t[:, :], nb.t[:, :], AF.Sqrt, [nb], [rstd], bias=1e-6, scale=1.0 / 512)
                self.recip(rstd.t[:, :], rstd.t[:, :], [rstd], [rstd])
                for h in range(4):
                    o = ob[h % 2]
                    self.stt(o.t[:, :], oblk.t[:, h, :], onw.t[:, h:h + 1], rstd.t[:, :], ALU.mult, ALU.mult, [oblk, onw, rstd], [o])
                    self.dma("pool", mixedT.t[h * 128:(h + 1) * 128, t0 + q0:t0 + q0 + 512], o.t[:, :], [o], [mixedT])


def _pp(v, nl):
    v = np.asarray(v, np.float32)
    n = v.shape[-1]
    return np.ascontiguousarray(v.reshape(nl, n // 128, 128).transpose(2, 0, 1).reshape(128, nl * (n // 128)))


def host_consts():
    c = {}
    half = 32
    inv = (10000.0 ** (-np.arange(half, dtype=np.float32) * 2.0 / 64)).astype(np.float32)
    ang = np.arange(S, dtype=np.float32)[None, :] * inv[:, None]
    c["rope_cos"] = np.ascontiguousarray(np.tile(np.cos(ang), (4, 1)).astype(np.float32))
    c["rope_sin"] = np.ascontiguousarray(np.tile(np.sin(ang), (4, 1)).astype(np.float32))
    t = np.linspace(0.0, 1.0, S, dtype=np.float32)[:, None]
    bands = 16
    angp = (2.0 * math.pi * np.arange(S, dtype=np.float32)[:, None] / S).astype(np.float32)
    f = np.linspace(1e-4, bands - 1, bands, dtype=np.float32)[None, :]
    z = np.concatenate([t, np.cos(f * angp), -np.sin(f * angp)], axis=-1).astype(np.float32)
    c["hy_zT"] = np.ascontiguousarray(z.T)
    c["hy_ntlin_pp"] = np.ascontiguousarray((-t[:, 0]).reshape(16, 128).T.astype(np.float32))
    mn, mx = math.log(1e-2) / 1.5, math.log(1e-2) / 0.3
    dl = np.abs(np.linspace(mn, mx, 512, dtype=np.float32))
    c["hy_delta_b"] = np.ascontiguousarray(np.tile(dl[None, :], (128, 1)).astype(np.float32))
    idx = np.arange(NFP, dtype=np.int64)
    ph_ = (np.outer(idx, idx) % 4096).astype(np.float64) * (2.0 * math.pi / 4096.0)
    valid = (idx <= 2048).astype(np.float64)
    Cm = np.cos(ph_) * valid[:, None] * valid[None, :]
    Sm = -np.sin(ph_) * valid[:, None] * valid[None, :]
    bf = ml_dtypes.bfloat16
    c["dft_Cnat"] = np.ascontiguousarray(Cm[:, :S].astype(np.float32).astype(bf))
    c["dft_Snat"] = np.ascontiguousarray(Sm[:, :S].astype(np.float32).astype(bf))
    c["dft_Cblk"] = np.ascontiguousarray(Cm[:S, :].reshape(16, 128, NF, 128).transpose(2, 1, 0, 3).astype(np.float32).astype(bf))
    c["dft_Sblk"] = np.ascontiguousarray(Sm[:S, :].reshape(16, 128, NF, 128).transpose(2, 1, 0, 3).astype(np.float32).astype(bf))
    wfv = np.where(idx <= 2048, 2.0, 0.0)
    wfv[0] = 1.0
    wfv[2048] = 1.0
    c["dft_wf_pp"] = np.ascontiguousarray((wfv / 4096.0).reshape(NF, 128).T.astype(np.float32))
    return c


def host_params(inp):
    p = {}
    p["mla_w_uq"] = np.ascontiguousarray(inp["mla_w_uq"], dtype=np.float32)
    p["mla_w_ukv"] = np.ascontiguousarray(inp["mla_w_ukv"], dtype=np.float32)
    p["mla_q_norm_pp"] = _pp(inp["mla_q_norm"], L)
    p["mla_kv_norm_pp"] = _pp(inp["mla_kv_norm"], L)
    p["mla_out_norm_pp"] = _pp(inp["mla_out_norm"], L)
    cwv = np.asarray(inp["ssd_conv_w"], np.float32)
    p["ssd_conv_w_pp"] = np.ascontiguousarray(cwv.reshape(L, 5, 8, 128).transpose(3, 0, 2, 1).reshape(128, L * 40))
    p["ssd_conv_b_pp"] = _pp(inp["ssd_conv_b"], L)
    p["ssd_dtb"] = np.ascontiguousarray(np.asarray(inp["ssd_dt_bias"], np.float32).reshape(L, 16).T)
    p["ssd_alog"] = np.ascontiguousarray(np.asarray(inp["ssd_a_log"], np.float32).reshape(L, 16).T)
    dd = np.asarray(inp["ssd_d"], np.float32)
    p["ssd_d_pp"] = np.ascontiguousarray(np.repeat(dd, 64, axis=1).reshape(L, 4, 128).transpose(2, 0, 1).reshape(128, L * 4))
    p["ssd_norm_pp"] = _pp(inp["ssd_norm"], L)
    hw = np.asarray(inp["hy_conv_w"], np.float32)
    p["hy_conv_w_pp"] = np.ascontiguousarray(hw.reshape(L, 3, 12, 128).transpose(3, 0, 2, 1).reshape(128, L * 36))
    p["hy_conv_b_pp"] = _pp(inp["hy_conv_b"], L)
    p["hy_w1"] = np.ascontiguousarray(inp["hy_w1"], dtype=np.float32)
    p["hy_w2"] = np.ascontiguousarray(inp["hy_w2"], dtype=np.float32)
    p["hy_w3"] = np.ascontiguousarray(inp["hy_w3"], dtype=np.float32)
    b12 = np.stack([np.asarray(inp["hy_b1"], np.float32), np.asarray(inp["hy_b2"], np.float32)], 1)
    p["hy_b_pp"] = np.ascontiguousarray(b12.transpose(2, 0, 1).reshape(64, L * 2))
    p["hy_freq_pp"] = np.ascontiguousarray(np.asarray(inp["hy_freq"], np.float32).transpose(2, 0, 1).reshape(64, L * 2))
    hbv = np.asarray(inp["hy_bias"], np.float32)
    p["hy_bias_pp"] = np.ascontiguousarray(hbv.reshape(L, 2, 4, 128).transpose(3, 0, 1, 2).reshape(128, L * 8))
    p["hy_out_norm_pp"] = _pp(inp["hy_out_norm"], L)
    p["dirsign"] = np.concatenate([-np.ones((8, 1), np.float32), np.ones((8, 1), np.float32)], 0)
    p["ndirmask"] = np.concatenate([np.zeros((8, 1), np.float32), -np.ones((8, 1), np.float32)], 0)
    return p


def build(cfg, shapes):
    nc = bass.Bass("TRN2", target_bir_lowering=False)
    ext = {}
    for name, (shape, dt) in shapes.items():
        ext[name] = nc.dram_tensor(name, list(shape), dt, kind="ExternalInput").ap()
    with ExitStack() as st:
        kb = KB(nc, st)
        kb.setup_consts()
        winb = kb.dram("winb", [L, D, INC], BF16)
        wrot = kb.dram("wrot", [L, D, 576], BF16)
        seg = {"qc": kb.dram("s_qc", [384, T], BF16), "kvc": kb.dram("s_kvc", [256, T], BF16),
               "kpe": kb.dram("s_kpe", [64, T], BF16), "rq": kb.dram("s_rq", [256, T], BF16),
               "rk": kb.dram("s_rk", [256, T], BF16), "rg": kb.dram("s_rg", [512, T], BF16),
               "mz": kb.dram("s_mz", [512, T], BF16), "xbc": kb.dram("s_xbc", [1024, T], BF16),
               "dt": kb.dram("s_dt", [16, T], F32), "hu": kb.dram("s_hu", [1536, T], BF16),
               "rv": kb.dram("s_rv", [T, 512], BF16)}
        xin = Tl(ext["x"], "x")
        if cfg.get("dbg"):
            kb.dbg = {"BC": Tl(nc.dram_tensor("d_BC", [16, S], F32, kind="ExternalOutput").ap()),
                      "dt_tok": Tl(nc.dram_tensor("d_dt_tok", [128, 256], F32, kind="ExternalOutput").ap()),
                      "bias_tok": Tl(nc.dram_tensor("d_bias_tok", [128, 256], F32, kind="ExternalOutput").ap()),
                      "xsT": Tl(nc.dram_tensor("d_xsT", [128, S], F32, kind="ExternalOutput").ap()),
                      "xdt": Tl(nc.dram_tensor("d_xdt", [128, 1024], BF16, kind="ExternalOutput").ap())}
        nseq = cfg.get("nseq", NSEQ)
        if cfg["mode"] == "mixtest":
            l = cfg["layer"]
            mixedT = Tl(nc.dram_tensor("mixedT", [D, T], BF16, kind="ExternalOutput").ap(), "mixedT")
            kb.cast_dram(Tl(winb.t[l], "x").__class__(winb.t[l]) if False else _sub(winb, winb.t[l]), ext["w_in"][l], D)
            kb.build_rot(l, ext["w_in"], wrot)
            kb.inproj(l, xin, winb, wrot, seg, ext, (0, nseq * S))
            for s in range(nseq):
                if "A" in cfg["groups"]:
                    kb.group_mla(l, s, seg, mixedT, ext, ext)
                if "B" in cfg["groups"]:
                    kb.group_ret(s, seg, mixedT)
                if "C" in cfg["groups"]:
                    kb.group_ssd(l, s, seg, mixedT, ext)
            if "D" in cfg["groups"]:
                Hs = kb.dram("Hs", [2, 2, NFP, 512], F32)
                hyu = kb.dram("hyu", [3, NSEQ * 512, S], F32)
                z1s = kb.dram("z1s", [NSEQ * 512, S], F32)
                kb.hyena_filter(l, ext, ext, Hs)
                kb.group_hyena(l, nseq, seg, mixedT, ext, ext, Hs, hyu, z1s)
        kb.P.finish()
        print("instructions:", kb.P.n_inst, "sems:", kb.P.nsem)
    return nc


def _sub(parent, ap):
    t = Tl(ap, parent.b.name)
    t.b = parent.b
    return t


def _group_ssd(self, l, s, seg, mixedT, prm):
    nc = self.nc
    t0 = s * S
    with Phase(self) as ph:
        xsT = self.sb(ph, [128, 4, S], F32, "xsT")
        BT = self.sb(ph, [128, 2, S], BF16, "BT")
        CT = self.sb(ph, [128, 2, S], BF16, "CT")
        mz = self.sb(ph, [128, 4, S], BF16, "mz")
        self.dma("sp", mz.t[:], seg["mz"].t[:, t0:t0 + S].rearrange("(c p) t -> p c t", p=128), [seg["mz"]], [mz])
        cw = self.sb(ph, [128, 40], F32, "cw")
        cb = self.sb(ph, [128, 8], F32, "cb")
        dpp = self.sb(ph, [128, 4], F32, "dpp")
        nw = self.sb(ph, [128, 4], F32, "nw")
        self.dma("sp", cw.t[:], prm["ssd_conv_w_pp"][:, l * 40:(l + 1) * 40], [], [cw])
        self.dma("sp", cb.t[:], prm["ssd_conv_b_pp"][:, l * 8:(l + 1) * 8], [], [cb])
        self.dma("sp", dpp.t[:], prm["ssd_d_pp"][:, l * 4:(l + 1) * 4], [], [dpp])
        self.dma("sp", nw.t[:], prm["ssd_norm_pp"][:, l * 4:(l + 1) * 4], [], [nw])
        dt_tok = self.sb(ph, [128, 16, 16], F32, "dt_tok")
        bias_tok = self.sb(ph, [128, 16, 16], F32, "bias_tok")
        BC = self.sb(ph, [16, S], F32, "BC")
        xdt = [self.sb(ph, [128, 16, 8, 128], BF16, "xdt%d" % d) for d in range(2)]
        for d in range(2):
            self.memset(xdt[d].t[:], 0.0, [xdt[d]])
        with Phase(self) as p2:
            raw = self.sb(p2, [128, 8, S], BF16, "raw")
            acc = self.sb(p2, [128, S], F32, "acc")
            self.dma("sp", raw.t[:], seg["xbc"].t[:, t0:t0 + S].rearrange("(c p) t -> p c t", p=128), [seg["xbc"]], [raw])
            for c in range(8):
                self.ts(acc.t[:, :], raw.t[:, c, :], cw.t[:, c * 5 + 2:c * 5 + 3], None, ALU.mult, None, [raw, cw], [acc])
                for k in (0, 1, 3, 4):
                    sh = k - 2
                    a0, a1 = max(0, -sh), S - max(0, sh)
                    self.stt(acc.t[:, a0:a1], raw.t[:, c, a0 + sh:a1 + sh], cw.t[:, c * 5 + k:c * 5 + k + 1], acc.t[:, a0:a1], ALU.mult, ALU.add, [raw, cw, acc], [acc])
                if c < 4:
                    dst, dtl = xsT.t[:, c, :], xsT
                elif c < 6:
                    dst, dtl = BT.t[:, c - 4, :], BT
                else:
                    dst, dtl = CT.t[:, c - 6, :], CT
                self.act(dst, acc.t[:, :], AF.Silu, [acc, cb], [dtl], bias=cb.t[:, c:c + 1])
        with Phase(self) as p2:
            dtr = self.sb(p2, [16, S], F32, "dtr")
            ax = self.sb(p2, [16, S], F32, "ax")
            dtv = self.sb(p2, [16, S], F32, "dtv")
            la = self.sb(p2, [16, S], F32, "la")
            cs = self.sb(p2, [16, S], F32, "cs")
            one16 = self.sb(p2, [16, S], F32, "one16")
            sm = self.sb(p2, [16, 8], F32, "sm")
            self.dma("sp", dtr.t[:], seg["dt"].t[:, t0:t0 + S], [seg["dt"]], [dtr])
            self.dma("sp", sm.t[:, 0:1], prm["ssd_dtb"][:, l:l + 1], [], [sm], allow_slow_non_contiguous=True)
            self.dma("sp", sm.t[:, 1:2], prm["ssd_alog"][:, l:l + 1], [], [sm], allow_slow_non_contiguous=True)
            self.dma("sp", sm.t[:, 2:3], prm["dirsign"][:, 0:1], [], [sm], allow_slow_non_contiguous=True)
            self.dma("sp", sm.t[:, 3:4], prm["ndirmask"][:, 0:1], [], [sm], allow_slow_non_contiguous=True)
            self.ts(dtr.t[:], dtr.t[:], sm.t[:, 0:1], None, ALU.add, None, [dtr, sm], [dtr])
            self.stt(ax.t[:], dtr.t[:], -1.0, dtr.t[:], ALU.mult, ALU.max, [dtr], [ax])
            self.act(ax.t[:], ax.t[:], AF.Exp, [ax], [ax], scale=-1.0)
            self.act(ax.t[:], ax.t[:], AF.Ln, [ax], [ax], bias=1.0)
            self.stt(dtv.t[:], dtr.t[:], 0.0, ax.t[:], ALU.max, ALU.add, [dtr, ax], [dtv])
            self.act(sm.t[:, 4:5], sm.t[:, 1:2], AF.Exp, [sm], [sm])
            self.ts(sm.t[:, 5:6], sm.t[:, 4:5], -1.0, None, ALU.mult, None, [sm], [sm])
            self.ts(la.t[:], dtv.t[:], sm.t[:, 5:6], None, ALU.mult, None, [dtv, sm], [la])
            self.memset(one16.t[:], 1.0, [one16])
            self.P.op("dve", lambda: nc.vector.tensor_tensor_scan(out=cs.t[:], data0=one16.t[:], data1=la.t[:], initial=0.0, op0=ALU.mult, op1=ALU.add), self._b([one16, la]), self._b([cs]))
            self.stt(BC.t[:], la.t[:], sm.t[:, 3:4], cs.t[:], ALU.mult, ALU.add, [la, sm, cs], [BC])
            self.ts(cs.t[:], BC.t[:], sm.t[:, 2:3], None, ALU.mult, None, [BC, sm], [cs])
            b6, b7 = self.banks[6], self.banks[7]
            for tc in range(16):
                self.tr(b6, b6.t[:, tc * 16:(tc + 1) * 16], dtv.t[0:16, tc * 128:(tc + 1) * 128], [dtv])
                self.tr(b7, b7.t[:, tc * 16:(tc + 1) * 16], cs.t[0:16, tc * 128:(tc + 1) * 128], [cs])
            self.copy(dt_tok.t[:], b6.t[:, 0:256].rearrange("p (a b) -> p a b", a=16), [b6], [dt_tok])
            self.copy(bias_tok.t[:], b7.t[:, 0:256].rearrange("p (a b) -> p a b", a=16), [b7], [bias_tok])
            for tc in range(16):
                bk = self.banks[4 + tc % 2]
                for c in range(4):
                    self.tr(bk, bk.t[:, c * 128:(c + 1) * 128], xsT.t[:, c, tc * 128:(tc + 1) * 128], [xsT])
                for d in range(2):
                    for h in range(8):
                        self.ts(xdt[d].t[:, tc, h, 64 * (h % 2):64 * (h % 2) + 64], bk.t[:, h * 64:(h + 1) * 64], dt_tok.t[:, tc, d * 8 + h:d * 8 + h + 1], None, ALU.mult, None, [bk, dt_tok], [xdt[d]])
        if getattr(self, "dbg", None) is not None:
            self.dma("pool", self.dbg["BC"].t[:, :], BC.t[:, :], [BC], [self.dbg["BC"]])
            self.dma("pool", self.dbg["dt_tok"].t[:, :], dt_tok.t[:].rearrange("p a b -> p (a b)"), [dt_tok], [self.dbg["dt_tok"]])
            self.dma("pool", self.dbg["bias_tok"].t[:, :], bias_tok.t[:].rearrange("p a b -> p (a b)"), [bias_tok], [self.dbg["bias_tok"]])
            self.dma("pool", self.dbg["xsT"].t[:, :], xsT.t[:, 0, :], [xsT], [self.dbg["xsT"]])
            self.dma("pool", self.dbg["xdt"].t[:, :], xdt[0].t[:, 0, :, :].rearrange("p a b -> p (a b)"), [xdt[0]], [self.dbg["xdt"]])
        Mf = self.sb(ph, [128, 896], BF16, "Mf")
        Mb = self.sb(ph, [128, 896], BF16, "Mb")
        self.memset(Mf.t[:], 1.0, [Mf])
        self.memset(Mb.t[:], 1.0, [Mb])
        self.P.op("pool", lambda: nc.gpsimd.affine_select(out=Mf.t[:], in_=Mf.t[:], pattern=[[1, 896]], compare_op=ALU.is_ge, fill=0.0, base=-384, channel_multiplier=-1), self._b([Mf]), self._b([Mf]))
        self.P.op("pool", lambda: nc.gpsimd.affine_select(out=Mb.t[:], in_=Mb.t[:], pattern=[[-1, 896]], compare_op=ALU.is_gt, fill=0.0, base=384, channel_multiplier=1), self._b([Mb]), self._b([Mb]))
        bcs = [[self.sb(ph, [128, 512], F32, "bcs") for _ in range(2)] for _ in range(2)]
        Lb = [self.sb(ph, [128, 512], F32, "L") for _ in range(6)]
        Pb = [self.sb(ph, [128, 512], BF16, "P") for _ in range(6)]
        ybuf = self.sb(ph, [128, 4, 512], F32, "ybuf")
        yv = self.sb(ph, [128, 512], F32, "yv")
        gate = self.sb(ph, [128, 512], F32, "gate")
        sq = self.sb(ph, [128, 4, 512], BF16, "sq")
        rstd = self.sb(ph, [128, 512], F32, "rstd")
        ob = [self.sb(ph, [128, 512], BF16, "ob") for _ in range(2)]
        li = 0
        for ib in range(4):
            i0 = ib * 512
            for pair in range(4):
                self.pump(3)
                g = pair // 2
                yb = self.banks[4 + pair % 2]
                for d in range(2):
                    for hh in range(2):
                        h = pair * 2 + hh
                        bb = self.banks[3]
                        self.mm(bb, bb.t[:, :], self.sel.t[:, d * 8 + h, :], BC.t[0:16, i0:i0 + 512], True, True, [self.sel, BC])
                        self.copy(bcs[d][hh].t[:, :], bb.t[:, :], [bb], [bcs[d][hh]])
                items = []
                for jc in range(16):
                    for d in range(2):
                        valid = (jc <= 4 * ib + 3) if d == 0 else (jc >= 4 * ib)
                        if valid:
                            for hh in range(2):
                                items.append((jc, d, hh))
                last_jc = -1
                for n, (jc, d, hh) in enumerate(items):
                    j0 = jc * 128
                    h = pair * 2 + hh
                    if jc != last_jc:
                        sbk = self.banks[(0, 1, 2, 7)[jc % 4]]
                        self.mm(sbk, sbk.t[:, :], BT.t[:, g, j0:j0 + 128], CT.t[:, g, i0:i0 + 512], True, True, [BT, CT])
                        last_jc = jc
                    diag = 4 * ib <= jc <= 4 * ib + 3
                    Lt = Lb[li % 6]
                    pb = Pb[li % 6]
                    li += 1
                    sgn = 1.0 if d == 0 else -1.0
                    bia = bias_tok.t[:, jc, d * 8 + h:d * 8 + h + 1]
                    if diag:
                        self.ts(Lt.t[:, :], bcs[d][hh].t[:, :], sgn, bia, ALU.mult, ALU.add, [bcs[d][hh], bias_tok], [Lt])
                        self.ts(Lt.t[:, :], Lt.t[:, :], 0.0, None, ALU.min, None, [Lt], [Lt])
                        self.act(Lt.t[:, :], Lt.t[:, :], AF.Exp, [Lt], [Lt])
                    else:
                        self.act(Lt.t[:, :], bcs[d][hh].t[:, :], AF.Exp, [bcs[d][hh], bias_tok], [Lt], bias=bia, scale=sgn)
                    self.tt(pb.t[:, :], sbk.t[:, :], Lt.t[:, :], ALU.mult, [sbk, Lt], [pb])
                    if diag:
                        m = jc - 4 * ib
                        M = Mf if d == 0 else Mb
                        self.tt(pb.t[:, :], pb.t[:, :], M.t[:, 384 - 128 * m:384 - 128 * m + 512], ALU.mult, [pb, M], [pb], eng="pool")
                    self.mm(yb, yb.t[:, :], xdt[d].t[:, jc, h, :], pb.t[:, :], n == 0, n == len(items) - 1, [xdt[d], pb])
                self.stt(yv.t[:, :], xsT.t[:, pair, i0:i0 + 512], dpp.t[:, pair:pair + 1], yb.t[:, :], ALU.mult, ALU.add, [xsT, dpp, yb], [yv])
                self.act(gate.t[:, :], mz.t[:, pair, i0:i0 + 512], AF.Silu, [mz], [gate])
                self.tt(ybuf.t[:, pair, :], yv.t[:, :], gate.t[:, :], ALU.mult, [yv, gate], [ybuf])
            for c in range(4):
                self.act(sq.t[:, c, :], ybuf.t[:, c, :], AF.Square, [ybuf], [sq])
            nb = self.banks[6]
            for c in range(4):
                self.mm(nb, nb.t[:, :], self.ones_bf.t[:, :], sq.t[:, c, :], c == 0, c == 3, [self.ones_bf, sq])
            self.act(rstd.t[:, :], nb.t[:, :], AF.Sqrt, [nb], [rstd], bias=1e-6, scale=1.0 / 512)
            self.recip(rstd.t[:, :], rstd.t[:, :], [rstd], [rstd])
            for c in range(4):
                o = ob[c % 2]
                self.stt(o.t[:, :], ybuf.t[:, c, :], nw.t[:, c:c + 1], rstd.t[:, :], ALU.mult, ALU.mult, [ybuf, nw, rstd], [o])
                self.dma("pool", mixedT.t[1024 + c * 128:1024 + (c + 1) * 128, t0 + i0:t0 + i0 + 512], o.t[:, :], [o], [mixedT])


KB.group_ssd = _group_ssd


TWO_PI = 2.0 * math.pi
MAGIC = 12582912.0


def _sin_rr(self, out, x, tmp, reads_x, writes_out, x_tl, tmp_tl):
    self.ts(tmp, x, 1.0 / TWO_PI, MAGIC, ALU.mult, ALU.add, [x_tl], [tmp_tl])
    self.ts(tmp, tmp, MAGIC, -TWO_PI, ALU.subtract, ALU.mult, [tmp_tl], [tmp_tl])
    self.tt(x, x, tmp, ALU.add, [x_tl, tmp_tl], [x_tl])
    self.ts(x, x, 3.1415925, -3.1415925, ALU.min, ALU.max, [x_tl], [x_tl])
    self.act(out, x, AF.Sin, [x_tl], writes_out)


def _hyena_filter(self, l, prm, cst, Hs):
    with Phase(self) as ph:
        zT = self.sb(ph, [33, S], F32, "zT")
        w1 = self.sb(ph, [33, 64], F32, "w1")
        w2 = self.sb(ph, [64, 64], F32, "w2")
        w3 = self.sb(ph, [64, 2048], F32, "w3")
        sm = self.sb(ph, [64, 4], F32, "sm")
        self.dma("sp", zT.t[:], cst["hy_zT"], [], [zT])
        self.dma("sp", w1.t[:], prm["hy_w1"][l], [], [w1])
        self.dma("sp", w2.t[:], prm["hy_w2"][l], [], [w2])
        self.dma("sp", w3.t[:], prm["hy_w3"][l], [], [w3])
        self.dma("sp", sm.t[:, 0:2], prm["hy_b_pp"][:, l * 2:l * 2 + 2], [], [sm])
        self.dma("sp", sm.t[:, 2:4], prm["hy_freq_pp"][:, l * 2:l * 2 + 2], [], [sm])
        ntl = self.sb(ph, [128, 16], F32, "ntl")
        dlb = self.sb(ph, [128, 512], F32, "dlb")
        wf = self.sb(ph, [128, NF], F32, "wf")
        self.dma("sp", ntl.t[:], cst["hy_ntlin_pp"], [], [ntl])
        self.dma("sp", dlb.t[:], cst["hy_delta_b"], [], [dlb])
        self.dma("sp", wf.t[:], cst["dft_wf_pp"], [], [wf])
        hid1 = self.sb(ph, [64, S], F32, "hid1")
        hid2 = self.sb(ph, [64, S], F32, "hid2")
        xa = self.sb(ph, [64, 512], F32, "xa")
        xb = self.sb(ph, [64, 512], F32, "xb")
        for tb in range(4):
            bk = self.banks[tb % 2]
            self.mm(bk, bk.t[0:64, :], w1.t[0:33, :], zT.t[0:33, tb * 512:(tb + 1) * 512], True, True, [w1, zT])
            self.ts(xa.t[:, :], bk.t[0:64, :], sm.t[:, 0:1], sm.t[:, 2:3], ALU.add, ALU.mult, [bk, sm], [xa])
            _sin_rr(self, hid1.t[:, tb * 512:(tb + 1) * 512], xa.t[:, :], xb.t[:, :], None, [hid1], xa, xb)
        for tb in range(4):
            bk = self.banks[tb % 2]
            self.mm(bk, bk.t[0:64, :], w2.t[0:64, :], hid1.t[0:64, tb * 512:(tb + 1) * 512], True, True, [w2, hid1])
            self.ts(xa.t[:, :], bk.t[0:64, :], sm.t[:, 1:2], sm.t[:, 3:4], ALU.add, ALU.mult, [bk, sm], [xa])
            _sin_rr(self, hid2.t[:, tb * 512:(tb + 1) * 512], xa.t[:, :], xb.t[:, :], None, [hid2], xa, xb)
        hsum = [self.sb(ph, [128, 16, 512], BF16, "hsum") for _ in range(2)]
        hdif = [self.sb(ph, [128, 16, 512], BF16, "hdif") for _ in range(2)]
        dec = self.sb(ph, [128, 512], F32, "dec")
        hb = self.sb(ph, [128, 512], F32, "hb")
        t1 = self.sb(ph, [128, 512], F32, "t1")
        for tc in range(16):
            self.act(dec.t[:, :], dlb.t[:, :], AF.Exp, [dlb, ntl], [dec], scale=ntl.t[:, tc:tc + 1])
            for o in range(2):
                bf_, bb_ = self.banks[2 * o], self.banks[2 * o + 1]
                self.mm(bf_, bf_.t[:, :], hid2.t[0:64, tc * 128:(tc + 1) * 128], w3.t[0:64, (2 * o) * 512:(2 * o + 1) * 512], True, True, [hid2, w3])
                self.mm(bb_, bb_.t[:, :], hid2.t[0:64, tc * 128:(tc + 1) * 128], w3.t[0:64, (2 * o + 1) * 512:(2 * o + 2) * 512], True, True, [hid2, w3])
                self.copy(hb.t[:, :], bb_.t[:, :], [bb_], [hb], eng="act")
                if tc == 0:
                    self.memset(hb.t[0:1, :], 0.0, [hb])
                self.tt(t1.t[:, :], bf_.t[:, :], hb.t[:, :], ALU.add, [bf_, hb], [t1])
                self.tt(hsum[o].t[:, tc, :], t1.t[:, :], dec.t[:, :], ALU.mult, [t1, dec], [hsum[o]])
                self.tt(t1.t[:, :], bf_.t[:, :], hb.t[:, :], ALU.subtract, [bf_, hb], [t1])
                self.tt(hdif[o].t[:, tc, :], t1.t[:, :], dec.t[:, :], ALU.mult, [t1, dec], [hdif[o]])
        Cb = [self.sb(ph, [128, 16, 128], BF16, "Cb") for _ in range(2)]
        Sb = [self.sb(ph, [128, 16, 128], BF16, "Sb") for _ in range(2)]
        ho = [self.sb(ph, [128, 512], F32, "ho") for _ in range(2)]
        n = 0
        for fc in range(NF):
            cb_, sb_ = Cb[fc % 2], Sb[fc % 2]
            self.dma("sp", cb_.t[:], cst["dft_Cblk"][fc], [], [cb_])
            self.dma("sp", sb_.t[:], cst["dft_Sblk"][fc], [], [sb_])
            for o in range(2):
                for ri, (tab, src) in enumerate(((cb_, hsum[o]), (sb_, hdif[o]))):
                    bk = self.banks[4 + n % 4]
                    for tc in range(16):
                        self.mm(bk, bk.t[:, :], tab.t[:, tc, :], src.t[:, tc, :], tc == 0, tc == 15, [tab, src])
                    h_ = ho[n % 2]
                    n += 1
                    self.ts(h_.t[:, :], bk.t[:, :], wf.t[:, fc:fc + 1], None, ALU.mult, None, [bk, wf], [h_])
                    self.dma("pool", Hs.t[o, ri, fc * 128:(fc + 1) * 128, :], h_.t[:, :], [h_], [Hs])


def _group_hyena(self, l, nseq, seg, mixedT, prm, cst, Hs, hyu, z1s):
    NCOL = nseq * 512
    with Phase(self) as ph:
        U = self.sb(ph, [128, 16, NCOL], BF16, "U")
        Yre = self.sb(ph, [128, NF, NCOL], BF16, "Yre")
        Yim = self.sb(ph, [128, NF, NCOL], BF16, "Yim")
        cw = self.sb(ph, [128, 36], F32, "cw")
        cb = self.sb(ph, [128, 12], F32, "cb")
        hbias = self.sb(ph, [128, 8], F32, "hbias")
        nw = self.sb(ph, [128, 4], F32, "nw")
        self.dma("sp", cw.t[:], prm["hy_conv_w_pp"][:, l * 36:(l + 1) * 36], [], [cw])
        self.dma("sp", cb.t[:], prm["hy_conv_b_pp"][:, l * 12:(l + 1) * 12], [], [cb])
        self.dma("sp", hbias.t[:], prm["hy_bias_pp"][:, l * 8:(l + 1) * 8], [], [hbias])
        self.dma("sp", nw.t[:], prm["hy_out_norm_pp"][:, l * 4:(l + 1) * 4], [], [nw])
        with Phase(self) as p2:
            raw = [self.sb(p2, [128, S], BF16, "raw") for _ in range(2)]
            acc = [self.sb(p2, [128, S], F32, "acc") for _ in range(2)]
            n = 0
            for j in range(3):
                for s in range(nseq):
                    for cc in range(4):
                        c = j * 4 + cc
                        r_, a_ = raw[n % 2], acc[n % 2]
                        n += 1
                        self.dma("sp", r_.t[:, :], seg["hu"].t[c * 128:(c + 1) * 128, s * S:(s + 1) * S], [seg["hu"]], [r_])
                        self.ts(a_.t[:, :], r_.t[:, :], cw.t[:, c * 3 + 1:c * 3 + 2], cb.t[:, c:c + 1], ALU.mult, ALU.add, [r_, cw, cb], [a_])
                        self.stt(a_.t[:, 1:S], r_.t[:, 0:S - 1], cw.t[:, c * 3:c * 3 + 1], a_.t[:, 1:S], ALU.mult, ALU.add, [r_, cw, a_], [a_])
                        self.stt(a_.t[:, 0:S - 1], r_.t[:, 1:S], cw.t[:, c * 3 + 2:c * 3 + 3], a_.t[:, 0:S - 1], ALU.mult, ALU.add, [r_, cw, a_], [a_])
                        self.dma("pool", hyu.t[j, (s * 4 + cc) * 128:(s * 4 + cc + 1) * 128, :], a_.t[:, :], [a_], [hyu])
                        if j == 0:
                            for tcg in range(4):
                                bk = self.banks[6 + tcg % 2]
                                for k in range(4):
                                    tc = tcg * 4 + k
                                    self.tr(bk, bk.t[:, k * 128:(k + 1) * 128], a_.t[:, tc * 128:(tc + 1) * 128], [a_])
                                self.copy(U.t[:, tcg * 4:tcg * 4 + 4, (s * 4 + cc) * 128:(s * 4 + cc + 1) * 128], bk.t[:].rearrange("p (a b) -> p a b", a=4), [bk], [U])
        Cb = [self.sb(ph, [128, 16, 128], BF16, "Cb") for _ in range(2)]
        Sb = [self.sb(ph, [128, 16, 128], BF16, "Sb") for _ in range(2)]
        Hre = [self.sb(ph, [128, 512], F32, "Hre") for _ in range(2)]
        Him = [self.sb(ph, [128, 512], F32, "Him") for _ in range(2)]
        Cn = self.sb(ph, [128, NF, 512], BF16, "Cn")
        Sn = self.sb(ph, [128, NF, 512], BF16, "Sn")
        ta = self.sb(ph, [128, 512], F32, "ta")
        tb_ = self.sb(ph, [128, 512], F32, "tb")
        zp = [self.sb(ph, [128, 512], F32, "zp") for _ in range(2)]
        xg = [self.sb(ph, [128, 512], F32, "xg") for _ in range(2)]
        zn = [self.sb(ph, [128, 512], F32, "zn") for _ in range(2)]
        zfin = self.sb(ph, [128, nseq * 4, 512], F32, "zfin")
        sq = self.sb(ph, [128, 4, 512], BF16, "sq")
        rstd = self.sb(ph, [128, 512], F32, "rstd")
        ob = [self.sb(ph, [128, 512], BF16, "ob") for _ in range(2)]
        for o in range(2):
            for fc in range(NF):
                self.pump(2)
                cb_, sb_ = Cb[fc % 2], Sb[fc % 2]
                hr, hi = Hre[fc % 2], Him[fc % 2]
                self.dma("sp", cb_.t[:], cst["dft_Cblk"][fc], [], [cb_])
                self.dma("sp", sb_.t[:], cst["dft_Sblk"][fc], [], [sb_])
                self.dma("sp", hr.t[:, :], Hs.t[o, 0, fc * 128:(fc + 1) * 128, :], [Hs], [hr])
                self.dma("sp", hi.t[:, :], Hs.t[o, 1, fc * 128:(fc + 1) * 128, :], [Hs], [hi])
                for s in range(nseq):
                    br, bi = self.banks[2 * (s % 2)], self.banks[2 * (s % 2) + 1]
                    for tc in range(16):
                        self.mm(br, br.t[:, :], cb_.t[:, tc, :], U.t[:, tc, s * 512:(s + 1) * 512], tc == 0, tc == 15, [cb_, U])
                    for tc in range(16):
                        self.mm(bi, bi.t[:, :], sb_.t[:, tc, :], U.t[:, tc, s * 512:(s + 1) * 512], tc == 0, tc == 15, [sb_, U])
                    self.tt(ta.t[:, :], br.t[:, :], hr.t[:, :], ALU.mult, [br, hr], [ta])
                    self.tt(tb_.t[:, :], bi.t[:, :], hi.t[:, :], ALU.mult, [bi, hi], [tb_])
                    self.tt(Yre.t[:, fc, s * 512:(s + 1) * 512], ta.t[:, :], tb_.t[:, :], ALU.subtract, [ta, tb_], [Yre])
                    self.tt(ta.t[:, :], br.t[:, :], hi.t[:, :], ALU.mult, [br, hi], [ta])
                    self.tt(tb_.t[:, :], bi.t[:, :], hr.t[:, :], ALU.mult, [bi, hr], [tb_])
                    self.tt(Yim.t[:, fc, s * 512:(s + 1) * 512], ta.t[:, :], tb_.t[:, :], ALU.add, [ta, tb_], [Yim])
            for tb in range(4):
                c0 = tb * 512
                self.dma("sp", Cn.t[:], cst["dft_Cnat"][:, c0:c0 + 512].rearrange("(f p) t -> p f t", p=128), [], [Cn])
                self.dma("sp", Sn.t[:], cst["dft_Snat"][:, c0:c0 + 512].rearrange("(f p) t -> p f t", p=128), [], [Sn])
                for sc in range(nseq * 4):
                    s, cc = sc // 4, sc % 4
                    bk = self.banks[4 + sc % 2]
                    for fc in range(NF):
                        self.mm(bk, bk.t[:, :], Yre.t[:, fc, sc * 128:(sc + 1) * 128], Cn.t[:, fc, :], fc == 0, False, [Yre, Cn])
                        self.mm(bk, bk.t[:, :], Yim.t[:, fc, sc * 128:(sc + 1) * 128], Sn.t[:, fc, :], False, fc == NF - 1, [Yim, Sn])
                    z_, x_, n_ = zp[sc % 2], xg[sc % 2], zn[sc % 2]
                    zsrc = hyu.t[0] if o == 0 else z1s.t
                    zsrc_tl = hyu if o == 0 else z1s
                    self.dma("sp", z_.t[:, :], zsrc[sc * 128:(sc + 1) * 128, c0:c0 + 512], [zsrc_tl], [z_])
                    self.dma("sp", x_.t[:, :], hyu.t[o + 1, sc * 128:(sc + 1) * 128, c0:c0 + 512], [hyu], [x_])
                    self.stt(n_.t[:, :], z_.t[:, :], hbias.t[:, o * 4 + cc:o * 4 + cc + 1], bk.t[:, :], ALU.mult, ALU.add, [z_, hbias, bk], [n_])
                    if o == 0:
                        self.tt(n_.t[:, :], n_.t[:, :], x_.t[:, :], ALU.mult, [n_, x_], [n_])
                        self.dma("pool", z1s.t[sc * 128:(sc + 1) * 128, c0:c0 + 512], n_.t[:, :], [n_], [z1s])
                        b6 = self.banks[6 + sc % 2]
                        for k in range(4):
                            self.tr(b6, b6.t[:, k * 128:(k + 1) * 128], n_.t[:, k * 128:(k + 1) * 128], [n_])
                        self.copy(U.t[:, tb * 4:tb * 4 + 4, sc * 128:(sc + 1) * 128], b6.t[:].rearrange("p (a b) -> p a b", a=4), [b6], [U])
                    else:
                        self.tt(zfin.t[:, sc, :], n_.t[:, :], x_.t[:, :], ALU.mult, [n_, x_], [zfin])
                if o == 1:
                    for s in range(nseq):
                        for cc in range(4):
                            self.act(sq.t[:, cc, :], zfin.t[:, s * 4 + cc, :], AF.Square, [zfin], [sq])
                        nb = self.banks[6]
                        for cc in range(4):
                            self.mm(nb, nb.t[:, :], self.ones_bf.t[:, :], sq.t[:, cc, :], cc == 0, cc == 3, [self.ones_bf, sq])
                        self.act(rstd.t[:, :], nb.t[:, :], AF.Sqrt, [nb], [rstd], bias=1e-6, scale=1.0 / 512)
                        self.recip(rstd.t[:, :], rstd.t[:, :], [rstd], [rstd])
                        for cc in range(4):
                            o_ = ob[cc % 2]
                            self.stt(o_.t[:, :], zfin.t[:, s * 4 + cc, :], nw.t[:, cc:cc + 1], rstd.t[:, :], ALU.mult, ALU.mult, [zfin, nw, rstd], [o_])
                            self.dma("pool", mixedT.t[1536 + cc * 128:1536 + (cc + 1) * 128, s * S + c0:s * S + c0 + 512], o_.t[:, :], [o_], [mixedT])


KB.hyena_filter = _hyena_filter
KB.group_hyena = _group_hyena


def _ln_rows(self, y, gB, bB, st6, mv, eps):
    nc = self.nc
    for q in range(4):
        self.P.op("dve", lambda q=q: nc.vector.bn_stats(out=st6.t[:, q, :], in_=y.t[:, q * 512:(q + 1) * 512]), self._b([y]), self._b([st6]))
    self.P.op("dve", lambda: nc.vector.bn_aggr(out=mv.t[:, 0:2], in_=st6.t[:].rearrange("p a b -> p (a b)")), self._b([st6]), self._b([mv]))
    self.act(mv.t[:, 2:3], mv.t[:, 1:2], AF.Sqrt, [mv], [mv], bias=eps, scale=1.0)
    self.recip(mv.t[:, 3:4], mv.t[:, 2:3], [mv], [mv])
    self.ts(y.t[:, :], y.t[:, :], mv.t[:, 0:1], mv.t[:, 3:4], ALU.subtract, ALU.mult, [y, mv], [y])
    self.tt(y.t[:, :], y.t[:, :], gB.t[:, :], ALU.mult, [y, gB], [y])
    self.tt(y.t[:, :], y.t[:, :], bB.t[:, :], ALU.add, [y, bB], [y])


def _outproj_ln(self, l, mixedT, woutb, xin, xout, prm, tok_range, wdep=None):
    with Phase(self) as ph:
        W = self.sb(ph, [128, 16, D], BF16, "Wout")
        self.dma("sp", W.t[:], woutb.t[l].rearrange("(ec p) d -> p ec d", p=128), [wdep or woutb], [W])
        gB = self.bcast_row(ph, prm["ln1_g"][l:l + 1, :], D, "gB")
        bB = self.bcast_row(ph, prm["ln1_b"][l:l + 1, :], D, "bB")
        mT = [self.sb(ph, [128, 16, 128], BF16, "mT") for _ in range(2)]
        xt = [self.sb(ph, [128, D], F32, "xt") for _ in range(2)]
        y = [self.sb(ph, [128, D], F32, "y") for _ in range(2)]
        st6 = self.sb(ph, [128, 4, 6], F32, "st6")
        mv = self.sb(ph, [128, 4], F32, "mv")
        for i, tok0 in enumerate(range(tok_range[0], tok_range[1], 128)):
            self.pump(2)
            m_, x_, y_ = mT[i % 2], xt[i % 2], y[i % 2]
            self.dma("sp", m_.t[:], mixedT.t[:, tok0:tok0 + 128].rearrange("(ec p) t -> p ec t", p=128), [mixedT], [m_])
            self.dma("sp", x_.t[:], xin.t[tok0:tok0 + 128, :], [xin], [x_])
            for q in range(4):
                bk = self.banks[(i * 4 + q) % 8]
                for ec in range(16):
                    self.mm(bk, bk.t[:, :], m_.t[:, ec, :], W.t[:, ec, q * 512:(q + 1) * 512], ec == 0, ec == 15, [m_, W])
                self.stt(y_.t[:, q * 512:(q + 1) * 512], x_.t[:, q * 512:(q + 1) * 512], ALPHA, bk.t[:, :], ALU.mult, ALU.add, [x_, bk], [y_])
            _ln_rows(self, y_, gB, bB, st6, mv, 1e-5)
            self.dma("pool", xout.t[tok0:tok0 + 128, :], y_.t[:, :], [y_], [xout])


def _wsl(W, r0, r1, c0, c1, pat):
    if isinstance(W, tuple) and W[0] == "dynflat":
        _, tens, v, kind = W
        if kind == "gu":
            return tens[c0 // 256][bass.ds(v, MSEG)].rearrange("(dc p f) -> p dc f", p=128, f=256)
        return tens[(c0 // 512) * 7 + r0 // 1024][bass.ds(v, MSEG)].rearrange("(a p f) -> p a f", p=128, f=512)
    if isinstance(W, tuple):
        _, t3, reg = W
        return t3[bass.ds(reg, 1), r0:r1, c0:c1].rearrange("o " + pat, p=128, o=1).rearrange("p o a b -> p (o a) b") if False else \
            t3[bass.ds(reg, 1), r0:r1, c0:c1].rearrange("1 " + pat, p=128)
    return W[r0:r1, c0:c1].rearrange(pat, p=128)


def _ffn(self, l, xin, xout, experts, F_, prm, tok_range, wr_ap=None, slot_mode=False, wdep=None):
    nc = self.nc
    wdep = wdep or self.wsrc
    nfc = F_ // 128
    nftiles = 7 if slot_mode else (8 if nfc % 8 == 0 else 4)
    nft = nfc // nftiles
    import os
    if os.environ.get("MOE_DBG", "") == "dense":
        wr_ap = None
    gated = wr_ap is not None
    with Phase(self) as ph:
        xT = self.sb(ph, [128, 16, 512], BF16, "xT")
        xtiles = [self.sb(ph, [128, D], F32, "xtile") for _ in range(2)]
        hT = self.sb(ph, [128, nfc, 512], BF16, "hT")
        acc = self.sb(ph, [128, 4, D], F32, "acc")
        wg = [self.sb(ph, [128, 16, 256], BF16, "wg") for _ in range(2)]
        wu = [self.sb(ph, [128, 16, 256], BF16, "wu") for _ in range(2)]
        wd = [self.sb(ph, [128, nft, 512], BF16, "wd") for _ in range(2)]
        sg = [self.sb(ph, [128, 512], F32, "sg") for _ in range(2)]
        if not slot_mode:
            gB = self.bcast_row(ph, prm["ln2_g"][l:l + 1, :], D, "gB2")
            bB = self.bcast_row(ph, prm["ln2_b"][l:l + 1, :], D, "bB2")
        st6 = self.sb(ph, [128, 4, 6], F32, "st6")
        mv = self.sb(ph, [128, 4], F32, "mv")
        G = self.sb(ph, [128, 4, 8], F32, "G")
        if gated:
            x32 = self.sb(ph, [128, 16, 128], F32, "x32")
            x32_keep = x32
            wr = self.sb(ph, [128, 16, 8], F32, "wr")
            if not os.environ.get("NOWR"):
                self.dma("sp", wr.t[:], wr_ap.rearrange("(dc p) e -> p dc e", p=128), [], [wr])
            lg = self.sb(ph, [128, 8], F32, "lg")
            srt = self.sb(ph, [128, 8], F32, "srt")
            gg = self.sb(ph, [128, 4], F32, "gg")
            g2t = self.sb(ph, [128, 8], F32, "g2t")
        else:
            x32 = None

        import os
        dbgm = os.environ.get("MOE_DBG", "")

        def router(ts_):
            if dbgm == "norouter":
                self.memset(G.t[:, ts_, :], 0.125, [G])
                return
            bk = self.banks[0]
            for dc in range(16):
                self.mm(bk, bk.t[:, 0:8], x32.t[:, dc, :], wr.t[:, dc, :], dc == 0, dc == 15, [x32, wr])
            self.copy(lg.t[:, :], bk.t[:, 0:8], [bk], [lg], eng="dve")
            self.P.op("dve", lambda: nc.vector.max(out=srt.t[:, :], in_=lg.t[:, :]), self._b([lg]), self._b([srt]))
            self.tt(gg.t[:, 0:1], srt.t[:, 1:2], srt.t[:, 0:1], ALU.subtract, [srt], [gg])
            self.act(gg.t[:, 1:2], gg.t[:, 0:1], AF.Sigmoid, [gg], [gg])
            self.ts(gg.t[:, 2:3], gg.t[:, 1:2], -1.0, 1.0, ALU.mult, ALU.add, [gg], [gg])
            self.ts(G.t[:, ts_, :], lg.t[:, :], srt.t[:, 0:1], gg.t[:, 2:3], ALU.is_equal, ALU.mult, [lg, srt, gg], [G])
            self.ts(g2t.t[:, :], lg.t[:, :], srt.t[:, 1:2], gg.t[:, 1:2], ALU.is_equal, ALU.mult, [lg, srt, gg], [g2t])
            self.tt(G.t[:, ts_, :], G.t[:, ts_, :], g2t.t[:, :], ALU.add, [G, g2t], [G])

        n1 = 0
        n2 = 0
        for tok0 in range(tok_range[0], tok_range[1], 512):
            self.load_xT(xin, tok0, xT, xtiles, x32=None if os.environ.get("NOX32") else x32, post=router if gated else None)
            exl = experts(tok0) if callable(experts) else experts
            for e, (Wg, Wu, Wd) in enumerate(exl):
                for fg in range(0 if os.environ.get("FFN_SKIP1") else nfc // 2):
                    self.pump(2)
                    g_, u_ = wg[n1 % 2], wu[n1 % 2]
                    n1 += 1
                    self.dma("sp", g_.t[:], _wsl(Wg, 0, D, fg * 256, (fg + 1) * 256, "(dc p) f -> p dc f"), [wdep], [g_])
                    self.dma("sp", u_.t[:], _wsl(Wu, 0, D, fg * 256, (fg + 1) * 256, "(dc p) f -> p dc f"), [wdep], [u_])
                    for j in range(2):
                        fc = fg * 2 + j
                        bg, bu = self.banks[fc % 2], self.banks[2 + fc % 2]
                        for dc in range(16):
                            self.mm(bg, bg.t[:, :], g_.t[:, dc, j * 128:(j + 1) * 128], xT.t[:, dc, :], dc == 0, dc == 15, [g_, xT])
                        for dc in range(16):
                            self.mm(bu, bu.t[:, :], u_.t[:, dc, j * 128:(j + 1) * 128], xT.t[:, dc, :], dc == 0, dc == 15, [u_, xT])
                        s_ = sg[fc % 2]
                        self.act(s_.t[:, :], bg.t[:, :], AF.Silu, [bg], [s_])
                        self.tt(hT.t[:, fc, :], s_.t[:, :], bu.t[:, :], ALU.mult, [s_, bu], [hT])
                for q in range(4):
                    if os.environ.get("FFN_SKIP2"):
                        for ts_ in range(4):
                            self.memset(acc.t[:, ts_, q * 512:(q + 1) * 512], 0.0, [acc])
                        continue
                    for ft in range(nftiles):
                        d_ = wd[n2 % 2]
                        n2 += 1
                        self.dma("sp", d_.t[:], _wsl(Wd, ft * nft * 128, (ft + 1) * nft * 128, q * 512, (q + 1) * 512, "(a p) d -> p a d"), [wdep], [d_])
                        for a in range(nft):
                            fc = ft * nft + a
                            for ts_ in range(4):
                                bk = self.banks[4 + ts_]
                                self.mm(bk, bk.t[:, :], hT.t[:, fc, ts_ * 128:(ts_ + 1) * 128], d_.t[:, a, :], fc == 0, fc == nfc - 1, [hT, d_])
                    for ts_ in range(4):
                        bk = self.banks[4 + ts_]
                        dst = acc.t[:, ts_, q * 512:(q + 1) * 512]
                        if not gated:
                            self.copy(dst, bk.t[:, :], [bk], [acc])
                        elif e == 0:
                            self.ts(dst, bk.t[:, :], G.t[:, ts_, e:e + 1], None, ALU.mult, None, [bk, G], [acc])
                        else:
                            self.stt(dst, bk.t[:, :], G.t[:, ts_, e:e + 1], dst, ALU.mult, ALU.add, [bk, G, acc], [acc])
            if slot_mode:
                for ts_ in range(4):
                    self.dma("pool", xout.t[tok0 + ts_ * 128:tok0 + (ts_ + 1) * 128, :], acc.t[:, ts_, :], [acc], [xout])
                continue
            for ts_ in range(4):
                x_ = xtiles[ts_ % 2]
                self.dma("sp", x_.t[:], xin.t[tok0 + ts_ * 128:tok0 + (ts_ + 1) * 128, :], [xin], [x_])
                self.stt(x_.t[:, :], x_.t[:, :], ALPHA, acc.t[:, ts_, :], ALU.mult, ALU.add, [x_, acc], [x_])
                _ln_rows(self, x_, gB, bB, st6, mv, 1e-5)
                self.dma("pool", xout.t[tok0 + ts_ * 128:tok0 + (ts_ + 1) * 128, :], x_.t[:, :], [x_], [xout])


KB.outproj_ln = _outproj_ln
KB.ffn = _ffn


def build_full(shapes, nseq=NSEQ, layers=(0, 1), ne=NE, skip_mixer=False):
    nc = bass.Bass("TRN2", target_bir_lowering=False)
    ext = {}
    for name, (shape, dt) in shapes.items():
        ext[name] = nc.dram_tensor(name, list(shape), dt, kind="ExternalInput").ap()
    out = Tl(nc.dram_tensor("out", [T, D], F32, kind="ExternalOutput").ap(), "out")
    with ExitStack() as st:
        kb = KB(nc, st)
        kb.setup_consts()
        kb.wsrc = Tl(None, "wsrc")
        winb = kb.dram("winb", [L, D, INC], BF16)
        wrot = kb.dram("wrot", [L, D, 576], BF16)
        woutb = kb.dram("woutb", [L, D, D], BF16)
        fg = kb.dram("fgb", [D, DFF], BF16)
        fu = kb.dram("fub", [D, DFF], BF16)
        fd = kb.dram("fdb", [DFF, D], BF16)
        seg = {"qc": kb.dram("s_qc", [384, T], BF16), "kvc": kb.dram("s_kvc", [256, T], BF16),
               "kpe": kb.dram("s_kpe", [64, T], BF16), "rq": kb.dram("s_rq", [256, T], BF16),
               "rk": kb.dram("s_rk", [256, T], BF16), "rg": kb.dram("s_rg", [512, T], BF16),
               "mz": kb.dram("s_mz", [512, T], BF16), "xbc": kb.dram("s_xbc", [1024, T], BF16),
               "dt": kb.dram("s_dt", [16, T], F32), "hu": kb.dram("s_hu", [1536, T], BF16),
               "rv": kb.dram("s_rv", [T, 512], BF16)}
        mixedT = kb.dram("mixedT", [D, T], BF16)
        Hs = kb.dram("Hs", [2, 2, NFP, 512], F32)
        hyu = kb.dram("hyu", [3, NSEQ * 512, S], F32)
        z1s = kb.dram("z1s", [NSEQ * 512, S], F32)
        xa = kb.dram("xa", [T, D], F32)
        xb = kb.dram("xb", [T, D], F32)
        xin = Tl(ext["x"], "x")
        rng = (0, nseq * S)
        import os
        if os.environ.get("RNG"):
            rng = (0, int(os.environ["RNG"]))
        kb.wmoe = Tl(None, "wmoe")
        woutL = [Tl(woutb.t[l], "wout%d" % l) for l in range(L)]
        winL = [Tl(winb.t[l], "win%d" % l) for l in range(L)]
        for l in layers:
            kb.cast_dram(winL[l], ext["w_in"][l], D, tag=None if l == layers[0] else "win%d" % l)
            kb.cast_dram(woutL[l], ext["w_out"][l], D, tag="wout%d" % l)
            if l == 0:
                for dst, nm, rows in ((fg, "ffn_w_gate", D), (fu, "ffn_w_up", D), (fd, "ffn_w_down", DFF)):
                    t_ = _sub(dst, dst.t)
                    t_.b = kb.wsrc.b
                    kb.cast_dram(t_, ext[nm][0], rows, tag="ffn")
        if 1 in layers:
            mg = [nc.dram_tensor("mgb%d" % i, [NE * MSEG], BF16).ap() for i in range(28)]
            mu = [nc.dram_tensor("mub%d" % i, [NE * MSEG], BF16).ap() for i in range(28)]
            md = [nc.dram_tensor("mdb%d" % i, [NE * MSEG], BF16).ap() for i in range(28)]
            for e in range(ne):
                for fg_ in range(28):
                    for tens, nm in ((mg, "moe_w_gate"), (mu, "moe_w_up")):
                        kb.defer("moe", lambda tens=tens, nm=nm, e=e, fg_=fg_: kb.dma(
                            "pool", tens[fg_][e * MSEG:(e + 1) * MSEG].rearrange("(r c) -> r c", c=256),
                            ext[nm][0, e][:, fg_ * 256:(fg_ + 1) * 256], [], [kb.wmoe]))
                for q in range(4):
                    for ft in range(7):
                        kb.defer("moe", lambda e=e, q=q, ft=ft: kb.dma(
                            "pool", md[q * 7 + ft][e * MSEG:(e + 1) * MSEG].rearrange("(r c) -> r c", c=512),
                            ext["moe_w_down"][0, e][ft * 1024:(ft + 1) * 1024, q * 512:(q + 1) * 512], [], [kb.wmoe]))
        cur = xin
        import os
        stages = os.environ.get("STAGES", "mix,op,ffn").split(",")
        for l in layers:
            if "mix" in stages:
                kb.flush_tag("win%d" % l)
                kb.build_rot(l, ext["w_in"], wrot)
                kb.inproj(l, cur, winb, wrot, seg, ext, rng, wdep=winL[l])
                for s in range(nseq):
                    kb.group_mla(l, s, seg, mixedT, ext, ext)
                    kb.group_ret(s, seg, mixedT)
                    kb.group_ssd(l, s, seg, mixedT, ext)
                kb.hyena_filter(l, ext, ext, Hs)
                kb.group_hyena(l, nseq, seg, mixedT, ext, ext, Hs, hyu, z1s)
            if "op" in stages:
                kb.flush_tag("wout%d" % l)
                kb.outproj_ln(l, mixedT, woutb, cur, xa, ext, rng, wdep=woutL[l])
            nxt = out if l == layers[-1] else xb
            if "ffn" not in stages:
                continue
            if l == 0:
                kb.flush_tag("ffn")
                kb.ffn(l, xa, nxt, [(fg.t, fu.t, fd.t)], DFF, ext, rng)
            else:
                if True:
                    kb.flush_tag("moe")
                    kb.moe_routed(l, xa, nxt, mg, mu, md, ext, rng[1], ext["moe_router"][0])
            cur = nxt
        kb.P.finish()
        print("instructions:", kb.P.n_inst, "sems:", kb.P.nsem)
    return nc


BIG = ("w_in", "w_out", "ffn_w_gate", "ffn_w_up", "ffn_w_down", "moe_router", "moe_w_gate", "moe_w_up", "moe_w_down",
       "ln1_g", "ln1_b", "ln2_g", "ln2_b")


def kernel(**inputs):
    common = {}
    for k in BIG:
        common[k] = np.ascontiguousarray(np.asarray(inputs[k], dtype=np.float32))
    common.update(host_consts())
    common.update(host_params(inputs))
    x = np.asarray(inputs["x"], dtype=np.float32)
    ncores = 8
    shapes = {k: (v.shape, BF16 if v.dtype == ml_dtypes.bfloat16 else F32) for k, v in common.items()}
    shapes["x"] = ((T, D), F32)
    nc = build_full(shapes)
    in_maps = []
    for c in range(ncores):
        m = dict(common)
        m["x"] = np.ascontiguousarray(x[c * NSEQ:(c + 1) * NSEQ].reshape(T, D))
        in_maps.append(m)
    res = run_bass_kernel_spmd(nc, in_maps, core_ids=list(range(ncores)))
    outs = [np.asarray(r["out"], dtype=np.float32).reshape(NSEQ, S, D) for r in res.results]
    return np.concatenate(outs, axis=0)


MSEG = 2048 * 256
NBLK = 24
I32 = mybir.dt.int32


def _moe_routed(self, l, xin, xout, mg, mu, md, prm, ntok, wr_ap):
    nc = self.nc
    NT = ntok // 128
    xslots = self.dram("xslots", [NBLK * 512, D], F32)
    yslots = self.dram("yslots", [NBLK * 512, D], F32)
    with Phase(self) as pr:
        M1 = self.sb(pr, [128, NT, 8], F32, "M1")
        M2 = self.sb(pr, [128, NT, 8], F32, "M2")
        LOC = self.sb(pr, [128, NT, 8], F32, "LOC")
        TMP = self.sb(pr, [128, NT, 8], F32, "TMPr")
        G12 = self.sb(pr, [128, NT, 2], F32, "G12")
        d1f = self.sb(pr, [128, NT], F32, "d1f")
        d2f = self.sb(pr, [128, NT], F32, "d2f")
        d1i = self.sb(pr, [128, NT], I32, "d1i")
        d2i = self.sb(pr, [128, NT], I32, "d2i")
        bei = self.sb(pr, [128, NBLK], I32, "bei")
        with Phase(self) as ph:
            xt = [self.sb(ph, [128, D], F32, "xt") for _ in range(2)]
            x32 = self.sb(ph, [128, 16, 128], F32, "x32")
            wr = self.sb(ph, [128, 16, 8], F32, "wr")
            self.dma("sp", wr.t[:], wr_ap.rearrange("(dc p) e -> p dc e", p=128), [], [wr])
            ltri = self.sb(ph, [128, 128], F32, "ltri")
            self.memset(ltri.t[:], 1.0, [ltri])
            self.P.op("pool", lambda: nc.gpsimd.affine_select(out=ltri.t[:], in_=ltri.t[:], pattern=[[1, 128]], compare_op=ALU.is_ge, fill=0.0, base=-1, channel_multiplier=-1), self._b([ltri]), self._b([ltri]))
            base = self.sb(ph, [128, 8], F32, "base")
            self.memset(base.t[:], 0.0, [base])
            lg = self.sb(ph, [128, 8], F32, "lg")
            srt = self.sb(ph, [128, 8], F32, "srt")
            gg = self.sb(ph, [128, 4], F32, "gg")
            ms = self.sb(ph, [128, 8], F32, "ms")
            for i in range(NT):
                x_ = xt[i % 2]
                self.dma("sp", x_.t[:], xin.t[i * 128:(i + 1) * 128, :], [xin], [x_])
                for j in range(4):
                    bk = self.banks[4 + j]
                    for k in range(4):
                        dc = 4 * j + k
                        self.tr(bk, bk.t[:, k * 128:(k + 1) * 128], x_.t[:, dc * 128:(dc + 1) * 128], [x_])
                    self.copy(x32.t[:, 4 * j:4 * j + 4, :], bk.t[:].rearrange("p (a b) -> p a b", a=4), [bk], [x32])
                bk = self.banks[0]
                for dc in range(16):
                    self.mm(bk, bk.t[:, 0:8], x32.t[:, dc, :], wr.t[:, dc, :], dc == 0, dc == 15, [x32, wr])
                self.copy(lg.t[:, :], bk.t[:, 0:8], [bk], [lg], eng="dve")
                self.P.op("dve", lambda: nc.vector.max(out=srt.t[:, :], in_=lg.t[:, :]), self._b([lg]), self._b([srt]))
                self.tt(gg.t[:, 0:1], srt.t[:, 1:2], srt.t[:, 0:1], ALU.subtract, [srt], [gg])
                self.act(G12.t[:, i, 1:2], gg.t[:, 0:1], AF.Sigmoid, [gg], [G12])
                self.ts(G12.t[:, i, 0:1], G12.t[:, i, 1:2], -1.0, 1.0, ALU.mult, ALU.add, [G12], [G12])
                self.ts(M1.t[:, i, :], lg.t[:, :], srt.t[:, 0:1], None, ALU.is_equal, None, [lg, srt], [M1])
                self.ts(M2.t[:, i, :], lg.t[:, :], srt.t[:, 1:2], None, ALU.is_equal, None, [lg, srt], [M2])
                self.tt(ms.t[:, :], M1.t[:, i, :], M2.t[:, i, :], ALU.add, [M1, M2], [ms])
                b1, b2 = self.banks[1], self.banks[2]
                self.mm(b1, b1.t[:, 0:8], ltri.t[:, :], ms.t[:, :], True, True, [ltri, ms])
                self.mm(b2, b2.t[:, 0:8], self.ones_f.t[:, :], ms.t[:, :], True, True, [self.ones_f, ms])
                self.tt(LOC.t[:, i, :], b1.t[:, 0:8], base.t[:, :], ALU.add, [b1, base], [LOC])
                self.tt(base.t[:, :], b2.t[:, 0:8], base.t[:, :], ALU.add, [b2, base], [base])
            pad = self.sb(ph, [128, 8], F32, "pad")
            pend = self.sb(ph, [128, 8], F32, "pend")
            pst = self.sb(ph, [128, 8], F32, "pst")
            one8 = self.sb(ph, [128, 8], F32, "one8")
            self.memset(one8.t[:], 1.0, [one8])
            self.ts(pad.t[:, :], base.t[:, :], 1.0 / 512, 0.4990234375, ALU.mult, ALU.add, [base], [pad])
            self.ts(pad.t[:, :], pad.t[:, :], MAGIC, None, ALU.add, None, [pad], [pad])
            self.ts(pad.t[:, :], pad.t[:, :], MAGIC, 512.0, ALU.subtract, ALU.mult, [pad], [pad])
            self.P.op("dve", lambda: nc.vector.tensor_tensor_scan(out=pend.t[:, :], data0=one8.t[:, :], data1=pad.t[:, :], initial=0.0, op0=ALU.mult, op1=ALU.add), self._b([one8, pad]), self._b([pend]))
            self.tt(pst.t[:, :], pend.t[:, :], pad.t[:, :], ALU.subtract, [pend, pad], [pst])
            for i in range(NT):
                self.tt(LOC.t[:, i, :], LOC.t[:, i, :], pst.t[:, :], ALU.add, [LOC, pst], [LOC])
            for Mx, df, di in ((M1, d1f, d1i), (M2, d2f, d2i)):
                self.tt(TMP.t[:], Mx.t[:], LOC.t[:], ALU.mult, [Mx, LOC], [TMP])
                self.P.op("dve", lambda df=df: nc.vector.tensor_reduce(out=df.t[:, :], in_=TMP.t[:], axis=mybir.AxisListType.X, op=ALU.add), self._b([TMP]), self._b([df]))
                self.copy(di.t[:, :], df.t[:, :], [df], [di], eng="dve")
            thr = self.sb(ph, [128, NBLK], F32, "thr")
            bef = self.sb(ph, [128, NBLK], F32, "bef")
            cmpt = self.sb(ph, [128, NBLK], F32, "cmpt")
            self.P.op("pool", lambda: nc.gpsimd.iota(thr.t[:], pattern=[[512, NBLK]], base=0, channel_multiplier=0, allow_small_or_imprecise_dtypes=True), [], [thr.b])
            self.memset(bef.t[:], 0.0, [bef])
            for e in range(8):
                self.ts(cmpt.t[:, :], thr.t[:, :], pend.t[:, e:e + 1], None, ALU.is_ge, None, [thr, pend], [cmpt])
                self.tt(bef.t[:, :], bef.t[:, :], cmpt.t[:, :], ALU.add, [bef, cmpt], [bef])
            self.ts(bef.t[:, :], bef.t[:, :], 7.0, float(MSEG), ALU.min, ALU.mult, [bef], [bef])
            self.copy(bei.t[:, :], bef.t[:, :], [bef], [bei], eng="dve")
            for i in range(NT):
                x_ = xt[i % 2]
                self.dma("sp", x_.t[:], xin.t[i * 128:(i + 1) * 128, :], [xin], [x_])
                for di in (d1i, d2i):
                    self.P.dma_raw("pool", lambda di=di, x_=x_, i=i: nc.gpsimd.indirect_dma_start(
                        out=xslots.t[:, :], out_offset=bass.IndirectOffsetOnAxis(ap=di.t[:, i:i + 1], axis=0), in_=x_.t[:, :], in_offset=None),
                        self._b([x_, di]), self._b([xslots]))
        self.P._deps("sp", self._b([bei]), [])
        cur_reg = [None]

        def experts(tok0):
            b = tok0 // 512
            if cur_reg[0] is not None:
                nc.sync.free_register(cur_reg[0])
            reg = nc.sync.alloc_register()
            nc.sync.reg_load(reg, bei.t[0:1, b:b + 1])
            r = nc.sync.snap(reg, donate=True, min_val=0, max_val=7 * MSEG)
            cur_reg[0] = reg
            return [(("dynflat", mg, r, "gu"), ("dynflat", mu, r, "gu"), ("dynflat", md, r, "d"))]

        self.ffn(l, xslots, yslots, experts, DFE, prm, (0, NBLK * 512), slot_mode=True, wdep=self.wmoe)
        if cur_reg[0] is not None:
            nc.sync.free_register(cur_reg[0])
        with Phase(self) as ph:
            gB = self.bcast_row(ph, prm["ln2_g"][l:l + 1, :], D, "gB2")
            bB = self.bcast_row(ph, prm["ln2_b"][l:l + 1, :], D, "bB2")
            st6 = self.sb(ph, [128, 4, 6], F32, "st6")
            mv = self.sb(ph, [128, 4], F32, "mv")
            xt = [self.sb(ph, [128, D], F32, "xt") for _ in range(2)]
            ya = [self.sb(ph, [128, D], F32, "ya") for _ in range(2)]
            yb = [self.sb(ph, [128, D], F32, "yb") for _ in range(2)]
            for i in range(NT):
                x_, a_, b_ = xt[i % 2], ya[i % 2], yb[i % 2]
                self.dma("sp", x_.t[:], xin.t[i * 128:(i + 1) * 128, :], [xin], [x_])
                for di, y_ in ((d1i, a_), (d2i, b_)):
                    self.P.dma_raw("pool", lambda di=di, y_=y_, i=i: nc.gpsimd.indirect_dma_start(
                        out=y_.t[:, :], out_offset=None, in_=yslots.t[:, :], in_offset=bass.IndirectOffsetOnAxis(ap=di.t[:, i:i + 1], axis=0)),
                        self._b([yslots, di]), self._b([y_]))
                self.ts(a_.t[:, :], a_.t[:, :], G12.t[:, i, 0:1], None, ALU.mult, None, [a_, G12], [a_])
                self.stt(a_.t[:, :], b_.t[:, :], G12.t[:, i, 1:2], a_.t[:, :], ALU.mult, ALU.add, [b_, G12, a_], [a_])
                self.stt(x_.t[:, :], x_.t[:, :], ALPHA, a_.t[:, :], ALU.mult, ALU.add, [x_, a_], [x_])
                _ln_rows(self, x_, gB, bB, st6, mv, 1e-5)
                self.dma("pool", xout.t[i * 128:(i + 1) * 128, :], x_.t[:, :], [x_], [xout])


KB.moe_routed = _moe_routed
```

```python
import math
from contextlib import ExitStack
import numpy as np
import ml_dtypes
import concourse.bass as bass
import concourse.mybir as mybir
from concourse.bass_utils import run_bass_kernel_spmd

F32 = mybir.dt.float32
BF16 = mybir.dt.bfloat16
AF = mybir.ActivationFunctionType
ALU = mybir.AluOpType
SEM_LIMIT = 8000

L = 2
D = 2048
S = 2048
NSEQ = 2
T = NSEQ * S
INC = 5328
DFF = 5632
DFE = 7168
NE = 8
ALPHA = (2 * L) ** 0.25
NF = 17
NFP = NF * 128
C_QC, C_KVC, C_KPE, C_RQ, C_RK, C_RV, C_RG, C_MZ, C_XBC, C_DT, C_HU = 0, 384, 640, 704, 960, 1216, 1728, 2240, 2752, 3776, 3792
RET_LG_F = [math.log1p(-2.0 ** (-5.0 - h)) for h in range(4)]
RET_LG_B = [math.log1p(-2.0 ** (-5.5 - h)) for h in range(4)]


class Buf:
    __slots__ = ("name", "w", "r")

    def __init__(self, name=""):
        self.name = name
        self.w = {}
        self.r = {}


class Prog:
    def __init__(self, nc, stack):
        self.nc = nc
        self.stack = stack
        self.eng = {"pe": nc.tensor, "dve": nc.vector, "act": nc.scalar, "pool": nc.gpsimd, "sp": nc.sync}
        self.cur_sem, self.cnt, self.sems, self.nsem = {}, {}, {}, 0
        for e in self.eng:
            self._new_eng_sem(e)
        self.waited = {e: {} for e in self.eng}
        self.nslots = 8
        self.slots = {q: [[self._new_sem("d%s%d" % (q, i)), 0] for i in range(self.nslots)] for q in ("sp", "pool", "act")}
        self.slot_i = {q: 0 for q in self.slots}
        self.n_inst = 0

    def _new_sem(self, name):
        self.nsem += 1
        key = "%s_%d" % (name, self.nsem)
        self.sems[key] = self.stack.enter_context(self.nc.semaphore(key))
        return key

    def _new_eng_sem(self, e):
        self.cur_sem[e] = self._new_sem("c" + e)
        self.cnt[e] = 0

    def _wait(self, e, tok):
        if tok is None:
            return
        key, val, src = tok
        if src == e and e == "pe":
            return
        w = self.waited[e]
        if w.get(key, 0) >= val:
            return
        self.eng[e].wait_ge(self.sems[key], val)
        w[key] = val

    def _deps(self, e, reads, writes):
        for b in reads:
            for t in b.w.values():
                self._wait(e, t)
        for b in writes:
            for t in b.w.values():
                self._wait(e, t)
            for t in b.r.values():
                if t[2] != e:
                    self._wait(e, t)

    def _record(self, tok, reads, writes):
        for b in reads:
            b.r[tok[0]] = tok
        for b in writes:
            b.w[tok[0]] = tok
            b.r = {}

    def op(self, e, fn, reads=(), writes=()):
        self._deps(e, reads, writes)
        ins = fn()
        self.n_inst += 1
        if self.cnt[e] >= SEM_LIMIT:
            self._new_eng_sem(e)
        self.cnt[e] += 1
        ins.then_inc(self.sems[self.cur_sem[e]], 1)
        tok = (self.cur_sem[e], self.cnt[e], e)
        self._record(tok, reads, writes)
        return tok

    def dma(self, q, out, in_, reads=(), writes=(), **kw):
        self._deps(q, reads, writes)
        i = self.slot_i[q]
        self.slot_i[q] = (i + 1) % self.nslots
        sl = self.slots[q][i]
        if sl[1] > 0:
            self._wait(q, (sl[0], sl[1], "dma"))
        if sl[1] + 16 > SEM_LIMIT:
            sl[0] = self._new_sem("d%s%d" % (q, i))
            sl[1] = 0
        sl[1] += 16
        self.eng[q].dma_start(out=out, in_=in_, **kw).then_inc(self.sems[sl[0]], 16)
        tok = (sl[0], sl[1], "dma")
        self._record(tok, reads, writes)
        self.n_inst += 1
        return tok

    def dma_raw(self, q, emit, reads=(), writes=()):
        self._deps(q, reads, writes)
        i = self.slot_i[q]
        self.slot_i[q] = (i + 1) % self.nslots
        sl = self.slots[q][i]
        if sl[1] > 0:
            self._wait(q, (sl[0], sl[1], "dma"))
        if sl[1] + 16 > SEM_LIMIT:
            sl[0] = self._new_sem("d%s%d" % (q, i))
            sl[1] = 0
        sl[1] += 16
        emit().then_inc(self.sems[sl[0]], 16)
        tok = (sl[0], sl[1], "dma")
        self._record(tok, reads, writes)
        self.n_inst += 1
        return tok

    def barrier(self):
        toks = [(self.cur_sem[e], self.cnt[e], e) for e in self.eng if self.cnt[e] > 0]
        for q in self.slots:
            for key, val in self.slots[q]:
                if val:
                    toks.append((key, val, "dma"))
        for e in self.eng:
            for t in toks:
                if t[2] != e:
                    self._wait(e, t)

    def finish(self):
        for q in self.slots:
            for key, val in self.slots[q]:
                if val:
                    self._wait("sp", (key, val, "dma"))


class Phase(ExitStack):
    def __init__(self, kb):
        super().__init__()
        self.kb = kb

    def __exit__(self, *a):
        self.kb.P.barrier()
        return super().__exit__(*a)


class Tl:
    __slots__ = ("t", "b")

    def __init__(self, t, name=""):
        self.t = t
        self.b = Buf(name)


class KB:
    def __init__(self, nc, st):
        self.nc = nc
        self.st = st
        self.P = Prog(nc, st)
        self.uid = 0
        self.banks = [Tl(st.enter_context(nc.psum_tensor("bank%d" % i, [128, 512], F32)), "bank%d" % i) for i in range(8)]
        self.evac_i = 0
        self.consts = {}

    def defer(self, tag, fn):
        if not hasattr(self, "pending"):
            self.pending = []
        self.pending.append((tag, fn))

    def pump(self, n=1):
        p = getattr(self, "pending", None)
        while p and n > 0:
            p.pop(0)[1]()
            n -= 1

    def flush_tag(self, tag):
        p = getattr(self, "pending", None)
        if not p:
            return
        last = -1
        for i, (t, _) in enumerate(p):
            if t == tag:
                last = i
        for _ in range(last + 1):
            p.pop(0)[1]()

    def sb(self, stack, shape, dt, name="t"):
        self.uid += 1
        nm = "%s_%d" % (name, self.uid)
        return Tl(stack.enter_context(self.nc.sbuf_tensor(nm, list(shape), dt)), nm)

    def dram(self, name, shape, dt):
        return Tl(self.nc.dram_tensor(name, list(shape), dt).ap(), name)

    def cst(self, val):
        if val not in self.consts:
            t = self.sb(self.st, [128, 1], F32, "cst")
            self.P.op("pool", lambda: self.nc.gpsimd.memset(t.t[:], float(val)), writes=[t.b])
            self.consts[val] = t
        return self.consts[val]

    @staticmethod
    def _b(xs):
        return [x.b for x in xs]

    def act(self, out, in_, func, reads, writes, bias=None, scale=None):
        kw = {}
        rd = list(reads)
        if bias is not None:
            if isinstance(bias, (int, float)):
                c = self.cst(bias)
                rd.append(c)
                bias = c.t[0:out.shape[0], 0:1]
            kw["bias"] = bias
        if scale is not None:
            kw["scale"] = scale
        return self.P.op("act", lambda: self.nc.scalar.activation(out=out, in_=in_, func=func, **kw), self._b(rd), self._b(writes))

    def tt(self, out, in0, in1, op, reads, writes, eng="dve"):
        e = self.nc.vector if eng == "dve" else self.nc.gpsimd
        return self.P.op(eng, lambda: e.tensor_tensor(out=out, in0=in0, in1=in1, op=op), self._b(reads), self._b(writes))

    def ts(self, out, in0, s1, s2, op0, op1, reads, writes, eng="dve"):
        e = self.nc.vector if eng == "dve" else self.nc.gpsimd
        if op1 is None:
            return self.P.op(eng, lambda: e.tensor_scalar(out=out, in0=in0, scalar1=s1, scalar2=None, op0=op0), self._b(reads), self._b(writes))
        return self.P.op(eng, lambda: e.tensor_scalar(out=out, in0=in0, scalar1=s1, scalar2=s2, op0=op0, op1=op1), self._b(reads), self._b(writes))

    def stt(self, out, in0, scalar, in1, op0, op1, reads, writes):
        return self.P.op("dve", lambda: self.nc.vector.scalar_tensor_tensor(out=out, in0=in0, scalar=scalar, in1=in1, op0=op0, op1=op1), self._b(reads), self._b(writes))

    def copy(self, out, in_, reads, writes, eng=None):
        if eng is None:
            self.evac_i += 1
            eng = "act" if self.evac_i % 2 else "dve"
        if eng == "act":
            return self.P.op("act", lambda: self.nc.scalar.copy(out=out, in_=in_), self._b(reads), self._b(writes))
        e = self.nc.vector if eng == "dve" else self.nc.gpsimd
        return self.P.op(eng, lambda: e.tensor_copy(out=out, in_=in_), self._b(reads), self._b(writes))

    def recip(self, out, in_, reads, writes):
        return self.P.op("dve", lambda: self.nc.vector.reciprocal(out=out, in_=in_), self._b(reads), self._b(writes))

    def memset(self, out, val, writes, eng="pool"):
        e = self.nc.vector if eng == "dve" else self.nc.gpsimd
        return self.P.op(eng, lambda: e.memset(out, float(val)), [], self._b(writes))

    def mm(self, bank, out, lhsT, rhs, start, stop, reads):
        return self.P.op("pe", lambda: self.nc.tensor.matmul(out, lhsT=lhsT, rhs=rhs, start=start, stop=stop), self._b(reads), [bank.b])

    def tr(self, bank, out, in_, reads):
        rd = list(reads) + [self.ident]
        return self.P.op("pe", lambda: self.nc.tensor.transpose(out=out, in_=in_, identity=self.ident.t[0:in_.shape[0], 0:in_.shape[0]]), self._b(rd), [bank.b])

    def dma(self, q, out, in_, reads, writes, **kw):
        return self.P.dma(q, out, in_, self._b(reads), self._b(writes), **kw)

    def setup_consts(self):
        nc = self.nc
        self.ident = self.sb(self.st, [128, 128], F32, "ident")
        self.memset(self.ident.t[:], 1.0, [self.ident])
        self.P.op("pool", lambda: nc.gpsimd.affine_select(out=self.ident.t[:], in_=self.ident.t[:], pattern=[[1, 128]], compare_op=ALU.is_equal, fill=0.0, base=0, channel_multiplier=-1), self._b([self.ident]), self._b([self.ident]))
        self.ones_bf = self.sb(self.st, [128, 128], BF16, "ones_bf")
        self.memset(self.ones_bf.t[:], 1.0, [self.ones_bf])
        self.ones_f = self.sb(self.st, [128, 128], F32, "ones_f")
        self.memset(self.ones_f.t[:], 1.0, [self.ones_f])
        for v in (1e-6, 1e-5, 1.0, 0.0):
            self.cst(v)
        self.sel = self.sb(self.st, [16, 16, 128], F32, "sel")
        self.memset(self.sel.t[:], 0.0, [self.sel])
        self.P.op("pool", lambda: nc.gpsimd.affine_select(out=self.sel.t[:], in_=self.sel.t[:], pattern=[[-1, 16], [0, 128]], compare_op=ALU.not_equal, fill=1.0, base=0, channel_multiplier=1), self._b([self.sel]), self._b([self.sel]))

    def bcast_row(self, stack, row_ap, n, name):
        out = self.sb(stack, [128, n], F32, name)
        with Phase(self) as p:
            rowt = self.sb(p, [1, n], F32, name + "_row")
            self.dma("sp", rowt.t[:], row_ap, [], [rowt])
            for c in range(0, n, 512):
                w = min(512, n - c)
                bk = self.banks[(c // 512) % 2]
                self.mm(bk, bk.t[:, 0:w], self.ones_f.t[0:1, :], rowt.t[0:1, c:c + w], True, True, [self.ones_f, rowt])
                self.copy(out.t[:, c:c + w], bk.t[:, 0:w], [bk], [out])
        return out

    def load_xT(self, src, tok0, xT, xtiles, ntt=4, x32=None, post=None):
        for ts_ in range(ntt):
            xt = xtiles[ts_ % len(xtiles)]
            self.dma("sp", xt.t[:], src.t[tok0 + ts_ * 128: tok0 + (ts_ + 1) * 128, :], [src], [xt])
            for j in range(4):
                bk = self.banks[4 + j]
                for k in range(4):
                    dc = 4 * j + k
                    self.tr(bk, bk.t[:, k * 128:(k + 1) * 128], xt.t[:, dc * 128:(dc + 1) * 128], [xt])
                bv = bk.t[:].rearrange("p (a b) -> p a b", a=4)
                ce = "act" if j % 2 else "dve"
                self.copy(xT.t[:, 4 * j:4 * j + 4, ts_ * 128:(ts_ + 1) * 128], bv, [bk], [xT], eng=ce)
                if x32 is not None:
                    self.copy(x32.t[:, 4 * j:4 * j + 4, :], bv, [bk], [x32], eng=ce)
            if post is not None:
                post(ts_)

    def cast_dram(self, dst, src_ap, rows, step=256, tag=None):
        if src_ap.shape[-1] > 5632:
            step = 128
        for r0 in range(0, rows, step):
            r1 = min(rows, r0 + step)
            if tag is None:
                self.dma("pool", dst.t[r0:r1, :], src_ap[r0:r1, :], [], [dst])
            else:
                self.defer(tag, lambda r0=r0, r1=r1: self.dma("pool", dst.t[r0:r1, :], src_ap[r0:r1, :], [], [dst]))

    def build_rot(self, l, w_in_ap, wrot):
        with Phase(self) as ph:
            src = self.sb(ph, [128, 16, 576], F32, "rsrc")
            dst = self.sb(ph, [128, 16, 576], BF16, "rdst")
            self.dma("sp", src.t[:], w_in_ap[l, :, C_KPE:C_KPE + 576].rearrange("(dc p) c -> p dc c", p=128), [], [src])
            for dc in range(16):
                sv = src.t[:, dc, :].rearrange("p (g two h) -> p g two h", two=2, h=32)
                dv = dst.t[:, dc, :].rearrange("p (g two h) -> p g two h", two=2, h=32)
                self.ts(dv[:, :, 0, :], sv[:, :, 1, :], -1.0, None, ALU.mult, None, [src], [dst])
                self.copy(dv[:, :, 1, :], sv[:, :, 0, :], [src], [dst], eng="dve")
            self.dma("pool", wrot.t[l].rearrange("(dc p) c -> p dc c", p=128), dst.t[:], [dst], [wrot])

    def inproj(self, l, xres, winb, wrot, seg, cst, tok_range, wdep=None):
        with Phase(self) as ph:
            xT = self.sb(ph, [128, 16, 512], BF16, "xT")
            xtiles = [self.sb(ph, [128, 2048], F32, "xtile") for _ in range(2)]
            wbuf = [self.sb(ph, [128, 16, 576], BF16, "wbuf") for _ in range(3)]
            rbuf = self.sb(ph, [128, 16, 576], BF16, "rbuf")
            cos = self.sb(ph, [128, 2048], F32, "cos")
            sin = self.sb(ph, [128, 2048], F32, "sin")
            self.dma("sp", cos.t[:], cst["rope_cos"], [], [cos])
            self.dma("sp", sin.t[:], cst["rope_sin"], [], [sin])
            self.dma("sp", rbuf.t[:], wrot.t[l].rearrange("(dc p) c -> p dc c", p=128), [wrot], [rbuf])
            stage = [self.sb(ph, [128, 512], BF16, "stg") for _ in range(4)]
            st32 = [self.sb(ph, [128, 512], F32, "st32") for _ in range(3)]
            stdt = self.sb(ph, [16, 512], F32, "stdt")
            sti = [0]
            bki = [0]
            groups = [(0, 384, "fm", [("qc", 0, 0, 128), ("qc", 128, 128, 128), ("qc", 256, 256, 128)]),
                      (384, 256, "fm", [("kvc", 0, 0, 128), ("kvc", 128, 128, 128)]),
                      (640, 576, "rope", [("kpe", 0, 0, 64, 1.0), ("rq", 0, 64, 128, 1.0), ("rq", 128, 192, 128, 1.0),
                                          ("rk", 0, 320, 128, 0.125), ("rk", 128, 448, 128, 0.125)]),
                      (1216, 512, "tm", None),
                      (1728, 512, "fm", [("rg", i * 128, i * 128, 128) for i in range(4)]),
                      (2240, 512, "fm", [("mz", i * 128, i * 128, 128) for i in range(4)]),
                      (2752, 512, "fm", [("xbc", i * 128, i * 128, 128) for i in range(4)]),
                      (3264, 512, "fm", [("xbc", 512 + i * 128, i * 128, 128) for i in range(4)]),
                      (3776, 16, "dt", None),
                      (3792, 512, "fm", [("hu", i * 128, i * 128, 128) for i in range(4)]),
                      (4304, 512, "fm", [("hu", 512 + i * 128, i * 128, 128) for i in range(4)]),
                      (4816, 512, "fm", [("hu", 1024 + i * 128, i * 128, 128) for i in range(4)])]

            def loadw(gi):
                c0, cw = groups[gi][0], groups[gi][1]
                wt = wbuf[gi % 3]
                self.dma("sp", wt.t[:, :, 0:cw], winb.t[l, :, c0:c0 + cw].rearrange("(dc p) c -> p dc c", p=128), [wdep or winb], [wt])

            for tok0 in range(tok_range[0], tok_range[1], 512):
                tpos = tok0 % S
                self.load_xT(xres, tok0, xT, xtiles)
                loadw(0)
                loadw(1)
                for gi, (c0, cw, kind, chunks) in enumerate(groups):
                    self.pump(2)
                    if gi + 2 < len(groups):
                        loadw(gi + 2)
                    wt = wbuf[gi % 3]
                    if kind == "fm":
                        for (sname, row0, lc, n) in chunks:
                            bk = self.banks[bki[0] % 4]
                            bki[0] += 1
                            for dc in range(16):
                                self.mm(bk, bk.t[0:n, :], wt.t[:, dc, lc:lc + n], xT.t[:, dc, :], dc == 0, dc == 15, [wt, xT])
                            sg = stage[sti[0] % 4]
                            sti[0] += 1
                            self.copy(sg.t[0:n, :], bk.t[0:n, :], [bk], [sg])
                            self.dma("pool", seg[sname].t[row0:row0 + n, tok0:tok0 + 512], sg.t[0:n, :], [sg], [seg[sname]])
                    elif kind == "dt":
                        bk = self.banks[bki[0] % 4]
                        bki[0] += 1
                        for dc in range(16):
                            self.mm(bk, bk.t[0:16, :], wt.t[:, dc, 0:16], xT.t[:, dc, :], dc == 0, dc == 15, [wt, xT])
                        self.copy(stdt.t[:], bk.t[0:16, :], [bk], [stdt])
                        self.dma("pool", seg["dt"].t[:, tok0:tok0 + 512], stdt.t[:], [stdt], [seg["dt"]])
                    elif kind == "tm":
                        for ts_ in range(4):
                            bk = self.banks[bki[0] % 4]
                            bki[0] += 1
                            for dc in range(16):
                                self.mm(bk, bk.t[:, :], xT.t[:, dc, ts_ * 128:(ts_ + 1) * 128], wt.t[:, dc, 0:512], dc == 0, dc == 15, [wt, xT])
                            sg = stage[sti[0] % 4]
                            sti[0] += 1
                            self.copy(sg.t[:, :], bk.t[:, :], [bk], [sg])
                            self.dma("pool", seg["rv"].t[tok0 + ts_ * 128: tok0 + (ts_ + 1) * 128, :], sg.t[:, :], [sg], [seg["rv"]])
                    else:
                        for (sname, row0, lc, n, scl) in chunks:
                            bA = self.banks[bki[0] % 4]
                            bB = self.banks[(bki[0] + 1) % 4]
                            bki[0] += 2
                            for dc in range(16):
                                self.mm(bA, bA.t[0:n, :], wt.t[:, dc, lc:lc + n], xT.t[:, dc, :], dc == 0, dc == 15, [wt, xT])
                            for dc in range(16):
                                self.mm(bB, bB.t[0:n, :], rbuf.t[:, dc, lc:lc + n], xT.t[:, dc, :], dc == 0, dc == 15, [rbuf, xT])
                            self.tt(st32[0].t[0:n, :], bA.t[0:n, :], cos.t[0:n, tpos:tpos + 512], ALU.mult, [bA, cos], [st32[0]])
                            self.tt(st32[1].t[0:n, :], bB.t[0:n, :], sin.t[0:n, tpos:tpos + 512], ALU.mult, [bB, sin], [st32[1]])
                            self.tt(st32[2].t[0:n, :], st32[0].t[0:n, :], st32[1].t[0:n, :], ALU.add, [st32[0], st32[1]], [st32[2]])
                            sg = stage[sti[0] % 4]
                            sti[0] += 1
                            self.act(sg.t[0:n, :], st32[2].t[0:n, :], AF.Copy, [st32[2]], [sg], scale=scl)
                            self.dma("pool", seg[sname].t[row0:row0 + n, tok0:tok0 + 512], sg.t[0:n, :], [sg], [seg[sname]])

    def rms_rstd(self, ph, src, nch, rows, cols, nfeat, eps, bank, tmp_sq, out_rstd):
        c0, c1 = cols
        for c in range(nch):
            self.act(tmp_sq.t[0:rows, c, :], src.t[0:rows, c, c0:c1], AF.Square, [src], [tmp_sq])
        for c in range(nch):
            self.mm(bank, bank.t[:, :], self.ones_bf.t[0:rows, :], tmp_sq.t[0:rows, c, :], c == 0, c == nch - 1, [self.ones_bf, tmp_sq])
        self.act(out_rstd.t[:, :], bank.t[:, :], AF.Sqrt, [bank], [out_rstd], bias=eps, scale=1.0 / nfeat)
        self.recip(out_rstd.t[:, :], out_rstd.t[:, :], [out_rstd], [out_rstd])

    def group_ret(self, s, seg, mixedT):
        nc = self.nc
        t0 = s * S
        with Phase(self) as ph:
            rq = self.sb(ph, [128, 2, S], BF16, "rq")
            rk = self.sb(ph, [128, 2, S], BF16, "rk")
            rg = self.sb(ph, [128, 4, S], BF16, "rg")
            V = self.sb(ph, [128, 16, 512], BF16, "rv")
            self.dma("sp", rq.t[:], seg["rq"].t[:, t0:t0 + S].rearrange("(c p) t -> p c t", p=128), [seg["rq"]], [rq])
            self.dma("sp", rk.t[:], seg["rk"].t[:, t0:t0 + S].rearrange("(c p) t -> p c t", p=128), [seg["rk"]], [rk])
            self.dma("sp", rg.t[:], seg["rg"].t[:, t0:t0 + S].rearrange("(c p) t -> p c t", p=128), [seg["rg"]], [rg])
            self.dma("sp", V.t[:], seg["rv"].t[t0:t0 + S, :].rearrange("(c p) e -> p c e", p=128), [seg["rv"]], [V])
            W = 3968
            strip = self.sb(ph, [128, 4, W], BF16, "strip")
            dl = self.sb(ph, [128, W], F32, "dl")
            tA = self.sb(ph, [128, W], F32, "tA")
            tB = self.sb(ph, [128, W], F32, "tB")
            self.P.op("pool", lambda: nc.gpsimd.iota(dl.t[:], pattern=[[1, W]], base=-1920, channel_multiplier=-1, allow_small_or_imprecise_dtypes=True), [], [dl.b])
            for h in range(4):
                self.ts(tA.t[:], dl.t[:], 0.0, RET_LG_F[h], ALU.max, ALU.mult, [dl], [tA])
                self.ts(tB.t[:], dl.t[:], 0.0, -RET_LG_B[h], ALU.min, ALU.mult, [dl], [tB])
                self.tt(tA.t[:], tA.t[:], tB.t[:], ALU.add, [tA, tB], [tA])
                self.act(strip.t[:, h, :], tA.t[:], AF.Exp, [tA], [strip])
            Pb = [self.sb(ph, [128, 512], BF16, "P") for _ in range(6)]
            ysb = self.sb(ph, [128, 512], F32, "ysb")
            ysq = self.sb(ph, [128, 512], F32, "ysq")
            mean = self.sb(ph, [128, 512], F32, "mean")
            var = self.sb(ph, [128, 512], F32, "var")
            gate = self.sb(ph, [128, 512], F32, "gate")
            ob = [self.sb(ph, [128, 512], BF16, "ob") for _ in range(2)]
            pi = 0
            for h in range(4):
                c, base = h // 2, 64 * (h % 2)
                for ib in range(4):
                    self.pump(3)
                    i0 = ib * 512
                    yb = self.banks[4 + (h * 4 + ib) % 2]
                    def S_(jc):
                        sbk = self.banks[jc % 4]
                        self.mm(sbk, sbk.t[:, :], rk.t[base:base + 64, c, jc * 128:jc * 128 + 128], rq.t[base:base + 64, c, i0:i0 + 512], True, True, [rk, rq])

                    S_(0)
                    for jc in range(16):
                        j0 = jc * 128
                        if jc + 1 < 16:
                            S_(jc + 1)
                        sbk = self.banks[jc % 4]
                        pb = Pb[pi % 6]
                        pi += 1
                        x0 = i0 - j0 + 1920
                        self.tt(pb.t[:, :], sbk.t[:, :], strip.t[:, h, x0:x0 + 512], ALU.mult, [sbk, strip], [pb])
                        self.mm(yb, yb.t[:, :], V.t[:, jc, h * 128:(h + 1) * 128], pb.t[:, :], jc == 0, jc == 15, [V, pb])
                    self.copy(ysb.t[:, :], yb.t[:, :], [yb], [ysb], eng="act")
                    self.act(ysq.t[:, :], yb.t[:, :], AF.Square, [yb], [ysq])
                    mb, vb = self.banks[6], self.banks[7]
                    self.mm(mb, mb.t[:, :], self.ones_f.t[:, :], ysb.t[:, :], True, True, [self.ones_f, ysb])
                    self.mm(vb, vb.t[:, :], self.ones_f.t[:, :], ysq.t[:, :], True, True, [self.ones_f, ysq])
                    self.act(mean.t[:, :], mb.t[:, :], AF.Copy, [mb], [mean], scale=1.0 / 128)
                    self.tt(var.t[:, :], mean.t[:, :], mean.t[:, :], ALU.mult, [mean], [var])
                    self.stt(var.t[:, :], vb.t[:, :], 1.0 / 128, var.t[:, :], ALU.mult, ALU.subtract, [vb, var], [var])
                    self.act(var.t[:, :], var.t[:, :], AF.Sqrt, [var], [var], bias=1e-6, scale=1.0)
                    self.recip(var.t[:, :], var.t[:, :], [var], [var])
                    self.tt(ysb.t[:, :], ysb.t[:, :], mean.t[:, :], ALU.subtract, [ysb, mean], [ysb])
                    self.tt(ysb.t[:, :], ysb.t[:, :], var.t[:, :], ALU.mult, [ysb, var], [ysb])
                    self.act(gate.t[:, :], rg.t[:, h, i0:i0 + 512], AF.Silu, [rg], [gate])
                    o = ob[(h * 4 + ib) % 2]
                    self.tt(o.t[:, :], ysb.t[:, :], gate.t[:, :], ALU.mult, [ysb, gate], [o])
                    self.dma("pool", mixedT.t[512 + h * 128: 512 + (h + 1) * 128, t0 + i0: t0 + i0 + 512], o.t[:, :], [o], [mixedT])

    def group_mla(self, l, s, seg, mixedT, prm, cst):
        t0 = s * S
        SC = (128 + 64) ** -0.5
        with Phase(self) as ph:
            qc = self.sb(ph, [128, 3, S], BF16, "qc")
            kvc = self.sb(ph, [128, 2, S], BF16, "kvc")
            kpe = self.sb(ph, [64, S], BF16, "kpe")
            self.dma("sp", qc.t[:], seg["qc"].t[:, t0:t0 + S].rearrange("(c p) t -> p c t", p=128), [seg["qc"]], [qc])
            self.dma("sp", kvc.t[:], seg["kvc"].t[:, t0:t0 + S].rearrange("(c p) t -> p c t", p=128), [seg["kvc"]], [kvc])
            self.dma("sp", kpe.t[:], seg["kpe"].t[:, t0:t0 + S], [seg["kpe"]], [kpe])
            cos = self.sb(ph, [64, S], F32, "cos")
            sin = self.sb(ph, [64, S], F32, "sin")
            self.dma("sp", cos.t[:], cst["rope_cos"][0:64, :], [], [cos])
            self.dma("sp", sin.t[:], cst["rope_sin"][0:64, :], [], [sin])
            wq32 = self.sb(ph, [128, 3, 768], F32, "wq32")
            wkv32 = self.sb(ph, [128, 2, 1024], F32, "wkv32")
            self.dma("sp", wq32.t[:], prm["mla_w_uq"][l].rearrange("(c p) e -> p c e", p=128), [], [wq32])
            self.dma("sp", wkv32.t[:], prm["mla_w_ukv"][l].rearrange("(c p) e -> p c e", p=128), [], [wkv32])
            wq = self.sb(ph, [128, 3, 768], BF16, "wq")
            wkv = self.sb(ph, [128, 2, 1024], BF16, "wkv")
            wqr = self.sb(ph, [128, 3, 4, 64], BF16, "wqr")
            self.copy(wq.t[:], wq32.t[:], [wq32], [wq], eng="dve")
            self.copy(wkv.t[:], wkv32.t[:], [wkv32], [wkv], eng="dve")
            for c in range(3):
                for h in range(4):
                    b0 = 192 * h + 128
                    self.ts(wqr.t[:, c, h, 0:32], wq32.t[:, c, b0 + 32:b0 + 64], -1.0, None, ALU.mult, None, [wq32], [wqr])
                    self.copy(wqr.t[:, c, h, 32:64], wq32.t[:, c, b0:b0 + 32], [wq32], [wqr], eng="dve")
            qnw = self.sb(ph, [128, 3], F32, "qnw")
            kvnw = self.sb(ph, [128, 2], F32, "kvnw")
            onw = self.sb(ph, [128, 4], F32, "onw")
            self.dma("sp", qnw.t[:], prm["mla_q_norm_pp"][:, l * 3:(l + 1) * 3], [], [qnw])
            self.dma("sp", kvnw.t[:], prm["mla_kv_norm_pp"][:, l * 2:(l + 1) * 2], [], [kvnw])
            self.dma("sp", onw.t[:], prm["mla_out_norm_pp"][:, l * 4:(l + 1) * 4], [], [onw])
            qn = self.sb(ph, [128, 3, S], BF16, "qn")
            kvn = self.sb(ph, [128, 2, S], BF16, "kvn")
            sq = self.sb(ph, [128, 4, 512], BF16, "sq")
            rstd = self.sb(ph, [128, 512], F32, "rstd")
            for tb in range(4):
                c0 = tb * 512
                self.rms_rstd(ph, qc, 3, 128, (c0, c0 + 512), 384.0, 1e-6, self.banks[0], sq, rstd)
                for c in range(3):
                    self.stt(qn.t[:, c, c0:c0 + 512], qc.t[:, c, c0:c0 + 512], qnw.t[:, c:c + 1], rstd.t[:, :], ALU.mult, ALU.mult, [qc, qnw, rstd], [qn])
                self.rms_rstd(ph, kvc, 2, 128, (c0, c0 + 512), 256.0, 1e-6, self.banks[1], sq, rstd)
                for c in range(2):
                    self.stt(kvn.t[:, c, c0:c0 + 512], kvc.t[:, c, c0:c0 + 512], kvnw.t[:, c:c + 1], rstd.t[:, :], ALU.mult, ALU.mult, [kvc, kvnw, rstd], [kvn])
            qhn = self.sb(ph, [128, 4, S], BF16, "qhn")
            qhp = self.sb(ph, [64, 4, S], BF16, "qhp")
            khn = self.sb(ph, [128, 4, S], BF16, "khn")
            Vt = self.sb(ph, [128, 16, 512], BF16, "Vt")
            t1 = self.sb(ph, [64, 512], F32, "t1")
            t2 = self.sb(ph, [64, 512], F32, "t2")
            bi = 0
            for tb in range(4):
                c0 = tb * 512
                for h in range(4):
                    bk = self.banks[bi % 4]; bi += 1
                    for c in range(3):
                        self.mm(bk, bk.t[:, :], wq.t[:, c, 192 * h:192 * h + 128], qn.t[:, c, c0:c0 + 512], c == 0, c == 2, [wq, qn])
                    self.copy(qhn.t[:, h, c0:c0 + 512], bk.t[:, :], [bk], [qhn])
                    bA = self.banks[bi % 4]; bi += 1
                    bB = self.banks[bi % 4]; bi += 1
                    for c in range(3):
                        self.mm(bA, bA.t[0:64, :], wq.t[:, c, 192 * h + 128:192 * h + 192], qn.t[:, c, c0:c0 + 512], c == 0, c == 2, [wq, qn])
                    for c in range(3):
                        self.mm(bB, bB.t[0:64, :], wqr.t[:, c, h, :], qn.t[:, c, c0:c0 + 512], c == 0, c == 2, [wqr, qn])
                    self.tt(t1.t[:, :], bA.t[0:64, :], cos.t[:, c0:c0 + 512], ALU.mult, [bA, cos], [t1])
                    self.tt(t2.t[:, :], bB.t[0:64, :], sin.t[:, c0:c0 + 512], ALU.mult, [bB, sin], [t2])
                    self.tt(qhp.t[:, h, c0:c0 + 512], t1.t[:, :], t2.t[:, :], ALU.add, [t1, t2], [qhp])
                    bk = self.banks[bi % 4]; bi += 1
                    for c in range(2):
                        self.mm(bk, bk.t[:, :], wkv.t[:, c, 256 * h:256 * h + 128], kvn.t[:, c, c0:c0 + 512], c == 0, c == 1, [wkv, kvn])
                    self.copy(khn.t[:, h, c0:c0 + 512], bk.t[:, :], [bk], [khn])
            for tc in range(16):
                bk = self.banks[bi % 4]; bi += 1
                for h in range(4):
                    for c in range(2):
                        self.mm(bk, bk.t[:, h * 128:(h + 1) * 128], kvn.t[:, c, tc * 128:(tc + 1) * 128], wkv.t[:, c, 256 * h + 128:256 * h + 256], c == 0, c == 1, [wkv, kvn])
                self.copy(Vt.t[:, tc, :], bk.t[:, :], [bk], [Vt])
            Pb = [self.sb(ph, [128, 512], BF16, "P") for _ in range(6)]
            oblk = self.sb(ph, [128, 4, 512], F32, "oblk")
            den = self.sb(ph, [128, 512], F32, "den")
            ob = [self.sb(ph, [128, 512], BF16, "ob") for _ in range(2)]
            pi = 0
            for qb in range(4):
                q0 = qb * 512
                for h in range(4):
                    self.pump(3)
                    ob_k, dn_k = self.banks[4 + 2 * (h % 2)], self.banks[5 + 2 * (h % 2)]
                    def S_(kc):
                        k0 = kc * 128
                        sbk = self.banks[kc % 4]
                        self.mm(sbk, sbk.t[:, :], khn.t[:, h, k0:k0 + 128], qhn.t[:, h, q0:q0 + 512], True, False, [khn, qhn])
                        self.mm(sbk, sbk.t[:, :], kpe.t[0:64, k0:k0 + 128], qhp.t[0:64, h, q0:q0 + 512], False, True, [kpe, qhp])

                    S_(0)
                    for kc in range(16):
                        if kc + 1 < 16:
                            S_(kc + 1)
                        sbk = self.banks[kc % 4]
                        pb = Pb[pi % 6]; pi += 1
                        self.act(pb.t[:, :], sbk.t[:, :], AF.Exp, [sbk], [pb], scale=SC)
                        self.mm(ob_k, ob_k.t[:, :], Vt.t[:, kc, h * 128:(h + 1) * 128], pb.t[:, :], kc == 0, kc == 15, [Vt, pb])
                        self.mm(dn_k, dn_k.t[:, :], self.ones_bf.t[:, :], pb.t[:, :], kc == 0, kc == 15, [self.ones_bf, pb])
                    self.recip(den.t[:, :], dn_k.t[:, :], [dn_k], [den])
                    self.tt(oblk.t[:, h, :], ob_k.t[:, :], den.t[:, :], ALU.mult, [ob_k, den], [oblk])
                for h in range(4):
                    self.act(sq.t[:, h, :], oblk.t[:, h, :], AF.Square, [oblk], [sq])
                nb = self.banks[3]
                for h in range(4):
                    self.mm(nb, nb.t[:, :], self.ones_bf.t[:, :], sq.t[:, h, :], h == 0, h == 3, [self.ones_bf, sq])
                self.act(rstd.t[:, :], nb.t[:, :], AF.Sqrt, [nb], [rstd], bias=1e-6, scale=1.0 / 512)
                self.recip(rstd.t[:, :], rstd.t[:, :], [rstd], [rstd])
                for h in range(4):
                    o = ob[h % 2]
                    self.stt(o.t[:, :], oblk.t[:, h, :], onw.t[:, h:h + 1], rstd.t[:, :], ALU.mult, ALU.mult, [oblk, onw, rstd], [o])
                    self.dma("pool", mixedT.t[h * 128:(h + 1) * 128, t0 + q0:t0 + q0 + 512], o.t[:, :], [o], [mixedT])


def _pp(v, nl):
    v = np.asarray(v, np.float32)
    n = v.shape[-1]
    return np.ascontiguousarray(v.reshape(nl, n // 128, 128).transpose(2, 0, 1).reshape(128, nl * (n // 128)))


def host_consts():
    c = {}
    half = 32
    inv = (10000.0 ** (-np.arange(half, dtype=np.float32) * 2.0 / 64)).astype(np.float32)
    ang = np.arange(S, dtype=np.float32)[None, :] * inv[:, None]
    c["rope_cos"] = np.ascontiguousarray(np.tile(np.cos(ang), (4, 1)).astype(np.float32))
    c["rope_sin"] = np.ascontiguousarray(np.tile(np.sin(ang), (4, 1)).astype(np.float32))
    t = np.linspace(0.0, 1.0, S, dtype=np.float32)[:, None]
    bands = 16
    angp = (2.0 * math.pi * np.arange(S, dtype=np.float32)[:, None] / S).astype(np.float32)
    f = np.linspace(1e-4, bands - 1, bands, dtype=np.float32)[None, :]
    z = np.concatenate([t, np.cos(f * angp), -np.sin(f * angp)], axis=-1).astype(np.float32)
    c["hy_zT"] = np.ascontiguousarray(z.T)
    c["hy_ntlin_pp"] = np.ascontiguousarray((-t[:, 0]).reshape(16, 128).T.astype(np.float32))
    mn, mx = math.log(1e-2) / 1.5, math.log(1e-2) / 0.3
    dl = np.abs(np.linspace(mn, mx, 512, dtype=np.float32))
    c["hy_delta_b"] = np.ascontiguousarray(np.tile(dl[None, :], (128, 1)).astype(np.float32))
    idx = np.arange(NFP, dtype=np.int64)
    ph_ = (np.outer(idx, idx) % 4096).astype(np.float64) * (2.0 * math.pi / 4096.0)
    valid = (idx <= 2048).astype(np.float64)
    Cm = np.cos(ph_) * valid[:, None] * valid[None, :]
    Sm = -np.sin(ph_) * valid[:, None] * valid[None, :]
    bf = ml_dtypes.bfloat16
    c["dft_Cnat"] = np.ascontiguousarray(Cm[:, :S].astype(np.float32).astype(bf))
    c["dft_Snat"] = np.ascontiguousarray(Sm[:, :S].astype(np.float32).astype(bf))
    c["dft_Cblk"] = np.ascontiguousarray(Cm[:S, :].reshape(16, 128, NF, 128).transpose(2, 1, 0, 3).astype(np.float32).astype(bf))
    c["dft_Sblk"] = np.ascontiguousarray(Sm[:S, :].reshape(16, 128, NF, 128).transpose(2, 1, 0, 3).astype(np.float32).astype(bf))
    wfv = np.where(idx <= 2048, 2.0, 0.0)
    wfv[0] = 1.0
    wfv[2048] = 1.0
    c["dft_wf_pp"] = np.ascontiguousarray((wfv / 4096.0).reshape(NF, 128).T.astype(np.float32))
    return c


def host_params(inp):
    p = {}
    p["mla_w_uq"] = np.ascontiguousarray(inp["mla_w_uq"], dtype=np.float32)
    p["mla_w_ukv"] = np.ascontiguousarray(inp["mla_w_ukv"], dtype=np.float32)
    p["mla_q_norm_pp"] = _pp(inp["mla_q_norm"], L)
    p["mla_kv_norm_pp"] = _pp(inp["mla_kv_norm"], L)
    p["mla_out_norm_pp"] = _pp(inp["mla_out_norm"], L)
    cwv = np.asarray(inp["ssd_conv_w"], np.float32)
    p["ssd_conv_w_pp"] = np.ascontiguousarray(cwv.reshape(L, 5, 8, 128).transpose(3, 0, 2, 1).reshape(128, L * 40))
    p["ssd_conv_b_pp"] = _pp(inp["ssd_conv_b"], L)
    p["ssd_dtb"] = np.ascontiguousarray(np.asarray(inp["ssd_dt_bias"], np.float32).reshape(L, 16).T)
    p["ssd_alog"] = np.ascontiguousarray(np.asarray(inp["ssd_a_log"], np.float32).reshape(L, 16).T)
    dd = np.asarray(inp["ssd_d"], np.float32)
    p["ssd_d_pp"] = np.ascontiguousarray(np.repeat(dd, 64, axis=1).reshape(L, 4, 128).transpose(2, 0, 1).reshape(128, L * 4))
    p["ssd_norm_pp"] = _pp(inp["ssd_norm"], L)
    hw = np.asarray(inp["hy_conv_w"], np.float32)
    p["hy_conv_w_pp"] = np.ascontiguousarray(hw.reshape(L, 3, 12, 128).transpose(3, 0, 2, 1).reshape(128, L * 36))
    p["hy_conv_b_pp"] = _pp(inp["hy_conv_b"], L)
    p["hy_w1"] = np.ascontiguousarray(inp["hy_w1"], dtype=np.float32)
    p["hy_w2"] = np.ascontiguousarray(inp["hy_w2"], dtype=np.float32)
    p["hy_w3"] = np.ascontiguousarray(inp["hy_w3"], dtype=np.float32)
    b12 = np.stack([np.asarray(inp["hy_b1"], np.float32), np.asarray(inp["hy_b2"], np.float32)], 1)
    p["hy_b_pp"] = np.ascontiguousarray(b12.transpose(2, 0, 1).reshape(64, L * 2))
    p["hy_freq_pp"] = np.ascontiguousarray(np.asarray(inp["hy_freq"], np.float32).transpose(2, 0, 1).reshape(64, L * 2))
    hbv = np.asarray(inp["hy_bias"], np.float32)
    p["hy_bias_pp"] = np.ascontiguousarray(hbv.reshape(L, 2, 4, 128).transpose(3, 0, 1, 2).reshape(128, L * 8))
    p["hy_out_norm_pp"] = _pp(inp["hy_out_norm"], L)
    p["dirsign"] = np.concatenate([-np.ones((8, 1), np.float32), np.ones((8, 1), np.float32)], 0)
    p["ndirmask"] = np.concatenate([np.zeros((8, 1), np.float32), -np.ones((8, 1), np.float32)], 0)
    return p


def build(cfg, shapes):
    nc = bass.Bass("TRN2", target_bir_lowering=False)
    ext = {}
    for name, (shape, dt) in shapes.items():
        ext[name] = nc.dram_tensor(name, list(shape), dt, kind="ExternalInput").ap()
    with ExitStack() as st:
        kb = KB(nc, st)
        kb.setup_consts()
        winb = kb.dram("winb", [L, D, INC], BF16)
        wrot = kb.dram("wrot", [L, D, 576], BF16)
        seg = {"qc": kb.dram("s_qc", [384, T], BF16), "kvc": kb.dram("s_kvc", [256, T], BF16),
               "kpe": kb.dram("s_kpe", [64, T], BF16), "rq": kb.dram("s_rq", [256, T], BF16),
               "rk": kb.dram("s_rk", [256, T], BF16), "rg": kb.dram("s_rg", [512, T], BF16),
               "mz": kb.dram("s_mz", [512, T], BF16), "xbc": kb.dram("s_xbc", [1024, T], BF16),
               "dt": kb.dram("s_dt", [16, T], F32), "hu": kb.dram("s_hu", [1536, T], BF16),
               "rv": kb.dram("s_rv", [T, 512], BF16)}
        xin = Tl(ext["x"], "x")
        if cfg.get("dbg"):
            kb.dbg = {"BC": Tl(nc.dram_tensor("d_BC", [16, S], F32, kind="ExternalOutput").ap()),
                      "dt_tok": Tl(nc.dram_tensor("d_dt_tok", [128, 256], F32, kind="ExternalOutput").ap()),
                      "bias_tok": Tl(nc.dram_tensor("d_bias_tok", [128, 256], F32, kind="ExternalOutput").ap()),
                      "xsT": Tl(nc.dram_tensor("d_xsT", [128, S], F32, kind="ExternalOutput").ap()),
                      "xdt": Tl(nc.dram_tensor("d_xdt", [128, 1024], BF16, kind="ExternalOutput").ap())}
        nseq = cfg.get("nseq", NSEQ)
        if cfg["mode"] == "mixtest":
            l = cfg["layer"]
            mixedT = Tl(nc.dram_tensor("mixedT", [D, T], BF16, kind="ExternalOutput").ap(), "mixedT")
            kb.cast_dram(Tl(winb.t[l], "x").__class__(winb.t[l]) if False else _sub(winb, winb.t[l]), ext["w_in"][l], D)
            kb.build_rot(l, ext["w_in"], wrot)
            kb.inproj(l, xin, winb, wrot, seg, ext, (0, nseq * S))
            for s in range(nseq):
                if "A" in cfg["groups"]:
                    kb.group_mla(l, s, seg, mixedT, ext, ext)
                if "B" in cfg["groups"]:
                    kb.group_ret(s, seg, mixedT)
                if "C" in cfg["groups"]:
                    kb.group_ssd(l, s, seg, mixedT, ext)
            if "D" in cfg["groups"]:
                Hs = kb.dram("Hs", [2, 2, NFP, 512], F32)
                hyu = kb.dram("hyu", [3, NSEQ * 512, S], F32)
                z1s = kb.dram("z1s", [NSEQ * 512, S], F32)
                kb.hyena_filter(l, ext, ext, Hs)
                kb.group_hyena(l, nseq, seg, mixedT, ext, ext, Hs, hyu, z1s)
        kb.P.finish()
        print("instructions:", kb.P.n_inst, "sems:", kb.P.nsem)
    return nc


def _sub(parent, ap):
    t = Tl(ap, parent.b.name)
    t.b = parent.b
    return t


def _group_ssd(self, l, s, seg, mixedT, prm):
    nc = self.nc
    t0 = s * S
    with Phase(self) as ph:
        xsT = self.sb(ph, [128, 4, S], F32, "xsT")
        BT = self.sb(ph, [128, 2, S], BF16, "BT")
        CT = self.sb(ph, [128, 2, S], BF16, "CT")
        mz = self.sb(ph, [128, 4, S], BF16, "mz")
        self.dma("sp", mz.t[:], seg["mz"].t[:, t0:t0 + S].rearrange("(c p) t -> p c t", p=128), [seg["mz"]], [mz])
        cw = self.sb(ph, [128, 40], F32, "cw")
        cb = self.sb(ph, [128, 8], F32, "cb")
        dpp = self.sb(ph, [128, 4], F32, "dpp")
        nw = self.sb(ph, [128, 4], F32, "nw")
        self.dma("sp", cw.t[:], prm["ssd_conv_w_pp"][:, l * 40:(l + 1) * 40], [], [cw])
        self.dma("sp", cb.t[:], prm["ssd_conv_b_pp"][:, l * 8:(l + 1) * 8], [], [cb])
        self.dma("sp", dpp.t[:], prm["ssd_d_pp"][:, l * 4:(l + 1) * 4], [], [dpp])
        self.dma("sp", nw.t[:], prm["ssd_norm_pp"][:, l * 4:(l + 1) * 4], [], [nw])
        dt_tok = self.sb(ph, [128, 16, 16], F32, "dt_tok")
        bias_tok = self.sb(ph, [128, 16, 16], F32, "bias_tok")
        BC = self.sb(ph, [16, S], F32, "BC")
        xdt = [self.sb(ph, [128, 16, 8, 128], BF16, "xdt%d" % d) for d in range(2)]
        for d in range(2):
            self.memset(xdt[d].t[:], 0.0, [xdt[d]])
        with Phase(self) as p2:
            raw = self.sb(p2, [128, 8, S], BF16, "raw")
            acc = self.sb(p2, [128, S], F32, "acc")
            self.dma("sp", raw.t[:], seg["xbc"].t[:, t0:t0 + S].rearrange("(c p) t -> p c t", p=128), [seg["xbc"]], [raw])
            for c in range(8):
                self.ts(acc.t[:, :], raw.t[:, c, :], cw.t[:, c * 5 + 2:c * 5 + 3], None, ALU.mult, None, [raw, cw], [acc])
                for k in (0, 1, 3, 4):
                    sh = k - 2
                    a0, a1 = max(0, -sh), S - max(0, sh)
                    self.stt(acc.t[:, a0:a1], raw.t[:, c, a0 + sh:a1 + sh], cw.t[:, c * 5 + k:c * 5 + k + 1], acc.t[:, a0:a1], ALU.mult, ALU.add, [raw, cw, acc], [acc])
                if c < 4:
                    dst, dtl = xsT.t[:, c, :], xsT
                elif c < 6:
                    dst, dtl = BT.t[:, c - 4, :], BT
                else:
                    dst, dtl = CT.t[:, c - 6, :], CT
                self.act(dst, acc.t[:, :], AF.Silu, [acc, cb], [dtl], bias=cb.t[:, c:c + 1])
        with Phase(self) as p2:
            dtr = self.sb(p2, [16, S], F32, "dtr")
            ax = self.sb(p2, [16, S], F32, "ax")
            dtv = self.sb(p2, [16, S], F32, "dtv")
            la = self.sb(p2, [16, S], F32, "la")
            cs = self.sb(p2, [16, S], F32, "cs")
            one16 = self.sb(p2, [16, S], F32, "one16")
            sm = self.sb(p2, [16, 8], F32, "sm")
            self.dma("sp", dtr.t[:], seg["dt"].t[:, t0:t0 + S], [seg["dt"]], [dtr])
            self.dma("sp", sm.t[:, 0:1], prm["ssd_dtb"][:, l:l + 1], [], [sm], allow_slow_non_contiguous=True)
            self.dma("sp", sm.t[:, 1:2], prm["ssd_alog"][:, l:l + 1], [], [sm], allow_slow_non_contiguous=True)
            self.dma("sp", sm.t[:, 2:3], prm["dirsign"][:, 0:1], [], [sm], allow_slow_non_contiguous=True)
            self.dma("sp", sm.t[:, 3:4], prm["ndirmask"][:, 0:1], [], [sm], allow_slow_non_contiguous=True)
            self.ts(dtr.t[:], dtr.t[:], sm.t[:, 0:1], None, ALU.add, None, [dtr, sm], [dtr])
            self.stt(ax.t[:], dtr.t[:], -1.0, dtr.t[:], ALU.mult, ALU.max, [dtr], [ax])
            self.act(ax.t[:], ax.t[:], AF.Exp, [ax], [ax], scale=-1.0)
            self.act(ax.t[:], ax.t[:], AF.Ln, [ax], [ax], bias=1.0)
            self.stt(dtv.t[:], dtr.t[:], 0.0, ax.t[:], ALU.max, ALU.add, [dtr, ax], [dtv])
            self.act(sm.t[:, 4:5], sm.t[:, 1:2], AF.Exp, [sm], [sm])
            self.ts(sm.t[:, 5:6], sm.t[:, 4:5], -1.0, None, ALU.mult, None, [sm], [sm])
            self.ts(la.t[:], dtv.t[:], sm.t[:, 5:6], None, ALU.mult, None, [dtv, sm], [la])
            self.memset(one16.t[:], 1.0, [one16])
            self.P.op("dve", lambda: nc.vector.tensor_tensor_scan(out=cs.t[:], data0=one16.t[:], data1=la.t[:], initial=0.0, op0=ALU.mult, op1=ALU.add), self._b([one16, la]), self._b([cs]))
            self.stt(BC.t[:], la.t[:], sm.t[:, 3:4], cs.t[:], ALU.mult, ALU.add, [la, sm, cs], [BC])
            self.ts(cs.t[:], BC.t[:], sm.t[:, 2:3], None, ALU.mult, None, [BC, sm], [cs])
            b6, b7 = self.banks[6], self.banks[7]
            for tc in range(16):
                self.tr(b6, b6.t[:, tc * 16:(tc + 1) * 16], dtv.t[0:16, tc * 128:(tc + 1) * 128], [dtv])
                self.tr(b7, b7.t[:, tc * 16:(tc + 1) * 16], cs.t[0:16, tc * 128:(tc + 1) * 128], [cs])
            self.copy(dt_tok.t[:], b6.t[:, 0:256].rearrange("p (a b) -> p a b", a=16), [b6], [dt_tok])
            self.copy(bias_tok.t[:], b7.t[:, 0:256].rearrange("p (a b) -> p a b", a=16), [b7], [bias_tok])
            for tc in range(16):
                bk = self.banks[4 + tc % 2]
                for c in range(4):
                    self.tr(bk, bk.t[:, c * 128:(c + 1) * 128], xsT.t[:, c, tc * 128:(tc + 1) * 128], [xsT])
                for d in range(2):
                    for h in range(8):
                        self.ts(xdt[d].t[:, tc, h, 64 * (h % 2):64 * (h % 2) + 64], bk.t[:, h * 64:(h + 1) * 64], dt_tok.t[:, tc, d * 8 + h:d * 8 + h + 1], None, ALU.mult, None, [bk, dt_tok], [xdt[d]])
        if getattr(self, "dbg", None) is not None:
            self.dma("pool", self.dbg["BC"].t[:, :], BC.t[:, :], [BC], [self.dbg["BC"]])
            self.dma("pool", self.dbg["dt_tok"].t[:, :], dt_tok.t[:].rearrange("p a b -> p (a b)"), [dt_tok], [self.dbg["dt_tok"]])
            self.dma("pool", self.dbg["bias_tok"].t[:, :], bias_tok.t[:].rearrange("p a b -> p (a b)"), [bias_tok], [self.dbg["bias_tok"]])
            self.dma("pool", self.dbg["xsT"].t[:, :], xsT.t[:, 0, :], [xsT], [self.dbg["xsT"]])
            self.dma("pool", self.dbg["xdt"].t[:, :], xdt[0].t[:, 0, :, :].rearrange("p a b -> p (a b)"), [xdt[0]], [self.dbg["xdt"]])
        Mf = self.sb(ph, [128, 896], BF16, "Mf")
        Mb = self.sb(ph, [128, 896], BF16, "Mb")
        self.memset(Mf.t[:], 1.0, [Mf])
        self.memset(Mb.t[:], 1.0, [Mb])
        self.P.op("pool", lambda: nc.gpsimd.affine_select(out=Mf.t[:], in_=Mf.t[:], pattern=[[1, 896]], compare_op=ALU.is_ge, fill=0.0, base=-384, channel_multiplier=-1), self._b([Mf]), self._b([Mf]))
        self.P.op("pool", lambda: nc.gpsimd.affine_select(out=Mb.t[:], in_=Mb.t[:], pattern=[[-1, 896]], compare_op=ALU.is_gt, fill=0.0, base=384, channel_multiplier=1), self._b([Mb]), self._b([Mb]))
        bcs = [[self.sb(ph, [128, 512], F32, "bcs") for _ in range(2)] for _ in range(2)]
        Lb = [self.sb(ph, [128, 512], F32, "L") for _ in range(6)]
        Pb = [self.sb(ph, [128, 512], BF16, "P") for _ in range(6)]
        ybuf = self.sb(ph, [128, 4, 512], F32, "ybuf")
        yv = self.sb(ph, [128, 512], F32, "yv")
        gate = self.sb(ph, [128, 512], F32, "gate")
        sq = self.sb(ph, [128, 4, 512], BF16, "sq")
        rstd = self.sb(ph, [128, 512], F32, "rstd")
        ob = [self.sb(ph, [128, 512], BF16, "ob") for _ in range(2)]
        li = 0
        for ib in range(4):
            i0 = ib * 512
            for pair in range(4):
                self.pump(3)
                g = pair // 2
                yb = self.banks[4 + pair % 2]
                for d in range(2):
                    for hh in range(2):
                        h = pair * 2 + hh
                        bb = self.banks[3]
                        self.mm(bb, bb.t[:, :], self.sel.t[:, d * 8 + h, :], BC.t[0:16, i0:i0 + 512], True, True, [self.sel, BC])
                        self.copy(bcs[d][hh].t[:, :], bb.t[:, :], [bb], [bcs[d][hh]])
                items = []
                for jc in range(16):
                    for d in range(2):
                        valid = (jc <= 4 * ib + 3) if d == 0 else (jc >= 4 * ib)
                        if valid:
                            for hh in range(2):
                                items.append((jc, d, hh))
                SB = (0, 1, 2, 7)

                def S_(jc):
                    sbk_ = self.banks[SB[jc % 4]]
                    self.mm(sbk_, sbk_.t[:, :], BT.t[:, g, jc * 128:jc * 128 + 128], CT.t[:, g, i0:i0 + 512], True, True, [BT, CT])

                jcs = sorted(set(it[0] for it in items))
                S_(jcs[0])
                last_jc = -1
                for n, (jc, d, hh) in enumerate(items):
                    j0 = jc * 128
                    h = pair * 2 + hh
                    if jc != last_jc:
                        k_ = jcs.index(jc)
                        if k_ + 1 < len(jcs):
                            S_(jcs[k_ + 1])
                        sbk = self.banks[SB[jc % 4]]
                        last_jc = jc
                    diag = 4 * ib <= jc <= 4 * ib + 3
                    Lt = Lb[li % 6]
                    pb = Pb[li % 6]
                    li += 1
                    sgn = 1.0 if d == 0 else -1.0
                    bia = bias_tok.t[:, jc, d * 8 + h:d * 8 + h + 1]
                    if diag:
                        self.ts(Lt.t[:, :], bcs[d][hh].t[:, :], sgn, bia, ALU.mult, ALU.add, [bcs[d][hh], bias_tok], [Lt])
                        self.ts(Lt.t[:, :], Lt.t[:, :], 0.0, None, ALU.min, None, [Lt], [Lt])
                        self.act(Lt.t[:, :], Lt.t[:, :], AF.Exp, [Lt], [Lt])
                    else:
                        self.act(Lt.t[:, :], bcs[d][hh].t[:, :], AF.Exp, [bcs[d][hh], bias_tok], [Lt], bias=bia, scale=sgn)
                    self.tt(pb.t[:, :], sbk.t[:, :], Lt.t[:, :], ALU.mult, [sbk, Lt], [pb])
                    if diag:
                        m = jc - 4 * ib
                        M = Mf if d == 0 else Mb
                        self.tt(pb.t[:, :], pb.t[:, :], M.t[:, 384 - 128 * m:384 - 128 * m + 512], ALU.mult, [pb, M], [pb], eng="pool")
                    self.mm(yb, yb.t[:, :], xdt[d].t[:, jc, h, :], pb.t[:, :], n == 0, n == len(items) - 1, [xdt[d], pb])
                self.stt(yv.t[:, :], xsT.t[:, pair, i0:i0 + 512], dpp.t[:, pair:pair + 1], yb.t[:, :], ALU.mult, ALU.add, [xsT, dpp, yb], [yv])
                self.act(gate.t[:, :], mz.t[:, pair, i0:i0 + 512], AF.Silu, [mz], [gate])
                self.tt(ybuf.t[:, pair, :], yv.t[:, :], gate.t[:, :], ALU.mult, [yv, gate], [ybuf])
            for c in range(4):
                self.act(sq.t[:, c, :], ybuf.t[:, c, :], AF.Square, [ybuf], [sq])
            nb = self.banks[6]
            for c in range(4):
                self.mm(nb, nb.t[:, :], self.ones_bf.t[:, :], sq.t[:, c, :], c == 0, c == 3, [self.ones_bf, sq])
            self.act(rstd.t[:, :], nb.t[:, :], AF.Sqrt, [nb], [rstd], bias=1e-6, scale=1.0 / 512)
            self.recip(rstd.t[:, :], rstd.t[:, :], [rstd], [rstd])
            for c in range(4):
                o = ob[c % 2]
                self.stt(o.t[:, :], ybuf.t[:, c, :], nw.t[:, c:c + 1], rstd.t[:, :], ALU.mult, ALU.mult, [ybuf, nw, rstd], [o])
                self.dma("pool", mixedT.t[1024 + c * 128:1024 + (c + 1) * 128, t0 + i0:t0 + i0 + 512], o.t[:, :], [o], [mixedT])


KB.group_ssd = _group_ssd


TWO_PI = 2.0 * math.pi
MAGIC = 12582912.0


def _sin_rr(self, out, x, tmp, reads_x, writes_out, x_tl, tmp_tl):
    self.ts(tmp, x, 1.0 / TWO_PI, MAGIC, ALU.mult, ALU.add, [x_tl], [tmp_tl])
    self.ts(tmp, tmp, MAGIC, -TWO_PI, ALU.subtract, ALU.mult, [tmp_tl], [tmp_tl])
    self.tt(x, x, tmp, ALU.add, [x_tl, tmp_tl], [x_tl])
    self.ts(x, x, 3.1415925, -3.1415925, ALU.min, ALU.max, [x_tl], [x_tl])
    self.act(out, x, AF.Sin, [x_tl], writes_out)


def _hyena_filter(self, l, prm, cst, Hs):
    with Phase(self) as ph:
        zT = self.sb(ph, [33, S], F32, "zT")
        w1 = self.sb(ph, [33, 64], F32, "w1")
        w2 = self.sb(ph, [64, 64], F32, "w2")
        w3 = self.sb(ph, [64, 2048], F32, "w3")
        sm = self.sb(ph, [64, 4], F32, "sm")
        self.dma("sp", zT.t[:], cst["hy_zT"], [], [zT])
        self.dma("sp", w1.t[:], prm["hy_w1"][l], [], [w1])
        self.dma("sp", w2.t[:], prm["hy_w2"][l], [], [w2])
        self.dma("sp", w3.t[:], prm["hy_w3"][l], [], [w3])
        self.dma("sp", sm.t[:, 0:2], prm["hy_b_pp"][:, l * 2:l * 2 + 2], [], [sm])
        self.dma("sp", sm.t[:, 2:4], prm["hy_freq_pp"][:, l * 2:l * 2 + 2], [], [sm])
        ntl = self.sb(ph, [128, 16], F32, "ntl")
        dlb = self.sb(ph, [128, 512], F32, "dlb")
        wf = self.sb(ph, [128, NF], F32, "wf")
        self.dma("sp", ntl.t[:], cst["hy_ntlin_pp"], [], [ntl])
        self.dma("sp", dlb.t[:], cst["hy_delta_b"], [], [dlb])
        self.dma("sp", wf.t[:], cst["dft_wf_pp"], [], [wf])
        hid1 = self.sb(ph, [64, S], F32, "hid1")
        hid2 = self.sb(ph, [64, S], F32, "hid2")
        xa = self.sb(ph, [64, 512], F32, "xa")
        xb = self.sb(ph, [64, 512], F32, "xb")
        for tb in range(4):
            bk = self.banks[tb % 2]
            self.mm(bk, bk.t[0:64, :], w1.t[0:33, :], zT.t[0:33, tb * 512:(tb + 1) * 512], True, True, [w1, zT])
            self.ts(xa.t[:, :], bk.t[0:64, :], sm.t[:, 0:1], sm.t[:, 2:3], ALU.add, ALU.mult, [bk, sm], [xa])
            _sin_rr(self, hid1.t[:, tb * 512:(tb + 1) * 512], xa.t[:, :], xb.t[:, :], None, [hid1], xa, xb)
        for tb in range(4):
            bk = self.banks[tb % 2]
            self.mm(bk, bk.t[0:64, :], w2.t[0:64, :], hid1.t[0:64, tb * 512:(tb + 1) * 512], True, True, [w2, hid1])
            self.ts(xa.t[:, :], bk.t[0:64, :], sm.t[:, 1:2], sm.t[:, 3:4], ALU.add, ALU.mult, [bk, sm], [xa])
            _sin_rr(self, hid2.t[:, tb * 512:(tb + 1) * 512], xa.t[:, :], xb.t[:, :], None, [hid2], xa, xb)
        hsum = [self.sb(ph, [128, 16, 512], BF16, "hsum") for _ in range(2)]
        hdif = [self.sb(ph, [128, 16, 512], BF16, "hdif") for _ in range(2)]
        dec = self.sb(ph, [128, 512], F32, "dec")
        hb = self.sb(ph, [128, 512], F32, "hb")
        t1 = self.sb(ph, [128, 512], F32, "t1")
        for tc in range(16):
            self.act(dec.t[:, :], dlb.t[:, :], AF.Exp, [dlb, ntl], [dec], scale=ntl.t[:, tc:tc + 1])
            for o in range(2):
                bf_, bb_ = self.banks[2 * o], self.banks[2 * o + 1]
                self.mm(bf_, bf_.t[:, :], hid2.t[0:64, tc * 128:(tc + 1) * 128], w3.t[0:64, (2 * o) * 512:(2 * o + 1) * 512], True, True, [hid2, w3])
                self.mm(bb_, bb_.t[:, :], hid2.t[0:64, tc * 128:(tc + 1) * 128], w3.t[0:64, (2 * o + 1) * 512:(2 * o + 2) * 512], True, True, [hid2, w3])
                self.copy(hb.t[:, :], bb_.t[:, :], [bb_], [hb], eng="act")
                if tc == 0:
                    self.memset(hb.t[0:1, :], 0.0, [hb])
                self.tt(t1.t[:, :], bf_.t[:, :], hb.t[:, :], ALU.add, [bf_, hb], [t1])
                self.tt(hsum[o].t[:, tc, :], t1.t[:, :], dec.t[:, :], ALU.mult, [t1, dec], [hsum[o]])
                self.tt(t1.t[:, :], bf_.t[:, :], hb.t[:, :], ALU.subtract, [bf_, hb], [t1])
                self.tt(hdif[o].t[:, tc, :], t1.t[:, :], dec.t[:, :], ALU.mult, [t1, dec], [hdif[o]])
        Cb = [self.sb(ph, [128, 16, 128], BF16, "Cb") for _ in range(2)]
        Sb = [self.sb(ph, [128, 16, 128], BF16, "Sb") for _ in range(2)]
        ho = [self.sb(ph, [128, 512], F32, "ho") for _ in range(2)]
        n = 0
        for fc in range(NF):
            cb_, sb_ = Cb[fc % 2], Sb[fc % 2]
            self.dma("sp", cb_.t[:], cst["dft_Cblk"][fc], [], [cb_])
            self.dma("sp", sb_.t[:], cst["dft_Sblk"][fc], [], [sb_])
            for o in range(2):
                for ri, (tab, src) in enumerate(((cb_, hsum[o]), (sb_, hdif[o]))):
                    bk = self.banks[4 + n % 4]
                    for tc in range(16):
                        self.mm(bk, bk.t[:, :], tab.t[:, tc, :], src.t[:, tc, :], tc == 0, tc == 15, [tab, src])
                    h_ = ho[n % 2]
                    n += 1
                    self.ts(h_.t[:, :], bk.t[:, :], wf.t[:, fc:fc + 1], None, ALU.mult, None, [bk, wf], [h_])
                    self.dma("pool", Hs.t[o, ri, fc * 128:(fc + 1) * 128, :], h_.t[:, :], [h_], [Hs])


def _group_hyena(self, l, nseq, seg, mixedT, prm, cst, Hs, hyu, z1s):
    NCOL = nseq * 512
    with Phase(self) as ph:
        U = self.sb(ph, [128, 16, NCOL], BF16, "U")
        Yre = self.sb(ph, [128, NF, NCOL], BF16, "Yre")
        Yim = self.sb(ph, [128, NF, NCOL], BF16, "Yim")
        cw = self.sb(ph, [128, 36], F32, "cw")
        cb = self.sb(ph, [128, 12], F32, "cb")
        hbias = self.sb(ph, [128, 8], F32, "hbias")
        nw = self.sb(ph, [128, 4], F32, "nw")
        self.dma("sp", cw.t[:], prm["hy_conv_w_pp"][:, l * 36:(l + 1) * 36], [], [cw])
        self.dma("sp", cb.t[:], prm["hy_conv_b_pp"][:, l * 12:(l + 1) * 12], [], [cb])
        self.dma("sp", hbias.t[:], prm["hy_bias_pp"][:, l * 8:(l + 1) * 8], [], [hbias])
        self.dma("sp", nw.t[:], prm["hy_out_norm_pp"][:, l * 4:(l + 1) * 4], [], [nw])
        with Phase(self) as p2:
            raw = [self.sb(p2, [128, S], BF16, "raw") for _ in range(2)]
            acc = [self.sb(p2, [128, S], F32, "acc") for _ in range(2)]
            n = 0
            for j in range(3):
                for s in range(nseq):
                    for cc in range(4):
                        c = j * 4 + cc
                        r_, a_ = raw[n % 2], acc[n % 2]
                        n += 1
                        self.dma("sp", r_.t[:, :], seg["hu"].t[c * 128:(c + 1) * 128, s * S:(s + 1) * S], [seg["hu"]], [r_])
                        self.ts(a_.t[:, :], r_.t[:, :], cw.t[:, c * 3 + 1:c * 3 + 2], cb.t[:, c:c + 1], ALU.mult, ALU.add, [r_, cw, cb], [a_])
                        self.stt(a_.t[:, 1:S], r_.t[:, 0:S - 1], cw.t[:, c * 3:c * 3 + 1], a_.t[:, 1:S], ALU.mult, ALU.add, [r_, cw, a_], [a_])
                        self.stt(a_.t[:, 0:S - 1], r_.t[:, 1:S], cw.t[:, c * 3 + 2:c * 3 + 3], a_.t[:, 0:S - 1], ALU.mult, ALU.add, [r_, cw, a_], [a_])
                        self.dma("pool", hyu.t[j, (s * 4 + cc) * 128:(s * 4 + cc + 1) * 128, :], a_.t[:, :], [a_], [hyu])
                        if j == 0:
                            for tcg in range(4):
                                bk = self.banks[6 + tcg % 2]
                                for k in range(4):
                                    tc = tcg * 4 + k
                                    self.tr(bk, bk.t[:, k * 128:(k + 1) * 128], a_.t[:, tc * 128:(tc + 1) * 128], [a_])
                                self.copy(U.t[:, tcg * 4:tcg * 4 + 4, (s * 4 + cc) * 128:(s * 4 + cc + 1) * 128], bk.t[:].rearrange("p (a b) -> p a b", a=4), [bk], [U])
        Cb = [self.sb(ph, [128, 16, 128], BF16, "Cb") for _ in range(2)]
        Sb = [self.sb(ph, [128, 16, 128], BF16, "Sb") for _ in range(2)]
        Hre = [self.sb(ph, [128, 512], F32, "Hre") for _ in range(2)]
        Him = [self.sb(ph, [128, 512], F32, "Him") for _ in range(2)]
        Cn = self.sb(ph, [128, NF, 512], BF16, "Cn")
        Sn = self.sb(ph, [128, NF, 512], BF16, "Sn")
        ta = self.sb(ph, [128, 512], F32, "ta")
        tb_ = self.sb(ph, [128, 512], F32, "tb")
        zp = [self.sb(ph, [128, 512], F32, "zp") for _ in range(2)]
        xg = [self.sb(ph, [128, 512], F32, "xg") for _ in range(2)]
        zn = [self.sb(ph, [128, 512], F32, "zn") for _ in range(2)]
        zfin = self.sb(ph, [128, nseq * 4, 512], F32, "zfin")
        sq = self.sb(ph, [128, 4, 512], BF16, "sq")
        rstd = self.sb(ph, [128, 512], F32, "rstd")
        ob = [self.sb(ph, [128, 512], BF16, "ob") for _ in range(2)]
        for o in range(2):
            for fc in range(NF):
                self.pump(2)
                cb_, sb_ = Cb[fc % 2], Sb[fc % 2]
                hr, hi = Hre[fc % 2], Him[fc % 2]
                self.dma("sp", cb_.t[:], cst["dft_Cblk"][fc], [], [cb_])
                self.dma("sp", sb_.t[:], cst["dft_Sblk"][fc], [], [sb_])
                self.dma("sp", hr.t[:, :], Hs.t[o, 0, fc * 128:(fc + 1) * 128, :], [Hs], [hr])
                self.dma("sp", hi.t[:, :], Hs.t[o, 1, fc * 128:(fc + 1) * 128, :], [Hs], [hi])
                for s in range(nseq):
                    br, bi = self.banks[2 * (s % 2)], self.banks[2 * (s % 2) + 1]
                    for tc in range(16):
                        self.mm(br, br.t[:, :], cb_.t[:, tc, :], U.t[:, tc, s * 512:(s + 1) * 512], tc == 0, tc == 15, [cb_, U])
                    for tc in range(16):
                        self.mm(bi, bi.t[:, :], sb_.t[:, tc, :], U.t[:, tc, s * 512:(s + 1) * 512], tc == 0, tc == 15, [sb_, U])
                    self.tt(ta.t[:, :], br.t[:, :], hr.t[:, :], ALU.mult, [br, hr], [ta])
                    self.tt(tb_.t[:, :], bi.t[:, :], hi.t[:, :], ALU.mult, [bi, hi], [tb_])
                    self.tt(Yre.t[:, fc, s * 512:(s + 1) * 512], ta.t[:, :], tb_.t[:, :], ALU.subtract, [ta, tb_], [Yre])
                    self.tt(ta.t[:, :], br.t[:, :], hi.t[:, :], ALU.mult, [br, hi], [ta])
                    self.tt(tb_.t[:, :], bi.t[:, :], hr.t[:, :], ALU.mult, [bi, hr], [tb_])
                    self.tt(Yim.t[:, fc, s * 512:(s + 1) * 512], ta.t[:, :], tb_.t[:, :], ALU.add, [ta, tb_], [Yim])
            for tb in range(4):
                c0 = tb * 512
                self.dma("sp", Cn.t[:], cst["dft_Cnat"][:, c0:c0 + 512].rearrange("(f p) t -> p f t", p=128), [], [Cn])
                self.dma("sp", Sn.t[:], cst["dft_Snat"][:, c0:c0 + 512].rearrange("(f p) t -> p f t", p=128), [], [Sn])
                for sc in range(nseq * 4):
                    s, cc = sc // 4, sc % 4
                    bk = self.banks[4 + sc % 2]
                    for fc in range(NF):
                        self.mm(bk, bk.t[:, :], Yre.t[:, fc, sc * 128:(sc + 1) * 128], Cn.t[:, fc, :], fc == 0, False, [Yre, Cn])
                        self.mm(bk, bk.t[:, :], Yim.t[:, fc, sc * 128:(sc + 1) * 128], Sn.t[:, fc, :], False, fc == NF - 1, [Yim, Sn])
                    z_, x_, n_ = zp[sc % 2], xg[sc % 2], zn[sc % 2]
                    zsrc = hyu.t[0] if o == 0 else z1s.t
                    zsrc_tl = hyu if o == 0 else z1s
                    self.dma("sp", z_.t[:, :], zsrc[sc * 128:(sc + 1) * 128, c0:c0 + 512], [zsrc_tl], [z_])
                    self.dma("sp", x_.t[:, :], hyu.t[o + 1, sc * 128:(sc + 1) * 128, c0:c0 + 512], [hyu], [x_])
                    self.stt(n_.t[:, :], z_.t[:, :], hbias.t[:, o * 4 + cc:o * 4 + cc + 1], bk.t[:, :], ALU.mult, ALU.add, [z_, hbias, bk], [n_])
                    if o == 0:
                        self.tt(n_.t[:, :], n_.t[:, :], x_.t[:, :], ALU.mult, [n_, x_], [n_])
                        self.dma("pool", z1s.t[sc * 128:(sc + 1) * 128, c0:c0 + 512], n_.t[:, :], [n_], [z1s])
                        b6 = self.banks[6 + sc % 2]
                        for k in range(4):
                            self.tr(b6, b6.t[:, k * 128:(k + 1) * 128], n_.t[:, k * 128:(k + 1) * 128], [n_])
                        self.copy(U.t[:, tb * 4:tb * 4 + 4, sc * 128:(sc + 1) * 128], b6.t[:].rearrange("p (a b) -> p a b", a=4), [b6], [U])
                    else:
                        self.tt(zfin.t[:, sc, :], n_.t[:, :], x_.t[:, :], ALU.mult, [n_, x_], [zfin])
                if o == 1:
                    for s in range(nseq):
                        for cc in range(4):
                            self.act(sq.t[:, cc, :], zfin.t[:, s * 4 + cc, :], AF.Square, [zfin], [sq])
                        nb = self.banks[6]
                        for cc in range(4):
                            self.mm(nb, nb.t[:, :], self.ones_bf.t[:, :], sq.t[:, cc, :], cc == 0, cc == 3, [self.ones_bf, sq])
                        self.act(rstd.t[:, :], nb.t[:, :], AF.Sqrt, [nb], [rstd], bias=1e-6, scale=1.0 / 512)
                        self.recip(rstd.t[:, :], rstd.t[:, :], [rstd], [rstd])
                        for cc in range(4):
                            o_ = ob[cc % 2]
                            self.stt(o_.t[:, :], zfin.t[:, s * 4 + cc, :], nw.t[:, cc:cc + 1], rstd.t[:, :], ALU.mult, ALU.mult, [zfin, nw, rstd], [o_])
                            self.dma("pool", mixedT.t[1536 + cc * 128:1536 + (cc + 1) * 128, s * S + c0:s * S + c0 + 512], o_.t[:, :], [o_], [mixedT])


KB.hyena_filter = _hyena_filter
KB.group_hyena = _group_hyena


def _ln_rows(self, y, gB, bB, st6, mv, eps):
    nc = self.nc
    for q in range(4):
        self.P.op("dve", lambda q=q: nc.vector.bn_stats(out=st6.t[:, q, :], in_=y.t[:, q * 512:(q + 1) * 512]), self._b([y]), self._b([st6]))
    self.P.op("dve", lambda: nc.vector.bn_aggr(out=mv.t[:, 0:2], in_=st6.t[:].rearrange("p a b -> p (a b)")), self._b([st6]), self._b([mv]))
    self.act(mv.t[:, 2:3], mv.t[:, 1:2], AF.Sqrt, [mv], [mv], bias=eps, scale=1.0)
    self.recip(mv.t[:, 3:4], mv.t[:, 2:3], [mv], [mv])
    self.ts(y.t[:, :], y.t[:, :], mv.t[:, 0:1], mv.t[:, 3:4], ALU.subtract, ALU.mult, [y, mv], [y])
    self.tt(y.t[:, :], y.t[:, :], gB.t[:, :], ALU.mult, [y, gB], [y])
    self.tt(y.t[:, :], y.t[:, :], bB.t[:, :], ALU.add, [y, bB], [y])


def _outproj_ln(self, l, mixedT, woutb, xin, xout, prm, tok_range, wdep=None):
    with Phase(self) as ph:
        W = self.sb(ph, [128, 16, D], BF16, "Wout")
        self.dma("sp", W.t[:], woutb.t[l].rearrange("(ec p) d -> p ec d", p=128), [wdep or woutb], [W])
        gB = self.bcast_row(ph, prm["ln1_g"][l:l + 1, :], D, "gB")
        bB = self.bcast_row(ph, prm["ln1_b"][l:l + 1, :], D, "bB")
        mT = [self.sb(ph, [128, 16, 128], BF16, "mT") for _ in range(2)]
        xt = [self.sb(ph, [128, D], F32, "xt") for _ in range(2)]
        y = [self.sb(ph, [128, D], F32, "y") for _ in range(2)]
        st6 = self.sb(ph, [128, 4, 6], F32, "st6")
        mv = self.sb(ph, [128, 4], F32, "mv")
        for i, tok0 in enumerate(range(tok_range[0], tok_range[1], 128)):
            self.pump(2)
            m_, x_, y_ = mT[i % 2], xt[i % 2], y[i % 2]
            self.dma("sp", m_.t[:], mixedT.t[:, tok0:tok0 + 128].rearrange("(ec p) t -> p ec t", p=128), [mixedT], [m_])
            self.dma("sp", x_.t[:], xin.t[tok0:tok0 + 128, :], [xin], [x_])
            for q in range(4):
                bk = self.banks[(i * 4 + q) % 8]
                for ec in range(16):
                    self.mm(bk, bk.t[:, :], m_.t[:, ec, :], W.t[:, ec, q * 512:(q + 1) * 512], ec == 0, ec == 15, [m_, W])
                self.stt(y_.t[:, q * 512:(q + 1) * 512], x_.t[:, q * 512:(q + 1) * 512], ALPHA, bk.t[:, :], ALU.mult, ALU.add, [x_, bk], [y_])
            _ln_rows(self, y_, gB, bB, st6, mv, 1e-5)
            self.dma("pool", xout.t[tok0:tok0 + 128, :], y_.t[:, :], [y_], [xout])


def _wsl(W, r0, r1, c0, c1, pat):
    if isinstance(W, tuple) and W[0] == "dynflat":
        _, tens, v, kind = W
        if kind == "gu":
            return tens[c0 // 256][bass.ds(v, MSEG)].rearrange("(dc p f) -> p dc f", p=128, f=256)
        return tens[(c0 // 512) * 7 + r0 // 1024][bass.ds(v, MSEG)].rearrange("(a p f) -> p a f", p=128, f=512)
    if isinstance(W, tuple):
        _, t3, reg = W
        return t3[bass.ds(reg, 1), r0:r1, c0:c1].rearrange("o " + pat, p=128, o=1).rearrange("p o a b -> p (o a) b") if False else \
            t3[bass.ds(reg, 1), r0:r1, c0:c1].rearrange("1 " + pat, p=128)
    return W[r0:r1, c0:c1].rearrange(pat, p=128)


def _ffn(self, l, xin, xout, experts, F_, prm, tok_range, wr_ap=None, slot_mode=False, wdep=None):
    nc = self.nc
    wdep = wdep or self.wsrc
    nfc = F_ // 128
    nftiles = 7 if slot_mode else (8 if nfc % 8 == 0 else 4)
    nft = nfc // nftiles
    import os
    if os.environ.get("MOE_DBG", "") == "dense":
        wr_ap = None
    gated = wr_ap is not None
    with Phase(self) as ph:
        xT = self.sb(ph, [128, 16, 512], BF16, "xT")
        xtiles = [self.sb(ph, [128, D], F32, "xtile") for _ in range(2)]
        hT = self.sb(ph, [128, nfc, 512], BF16, "hT")
        acc = self.sb(ph, [128, 4, D], F32, "acc")
        wg = [self.sb(ph, [128, 16, 256], BF16, "wg") for _ in range(2)]
        wu = [self.sb(ph, [128, 16, 256], BF16, "wu") for _ in range(2)]
        wd = [self.sb(ph, [128, nft, 512], BF16, "wd") for _ in range(2)]
        sg = [self.sb(ph, [128, 512], F32, "sg") for _ in range(2)]
        if not slot_mode:
            gB = self.bcast_row(ph, prm["ln2_g"][l:l + 1, :], D, "gB2")
            bB = self.bcast_row(ph, prm["ln2_b"][l:l + 1, :], D, "bB2")
        st6 = self.sb(ph, [128, 4, 6], F32, "st6")
        mv = self.sb(ph, [128, 4], F32, "mv")
        G = self.sb(ph, [128, 4, 8], F32, "G")
        if gated:
            x32 = self.sb(ph, [128, 16, 128], F32, "x32")
            x32_keep = x32
            wr = self.sb(ph, [128, 16, 8], F32, "wr")
            if not os.environ.get("NOWR"):
                self.dma("sp", wr.t[:], wr_ap.rearrange("(dc p) e -> p dc e", p=128), [], [wr])
            lg = self.sb(ph, [128, 8], F32, "lg")
            srt = self.sb(ph, [128, 8], F32, "srt")
            gg = self.sb(ph, [128, 4], F32, "gg")
            g2t = self.sb(ph, [128, 8], F32, "g2t")
        else:
            x32 = None

        import os
        dbgm = os.environ.get("MOE_DBG", "")

        def router(ts_):
            if dbgm == "norouter":
                self.memset(G.t[:, ts_, :], 0.125, [G])
                return
            bk = self.banks[0]
            for dc in range(16):
                self.mm(bk, bk.t[:, 0:8], x32.t[:, dc, :], wr.t[:, dc, :], dc == 0, dc == 15, [x32, wr])
            self.copy(lg.t[:, :], bk.t[:, 0:8], [bk], [lg], eng="dve")
            self.P.op("dve", lambda: nc.vector.max(out=srt.t[:, :], in_=lg.t[:, :]), self._b([lg]), self._b([srt]))
            self.tt(gg.t[:, 0:1], srt.t[:, 1:2], srt.t[:, 0:1], ALU.subtract, [srt], [gg])
            self.act(gg.t[:, 1:2], gg.t[:, 0:1], AF.Sigmoid, [gg], [gg])
            self.ts(gg.t[:, 2:3], gg.t[:, 1:2], -1.0, 1.0, ALU.mult, ALU.add, [gg], [gg])
            self.ts(G.t[:, ts_, :], lg.t[:, :], srt.t[:, 0:1], gg.t[:, 2:3], ALU.is_equal, ALU.mult, [lg, srt, gg], [G])
            self.ts(g2t.t[:, :], lg.t[:, :], srt.t[:, 1:2], gg.t[:, 1:2], ALU.is_equal, ALU.mult, [lg, srt, gg], [g2t])
            self.tt(G.t[:, ts_, :], G.t[:, ts_, :], g2t.t[:, :], ALU.add, [G, g2t], [G])

        n1 = 0
        n2 = 0
        for tok0 in range(tok_range[0], tok_range[1], 512):
            self.load_xT(xin, tok0, xT, xtiles, x32=None if os.environ.get("NOX32") else x32, post=router if gated else None)
            exl = experts(tok0) if callable(experts) else experts
            for e, (Wg, Wu, Wd) in enumerate(exl):
                for fg in range(0 if os.environ.get("FFN_SKIP1") else nfc // 2):
                    self.pump(2)
                    g_, u_ = wg[n1 % 2], wu[n1 % 2]
                    n1 += 1
                    self.dma("sp", g_.t[:], _wsl(Wg, 0, D, fg * 256, (fg + 1) * 256, "(dc p) f -> p dc f"), [wdep], [g_])
                    self.dma("sp", u_.t[:], _wsl(Wu, 0, D, fg * 256, (fg + 1) * 256, "(dc p) f -> p dc f"), [wdep], [u_])
                    for j in range(2):
                        fc = fg * 2 + j
                        bg, bu = self.banks[fc % 2], self.banks[2 + fc % 2]
                        for dc in range(16):
                            self.mm(bg, bg.t[:, :], g_.t[:, dc, j * 128:(j + 1) * 128], xT.t[:, dc, :], dc == 0, dc == 15, [g_, xT])
                        for dc in range(16):
                            self.mm(bu, bu.t[:, :], u_.t[:, dc, j * 128:(j + 1) * 128], xT.t[:, dc, :], dc == 0, dc == 15, [u_, xT])
                        s_ = sg[fc % 2]
                        self.act(s_.t[:, :], bg.t[:, :], AF.Silu, [bg], [s_])
                        self.tt(hT.t[:, fc, :], s_.t[:, :], bu.t[:, :], ALU.mult, [s_, bu], [hT])
                for q in range(4):
                    if os.environ.get("FFN_SKIP2"):
                        for ts_ in range(4):
                            self.memset(acc.t[:, ts_, q * 512:(q + 1) * 512], 0.0, [acc])
                        continue
                    for ft in range(nftiles):
                        d_ = wd[n2 % 2]
                        n2 += 1
                        self.dma("sp", d_.t[:], _wsl(Wd, ft * nft * 128, (ft + 1) * nft * 128, q * 512, (q + 1) * 512, "(a p) d -> p a d"), [wdep], [d_])
                        for a in range(nft):
                            fc = ft * nft + a
                            for ts_ in range(4):
                                bk = self.banks[4 + ts_]
                                self.mm(bk, bk.t[:, :], hT.t[:, fc, ts_ * 128:(ts_ + 1) * 128], d_.t[:, a, :], fc == 0, fc == nfc - 1, [hT, d_])
                    for ts_ in range(4):
                        bk = self.banks[4 + ts_]
                        dst = acc.t[:, ts_, q * 512:(q + 1) * 512]
                        if not gated:
                            self.copy(dst, bk.t[:, :], [bk], [acc])
                        elif e == 0:
                            self.ts(dst, bk.t[:, :], G.t[:, ts_, e:e + 1], None, ALU.mult, None, [bk, G], [acc])
                        else:
                            self.stt(dst, bk.t[:, :], G.t[:, ts_, e:e + 1], dst, ALU.mult, ALU.add, [bk, G, acc], [acc])
            if slot_mode:
                for ts_ in range(4):
                    self.dma("pool", xout.t[tok0 + ts_ * 128:tok0 + (ts_ + 1) * 128, :], acc.t[:, ts_, :], [acc], [xout])
                continue
            for ts_ in range(4):
                x_ = xtiles[ts_ % 2]
                self.dma("sp", x_.t[:], xin.t[tok0 + ts_ * 128:tok0 + (ts_ + 1) * 128, :], [xin], [x_])
                self.stt(x_.t[:, :], x_.t[:, :], ALPHA, acc.t[:, ts_, :], ALU.mult, ALU.add, [x_, acc], [x_])
                _ln_rows(self, x_, gB, bB, st6, mv, 1e-5)
                self.dma("pool", xout.t[tok0 + ts_ * 128:tok0 + (ts_ + 1) * 128, :], x_.t[:, :], [x_], [xout])


KB.outproj_ln = _outproj_ln
KB.ffn = _ffn


def build_full(shapes, nseq=NSEQ, layers=(0, 1), ne=NE, skip_mixer=False):
    nc = bass.Bass("TRN2", target_bir_lowering=False)
    ext = {}
    for name, (shape, dt) in shapes.items():
        ext[name] = nc.dram_tensor(name, list(shape), dt, kind="ExternalInput").ap()
    out = Tl(nc.dram_tensor("out", [T, D], F32, kind="ExternalOutput").ap(), "out")
    with ExitStack() as st:
        kb = KB(nc, st)
        kb.setup_consts()
        kb.wsrc = Tl(None, "wsrc")
        winb = kb.dram("winb", [L, D, INC], BF16)
        wrot = kb.dram("wrot", [L, D, 576], BF16)
        woutb = kb.dram("woutb", [L, D, D], BF16)
        fg = kb.dram("fgb", [D, DFF], BF16)
        fu = kb.dram("fub", [D, DFF], BF16)
        fd = kb.dram("fdb", [DFF, D], BF16)
        seg = {"qc": kb.dram("s_qc", [384, T], BF16), "kvc": kb.dram("s_kvc", [256, T], BF16),
               "kpe": kb.dram("s_kpe", [64, T], BF16), "rq": kb.dram("s_rq", [256, T], BF16),
               "rk": kb.dram("s_rk", [256, T], BF16), "rg": kb.dram("s_rg", [512, T], BF16),
               "mz": kb.dram("s_mz", [512, T], BF16), "xbc": kb.dram("s_xbc", [1024, T], BF16),
               "dt": kb.dram("s_dt", [16, T], F32), "hu": kb.dram("s_hu", [1536, T], BF16),
               "rv": kb.dram("s_rv", [T, 512], BF16)}
        mixedT = kb.dram("mixedT", [D, T], BF16)
        Hs = kb.dram("Hs", [2, 2, NFP, 512], F32)
        hyu = kb.dram("hyu", [3, NSEQ * 512, S], F32)
        z1s = kb.dram("z1s", [NSEQ * 512, S], F32)
        xa = kb.dram("xa", [T, D], F32)
        xb = kb.dram("xb", [T, D], F32)
        xin = Tl(ext["x"], "x")
        rng = (0, nseq * S)
        import os
        if os.environ.get("RNG"):
            rng = (0, int(os.environ["RNG"]))
        kb.wmoe = Tl(None, "wmoe")
        woutL = [Tl(woutb.t[l], "wout%d" % l) for l in range(L)]
        winL = [Tl(winb.t[l], "win%d" % l) for l in range(L)]
        for l in layers:
            kb.cast_dram(winL[l], ext["w_in"][l], D, tag=None if l == layers[0] else "win%d" % l)
            kb.cast_dram(woutL[l], ext["w_out"][l], D, tag="wout%d" % l)
            if l == 0:
                for dst, nm, rows in ((fg, "ffn_w_gate", D), (fu, "ffn_w_up", D), (fd, "ffn_w_down", DFF)):
                    t_ = _sub(dst, dst.t)
                    t_.b = kb.wsrc.b
                    kb.cast_dram(t_, ext[nm][0], rows, tag="ffn")
        if 1 in layers:
            mg = [nc.dram_tensor("mgb%d" % i, [NE * MSEG], BF16).ap() for i in range(28)]
            mu = [nc.dram_tensor("mub%d" % i, [NE * MSEG], BF16).ap() for i in range(28)]
            md = [nc.dram_tensor("mdb%d" % i, [NE * MSEG], BF16).ap() for i in range(28)]
            for e in range(ne):
                for fg_ in range(28):
                    for tens, nm in ((mg, "moe_w_gate"), (mu, "moe_w_up")):
                        kb.defer("moe", lambda tens=tens, nm=nm, e=e, fg_=fg_: kb.dma(
                            "pool", tens[fg_][e * MSEG:(e + 1) * MSEG].rearrange("(r c) -> r c", c=256),
                            ext[nm][0, e][:, fg_ * 256:(fg_ + 1) * 256], [], [kb.wmoe]))
                for q in range(4):
                    for ft in range(7):
                        kb.defer("moe", lambda e=e, q=q, ft=ft: kb.dma(
                            "pool", md[q * 7 + ft][e * MSEG:(e + 1) * MSEG].rearrange("(r c) -> r c", c=512),
                            ext["moe_w_down"][0, e][ft * 1024:(ft + 1) * 1024, q * 512:(q + 1) * 512], [], [kb.wmoe]))
        cur = xin
        import os
        stages = os.environ.get("STAGES", "mix,op,ffn").split(",")
        for l in layers:
            if "mix" in stages:
                kb.flush_tag("win%d" % l)
                kb.build_rot(l, ext["w_in"], wrot)
                kb.inproj(l, cur, winb, wrot, seg, ext, rng, wdep=winL[l])
                for s in range(nseq):
                    kb.group_mla(l, s, seg, mixedT, ext, ext)
                    kb.group_ret(s, seg, mixedT)
                    kb.group_ssd(l, s, seg, mixedT, ext)
                kb.hyena_filter(l, ext, ext, Hs)
                kb.group_hyena(l, nseq, seg, mixedT, ext, ext, Hs, hyu, z1s)
            if "op" in stages:
                kb.flush_tag("wout%d" % l)
                kb.outproj_ln(l, mixedT, woutb, cur, xa, ext, rng, wdep=woutL[l])
            nxt = out if l == layers[-1] else xb
            if "ffn" not in stages:
                continue
            if l == 0:
                kb.flush_tag("ffn")
                kb.ffn(l, xa, nxt, [(fg.t, fu.t, fd.t)], DFF, ext, rng)
            else:
                if True:
                    kb.flush_tag("moe")
                    kb.moe_routed(l, xa, nxt, mg, mu, md, ext, rng[1], ext["moe_router"][0])
            cur = nxt
        kb.P.finish()
        print("instructions:", kb.P.n_inst, "sems:", kb.P.nsem)
    return nc


BIG = ("w_in", "w_out", "ffn_w_gate", "ffn_w_up", "ffn_w_down", "moe_router", "moe_w_gate", "moe_w_up", "moe_w_down",
       "ln1_g", "ln1_b", "ln2_g", "ln2_b")


def kernel(**inputs):
    common = {}
    for k in BIG:
        common[k] = np.ascontiguousarray(np.asarray(inputs[k], dtype=np.float32))
    common.update(host_consts())
    common.update(host_params(inputs))
    x = np.asarray(inputs["x"], dtype=np.float32)
    ncores = 8
    shapes = {k: (v.shape, BF16 if v.dtype == ml_dtypes.bfloat16 else F32) for k, v in common.items()}
    shapes["x"] = ((T, D), F32)
    nc = build_full(shapes)
    in_maps = []
    for c in range(ncores):
        m = dict(common)
        m["x"] = np.ascontiguousarray(x[c * NSEQ:(c + 1) * NSEQ].reshape(T, D))
        in_maps.append(m)
    res = run_bass_kernel_spmd(nc, in_maps, core_ids=list(range(ncores)))
    outs = [np.asarray(r["out"], dtype=np.float32).reshape(NSEQ, S, D) for r in res.results]
    return np.concatenate(outs, axis=0)


MSEG = 2048 * 256
NBLK = 24
I32 = mybir.dt.int32


def _moe_routed(self, l, xin, xout, mg, mu, md, prm, ntok, wr_ap):
    nc = self.nc
    NT = ntok // 128
    xslots = self.dram("xslots", [NBLK * 512, D], F32)
    yslots = self.dram("yslots", [NBLK * 512, D], F32)
    with Phase(self) as pr:
        M1 = self.sb(pr, [128, NT, 8], F32, "M1")
        M2 = self.sb(pr, [128, NT, 8], F32, "M2")
        LOC = self.sb(pr, [128, NT, 8], F32, "LOC")
        TMP = self.sb(pr, [128, NT, 8], F32, "TMPr")
        G12 = self.sb(pr, [128, NT, 2], F32, "G12")
        d1f = self.sb(pr, [128, NT], F32, "d1f")
        d2f = self.sb(pr, [128, NT], F32, "d2f")
        d1i = self.sb(pr, [128, NT], I32, "d1i")
        d2i = self.sb(pr, [128, NT], I32, "d2i")
        bei = self.sb(pr, [128, NBLK], I32, "bei")
        with Phase(self) as ph:
            xt = [self.sb(ph, [128, D], F32, "xt") for _ in range(2)]
            x32 = self.sb(ph, [128, 16, 128], F32, "x32")
            wr = self.sb(ph, [128, 16, 8], F32, "wr")
            self.dma("sp", wr.t[:], wr_ap.rearrange("(dc p) e -> p dc e", p=128), [], [wr])
            ltri = self.sb(ph, [128, 128], F32, "ltri")
            self.memset(ltri.t[:], 1.0, [ltri])
            self.P.op("pool", lambda: nc.gpsimd.affine_select(out=ltri.t[:], in_=ltri.t[:], pattern=[[1, 128]], compare_op=ALU.is_ge, fill=0.0, base=-1, channel_multiplier=-1), self._b([ltri]), self._b([ltri]))
            base = self.sb(ph, [128, 8], F32, "base")
            self.memset(base.t[:], 0.0, [base])
            lg = self.sb(ph, [128, 8], F32, "lg")
            srt = self.sb(ph, [128, 8], F32, "srt")
            gg = self.sb(ph, [128, 4], F32, "gg")
            ms = self.sb(ph, [128, 8], F32, "ms")
            for i in range(NT):
                x_ = xt[i % 2]
                self.dma("sp", x_.t[:], xin.t[i * 128:(i + 1) * 128, :], [xin], [x_])
                for j in range(4):
                    bk = self.banks[4 + j]
                    for k in range(4):
                        dc = 4 * j + k
                        self.tr(bk, bk.t[:, k * 128:(k + 1) * 128], x_.t[:, dc * 128:(dc + 1) * 128], [x_])
                    self.copy(x32.t[:, 4 * j:4 * j + 4, :], bk.t[:].rearrange("p (a b) -> p a b", a=4), [bk], [x32])
                bk = self.banks[0]
                for dc in range(16):
                    self.mm(bk, bk.t[:, 0:8], x32.t[:, dc, :], wr.t[:, dc, :], dc == 0, dc == 15, [x32, wr])
                self.copy(lg.t[:, :], bk.t[:, 0:8], [bk], [lg], eng="dve")
                self.P.op("dve", lambda: nc.vector.max(out=srt.t[:, :], in_=lg.t[:, :]), self._b([lg]), self._b([srt]))
                self.tt(gg.t[:, 0:1], srt.t[:, 1:2], srt.t[:, 0:1], ALU.subtract, [srt], [gg])
                self.act(G12.t[:, i, 1:2], gg.t[:, 0:1], AF.Sigmoid, [gg], [G12])
                self.ts(G12.t[:, i, 0:1], G12.t[:, i, 1:2], -1.0, 1.0, ALU.mult, ALU.add, [G12], [G12])
                self.ts(M1.t[:, i, :], lg.t[:, :], srt.t[:, 0:1], None, ALU.is_equal, None, [lg, srt], [M1])
                self.ts(M2.t[:, i, :], lg.t[:, :], srt.t[:, 1:2], None, ALU.is_equal, None, [lg, srt], [M2])
                self.tt(ms.t[:, :], M1.t[:, i, :], M2.t[:, i, :], ALU.add, [M1, M2], [ms])
                b1, b2 = self.banks[1], self.banks[2]
                self.mm(b1, b1.t[:, 0:8], ltri.t[:, :], ms.t[:, :], True, True, [ltri, ms])
                self.mm(b2, b2.t[:, 0:8], self.ones_f.t[:, :], ms.t[:, :], True, True, [self.ones_f, ms])
                self.tt(LOC.t[:, i, :], b1.t[:, 0:8], base.t[:, :], ALU.add, [b1, base], [LOC])
                self.tt(base.t[:, :], b2.t[:, 0:8], base.t[:, :], ALU.add, [b2, base], [base])
            pad = self.sb(ph, [128, 8], F32, "pad")
            pend = self.sb(ph, [128, 8], F32, "pend")
            pst = self.sb(ph, [128, 8], F32, "pst")
            one8 = self.sb(ph, [128, 8], F32, "one8")
            self.memset(one8.t[:], 1.0, [one8])
            self.ts(pad.t[:, :], base.t[:, :], 1.0 / 512, 0.4990234375, ALU.mult, ALU.add, [base], [pad])
            self.ts(pad.t[:, :], pad.t[:, :], MAGIC, None, ALU.add, None, [pad], [pad])
            self.ts(pad.t[:, :], pad.t[:, :], MAGIC, 512.0, ALU.subtract, ALU.mult, [pad], [pad])
            self.P.op("dve", lambda: nc.vector.tensor_tensor_scan(out=pend.t[:, :], data0=one8.t[:, :], data1=pad.t[:, :], initial=0.0, op0=ALU.mult, op1=ALU.add), self._b([one8, pad]), self._b([pend]))
            self.tt(pst.t[:, :], pend.t[:, :], pad.t[:, :], ALU.subtract, [pend, pad], [pst])
            for i in range(NT):
                self.tt(LOC.t[:, i, :], LOC.t[:, i, :], pst.t[:, :], ALU.add, [LOC, pst], [LOC])
            for Mx, df, di in ((M1, d1f, d1i), (M2, d2f, d2i)):
                self.tt(TMP.t[:], Mx.t[:], LOC.t[:], ALU.mult, [Mx, LOC], [TMP])
                self.P.op("dve", lambda df=df: nc.vector.tensor_reduce(out=df.t[:, :], in_=TMP.t[:], axis=mybir.AxisListType.X, op=ALU.add), self._b([TMP]), self._b([df]))
                self.copy(di.t[:, :], df.t[:, :], [df], [di], eng="dve")
            thr = self.sb(ph, [128, NBLK], F32, "thr")
            bef = self.sb(ph, [128, NBLK], F32, "bef")
            cmpt = self.sb(ph, [128, NBLK], F32, "cmpt")
            self.P.op("pool", lambda: nc.gpsimd.iota(thr.t[:], pattern=[[512, NBLK]], base=0, channel_multiplier=0, allow_small_or_imprecise_dtypes=True), [], [thr.b])
            self.memset(bef.t[:], 0.0, [bef])
            for e in range(8):
                self.ts(cmpt.t[:, :], thr.t[:, :], pend.t[:, e:e + 1], None, ALU.is_ge, None, [thr, pend], [cmpt])
                self.tt(bef.t[:, :], bef.t[:, :], cmpt.t[:, :], ALU.add, [bef, cmpt], [bef])
            self.ts(bef.t[:, :], bef.t[:, :], 7.0, float(MSEG), ALU.min, ALU.mult, [bef], [bef])
            self.copy(bei.t[:, :], bef.t[:, :], [bef], [bei], eng="dve")
            for i in range(NT):
                x_ = xt[i % 2]
                self.dma("sp", x_.t[:], xin.t[i * 128:(i + 1) * 128, :], [xin], [x_])
                for di in (d1i, d2i):
                    self.P.dma_raw("pool", lambda di=di, x_=x_, i=i: nc.gpsimd.indirect_dma_start(
                        out=xslots.t[:, :], out_offset=bass.IndirectOffsetOnAxis(ap=di.t[:, i:i + 1], axis=0), in_=x_.t[:, :], in_offset=None),
                        self._b([x_, di]), self._b([xslots]))
        self.P._deps("sp", self._b([bei]), [])
        cur_reg = [None]

        def experts(tok0):
            b = tok0 // 512
            if cur_reg[0] is not None:
                nc.sync.free_register(cur_reg[0])
            reg = nc.sync.alloc_register()
            nc.sync.reg_load(reg, bei.t[0:1, b:b + 1])
            r = nc.sync.snap(reg, donate=True, min_val=0, max_val=7 * MSEG)
            cur_reg[0] = reg
            return [(("dynflat", mg, r, "gu"), ("dynflat", mu, r, "gu"), ("dynflat", md, r, "d"))]

        self.ffn(l, xslots, yslots, experts, DFE, prm, (0, NBLK * 512), slot_mode=True, wdep=self.wmoe)
        if cur_reg[0] is not None:
            nc.sync.free_register(cur_reg[0])
        with Phase(self) as ph:
            gB = self.bcast_row(ph, prm["ln2_g"][l:l + 1, :], D, "gB2")
            bB = self.bcast_row(ph, prm["ln2_b"][l:l + 1, :], D, "bB2")
            st6 = self.sb(ph, [128, 4, 6], F32, "st6")
            mv = self.sb(ph, [128, 4], F32, "mv")
            xt = [self.sb(ph, [128, D], F32, "xt") for _ in range(2)]
            ya = [self.sb(ph, [128, D], F32, "ya") for _ in range(2)]
            yb = [self.sb(ph, [128, D], F32, "yb") for _ in range(2)]
            for i in range(NT):
                x_, a_, b_ = xt[i % 2], ya[i % 2], yb[i % 2]
                self.dma("sp", x_.t[:], xin.t[i * 128:(i + 1) * 128, :], [xin], [x_])
                for di, y_ in ((d1i, a_), (d2i, b_)):
                    self.P.dma_raw("pool", lambda di=di, y_=y_, i=i: nc.gpsimd.indirect_dma_start(
                        out=y_.t[:, :], out_offset=None, in_=yslots.t[:, :], in_offset=bass.IndirectOffsetOnAxis(ap=di.t[:, i:i + 1], axis=0)),
                        self._b([yslots, di]), self._b([y_]))
                self.ts(a_.t[:, :], a_.t[:, :], G12.t[:, i, 0:1], None, ALU.mult, None, [a_, G12], [a_])
                self.stt(a_.t[:, :], b_.t[:, :], G12.t[:, i, 1:2], a_.t[:, :], ALU.mult, ALU.add, [b_, G12, a_], [a_])
                self.stt(x_.t[:, :], x_.t[:, :], ALPHA, a_.t[:, :], ALU.mult, ALU.add, [x_, a_], [x_])
                _ln_rows(self, x_, gB, bB, st6, mv, 1e-5)
                self.dma("pool", xout.t[i * 128:(i + 1) * 128, :], x_.t[:, :], [x_], [xout])


KB.moe_routed = _moe_routed
```

```python
import math
from contextlib import ExitStack
import numpy as np
import ml_dtypes
import concourse.bass as bass
import concourse.mybir as mybir
from concourse.bass_utils import run_bass_kernel_spmd

F32 = mybir.dt.float32
BF16 = mybir.dt.bfloat16
AF = mybir.ActivationFunctionType
ALU = mybir.AluOpType
SEM_LIMIT = 8000

L = 2
D = 2048
S = 2048
NSEQ = 2
T = NSEQ * S
INC = 5328
DFF = 5632
DFE = 7168
NE = 8
ALPHA = (2 * L) ** 0.25
NF = 17
NFP = NF * 128
C_QC, C_KVC, C_KPE, C_RQ, C_RK, C_RV, C_RG, C_MZ, C_XBC, C_DT, C_HU = 0, 384, 640, 704, 960, 1216, 1728, 2240, 2752, 3776, 3792
RET_LG_F = [math.log1p(-2.0 ** (-5.0 - h)) for h in range(4)]
RET_LG_B = [math.log1p(-2.0 ** (-5.5 - h)) for h in range(4)]


class Buf:
    __slots__ = ("name", "w", "r")

    def __init__(self, name=""):
        self.name = name
        self.w = {}
        self.r = {}


class Prog:
    def __init__(self, nc, stack):
        self.nc = nc
        self.stack = stack
        self.eng = {"pe": nc.tensor, "dve": nc.vector, "act": nc.scalar, "pool": nc.gpsimd, "sp": nc.sync}
        self.cur_sem, self.cnt, self.sems, self.nsem = {}, {}, {}, 0
        for e in self.eng:
            self._new_eng_sem(e)
        self.waited = {e: {} for e in self.eng}
        self.nslots = 8
        self.slots = {q: [[self._new_sem("d%s%d" % (q, i)), 0] for i in range(self.nslots)] for q in ("sp", "pool", "act")}
        self.slot_i = {q: 0 for q in self.slots}
        self.n_inst = 0

    def _new_sem(self, name):
        self.nsem += 1
        key = "%s_%d" % (name, self.nsem)
        self.sems[key] = self.stack.enter_context(self.nc.semaphore(key))
        return key

    def _new_eng_sem(self, e):
        self.cur_sem[e] = self._new_sem("c" + e)
        self.cnt[e] = 0

    def _wait(self, e, tok):
        if tok is None:
            return
        key, val, src = tok
        if src == e and e == "pe":
            return
        w = self.waited[e]
        if w.get(key, 0) >= val:
            return
        self.eng[e].wait_ge(self.sems[key], val)
        w[key] = val

    def _deps(self, e, reads, writes):
        for b in reads:
            for t in b.w.values():
                self._wait(e, t)
        for b in writes:
            for t in b.w.values():
                self._wait(e, t)
            for t in b.r.values():
                if t[2] != e:
                    self._wait(e, t)

    def _record(self, tok, reads, writes):
        for b in reads:
            b.r[tok[0]] = tok
        for b in writes:
            b.w[tok[0]] = tok
            b.r = {}

    def op(self, e, fn, reads=(), writes=()):
        self._deps(e, reads, writes)
        ins = fn()
        self.n_inst += 1
        if self.cnt[e] >= SEM_LIMIT:
            self._new_eng_sem(e)
        self.cnt[e] += 1
        ins.then_inc(self.sems[self.cur_sem[e]], 1)
        tok = (self.cur_sem[e], self.cnt[e], e)
        self._record(tok, reads, writes)
        return tok

    def dma(self, q, out, in_, reads=(), writes=(), **kw):
        self._deps(q, reads, writes)
        i = self.slot_i[q]
        self.slot_i[q] = (i + 1) % self.nslots
        sl = self.slots[q][i]
        if sl[1] > 0:
            self._wait(q, (sl[0], sl[1], "dma"))
        if sl[1] + 16 > SEM_LIMIT:
            sl[0] = self._new_sem("d%s%d" % (q, i))
            sl[1] = 0
        sl[1] += 16
        self.eng[q].dma_start(out=out, in_=in_, **kw).then_inc(self.sems[sl[0]], 16)
        tok = (sl[0], sl[1], "dma")
        self._record(tok, reads, writes)
        self.n_inst += 1
        return tok

    def dma_raw(self, q, emit, reads=(), writes=()):
        self._deps(q, reads, writes)
        i = self.slot_i[q]
        self.slot_i[q] = (i + 1) % self.nslots
        sl = self.slots[q][i]
        if sl[1] > 0:
            self._wait(q, (sl[0], sl[1], "dma"))
        if sl[1] + 16 > SEM_LIMIT:
            sl[0] = self._new_sem("d%s%d" % (q, i))
            sl[1] = 0
        sl[1] += 16
        emit().then_inc(self.sems[sl[0]], 16)
        tok = (sl[0], sl[1], "dma")
        self._record(tok, reads, writes)
        self.n_inst += 1
        return tok

    def barrier(self):
        toks = [(self.cur_sem[e], self.cnt[e], e) for e in self.eng if self.cnt[e] > 0]
        for q in self.slots:
            for key, val in self.slots[q]:
                if val:
                    toks.append((key, val, "dma"))
        for e in self.eng:
            for t in toks:
                if t[2] != e:
                    self._wait(e, t)

    def finish(self):
        for q in self.slots:
            for key, val in self.slots[q]:
                if val:
                    self._wait("sp", (key, val, "dma"))


class Phase(ExitStack):
    def __init__(self, kb):
        super().__init__()
        self.kb = kb

    def __exit__(self, *a):
        self.kb.P.barrier()
        return super().__exit__(*a)


class Tl:
    __slots__ = ("t", "b")

    def __init__(self, t, name=""):
        self.t = t
        self.b = Buf(name)


class KB:
    def __init__(self, nc, st):
        self.nc = nc
        self.st = st
        self.P = Prog(nc, st)
        self.uid = 0
        self.banks = [Tl(st.enter_context(nc.psum_tensor("bank%d" % i, [128, 512], F32)), "bank%d" % i) for i in range(8)]
        self.evac_i = 0
        self.consts = {}

    def defer(self, tag, fn):
        if not hasattr(self, "pending"):
            self.pending = []
        self.pending.append((tag, fn))

    def pump(self, n=1):
        p = getattr(self, "pending", None)
        while p and n > 0:
            p.pop(0)[1]()
            n -= 1

    def flush_tag(self, tag):
        p = getattr(self, "pending", None)
        if not p:
            return
        last = -1
        for i, (t, _) in enumerate(p):
            if t == tag:
                last = i
        for _ in range(last + 1):
            p.pop(0)[1]()

    def sb(self, stack, shape, dt, name="t"):
        self.uid += 1
        nm = "%s_%d" % (name, self.uid)
        return Tl(stack.enter_context(self.nc.sbuf_tensor(nm, list(shape), dt)), nm)

    def dram(self, name, shape, dt):
        return Tl(self.nc.dram_tensor(name, list(shape), dt).ap(), name)

    def cst(self, val):
        if val not in self.consts:
            t = self.sb(self.st, [128, 1], F32, "cst")
            self.P.op("pool", lambda: self.nc.gpsimd.memset(t.t[:], float(val)), writes=[t.b])
            self.consts[val] = t
        return self.consts[val]

    @staticmethod
    def _b(xs):
        return [x.b for x in xs]

    def act(self, out, in_, func, reads, writes, bias=None, scale=None):
        kw = {}
        rd = list(reads)
        if bias is not None:
            if isinstance(bias, (int, float)):
                c = self.cst(bias)
                rd.append(c)
                bias = c.t[0:out.shape[0], 0:1]
            kw["bias"] = bias
        if scale is not None:
            kw["scale"] = scale
        return self.P.op("act", lambda: self.nc.scalar.activation(out=out, in_=in_, func=func, **kw), self._b(rd), self._b(writes))

    def tt(self, out, in0, in1, op, reads, writes, eng="dve"):
        e = self.nc.vector if eng == "dve" else self.nc.gpsimd
        return self.P.op(eng, lambda: e.tensor_tensor(out=out, in0=in0, in1=in1, op=op), self._b(reads), self._b(writes))

    def ts(self, out, in0, s1, s2, op0, op1, reads, writes, eng="dve"):
        e = self.nc.vector if eng == "dve" else self.nc.gpsimd
        if op1 is None:
            return self.P.op(eng, lambda: e.tensor_scalar(out=out, in0=in0, scalar1=s1, scalar2=None, op0=op0), self._b(reads), self._b(writes))
        return self.P.op(eng, lambda: e.tensor_scalar(out=out, in0=in0, scalar1=s1, scalar2=s2, op0=op0, op1=op1), self._b(reads), self._b(writes))

    def stt(self, out, in0, scalar, in1, op0, op1, reads, writes):
        return self.P.op("dve", lambda: self.nc.vector.scalar_tensor_tensor(out=out, in0=in0, scalar=scalar, in1=in1, op0=op0, op1=op1), self._b(reads), self._b(writes))

    def copy(self, out, in_, reads, writes, eng=None):
        if eng is None:
            self.evac_i += 1
            eng = "act" if self.evac_i % 2 else "dve"
        if eng == "act":
            return self.P.op("act", lambda: self.nc.scalar.copy(out=out, in_=in_), self._b(reads), self._b(writes))
        e = self.nc.vector if eng == "dve" else self.nc.gpsimd
        return self.P.op(eng, lambda: e.tensor_copy(out=out, in_=in_), self._b(reads), self._b(writes))

    def recip(self, out, in_, reads, writes):
        return self.P.op("dve", lambda: self.nc.vector.reciprocal(out=out, in_=in_), self._b(reads), self._b(writes))

    def memset(self, out, val, writes, eng="pool"):
        e = self.nc.vector if eng == "dve" else self.nc.gpsimd
        return self.P.op(eng, lambda: e.memset(out, float(val)), [], self._b(writes))

    def mm(self, bank, out, lhsT, rhs, start, stop, reads):
        return self.P.op("pe", lambda: self.nc.tensor.matmul(out, lhsT=lhsT, rhs=rhs, start=start, stop=stop), self._b(reads), [bank.b])

    def tr(self, bank, out, in_, reads):
        rd = list(reads) + [self.ident]
        return self.P.op("pe", lambda: self.nc.tensor.transpose(out=out, in_=in_, identity=self.ident.t[0:in_.shape[0], 0:in_.shape[0]]), self._b(rd), [bank.b])

    def dma(self, q, out, in_, reads, writes, **kw):
        return self.P.dma(q, out, in_, self._b(reads), self._b(writes), **kw)

    def setup_consts(self):
        nc = self.nc
        self.ident = self.sb(self.st, [128, 128], F32, "ident")
        self.memset(self.ident.t[:], 1.0, [self.ident])
        self.P.op("pool", lambda: nc.gpsimd.affine_select(out=self.ident.t[:], in_=self.ident.t[:], pattern=[[1, 128]], compare_op=ALU.is_equal, fill=0.0, base=0, channel_multiplier=-1), self._b([self.ident]), self._b([self.ident]))
        self.ones_bf = self.sb(self.st, [128, 128], BF16, "ones_bf")
        self.memset(self.ones_bf.t[:], 1.0, [self.ones_bf])
        self.ones_f = self.sb(self.st, [128, 128], F32, "ones_f")
        self.memset(self.ones_f.t[:], 1.0, [self.ones_f])
        for v in (1e-6, 1e-5, 1.0, 0.0):
            self.cst(v)
        self.sel = self.sb(self.st, [16, 16, 128], F32, "sel")
        self.memset(self.sel.t[:], 0.0, [self.sel])
        self.P.op("pool", lambda: nc.gpsimd.affine_select(out=self.sel.t[:], in_=self.sel.t[:], pattern=[[-1, 16], [0, 128]], compare_op=ALU.not_equal, fill=1.0, base=0, channel_multiplier=1), self._b([self.sel]), self._b([self.sel]))

    def bcast_row(self, stack, row_ap, n, name):
        out = self.sb(stack, [128, n], F32, name)
        with Phase(self) as p:
            rowt = self.sb(p, [1, n], F32, name + "_row")
            self.dma("sp", rowt.t[:], row_ap, [], [rowt])
            for c in range(0, n, 512):
                w = min(512, n - c)
                bk = self.banks[(c // 512) % 2]
                self.mm(bk, bk.t[:, 0:w], self.ones_f.t[0:1, :], rowt.t[0:1, c:c + w], True, True, [self.ones_f, rowt])
                self.copy(out.t[:, c:c + w], bk.t[:, 0:w], [bk], [out])
        return out

    def load_xT(self, src, tok0, xT, xtiles, ntt=4, x32=None, post=None):
        for ts_ in range(ntt):
            xt = xtiles[ts_ % len(xtiles)]
            self.dma("sp", xt.t[:], src.t[tok0 + ts_ * 128: tok0 + (ts_ + 1) * 128, :], [src], [xt])
            for j in range(4):
                bk = self.banks[4 + j]
                for k in range(4):
                    dc = 4 * j + k
                    self.tr(bk, bk.t[:, k * 128:(k + 1) * 128], xt.t[:, dc * 128:(dc + 1) * 128], [xt])
                bv = bk.t[:].rearrange("p (a b) -> p a b", a=4)
                ce = "act" if j % 2 else "dve"
                self.copy(xT.t[:, 4 * j:4 * j + 4, ts_ * 128:(ts_ + 1) * 128], bv, [bk], [xT], eng=ce)
                if x32 is not None:
                    self.copy(x32.t[:, 4 * j:4 * j + 4, :], bv, [bk], [x32], eng=ce)
            if post is not None:
                post(ts_)

    def cast_dram(self, dst, src_ap, rows, step=256, tag=None):
        if src_ap.shape[-1] > 5632:
            step = 128
        for r0 in range(0, rows, step):
            r1 = min(rows, r0 + step)
            if tag is None:
                self.dma("pool", dst.t[r0:r1, :], src_ap[r0:r1, :], [], [dst])
            else:
                self.defer(tag, lambda r0=r0, r1=r1: self.dma("pool", dst.t[r0:r1, :], src_ap[r0:r1, :], [], [dst]))

    def build_rot(self, l, w_in_ap, wrot):
        with Phase(self) as ph:
            src = self.sb(ph, [128, 16, 576], F32, "rsrc")
            dst = self.sb(ph, [128, 16, 576], BF16, "rdst")
            self.dma("sp", src.t[:], w_in_ap[l, :, C_KPE:C_KPE + 576].rearrange("(dc p) c -> p dc c", p=128), [], [src])
            for dc in range(16):
                sv = src.t[:, dc, :].rearrange("p (g two h) -> p g two h", two=2, h=32)
                dv = dst.t[:, dc, :].rearrange("p (g two h) -> p g two h", two=2, h=32)
                self.ts(dv[:, :, 0, :], sv[:, :, 1, :], -1.0, None, ALU.mult, None, [src], [dst])
                self.copy(dv[:, :, 1, :], sv[:, :, 0, :], [src], [dst], eng="dve")
            self.dma("pool", wrot.t[l].rearrange("(dc p) c -> p dc c", p=128), dst.t[:], [dst], [wrot])

    def inproj(self, l, xres, winb, wrot, seg, cst, tok_range, wdep=None):
        with Phase(self) as ph:
            xT = self.sb(ph, [128, 16, 512], BF16, "xT")
            xtiles = [self.sb(ph, [128, 2048], F32, "xtile") for _ in range(2)]
            wbuf = [self.sb(ph, [128, 16, 576], BF16, "wbuf") for _ in range(3)]
            rbuf = self.sb(ph, [128, 16, 576], BF16, "rbuf")
            cos = self.sb(ph, [128, 2048], F32, "cos")
            sin = self.sb(ph, [128, 2048], F32, "sin")
            self.dma("sp", cos.t[:], cst["rope_cos"], [], [cos])
            self.dma("sp", sin.t[:], cst["rope_sin"], [], [sin])
            self.dma("sp", rbuf.t[:], wrot.t[l].rearrange("(dc p) c -> p dc c", p=128), [wrot], [rbuf])
            stage = [self.sb(ph, [128, 512], BF16, "stg") for _ in range(4)]
            st32 = [self.sb(ph, [128, 512], F32, "st32") for _ in range(3)]
            stdt = self.sb(ph, [16, 512], F32, "stdt")
            sti = [0]
            bki = [0]
            groups = [(0, 384, "fm", [("qc", 0, 0, 128), ("qc", 128, 128, 128), ("qc", 256, 256, 128)]),
                      (384, 256, "fm", [("kvc", 0, 0, 128), ("kvc", 128, 128, 128)]),
                      (640, 576, "rope", [("kpe", 0, 0, 64, 1.0), ("rq", 0, 64, 128, 1.0), ("rq", 128, 192, 128, 1.0),
                                          ("rk", 0, 320, 128, 0.125), ("rk", 128, 448, 128, 0.125)]),
                      (1216, 512, "tm", None),
                      (1728, 512, "fm", [("rg", i * 128, i * 128, 128) for i in range(4)]),
                      (2240, 512, "fm", [("mz", i * 128, i * 128, 128) for i in range(4)]),
                      (2752, 512, "fm", [("xbc", i * 128, i * 128, 128) for i in range(4)]),
                      (3264, 512, "fm", [("xbc", 512 + i * 128, i * 128, 128) for i in range(4)]),
                      (3776, 16, "dt", None),
                      (3792, 512, "fm", [("hu", i * 128, i * 128, 128) for i in range(4)]),
                      (4304, 512, "fm", [("hu", 512 + i * 128, i * 128, 128) for i in range(4)]),
                      (4816, 512, "fm", [("hu", 1024 + i * 128, i * 128, 128) for i in range(4)])]

            def loadw(gi):
                c0, cw = groups[gi][0], groups[gi][1]
                wt = wbuf[gi % 3]
                self.dma("sp", wt.t[:, :, 0:cw], winb.t[l, :, c0:c0 + cw].rearrange("(dc p) c -> p dc c", p=128), [wdep or winb], [wt])

            for tok0 in range(tok_range[0], tok_range[1], 512):
                tpos = tok0 % S
                self.load_xT(xres, tok0, xT, xtiles)
                loadw(0)
                loadw(1)
                for gi, (c0, cw, kind, chunks) in enumerate(groups):
                    self.pump(1)
                    if gi + 2 < len(groups):
                        loadw(gi + 2)
                    wt = wbuf[gi % 3]
                    if kind == "fm":
                        for (sname, row0, lc, n) in chunks:
                            bk = self.banks[bki[0] % 4]
                            bki[0] += 1
                            for dc in range(16):
                                self.mm(bk, bk.t[0:n, :], wt.t[:, dc, lc:lc + n], xT.t[:, dc, :], dc == 0, dc == 15, [wt, xT])
                            sg = stage[sti[0] % 4]
                            sti[0] += 1
                            self.copy(sg.t[0:n, :], bk.t[0:n, :], [bk], [sg])
                            self.dma("pool", seg[sname].t[row0:row0 + n, tok0:tok0 + 512], sg.t[0:n, :], [sg], [seg[sname]])
                    elif kind == "dt":
                        bk = self.banks[bki[0] % 4]
                        bki[0] += 1
                        for dc in range(16):
                            self.mm(bk, bk.t[0:16, :], wt.t[:, dc, 0:16], xT.t[:, dc, :], dc == 0, dc == 15, [wt, xT])
                        self.copy(stdt.t[:], bk.t[0:16, :], [bk], [stdt])
                        self.dma("pool", seg["dt"].t[:, tok0:tok0 + 512], stdt.t[:], [stdt], [seg["dt"]])
                    elif kind == "tm":
                        for ts_ in range(4):
                            bk = self.banks[bki[0] % 4]
                            bki[0] += 1
                            for dc in range(16):
                                self.mm(bk, bk.t[:, :], xT.t[:, dc, ts_ * 128:(ts_ + 1) * 128], wt.t[:, dc, 0:512], dc == 0, dc == 15, [wt, xT])
                            sg = stage[sti[0] % 4]
                            sti[0] += 1
                            self.copy(sg.t[:, :], bk.t[:, :], [bk], [sg])
                            self.dma("pool", seg["rv"].t[tok0 + ts_ * 128: tok0 + (ts_ + 1) * 128, :], sg.t[:, :], [sg], [seg["rv"]])
                    else:
                        for (sname, row0, lc, n, scl) in chunks:
                            bA = self.banks[bki[0] % 4]
                            bB = self.banks[(bki[0] + 1) % 4]
                            bki[0] += 2
                            for dc in range(16):
                                self.mm(bA, bA.t[0:n, :], wt.t[:, dc, lc:lc + n], xT.t[:, dc, :], dc == 0, dc == 15, [wt, xT])
                            for dc in range(16):
                                self.mm(bB, bB.t[0:n, :], rbuf.t[:, dc, lc:lc + n], xT.t[:, dc, :], dc == 0, dc == 15, [rbuf, xT])
                            self.tt(st32[0].t[0:n, :], bA.t[0:n, :], cos.t[0:n, tpos:tpos + 512], ALU.mult, [bA, cos], [st32[0]])
                            self.tt(st32[1].t[0:n, :], bB.t[0:n, :], sin.t[0:n, tpos:tpos + 512], ALU.mult, [bB, sin], [st32[1]])
                            self.tt(st32[2].t[0:n, :], st32[0].t[0:n, :], st32[1].t[0:n, :], ALU.add, [st32[0], st32[1]], [st32[2]])
                            sg = stage[sti[0] % 4]
                            sti[0] += 1
                            self.act(sg.t[0:n, :], st32[2].t[0:n, :], AF.Copy, [st32[2]], [sg], scale=scl)
                            self.dma("pool", seg[sname].t[row0:row0 + n, tok0:tok0 + 512], sg.t[0:n, :], [sg], [seg[sname]])

    def rms_rstd(self, ph, src, nch, rows, cols, nfeat, eps, bank, tmp_sq, out_rstd):
        c0, c1 = cols
        for c in range(nch):
            self.act(tmp_sq.t[0:rows, c, :], src.t[0:rows, c, c0:c1], AF.Square, [src], [tmp_sq])
        for c in range(nch):
            self.mm(bank, bank.t[:, :], self.ones_bf.t[0:rows, :], tmp_sq.t[0:rows, c, :], c == 0, c == nch - 1, [self.ones_bf, tmp_sq])
        self.act(out_rstd.t[:, :], bank.t[:, :], AF.Sqrt, [bank], [out_rstd], bias=eps, scale=1.0 / nfeat)
        self.recip(out_rstd.t[:, :], out_rstd.t[:, :], [out_rstd], [out_rstd])

    def group_ret(self, s, seg, mixedT):
        nc = self.nc
        t0 = s * S
        with Phase(self) as ph:
            rq = self.sb(ph, [128, 2, S], BF16, "rq")
            rk = self.sb(ph, [128, 2, S], BF16, "rk")
            rg = self.sb(ph, [128, 4, S], BF16, "rg")
            V = self.sb(ph, [128, 16, 512], BF16, "rv")
            self.dma("sp", rq.t[:], seg["rq"].t[:, t0:t0 + S].rearrange("(c p) t -> p c t", p=128), [seg["rq"]], [rq])
            self.dma("sp", rk.t[:], seg["rk"].t[:, t0:t0 + S].rearrange("(c p) t -> p c t", p=128), [seg["rk"]], [rk])
            self.dma("sp", rg.t[:], seg["rg"].t[:, t0:t0 + S].rearrange("(c p) t -> p c t", p=128), [seg["rg"]], [rg])
            self.dma("sp", V.t[:], seg["rv"].t[t0:t0 + S, :].rearrange("(c p) e -> p c e", p=128), [seg["rv"]], [V])
            W = 3968
            strip = self.sb(ph, [128, 4, W], BF16, "strip")
            dl = self.sb(ph, [128, W], F32, "dl")
            tA = self.sb(ph, [128, W], F32, "tA")
            tB = self.sb(ph, [128, W], F32, "tB")
            self.P.op("pool", lambda: nc.gpsimd.iota(dl.t[:], pattern=[[1, W]], base=-1920, channel_multiplier=-1, allow_small_or_imprecise_dtypes=True), [], [dl.b])
            for h in range(4):
                self.ts(tA.t[:], dl.t[:], 0.0, RET_LG_F[h], ALU.max, ALU.mult, [dl], [tA])
                self.ts(tB.t[:], dl.t[:], 0.0, -RET_LG_B[h], ALU.min, ALU.mult, [dl], [tB])
                self.tt(tA.t[:], tA.t[:], tB.t[:], ALU.add, [tA, tB], [tA])
                self.act(strip.t[:, h, :], tA.t[:], AF.Exp, [tA], [strip])
            Pb = [self.sb(ph, [128, 512], BF16, "P") for _ in range(6)]
            ysb = self.sb(ph, [128, 512], F32, "ysb")
            ysq = self.sb(ph, [128, 512], F32, "ysq")
            mean = self.sb(ph, [128, 512], F32, "mean")
            var = self.sb(ph, [128, 512], F32, "var")
            gate = self.sb(ph, [128, 512], F32, "gate")
            ob = [self.sb(ph, [128, 512], BF16, "ob") for _ in range(2)]
            pi = 0
            for h in range(4):
                c, base = h // 2, 64 * (h % 2)
                for ib in range(4):
                    self.pump(4)
                    i0 = ib * 512
                    yb = self.banks[4 + (h * 4 + ib) % 2]
                    def S_(jc):
                        sbk = self.banks[jc % 4]
                        self.mm(sbk, sbk.t[:, :], rk.t[base:base + 64, c, jc * 128:jc * 128 + 128], rq.t[base:base + 64, c, i0:i0 + 512], True, True, [rk, rq])

                    S_(0)
                    for jc in range(16):
                        j0 = jc * 128
                        if jc + 1 < 16:
                            S_(jc + 1)
                        sbk = self.banks[jc % 4]
                        pb = Pb[pi % 6]
                        pi += 1
                        x0 = i0 - j0 + 1920
                        self.tt(pb.t[:, :], sbk.t[:, :], strip.t[:, h, x0:x0 + 512], ALU.mult, [sbk, strip], [pb])
                        self.mm(yb, yb.t[:, :], V.t[:, jc, h * 128:(h + 1) * 128], pb.t[:, :], jc == 0, jc == 15, [V, pb])
                    self.copy(ysb.t[:, :], yb.t[:, :], [yb], [ysb], eng="act")
                    self.act(ysq.t[:, :], yb.t[:, :], AF.Square, [yb], [ysq])
                    mb, vb = self.banks[6], self.banks[7]
                    self.mm(mb, mb.t[:, :], self.ones_f.t[:, :], ysb.t[:, :], True, True, [self.ones_f, ysb])
                    self.mm(vb, vb.t[:, :], self.ones_f.t[:, :], ysq.t[:, :], True, True, [self.ones_f, ysq])
                    self.act(mean.t[:, :], mb.t[:, :], AF.Copy, [mb], [mean], scale=1.0 / 128)
                    self.tt(var.t[:, :], mean.t[:, :], mean.t[:, :], ALU.mult, [mean], [var])
                    self.stt(var.t[:, :], vb.t[:, :], 1.0 / 128, var.t[:, :], ALU.mult, ALU.subtract, [vb, var], [var])
                    self.act(var.t[:, :], var.t[:, :], AF.Sqrt, [var], [var], bias=1e-6, scale=1.0)
                    self.recip(var.t[:, :], var.t[:, :], [var], [var])
                    self.tt(ysb.t[:, :], ysb.t[:, :], mean.t[:, :], ALU.subtract, [ysb, mean], [ysb])
                    self.tt(ysb.t[:, :], ysb.t[:, :], var.t[:, :], ALU.mult, [ysb, var], [ysb])
                    self.act(gate.t[:, :], rg.t[:, h, i0:i0 + 512], AF.Silu, [rg], [gate])
                    o = ob[(h * 4 + ib) % 2]
                    self.tt(o.t[:, :], ysb.t[:, :], gate.t[:, :], ALU.mult, [ysb, gate], [o])
                    self.dma("pool", mixedT.t[512 + h * 128: 512 + (h + 1) * 128, t0 + i0: t0 + i0 + 512], o.t[:, :], [o], [mixedT])

    def group_mla(self, l, s, seg, mixedT, prm, cst):
        t0 = s * S
        SC = (128 + 64) ** -0.5
        with Phase(self) as ph:
            qc = self.sb(ph, [128, 3, S], BF16, "qc")
            kvc = self.sb(ph, [128, 2, S], BF16, "kvc")
            kpe = self.sb(ph, [64, S], BF16, "kpe")
            self.dma("sp", qc.t[:], seg["qc"].t[:, t0:t0 + S].rearrange("(c p) t -> p c t", p=128), [seg["qc"]], [qc])
            self.dma("sp", kvc.t[:], seg["kvc"].t[:, t0:t0 + S].rearrange("(c p) t -> p c t", p=128), [seg["kvc"]], [kvc])
            self.dma("sp", kpe.t[:], seg["kpe"].t[:, t0:t0 + S], [seg["kpe"]], [kpe])
            cos = self.sb(ph, [64, S], F32, "cos")
            sin = self.sb(ph, [64, S], F32, "sin")
            self.dma("sp", cos.t[:], cst["rope_cos"][0:64, :], [], [cos])
            self.dma("sp", sin.t[:], cst["rope_sin"][0:64, :], [], [sin])
            wq32 = self.sb(ph, [128, 3, 768], F32, "wq32")
            wkv32 = self.sb(ph, [128, 2, 1024], F32, "wkv32")
            self.dma("sp", wq32.t[:], prm["mla_w_uq"][l].rearrange("(c p) e -> p c e", p=128), [], [wq32])
            self.dma("sp", wkv32.t[:], prm["mla_w_ukv"][l].rearrange("(c p) e -> p c e", p=128), [], [wkv32])
            wq = self.sb(ph, [128, 3, 768], BF16, "wq")
            wkv = self.sb(ph, [128, 2, 1024], BF16, "wkv")
            wqr = self.sb(ph, [128, 3, 4, 64], BF16, "wqr")
            self.copy(wq.t[:], wq32.t[:], [wq32], [wq], eng="dve")
            self.copy(wkv.t[:], wkv32.t[:], [wkv32], [wkv], eng="dve")
            for c in range(3):
                for h in range(4):
                    b0 = 192 * h + 128
                    self.ts(wqr.t[:, c, h, 0:32], wq32.t[:, c, b0 + 32:b0 + 64], -1.0, None, ALU.mult, None, [wq32], [wqr])
                    self.copy(wqr.t[:, c, h, 32:64], wq32.t[:, c, b0:b0 + 32], [wq32], [wqr], eng="dve")
            qnw = self.sb(ph, [128, 3], F32, "qnw")
            kvnw = self.sb(ph, [128, 2], F32, "kvnw")
            onw = self.sb(ph, [128, 4], F32, "onw")
            self.dma("sp", qnw.t[:], prm["mla_q_norm_pp"][:, l * 3:(l + 1) * 3], [], [qnw])
            self.dma("sp", kvnw.t[:], prm["mla_kv_norm_pp"][:, l * 2:(l + 1) * 2], [], [kvnw])
            self.dma("sp", onw.t[:], prm["mla_out_norm_pp"][:, l * 4:(l + 1) * 4], [], [onw])
            qn = self.sb(ph, [128, 3, S], BF16, "qn")
            kvn = self.sb(ph, [128, 2, S], BF16, "kvn")
            sq = self.sb(ph, [128, 4, 512], BF16, "sq")
            rstd = self.sb(ph, [128, 512], F32, "rstd")
            for tb in range(4):
                c0 = tb * 512
                self.rms_rstd(ph, qc, 3, 128, (c0, c0 + 512), 384.0, 1e-6, self.banks[0], sq, rstd)
                for c in range(3):
                    self.stt(qn.t[:, c, c0:c0 + 512], qc.t[:, c, c0:c0 + 512], qnw.t[:, c:c + 1], rstd.t[:, :], ALU.mult, ALU.mult, [qc, qnw, rstd], [qn])
                self.rms_rstd(ph, kvc, 2, 128, (c0, c0 + 512), 256.0, 1e-6, self.banks[1], sq, rstd)
                for c in range(2):
                    self.stt(kvn.t[:, c, c0:c0 + 512], kvc.t[:, c, c0:c0 + 512], kvnw.t[:, c:c + 1], rstd.t[:, :], ALU.mult, ALU.mult, [kvc, kvnw, rstd], [kvn])
            qhn = self.sb(ph, [128, 4, S], BF16, "qhn")
            qhp = self.sb(ph, [64, 4, S], BF16, "qhp")
            khn = self.sb(ph, [128, 4, S], BF16, "khn")
            Vt = self.sb(ph, [128, 16, 512], BF16, "Vt")
            t1 = self.sb(ph, [64, 512], F32, "t1")
            t2 = self.sb(ph, [64, 512], F32, "t2")
            bi = 0
            for tb in range(4):
                c0 = tb * 512
                for h in range(4):
                    bk = self.banks[bi % 4]; bi += 1
                    for c in range(3):
                        self.mm(bk, bk.t[:, :], wq.t[:, c, 192 * h:192 * h + 128], qn.t[:, c, c0:c0 + 512], c == 0, c == 2, [wq, qn])
                    self.copy(qhn.t[:, h, c0:c0 + 512], bk.t[:, :], [bk], [qhn])
                    bA = self.banks[bi % 4]; bi += 1
                    bB = self.banks[bi % 4]; bi += 1
                    for c in range(3):
                        self.mm(bA, bA.t[0:64, :], wq.t[:, c, 192 * h + 128:192 * h + 192], qn.t[:, c, c0:c0 + 512], c == 0, c == 2, [wq, qn])
                    for c in range(3):
                        self.mm(bB, bB.t[0:64, :], wqr.t[:, c, h, :], qn.t[:, c, c0:c0 + 512], c == 0, c == 2, [wqr, qn])
                    self.tt(t1.t[:, :], bA.t[0:64, :], cos.t[:, c0:c0 + 512], ALU.mult, [bA, cos], [t1])
                    self.tt(t2.t[:, :], bB.t[0:64, :], sin.t[:, c0:c0 + 512], ALU.mult, [bB, sin], [t2])
                    self.tt(qhp.t[:, h, c0:c0 + 512], t1.t[:, :], t2.t[:, :], ALU.add, [t1, t2], [qhp])
                    bk = self.banks[bi % 4]; bi += 1
                    for c in range(2):
                        self.mm(bk, bk.t[:, :], wkv.t[:, c, 256 * h:256 * h + 128], kvn.t[:, c, c0:c0 + 512], c == 0, c == 1, [wkv, kvn])
                    self.copy(khn.t[:, h, c0:c0 + 512], bk.t[:, :], [bk], [khn])
            for tc in range(16):
                bk = self.banks[bi % 4]; bi += 1
                for h in range(4):
                    for c in range(2):
                        self.mm(bk, bk.t[:, h * 128:(h + 1) * 128], kvn.t[:, c, tc * 128:(tc + 1) * 128], wkv.t[:, c, 256 * h + 128:256 * h + 256], c == 0, c == 1, [wkv, kvn])
                self.copy(Vt.t[:, tc, :], bk.t[:, :], [bk], [Vt])
            Pb = [self.sb(ph, [128, 512], BF16, "P") for _ in range(6)]
            oblk = self.sb(ph, [128, 4, 512], F32, "oblk")
            den = self.sb(ph, [128, 512], F32, "den")
            ob = [self.sb(ph, [128, 512], BF16, "ob") for _ in range(2)]
            pi = 0
            for qb in range(4):
                q0 = qb * 512
                for h in range(4):
                    self.pump(4)
                    ob_k, dn_k = self.banks[4 + 2 * (h % 2)], self.banks[5 + 2 * (h % 2)]
                    def S_(kc):
                        k0 = kc * 128
                        sbk = self.banks[kc % 4]
                        self.mm(sbk, sbk.t[:, :], khn.t[:, h, k0:k0 + 128], qhn.t[:, h, q0:q0 + 512], True, False, [khn, qhn])
                        self.mm(sbk, sbk.t[:, :], kpe.t[0:64, k0:k0 + 128], qhp.t[0:64, h, q0:q0 + 512], False, True, [kpe, qhp])

                    S_(0)
                    for kc in range(16):
                        if kc + 1 < 16:
                            S_(kc + 1)
                        sbk = self.banks[kc % 4]
                        pb = Pb[pi % 6]; pi += 1
                        self.act(pb.t[:, :], sbk.t[:, :], AF.Exp, [sbk], [pb], scale=SC)
                        self.mm(ob_k, ob_k.t[:, :], Vt.t[:, kc, h * 128:(h + 1) * 128], pb.t[:, :], kc == 0, kc == 15, [Vt, pb])
                        self.mm(dn_k, dn_k.t[:, :], self.ones_bf.t[:, :], pb.t[:, :], kc == 0, kc == 15, [self.ones_bf, pb])
                    self.recip(den.t[:, :], dn_k.t[:, :], [dn_k], [den])
                    self.tt(oblk.t[:, h, :], ob_k.t[:, :], den.t[:, :], ALU.mult, [ob_k, den], [oblk])
                for h in range(4):
                    self.act(sq.t[:, h, :], oblk.t[:, h, :], AF.Square, [oblk], [sq])
                nb = self.banks[3]
                for h in range(4):
                    self.mm(nb, nb.t[:, :], self.ones_bf.t[:, :], sq.t[:, h, :], h == 0, h == 3, [self.ones_bf, sq])
                self.act(rstd.t[:, :], nb.t[:, :], AF.Sqrt, [nb], [rstd], bias=1e-6, scale=1.0 / 512)
                self.recip(rstd.t[:, :], rstd.t[:, :], [rstd], [rstd])
                for h in range(4):
                    o = ob[h % 2]
                    self.stt(o.t[:, :], oblk.t[:, h, :], onw.t[:, h:h + 1], rstd.t[:, :], ALU.mult, ALU.mult, [oblk, onw, rstd], [o])
                    self.dma("pool", mixedT.t[h * 128:(h + 1) * 128, t0 + q0:t0 + q0 + 512], o.t[:, :], [o], [mixedT])


def _pp(v, nl):
    v = np.asarray(v, np.float32)
    n = v.shape[-1]
    return np.ascontiguousarray(v.reshape(nl, n // 128, 128).transpose(2, 0, 1).reshape(128, nl * (n // 128)))


def host_consts():
    c = {}
    half = 32
    inv = (10000.0 ** (-np.arange(half, dtype=np.float32) * 2.0 / 64)).astype(np.float32)
    ang = np.arange(S, dtype=np.float32)[None, :] * inv[:, None]
    c["rope_cos"] = np.ascontiguousarray(np.tile(np.cos(ang), (4, 1)).astype(np.float32))
    c["rope_sin"] = np.ascontiguousarray(np.tile(np.sin(ang), (4, 1)).astype(np.float32))
    t = np.linspace(0.0, 1.0, S, dtype=np.float32)[:, None]
    bands = 16
    angp = (2.0 * math.pi * np.arange(S, dtype=np.float32)[:, None] / S).astype(np.float32)
    f = np.linspace(1e-4, bands - 1, bands, dtype=np.float32)[None, :]
    z = np.concatenate([t, np.cos(f * angp), -np.sin(f * angp)], axis=-1).astype(np.float32)
    c["hy_zT"] = np.ascontiguousarray(z.T)
    c["hy_ntlin_pp"] = np.ascontiguousarray((-t[:, 0]).reshape(16, 128).T.astype(np.float32))
    mn, mx = math.log(1e-2) / 1.5, math.log(1e-2) / 0.3
    dl = np.abs(np.linspace(mn, mx, 512, dtype=np.float32))
    c["hy_delta_b"] = np.ascontiguousarray(np.tile(dl[None, :], (128, 1)).astype(np.float32))
    idx = np.arange(NFP, dtype=np.int64)
    ph_ = (np.outer(idx, idx) % 4096).astype(np.float64) * (2.0 * math.pi / 4096.0)
    valid = (idx <= 2048).astype(np.float64)
    Cm = np.cos(ph_) * valid[:, None] * valid[None, :]
    Sm = -np.sin(ph_) * valid[:, None] * valid[None, :]
    bf = ml_dtypes.bfloat16
    c["dft_Cnat"] = np.ascontiguousarray(Cm[:, :S].astype(np.float32).astype(bf))
    c["dft_Snat"] = np.ascontiguousarray(Sm[:, :S].astype(np.float32).astype(bf))
    c["dft_Cblk"] = np.ascontiguousarray(Cm[:S, :].reshape(16, 128, NF, 128).transpose(2, 1, 0, 3).astype(np.float32).astype(bf))
    c["dft_Sblk"] = np.ascontiguousarray(Sm[:S, :].reshape(16, 128, NF, 128).transpose(2, 1, 0, 3).astype(np.float32).astype(bf))
    wfv = np.where(idx <= 2048, 2.0, 0.0)
    wfv[0] = 1.0
    wfv[2048] = 1.0
    c["dft_wf_pp"] = np.ascontiguousarray((wfv / 4096.0).reshape(NF, 128).T.astype(np.float32))
    return c


def host_params(inp):
    p = {}
    p["mla_w_uq"] = np.ascontiguousarray(inp["mla_w_uq"], dtype=np.float32)
    p["mla_w_ukv"] = np.ascontiguousarray(inp["mla_w_ukv"], dtype=np.float32)
    p["mla_q_norm_pp"] = _pp(inp["mla_q_norm"], L)
    p["mla_kv_norm_pp"] = _pp(inp["mla_kv_norm"], L)
    p["mla_out_norm_pp"] = _pp(inp["mla_out_norm"], L)
    cwv = np.asarray(inp["ssd_conv_w"], np.float32)
    p["ssd_conv_w_pp"] = np.ascontiguousarray(cwv.reshape(L, 5, 8, 128).transpose(3, 0, 2, 1).reshape(128, L * 40))
    p["ssd_conv_b_pp"] = _pp(inp["ssd_conv_b"], L)
    p["ssd_dtb"] = np.ascontiguousarray(np.asarray(inp["ssd_dt_bias"], np.float32).reshape(L, 16).T)
    p["ssd_alog"] = np.ascontiguousarray(np.asarray(inp["ssd_a_log"], np.float32).reshape(L, 16).T)
    dd = np.asarray(inp["ssd_d"], np.float32)
    p["ssd_d_pp"] = np.ascontiguousarray(np.repeat(dd, 64, axis=1).reshape(L, 4, 128).transpose(2, 0, 1).reshape(128, L * 4))
    p["ssd_norm_pp"] = _pp(inp["ssd_norm"], L)
    hw = np.asarray(inp["hy_conv_w"], np.float32)
    p["hy_conv_w_pp"] = np.ascontiguousarray(hw.reshape(L, 3, 12, 128).transpose(3, 0, 2, 1).reshape(128, L * 36))
    p["hy_conv_b_pp"] = _pp(inp["hy_conv_b"], L)
    p["hy_w1"] = np.ascontiguousarray(inp["hy_w1"], dtype=np.float32)
    p["hy_w2"] = np.ascontiguousarray(inp["hy_w2"], dtype=np.float32)
    p["hy_w3"] = np.ascontiguousarray(inp["hy_w3"], dtype=np.float32)
    b12 = np.stack([np.asarray(inp["hy_b1"], np.float32), np.asarray(inp["hy_b2"], np.float32)], 1)
    p["hy_b_pp"] = np.ascontiguousarray(b12.transpose(2, 0, 1).reshape(64, L * 2))
    p["hy_freq_pp"] = np.ascontiguousarray(np.asarray(inp["hy_freq"], np.float32).transpose(2, 0, 1).reshape(64, L * 2))
    hbv = np.asarray(inp["hy_bias"], np.float32)
    p["hy_bias_pp"] = np.ascontiguousarray(hbv.reshape(L, 2, 4, 128).transpose(3, 0, 1, 2).reshape(128, L * 8))
    p["hy_out_norm_pp"] = _pp(inp["hy_out_norm"], L)
    p["dirsign"] = np.concatenate([-np.ones((8, 1), np.float32), np.ones((8, 1), np.float32)], 0)
    p["ndirmask"] = np.concatenate([np.zeros((8, 1), np.float32), -np.ones((8, 1), np.float32)], 0)
    return p


def build(cfg, shapes):
    nc = bass.Bass("TRN2", target_bir_lowering=False)
    ext = {}
    for name, (shape, dt) in shapes.items():
        ext[name] = nc.dram_tensor(name, list(shape), dt, kind="ExternalInput").ap()
    with ExitStack() as st:
        kb = KB(nc, st)
        kb.setup_consts()
        winb = kb.dram("winb", [L, D, INC], BF16)
        wrot = kb.dram("wrot", [L, D, 576], BF16)
        seg = {"qc": kb.dram("s_qc", [384, T], BF16), "kvc": kb.dram("s_kvc", [256, T], BF16),
               "kpe": kb.dram("s_kpe", [64, T], BF16), "rq": kb.dram("s_rq", [256, T], BF16),
               "rk": kb.dram("s_rk", [256, T], BF16), "rg": kb.dram("s_rg", [512, T], BF16),
               "mz": kb.dram("s_mz", [512, T], BF16), "xbc": kb.dram("s_xbc", [1024, T], BF16),
               "dt": kb.dram("s_dt", [16, T], F32), "hu": kb.dram("s_hu", [1536, T], BF16),
               "rv": kb.dram("s_rv", [T, 512], BF16)}
        xin = Tl(ext["x"], "x")
        if cfg.get("dbg"):
            kb.dbg = {"BC": Tl(nc.dram_tensor("d_BC", [16, S], F32, kind="ExternalOutput").ap()),
                      "dt_tok": Tl(nc.dram_tensor("d_dt_tok", [128, 256], F32, kind="ExternalOutput").ap()),
                      "bias_tok": Tl(nc.dram_tensor("d_bias_tok", [128, 256], F32, kind="ExternalOutput").ap()),
                      "xsT": Tl(nc.dram_tensor("d_xsT", [128, S], F32, kind="ExternalOutput").ap()),
                      "xdt": Tl(nc.dram_tensor("d_xdt", [128, 1024], BF16, kind="ExternalOutput").ap())}
        nseq = cfg.get("nseq", NSEQ)
        if cfg["mode"] == "mixtest":
            l = cfg["layer"]
            mixedT = Tl(nc.dram_tensor("mixedT", [D, T], BF16, kind="ExternalOutput").ap(), "mixedT")
            kb.cast_dram(Tl(winb.t[l], "x").__class__(winb.t[l]) if False else _sub(winb, winb.t[l]), ext["w_in"][l], D)
            kb.build_rot(l, ext["w_in"], wrot)
            kb.inproj(l, xin, winb, wrot, seg, ext, (0, nseq * S))
            for s in range(nseq):
                if "A" in cfg["groups"]:
                    kb.group_mla(l, s, seg, mixedT, ext, ext)
                if "B" in cfg["groups"]:
                    kb.group_ret(s, seg, mixedT)
                if "C" in cfg["groups"]:
                    kb.group_ssd(l, s, seg, mixedT, ext)
            if "D" in cfg["groups"]:
                Hs = kb.dram("Hs", [2, 2, NFP, 512], F32)
                hyu = kb.dram("hyu", [3, NSEQ * 512, S], F32)
                z1s = kb.dram("z1s", [NSEQ * 512, S], F32)
                kb.hyena_filter(l, ext, ext, Hs)
                kb.group_hyena(l, nseq, seg, mixedT, ext, ext, Hs, hyu, z1s)
        kb.P.finish()
        print("instructions:", kb.P.n_inst, "sems:", kb.P.nsem)
    return nc


def _sub(parent, ap):
    t = Tl(ap, parent.b.name)
    t.b = parent.b
    return t


def _group_ssd(self, l, s, seg, mixedT, prm):
    nc = self.nc
    t0 = s * S
    with Phase(self) as ph:
        xsT = self.sb(ph, [128, 4, S], F32, "xsT")
        BT = self.sb(ph, [128, 2, S], BF16, "BT")
        CT = self.sb(ph, [128, 2, S], BF16, "CT")
        mz = self.sb(ph, [128, 4, S], BF16, "mz")
        self.dma("sp", mz.t[:], seg["mz"].t[:, t0:t0 + S].rearrange("(c p) t -> p c t", p=128), [seg["mz"]], [mz])
        cw = self.sb(ph, [128, 40], F32, "cw")
        cb = self.sb(ph, [128, 8], F32, "cb")
        dpp = self.sb(ph, [128, 4], F32, "dpp")
        nw = self.sb(ph, [128, 4], F32, "nw")
        self.dma("sp", cw.t[:], prm["ssd_conv_w_pp"][:, l * 40:(l + 1) * 40], [], [cw])
        self.dma("sp", cb.t[:], prm["ssd_conv_b_pp"][:, l * 8:(l + 1) * 8], [], [cb])
        self.dma("sp", dpp.t[:], prm["ssd_d_pp"][:, l * 4:(l + 1) * 4], [], [dpp])
        self.dma("sp", nw.t[:], prm["ssd_norm_pp"][:, l * 4:(l + 1) * 4], [], [nw])
        dt_tok = self.sb(ph, [128, 16, 16], F32, "dt_tok")
        bias_tok = self.sb(ph, [128, 16, 16], F32, "bias_tok")
        BC = self.sb(ph, [16, S], F32, "BC")
        xdt = [self.sb(ph, [128, 16, 8, 128], BF16, "xdt%d" % d) for d in range(2)]
        for d in range(2):
            self.memset(xdt[d].t[:], 0.0, [xdt[d]])
        with Phase(self) as p2:
            raw = self.sb(p2, [128, 8, S], BF16, "raw")
            acc = self.sb(p2, [128, S], F32, "acc")
            self.dma("sp", raw.t[:], seg["xbc"].t[:, t0:t0 + S].rearrange("(c p) t -> p c t", p=128), [seg["xbc"]], [raw])
            for c in range(8):
                self.ts(acc.t[:, :], raw.t[:, c, :], cw.t[:, c * 5 + 2:c * 5 + 3], None, ALU.mult, None, [raw, cw], [acc])
                for k in (0, 1, 3, 4):
                    sh = k - 2
                    a0, a1 = max(0, -sh), S - max(0, sh)
                    self.stt(acc.t[:, a0:a1], raw.t[:, c, a0 + sh:a1 + sh], cw.t[:, c * 5 + k:c * 5 + k + 1], acc.t[:, a0:a1], ALU.mult, ALU.add, [raw, cw, acc], [acc])
                if c < 4:
                    dst, dtl = xsT.t[:, c, :], xsT
                elif c < 6:
                    dst, dtl = BT.t[:, c - 4, :], BT
                else:
                    dst, dtl = CT.t[:, c - 6, :], CT
                self.act(dst, acc.t[:, :], AF.Silu, [acc, cb], [dtl], bias=cb.t[:, c:c + 1])
        with Phase(self) as p2:
            dtr = self.sb(p2, [16, S], F32, "dtr")
            ax = self.sb(p2, [16, S], F32, "ax")
            dtv = self.sb(p2, [16, S], F32, "dtv")
            la = self.sb(p2, [16, S], F32, "la")
            cs = self.sb(p2, [16, S], F32, "cs")
            one16 = self.sb(p2, [16, S], F32, "one16")
            sm = self.sb(p2, [16, 8], F32, "sm")
            self.dma("sp", dtr.t[:], seg["dt"].t[:, t0:t0 + S], [seg["dt"]], [dtr])
            self.dma("sp", sm.t[:, 0:1], prm["ssd_dtb"][:, l:l + 1], [], [sm], allow_slow_non_contiguous=True)
            self.dma("sp", sm.t[:, 1:2], prm["ssd_alog"][:, l:l + 1], [], [sm], allow_slow_non_contiguous=True)
            self.dma("sp", sm.t[:, 2:3], prm["dirsign"][:, 0:1], [], [sm], allow_slow_non_contiguous=True)
            self.dma("sp", sm.t[:, 3:4], prm["ndirmask"][:, 0:1], [], [sm], allow_slow_non_contiguous=True)
            self.ts(dtr.t[:], dtr.t[:], sm.t[:, 0:1], None, ALU.add, None, [dtr, sm], [dtr])
            self.stt(ax.t[:], dtr.t[:], -1.0, dtr.t[:], ALU.mult, ALU.max, [dtr], [ax])
            self.act(ax.t[:], ax.t[:], AF.Exp, [ax], [ax], scale=-1.0)
            self.act(ax.t[:], ax.t[:], AF.Ln, [ax], [ax], bias=1.0)
            self.stt(dtv.t[:], dtr.t[:], 0.0, ax.t[:], ALU.max, ALU.add, [dtr, ax], [dtv])
            self.act(sm.t[:, 4:5], sm.t[:, 1:2], AF.Exp, [sm], [sm])
            self.ts(sm.t[:, 5:6], sm.t[:, 4:5], -1.0, None, ALU.mult, None, [sm], [sm])
            self.ts(la.t[:], dtv.t[:], sm.t[:, 5:6], None, ALU.mult, None, [dtv, sm], [la])
            self.memset(one16.t[:], 1.0, [one16])
            self.P.op("dve", lambda: nc.vector.tensor_tensor_scan(out=cs.t[:], data0=one16.t[:], data1=la.t[:], initial=0.0, op0=ALU.mult, op1=ALU.add), self._b([one16, la]), self._b([cs]))
            self.stt(BC.t[:], la.t[:], sm.t[:, 3:4], cs.t[:], ALU.mult, ALU.add, [la, sm, cs], [BC])
            self.ts(cs.t[:], BC.t[:], sm.t[:, 2:3], None, ALU.mult, None, [BC, sm], [cs])
            b6, b7 = self.banks[6], self.banks[7]
            for tc in range(16):
                self.tr(b6, b6.t[:, tc * 16:(tc + 1) * 16], dtv.t[0:16, tc * 128:(tc + 1) * 128], [dtv])
                self.tr(b7, b7.t[:, tc * 16:(tc + 1) * 16], cs.t[0:16, tc * 128:(tc + 1) * 128], [cs])
            self.copy(dt_tok.t[:], b6.t[:, 0:256].rearrange("p (a b) -> p a b", a=16), [b6], [dt_tok])
            self.copy(bias_tok.t[:], b7.t[:, 0:256].rearrange("p (a b) -> p a b", a=16), [b7], [bias_tok])
            for tc in range(16):
                bk = self.banks[4 + tc % 2]
                for c in range(4):
                    self.tr(bk, bk.t[:, c * 128:(c + 1) * 128], xsT.t[:, c, tc * 128:(tc + 1) * 128], [xsT])
                for d in range(2):
                    for h in range(8):
                        self.ts(xdt[d].t[:, tc, h, 64 * (h % 2):64 * (h % 2) + 64], bk.t[:, h * 64:(h + 1) * 64], dt_tok.t[:, tc, d * 8 + h:d * 8 + h + 1], None, ALU.mult, None, [bk, dt_tok], [xdt[d]])
        if getattr(self, "dbg", None) is not None:
            self.dma("pool", self.dbg["BC"].t[:, :], BC.t[:, :], [BC], [self.dbg["BC"]])
            self.dma("pool", self.dbg["dt_tok"].t[:, :], dt_tok.t[:].rearrange("p a b -> p (a b)"), [dt_tok], [self.dbg["dt_tok"]])
            self.dma("pool", self.dbg["bias_tok"].t[:, :], bias_tok.t[:].rearrange("p a b -> p (a b)"), [bias_tok], [self.dbg["bias_tok"]])
            self.dma("pool", self.dbg["xsT"].t[:, :], xsT.t[:, 0, :], [xsT], [self.dbg["xsT"]])
            self.dma("pool", self.dbg["xdt"].t[:, :], xdt[0].t[:, 0, :, :].rearrange("p a b -> p (a b)"), [xdt[0]], [self.dbg["xdt"]])
        Mf = self.sb(ph, [128, 896], BF16, "Mf")
        Mb = self.sb(ph, [128, 896], BF16, "Mb")
        self.memset(Mf.t[:], 1.0, [Mf])
        self.memset(Mb.t[:], 1.0, [Mb])
        self.P.op("pool", lambda: nc.gpsimd.affine_select(out=Mf.t[:], in_=Mf.t[:], pattern=[[1, 896]], compare_op=ALU.is_ge, fill=0.0, base=-384, channel_multiplier=-1), self._b([Mf]), self._b([Mf]))
        self.P.op("pool", lambda: nc.gpsimd.affine_select(out=Mb.t[:], in_=Mb.t[:], pattern=[[-1, 896]], compare_op=ALU.is_gt, fill=0.0, base=384, channel_multiplier=1), self._b([Mb]), self._b([Mb]))
        bcs = [[self.sb(ph, [128, 512], F32, "bcs") for _ in range(2)] for _ in range(2)]
        Lb = [self.sb(ph, [128, 512], F32, "L") for _ in range(6)]
        Pb = [self.sb(ph, [128, 512], BF16, "P") for _ in range(6)]
        ybuf = self.sb(ph, [128, 4, 512], F32, "ybuf")
        yv = self.sb(ph, [128, 512], F32, "yv")
        gate = self.sb(ph, [128, 512], F32, "gate")
        sq = self.sb(ph, [128, 4, 512], BF16, "sq")
        rstd = self.sb(ph, [128, 512], F32, "rstd")
        ob = [self.sb(ph, [128, 512], BF16, "ob") for _ in range(2)]
        li = 0
        for ib in range(4):
            i0 = ib * 512
            for pair in range(4):
                self.pump(4)
                g = pair // 2
                yb = self.banks[4 + pair % 2]
                for d in range(2):
                    for hh in range(2):
                        h = pair * 2 + hh
                        bb = self.banks[3]
                        self.mm(bb, bb.t[:, :], self.sel.t[:, d * 8 + h, :], BC.t[0:16, i0:i0 + 512], True, True, [self.sel, BC])
                        self.copy(bcs[d][hh].t[:, :], bb.t[:, :], [bb], [bcs[d][hh]])
                items = []
                for jc in range(16):
                    for d in range(2):
                        valid = (jc <= 4 * ib + 3) if d == 0 else (jc >= 4 * ib)
                        if valid:
                            for hh in range(2):
                                items.append((jc, d, hh))
                SB = (0, 1, 2, 7)

                def S_(jc):
                    sbk_ = self.banks[SB[jc % 4]]
                    self.mm(sbk_, sbk_.t[:, :], BT.t[:, g, jc * 128:jc * 128 + 128], CT.t[:, g, i0:i0 + 512], True, True, [BT, CT])

                jcs = sorted(set(it[0] for it in items))
                S_(jcs[0])
                last_jc = -1
                for n, (jc, d, hh) in enumerate(items):
                    j0 = jc * 128
                    h = pair * 2 + hh
                    if jc != last_jc:
                        k_ = jcs.index(jc)
                        if k_ + 1 < len(jcs):
                            S_(jcs[k_ + 1])
                        sbk = self.banks[SB[jc % 4]]
                        last_jc = jc
                    diag = 4 * ib <= jc <= 4 * ib + 3
                    Lt = Lb[li % 6]
                    pb = Pb[li % 6]
                    li += 1
                    sgn = 1.0 if d == 0 else -1.0
                    bia = bias_tok.t[:, jc, d * 8 + h:d * 8 + h + 1]
                    if diag:
                        self.ts(Lt.t[:, :], bcs[d][hh].t[:, :], sgn, bia, ALU.mult, ALU.add, [bcs[d][hh], bias_tok], [Lt])
                        self.ts(Lt.t[:, :], Lt.t[:, :], 0.0, None, ALU.min, None, [Lt], [Lt])
                        self.act(Lt.t[:, :], Lt.t[:, :], AF.Exp, [Lt], [Lt])
                    else:
                        self.act(Lt.t[:, :], bcs[d][hh].t[:, :], AF.Exp, [bcs[d][hh], bias_tok], [Lt], bias=bia, scale=sgn)
                    self.tt(pb.t[:, :], sbk.t[:, :], Lt.t[:, :], ALU.mult, [sbk, Lt], [pb])
                    if diag:
                        m = jc - 4 * ib
                        M = Mf if d == 0 else Mb
                        self.tt(pb.t[:, :], pb.t[:, :], M.t[:, 384 - 128 * m:384 - 128 * m + 512], ALU.mult, [pb, M], [pb], eng="pool")
                    self.mm(yb, yb.t[:, :], xdt[d].t[:, jc, h, :], pb.t[:, :], n == 0, n == len(items) - 1, [xdt[d], pb])
                self.stt(yv.t[:, :], xsT.t[:, pair, i0:i0 + 512], dpp.t[:, pair:pair + 1], yb.t[:, :], ALU.mult, ALU.add, [xsT, dpp, yb], [yv])
                self.act(gate.t[:, :], mz.t[:, pair, i0:i0 + 512], AF.Silu, [mz], [gate])
                self.tt(ybuf.t[:, pair, :], yv.t[:, :], gate.t[:, :], ALU.mult, [yv, gate], [ybuf])
            for c in range(4):
                self.act(sq.t[:, c, :], ybuf.t[:, c, :], AF.Square, [ybuf], [sq])
            nb = self.banks[6]
            for c in range(4):
                self.mm(nb, nb.t[:, :], self.ones_bf.t[:, :], sq.t[:, c, :], c == 0, c == 3, [self.ones_bf, sq])
            self.act(rstd.t[:, :], nb.t[:, :], AF.Sqrt, [nb], [rstd], bias=1e-6, scale=1.0 / 512)
            self.recip(rstd.t[:, :], rstd.t[:, :], [rstd], [rstd])
            for c in range(4):
                o = ob[c % 2]
                self.stt(o.t[:, :], ybuf.t[:, c, :], nw.t[:, c:c + 1], rstd.t[:, :], ALU.mult, ALU.mult, [ybuf, nw, rstd], [o])
                self.dma("pool", mixedT.t[1024 + c * 128:1024 + (c + 1) * 128, t0 + i0:t0 + i0 + 512], o.t[:, :], [o], [mixedT])


KB.group_ssd = _group_ssd


TWO_PI = 2.0 * math.pi
MAGIC = 12582912.0


def _sin_rr(self, out, x, tmp, reads_x, writes_out, x_tl, tmp_tl):
    self.ts(tmp, x, 1.0 / TWO_PI, MAGIC, ALU.mult, ALU.add, [x_tl], [tmp_tl])
    self.ts(tmp, tmp, MAGIC, -TWO_PI, ALU.subtract, ALU.mult, [tmp_tl], [tmp_tl])
    self.tt(x, x, tmp, ALU.add, [x_tl, tmp_tl], [x_tl])
    self.ts(x, x, 3.1415925, -3.1415925, ALU.min, ALU.max, [x_tl], [x_tl])
    self.act(out, x, AF.Sin, [x_tl], writes_out)


def _hyena_filter(self, l, prm, cst, Hs):
    with Phase(self) as ph:
        zT = self.sb(ph, [33, S], F32, "zT")
        w1 = self.sb(ph, [33, 64], F32, "w1")
        w2 = self.sb(ph, [64, 64], F32, "w2")
        w3 = self.sb(ph, [64, 2048], F32, "w3")
        sm = self.sb(ph, [64, 4], F32, "sm")
        self.dma("sp", zT.t[:], cst["hy_zT"], [], [zT])
        self.dma("sp", w1.t[:], prm["hy_w1"][l], [], [w1])
        self.dma("sp", w2.t[:], prm["hy_w2"][l], [], [w2])
        self.dma("sp", w3.t[:], prm["hy_w3"][l], [], [w3])
        self.dma("sp", sm.t[:, 0:2], prm["hy_b_pp"][:, l * 2:l * 2 + 2], [], [sm])
        self.dma("sp", sm.t[:, 2:4], prm["hy_freq_pp"][:, l * 2:l * 2 + 2], [], [sm])
        ntl = self.sb(ph, [128, 16], F32, "ntl")
        dlb = self.sb(ph, [128, 512], F32, "dlb")
        wf = self.sb(ph, [128, NF], F32, "wf")
        self.dma("sp", ntl.t[:], cst["hy_ntlin_pp"], [], [ntl])
        self.dma("sp", dlb.t[:], cst["hy_delta_b"], [], [dlb])
        self.dma("sp", wf.t[:], cst["dft_wf_pp"], [], [wf])
        hid1 = self.sb(ph, [64, S], F32, "hid1")
        hid2 = self.sb(ph, [64, S], F32, "hid2")
        xa = self.sb(ph, [64, 512], F32, "xa")
        xb = self.sb(ph, [64, 512], F32, "xb")
        for tb in range(4):
            bk = self.banks[tb % 2]
            self.mm(bk, bk.t[0:64, :], w1.t[0:33, :], zT.t[0:33, tb * 512:(tb + 1) * 512], True, True, [w1, zT])
            self.ts(xa.t[:, :], bk.t[0:64, :], sm.t[:, 0:1], sm.t[:, 2:3], ALU.add, ALU.mult, [bk, sm], [xa])
            _sin_rr(self, hid1.t[:, tb * 512:(tb + 1) * 512], xa.t[:, :], xb.t[:, :], None, [hid1], xa, xb)
        for tb in range(4):
            bk = self.banks[tb % 2]
            self.mm(bk, bk.t[0:64, :], w2.t[0:64, :], hid1.t[0:64, tb * 512:(tb + 1) * 512], True, True, [w2, hid1])
            self.ts(xa.t[:, :], bk.t[0:64, :], sm.t[:, 1:2], sm.t[:, 3:4], ALU.add, ALU.mult, [bk, sm], [xa])
            _sin_rr(self, hid2.t[:, tb * 512:(tb + 1) * 512], xa.t[:, :], xb.t[:, :], None, [hid2], xa, xb)
        hsum = [self.sb(ph, [128, 16, 512], BF16, "hsum") for _ in range(2)]
        hdif = [self.sb(ph, [128, 16, 512], BF16, "hdif") for _ in range(2)]
        dec = self.sb(ph, [128, 512], F32, "dec")
        hb = self.sb(ph, [128, 512], F32, "hb")
        t1 = self.sb(ph, [128, 512], F32, "t1")
        for tc in range(16):
            self.act(dec.t[:, :], dlb.t[:, :], AF.Exp, [dlb, ntl], [dec], scale=ntl.t[:, tc:tc + 1])
            for o in range(2):
                bf_, bb_ = self.banks[2 * o], self.banks[2 * o + 1]
                self.mm(bf_, bf_.t[:, :], hid2.t[0:64, tc * 128:(tc + 1) * 128], w3.t[0:64, (2 * o) * 512:(2 * o + 1) * 512], True, True, [hid2, w3])
                self.mm(bb_, bb_.t[:, :], hid2.t[0:64, tc * 128:(tc + 1) * 128], w3.t[0:64, (2 * o + 1) * 512:(2 * o + 2) * 512], True, True, [hid2, w3])
                self.copy(hb.t[:, :], bb_.t[:, :], [bb_], [hb], eng="act")
                if tc == 0:
                    self.memset(hb.t[0:1, :], 0.0, [hb])
                self.tt(t1.t[:, :], bf_.t[:, :], hb.t[:, :], ALU.add, [bf_, hb], [t1])
                self.tt(hsum[o].t[:, tc, :], t1.t[:, :], dec.t[:, :], ALU.mult, [t1, dec], [hsum[o]])
                self.tt(t1.t[:, :], bf_.t[:, :], hb.t[:, :], ALU.subtract, [bf_, hb], [t1])
                self.tt(hdif[o].t[:, tc, :], t1.t[:, :], dec.t[:, :], ALU.mult, [t1, dec], [hdif[o]])
        Cb = [self.sb(ph, [128, 16, 128], BF16, "Cb") for _ in range(2)]
        Sb = [self.sb(ph, [128, 16, 128], BF16, "Sb") for _ in range(2)]
        ho = [self.sb(ph, [128, 512], F32, "ho") for _ in range(2)]
        n = 0
        for fc in range(NF):
            cb_, sb_ = Cb[fc % 2], Sb[fc % 2]
            self.dma("sp", cb_.t[:], cst["dft_Cblk"][fc], [], [cb_])
            self.dma("sp", sb_.t[:], cst["dft_Sblk"][fc], [], [sb_])
            for o in range(2):
                for ri, (tab, src) in enumerate(((cb_, hsum[o]), (sb_, hdif[o]))):
                    bk = self.banks[4 + n % 4]
                    for tc in range(16):
                        self.mm(bk, bk.t[:, :], tab.t[:, tc, :], src.t[:, tc, :], tc == 0, tc == 15, [tab, src])
                    h_ = ho[n % 2]
                    n += 1
                    self.ts(h_.t[:, :], bk.t[:, :], wf.t[:, fc:fc + 1], None, ALU.mult, None, [bk, wf], [h_])
                    self.dma("pool", Hs.t[o, ri, fc * 128:(fc + 1) * 128, :], h_.t[:, :], [h_], [Hs])


def _group_hyena(self, l, nseq, seg, mixedT, prm, cst, Hs, hyu, z1s):
    NCOL = nseq * 512
    with Phase(self) as ph:
        U = self.sb(ph, [128, 16, NCOL], BF16, "U")
        Yre = self.sb(ph, [128, NF, NCOL], BF16, "Yre")
        Yim = self.sb(ph, [128, NF, NCOL], BF16, "Yim")
        cw = self.sb(ph, [128, 36], F32, "cw")
        cb = self.sb(ph, [128, 12], F32, "cb")
        hbias = self.sb(ph, [128, 8], F32, "hbias")
        nw = self.sb(ph, [128, 4], F32, "nw")
        self.dma("sp", cw.t[:], prm["hy_conv_w_pp"][:, l * 36:(l + 1) * 36], [], [cw])
        self.dma("sp", cb.t[:], prm["hy_conv_b_pp"][:, l * 12:(l + 1) * 12], [], [cb])
        self.dma("sp", hbias.t[:], prm["hy_bias_pp"][:, l * 8:(l + 1) * 8], [], [hbias])
        self.dma("sp", nw.t[:], prm["hy_out_norm_pp"][:, l * 4:(l + 1) * 4], [], [nw])
        with Phase(self) as p2:
            raw = [self.sb(p2, [128, S], BF16, "raw") for _ in range(2)]
            acc = [self.sb(p2, [128, S], F32, "acc") for _ in range(2)]
            n = 0
            for j in range(3):
                for s in range(nseq):
                    for cc in range(4):
                        c = j * 4 + cc
                        r_, a_ = raw[n % 2], acc[n % 2]
                        n += 1
                        self.dma("sp", r_.t[:, :], seg["hu"].t[c * 128:(c + 1) * 128, s * S:(s + 1) * S], [seg["hu"]], [r_])
                        self.ts(a_.t[:, :], r_.t[:, :], cw.t[:, c * 3 + 1:c * 3 + 2], cb.t[:, c:c + 1], ALU.mult, ALU.add, [r_, cw, cb], [a_])
                        self.stt(a_.t[:, 1:S], r_.t[:, 0:S - 1], cw.t[:, c * 3:c * 3 + 1], a_.t[:, 1:S], ALU.mult, ALU.add, [r_, cw, a_], [a_])
                        self.stt(a_.t[:, 0:S - 1], r_.t[:, 1:S], cw.t[:, c * 3 + 2:c * 3 + 3], a_.t[:, 0:S - 1], ALU.mult, ALU.add, [r_, cw, a_], [a_])
                        self.dma("pool", hyu.t[j, (s * 4 + cc) * 128:(s * 4 + cc + 1) * 128, :], a_.t[:, :], [a_], [hyu])
                        if j == 0:
                            for tcg in range(4):
                                bk = self.banks[6 + tcg % 2]
                                for k in range(4):
                                    tc = tcg * 4 + k
                                    self.tr(bk, bk.t[:, k * 128:(k + 1) * 128], a_.t[:, tc * 128:(tc + 1) * 128], [a_])
                                self.copy(U.t[:, tcg * 4:tcg * 4 + 4, (s * 4 + cc) * 128:(s * 4 + cc + 1) * 128], bk.t[:].rearrange("p (a b) -> p a b", a=4), [bk], [U])
        Cb = [self.sb(ph, [128, 16, 128], BF16, "Cb") for _ in range(2)]
        Sb = [self.sb(ph, [128, 16, 128], BF16, "Sb") for _ in range(2)]
        Hre = [self.sb(ph, [128, 512], F32, "Hre") for _ in range(2)]
        Him = [self.sb(ph, [128, 512], F32, "Him") for _ in range(2)]
        Cn = self.sb(ph, [128, NF, 512], BF16, "Cn")
        Sn = self.sb(ph, [128, NF, 512], BF16, "Sn")
        ta = self.sb(ph, [128, 512], F32, "ta")
        tb_ = self.sb(ph, [128, 512], F32, "tb")
        zp = [self.sb(ph, [128, 512], F32, "zp") for _ in range(2)]
        xg = [self.sb(ph, [128, 512], F32, "xg") for _ in range(2)]
        zn = [self.sb(ph, [128, 512], F32, "zn") for _ in range(2)]
        zfin = self.sb(ph, [128, nseq * 4, 512], F32, "zfin")
        sq = self.sb(ph, [128, 4, 512], BF16, "sq")
        rstd = self.sb(ph, [128, 512], F32, "rstd")
        ob = [self.sb(ph, [128, 512], BF16, "ob") for _ in range(2)]
        for o in range(2):
            for fc in range(NF):
                self.pump(2)
                cb_, sb_ = Cb[fc % 2], Sb[fc % 2]
                hr, hi = Hre[fc % 2], Him[fc % 2]
                self.dma("sp", cb_.t[:], cst["dft_Cblk"][fc], [], [cb_])
                self.dma("sp", sb_.t[:], cst["dft_Sblk"][fc], [], [sb_])
                self.dma("sp", hr.t[:, :], Hs.t[o, 0, fc * 128:(fc + 1) * 128, :], [Hs], [hr])
                self.dma("sp", hi.t[:, :], Hs.t[o, 1, fc * 128:(fc + 1) * 128, :], [Hs], [hi])
                for s in range(nseq):
                    br, bi = self.banks[2 * (s % 2)], self.banks[2 * (s % 2) + 1]
                    for tc in range(16):
                        self.mm(br, br.t[:, :], cb_.t[:, tc, :], U.t[:, tc, s * 512:(s + 1) * 512], tc == 0, tc == 15, [cb_, U])
                    for tc in range(16):
                        self.mm(bi, bi.t[:, :], sb_.t[:, tc, :], U.t[:, tc, s * 512:(s + 1) * 512], tc == 0, tc == 15, [sb_, U])
                    self.tt(ta.t[:, :], br.t[:, :], hr.t[:, :], ALU.mult, [br, hr], [ta])
                    self.tt(tb_.t[:, :], bi.t[:, :], hi.t[:, :], ALU.mult, [bi, hi], [tb_])
                    self.tt(Yre.t[:, fc, s * 512:(s + 1) * 512], ta.t[:, :], tb_.t[:, :], ALU.subtract, [ta, tb_], [Yre])
                    self.tt(ta.t[:, :], br.t[:, :], hi.t[:, :], ALU.mult, [br, hi], [ta])
                    self.tt(tb_.t[:, :], bi.t[:, :], hr.t[:, :], ALU.mult, [bi, hr], [tb_])
                    self.tt(Yim.t[:, fc, s * 512:(s + 1) * 512], ta.t[:, :], tb_.t[:, :], ALU.add, [ta, tb_], [Yim])
            for tb in range(4):
                c0 = tb * 512
                self.dma("sp", Cn.t[:], cst["dft_Cnat"][:, c0:c0 + 512].rearrange("(f p) t -> p f t", p=128), [], [Cn])
                self.dma("sp", Sn.t[:], cst["dft_Snat"][:, c0:c0 + 512].rearrange("(f p) t -> p f t", p=128), [], [Sn])
                for sc in range(nseq * 4):
                    s, cc = sc // 4, sc % 4
                    bk = self.banks[4 + sc % 2]
                    for fc in range(NF):
                        self.mm(bk, bk.t[:, :], Yre.t[:, fc, sc * 128:(sc + 1) * 128], Cn.t[:, fc, :], fc == 0, False, [Yre, Cn])
                        self.mm(bk, bk.t[:, :], Yim.t[:, fc, sc * 128:(sc + 1) * 128], Sn.t[:, fc, :], False, fc == NF - 1, [Yim, Sn])
                    z_, x_, n_ = zp[sc % 2], xg[sc % 2], zn[sc % 2]
                    zsrc = hyu.t[0] if o == 0 else z1s.t
                    zsrc_tl = hyu if o == 0 else z1s
                    self.dma("sp", z_.t[:, :], zsrc[sc * 128:(sc + 1) * 128, c0:c0 + 512], [zsrc_tl], [z_])
                    self.dma("sp", x_.t[:, :], hyu.t[o + 1, sc * 128:(sc + 1) * 128, c0:c0 + 512], [hyu], [x_])
                    self.stt(n_.t[:, :], z_.t[:, :], hbias.t[:, o * 4 + cc:o * 4 + cc + 1], bk.t[:, :], ALU.mult, ALU.add, [z_, hbias, bk], [n_])
                    if o == 0:
                        self.tt(n_.t[:, :], n_.t[:, :], x_.t[:, :], ALU.mult, [n_, x_], [n_])
                        self.dma("pool", z1s.t[sc * 128:(sc + 1) * 128, c0:c0 + 512], n_.t[:, :], [n_], [z1s])
                        b6 = self.banks[6 + sc % 2]
                        for k in range(4):
                            self.tr(b6, b6.t[:, k * 128:(k + 1) * 128], n_.t[:, k * 128:(k + 1) * 128], [n_])
                        self.copy(U.t[:, tb * 4:tb * 4 + 4, sc * 128:(sc + 1) * 128], b6.t[:].rearrange("p (a b) -> p a b", a=4), [b6], [U])
                    else:
                        self.tt(zfin.t[:, sc, :], n_.t[:, :], x_.t[:, :], ALU.mult, [n_, x_], [zfin])
                if o == 1:
                    for s in range(nseq):
                        for cc in range(4):
                            self.act(sq.t[:, cc, :], zfin.t[:, s * 4 + cc, :], AF.Square, [zfin], [sq])
                        nb = self.banks[6]
                        for cc in range(4):
                            self.mm(nb, nb.t[:, :], self.ones_bf.t[:, :], sq.t[:, cc, :], cc == 0, cc == 3, [self.ones_bf, sq])
                        self.act(rstd.t[:, :], nb.t[:, :], AF.Sqrt, [nb], [rstd], bias=1e-6, scale=1.0 / 512)
                        self.recip(rstd.t[:, :], rstd.t[:, :], [rstd], [rstd])
                        for cc in range(4):
                            o_ = ob[cc % 2]
                            self.stt(o_.t[:, :], zfin.t[:, s * 4 + cc, :], nw.t[:, cc:cc + 1], rstd.t[:, :], ALU.mult, ALU.mult, [zfin, nw, rstd], [o_])
                            self.dma("pool", mixedT.t[1536 + cc * 128:1536 + (cc + 1) * 128, s * S + c0:s * S + c0 + 512], o_.t[:, :], [o_], [mixedT])


KB.hyena_filter = _hyena_filter
KB.group_hyena = _group_hyena


def _ln_rows(self, y, gB, bB, st6, mv, eps):
    nc = self.nc
    for q in range(4):
        self.P.op("dve", lambda q=q: nc.vector.bn_stats(out=st6.t[:, q, :], in_=y.t[:, q * 512:(q + 1) * 512]), self._b([y]), self._b([st6]))
    self.P.op("dve", lambda: nc.vector.bn_aggr(out=mv.t[:, 0:2], in_=st6.t[:].rearrange("p a b -> p (a b)")), self._b([st6]), self._b([mv]))
    self.act(mv.t[:, 2:3], mv.t[:, 1:2], AF.Sqrt, [mv], [mv], bias=eps, scale=1.0)
    self.recip(mv.t[:, 3:4], mv.t[:, 2:3], [mv], [mv])
    self.ts(y.t[:, :], y.t[:, :], mv.t[:, 0:1], mv.t[:, 3:4], ALU.subtract, ALU.mult, [y, mv], [y])
    self.tt(y.t[:, :], y.t[:, :], gB.t[:, :], ALU.mult, [y, gB], [y])
    self.tt(y.t[:, :], y.t[:, :], bB.t[:, :], ALU.add, [y, bB], [y])


def _outproj_ln(self, l, mixedT, woutb, xin, xout, prm, tok_range, wdep=None):
    with Phase(self) as ph:
        W = self.sb(ph, [128, 16, D], BF16, "Wout")
        self.dma("sp", W.t[:], woutb.t[l].rearrange("(ec p) d -> p ec d", p=128), [wdep or woutb], [W])
        gB = self.bcast_row(ph, prm["ln1_g"][l:l + 1, :], D, "gB")
        bB = self.bcast_row(ph, prm["ln1_b"][l:l + 1, :], D, "bB")
        mT = [self.sb(ph, [128, 16, 128], BF16, "mT") for _ in range(2)]
        xt = [self.sb(ph, [128, D], F32, "xt") for _ in range(2)]
        y = [self.sb(ph, [128, D], F32, "y") for _ in range(2)]
        st6 = self.sb(ph, [128, 4, 6], F32, "st6")
        mv = self.sb(ph, [128, 4], F32, "mv")
        for i, tok0 in enumerate(range(tok_range[0], tok_range[1], 128)):
            self.pump(2)
            m_, x_, y_ = mT[i % 2], xt[i % 2], y[i % 2]
            self.dma("sp", m_.t[:], mixedT.t[:, tok0:tok0 + 128].rearrange("(ec p) t -> p ec t", p=128), [mixedT], [m_])
            self.dma("sp", x_.t[:], xin.t[tok0:tok0 + 128, :], [xin], [x_])
            for q in range(4):
                bk = self.banks[(i * 4 + q) % 8]
                for ec in range(16):
                    self.mm(bk, bk.t[:, :], m_.t[:, ec, :], W.t[:, ec, q * 512:(q + 1) * 512], ec == 0, ec == 15, [m_, W])
                self.stt(y_.t[:, q * 512:(q + 1) * 512], x_.t[:, q * 512:(q + 1) * 512], ALPHA, bk.t[:, :], ALU.mult, ALU.add, [x_, bk], [y_])
            _ln_rows(self, y_, gB, bB, st6, mv, 1e-5)
            self.dma("pool", xout.t[tok0:tok0 + 128, :], y_.t[:, :], [y_], [xout])


def _wsl(W, r0, r1, c0, c1, pat):
    if isinstance(W, tuple) and W[0] == "dynflat":
        _, tens, v, kind = W
        if kind == "gu":
            return tens[c0 // 256][bass.ds(v, MSEG)].rearrange("(dc p f) -> p dc f", p=128, f=256)
        return tens[(c0 // 512) * 7 + r0 // 1024][bass.ds(v, MSEG)].rearrange("(a p f) -> p a f", p=128, f=512)
    if isinstance(W, tuple):
        _, t3, reg = W
        return t3[bass.ds(reg, 1), r0:r1, c0:c1].rearrange("o " + pat, p=128, o=1).rearrange("p o a b -> p (o a) b") if False else \
            t3[bass.ds(reg, 1), r0:r1, c0:c1].rearrange("1 " + pat, p=128)
    return W[r0:r1, c0:c1].rearrange(pat, p=128)


def _ffn(self, l, xin, xout, experts, F_, prm, tok_range, wr_ap=None, slot_mode=False, wdep=None):
    nc = self.nc
    wdep = wdep or self.wsrc
    nfc = F_ // 128
    nftiles = 7 if slot_mode else (8 if nfc % 8 == 0 else 4)
    nft = nfc // nftiles
    import os
    if os.environ.get("MOE_DBG", "") == "dense":
        wr_ap = None
    gated = wr_ap is not None
    with Phase(self) as ph:
        xT = self.sb(ph, [128, 16, 512], BF16, "xT")
        xtiles = [self.sb(ph, [128, D], F32, "xtile") for _ in range(2)]
        hT = self.sb(ph, [128, nfc, 512], BF16, "hT")
        acc = self.sb(ph, [128, 4, D], F32, "acc")
        wg = [self.sb(ph, [128, 16, 256], BF16, "wg") for _ in range(2)]
        wu = [self.sb(ph, [128, 16, 256], BF16, "wu") for _ in range(2)]
        wd = [self.sb(ph, [128, nft, 512], BF16, "wd") for _ in range(2)]
        sg = [self.sb(ph, [128, 512], F32, "sg") for _ in range(2)]
        if not slot_mode:
            gB = self.bcast_row(ph, prm["ln2_g"][l:l + 1, :], D, "gB2")
            bB = self.bcast_row(ph, prm["ln2_b"][l:l + 1, :], D, "bB2")
        st6 = self.sb(ph, [128, 4, 6], F32, "st6")
        mv = self.sb(ph, [128, 4], F32, "mv")
        G = self.sb(ph, [128, 4, 8], F32, "G")
        if gated:
            x32 = self.sb(ph, [128, 16, 128], F32, "x32")
            x32_keep = x32
            wr = self.sb(ph, [128, 16, 8], F32, "wr")
            if not os.environ.get("NOWR"):
                self.dma("sp", wr.t[:], wr_ap.rearrange("(dc p) e -> p dc e", p=128), [], [wr])
            lg = self.sb(ph, [128, 8], F32, "lg")
            srt = self.sb(ph, [128, 8], F32, "srt")
            gg = self.sb(ph, [128, 4], F32, "gg")
            g2t = self.sb(ph, [128, 8], F32, "g2t")
        else:
            x32 = None

        import os
        dbgm = os.environ.get("MOE_DBG", "")

        def router(ts_):
            if dbgm == "norouter":
                self.memset(G.t[:, ts_, :], 0.125, [G])
                return
            bk = self.banks[0]
            for dc in range(16):
                self.mm(bk, bk.t[:, 0:8], x32.t[:, dc, :], wr.t[:, dc, :], dc == 0, dc == 15, [x32, wr])
            self.copy(lg.t[:, :], bk.t[:, 0:8], [bk], [lg], eng="dve")
            self.P.op("dve", lambda: nc.vector.max(out=srt.t[:, :], in_=lg.t[:, :]), self._b([lg]), self._b([srt]))
            self.tt(gg.t[:, 0:1], srt.t[:, 1:2], srt.t[:, 0:1], ALU.subtract, [srt], [gg])
            self.act(gg.t[:, 1:2], gg.t[:, 0:1], AF.Sigmoid, [gg], [gg])
            self.ts(gg.t[:, 2:3], gg.t[:, 1:2], -1.0, 1.0, ALU.mult, ALU.add, [gg], [gg])
            self.ts(G.t[:, ts_, :], lg.t[:, :], srt.t[:, 0:1], gg.t[:, 2:3], ALU.is_equal, ALU.mult, [lg, srt, gg], [G])
            self.ts(g2t.t[:, :], lg.t[:, :], srt.t[:, 1:2], gg.t[:, 1:2], ALU.is_equal, ALU.mult, [lg, srt, gg], [g2t])
            self.tt(G.t[:, ts_, :], G.t[:, ts_, :], g2t.t[:, :], ALU.add, [G, g2t], [G])

        n1 = 0
        n2 = 0
        for tok0 in range(tok_range[0], tok_range[1], 512):
            self.load_xT(xin, tok0, xT, xtiles, x32=None if os.environ.get("NOX32") else x32, post=router if gated else None)
            exl = experts(tok0) if callable(experts) else experts
            for e, (Wg, Wu, Wd) in enumerate(exl):
                for fg in range(0 if os.environ.get("FFN_SKIP1") else nfc // 2):
                    self.pump(2)
                    g_, u_ = wg[n1 % 2], wu[n1 % 2]
                    n1 += 1
                    self.dma("sp", g_.t[:], _wsl(Wg, 0, D, fg * 256, (fg + 1) * 256, "(dc p) f -> p dc f"), [wdep], [g_])
                    self.dma("sp", u_.t[:], _wsl(Wu, 0, D, fg * 256, (fg + 1) * 256, "(dc p) f -> p dc f"), [wdep], [u_])
                    for j in range(2):
                        fc = fg * 2 + j
                        bg, bu = self.banks[fc % 2], self.banks[2 + fc % 2]
                        for dc in range(16):
                            self.mm(bg, bg.t[:, :], g_.t[:, dc, j * 128:(j + 1) * 128], xT.t[:, dc, :], dc == 0, dc == 15, [g_, xT])
                        for dc in range(16):
                            self.mm(bu, bu.t[:, :], u_.t[:, dc, j * 128:(j + 1) * 128], xT.t[:, dc, :], dc == 0, dc == 15, [u_, xT])
                        s_ = sg[fc % 2]
                        self.act(s_.t[:, :], bg.t[:, :], AF.Silu, [bg], [s_])
                        self.tt(hT.t[:, fc, :], s_.t[:, :], bu.t[:, :], ALU.mult, [s_, bu], [hT])
                for q in range(4):
                    if os.environ.get("FFN_SKIP2"):
                        for ts_ in range(4):
                            self.memset(acc.t[:, ts_, q * 512:(q + 1) * 512], 0.0, [acc])
                        continue
                    for ft in range(nftiles):
                        d_ = wd[n2 % 2]
                        n2 += 1
                        self.dma("sp", d_.t[:], _wsl(Wd, ft * nft * 128, (ft + 1) * nft * 128, q * 512, (q + 1) * 512, "(a p) d -> p a d"), [wdep], [d_])
                        for a in range(nft):
                            fc = ft * nft + a
                            for ts_ in range(4):
                                bk = self.banks[4 + ts_]
                                self.mm(bk, bk.t[:, :], hT.t[:, fc, ts_ * 128:(ts_ + 1) * 128], d_.t[:, a, :], fc == 0, fc == nfc - 1, [hT, d_])
                    for ts_ in range(4):
                        bk = self.banks[4 + ts_]
                        dst = acc.t[:, ts_, q * 512:(q + 1) * 512]
                        if not gated:
                            self.copy(dst, bk.t[:, :], [bk], [acc])
                        elif e == 0:
                            self.ts(dst, bk.t[:, :], G.t[:, ts_, e:e + 1], None, ALU.mult, None, [bk, G], [acc])
                        else:
                            self.stt(dst, bk.t[:, :], G.t[:, ts_, e:e + 1], dst, ALU.mult, ALU.add, [bk, G, acc], [acc])
            if slot_mode:
                for ts_ in range(4):
                    self.dma("pool", xout.t[tok0 + ts_ * 128:tok0 + (ts_ + 1) * 128, :], acc.t[:, ts_, :], [acc], [xout])
                continue
            for ts_ in range(4):
                x_ = xtiles[ts_ % 2]
                self.dma("sp", x_.t[:], xin.t[tok0 + ts_ * 128:tok0 + (ts_ + 1) * 128, :], [xin], [x_])
                self.stt(x_.t[:, :], x_.t[:, :], ALPHA, acc.t[:, ts_, :], ALU.mult, ALU.add, [x_, acc], [x_])
                _ln_rows(self, x_, gB, bB, st6, mv, 1e-5)
                self.dma("pool", xout.t[tok0 + ts_ * 128:tok0 + (ts_ + 1) * 128, :], x_.t[:, :], [x_], [xout])


KB.outproj_ln = _outproj_ln
KB.ffn = _ffn


def build_full(shapes, nseq=NSEQ, layers=(0, 1), ne=NE, skip_mixer=False):
    nc = bass.Bass("TRN2", target_bir_lowering=False)
    ext = {}
    for name, (shape, dt) in shapes.items():
        ext[name] = nc.dram_tensor(name, list(shape), dt, kind="ExternalInput").ap()
    out = Tl(nc.dram_tensor("out", [T, D], F32, kind="ExternalOutput").ap(), "out")
    with ExitStack() as st:
        kb = KB(nc, st)
        kb.setup_consts()
        kb.wsrc = Tl(None, "wsrc")
        winb = kb.dram("winb", [L, D, INC], BF16)
        wrot = kb.dram("wrot", [L, D, 576], BF16)
        woutb = kb.dram("woutb", [L, D, D], BF16)
        fg = kb.dram("fgb", [D, DFF], BF16)
        fu = kb.dram("fub", [D, DFF], BF16)
        fd = kb.dram("fdb", [DFF, D], BF16)
        seg = {"qc": kb.dram("s_qc", [384, T], BF16), "kvc": kb.dram("s_kvc", [256, T], BF16),
               "kpe": kb.dram("s_kpe", [64, T], BF16), "rq": kb.dram("s_rq", [256, T], BF16),
               "rk": kb.dram("s_rk", [256, T], BF16), "rg": kb.dram("s_rg", [512, T], BF16),
               "mz": kb.dram("s_mz", [512, T], BF16), "xbc": kb.dram("s_xbc", [1024, T], BF16),
               "dt": kb.dram("s_dt", [16, T], F32), "hu": kb.dram("s_hu", [1536, T], BF16),
               "rv": kb.dram("s_rv", [T, 512], BF16)}
        mixedT = kb.dram("mixedT", [D, T], BF16)
        Hs = kb.dram("Hs", [2, 2, NFP, 512], F32)
        hyu = kb.dram("hyu", [3, NSEQ * 512, S], F32)
        z1s = kb.dram("z1s", [NSEQ * 512, S], F32)
        xa = kb.dram("xa", [T, D], F32)
        xb = kb.dram("xb", [T, D], F32)
        xin = Tl(ext["x"], "x")
        rng = (0, nseq * S)
        import os
        if os.environ.get("RNG"):
            rng = (0, int(os.environ["RNG"]))
        kb.wmoe = Tl(None, "wmoe")
        woutL = [Tl(woutb.t[l], "wout%d" % l) for l in range(L)]
        winL = [Tl(winb.t[l], "win%d" % l) for l in range(L)]
        for l in layers:
            kb.cast_dram(winL[l], ext["w_in"][l], D, tag=None if l == layers[0] else "win%d" % l)
            kb.cast_dram(woutL[l], ext["w_out"][l], D, tag="wout%d" % l)
            if l == 0:
                for dst, nm, rows in ((fg, "ffn_w_gate", D), (fu, "ffn_w_up", D), (fd, "ffn_w_down", DFF)):
                    t_ = _sub(dst, dst.t)
                    t_.b = kb.wsrc.b
                    kb.cast_dram(t_, ext[nm][0], rows, tag="ffn")
        if 1 in layers:
            mg = [nc.dram_tensor("mgb%d" % i, [NE * MSEG], BF16).ap() for i in range(28)]
            mu = [nc.dram_tensor("mub%d" % i, [NE * MSEG], BF16).ap() for i in range(28)]
            md = [nc.dram_tensor("mdb%d" % i, [NE * MSEG], BF16).ap() for i in range(28)]
            for e in range(ne):
                for fg_ in range(28):
                    for tens, nm in ((mg, "moe_w_gate"), (mu, "moe_w_up")):
                        kb.defer("moe", lambda tens=tens, nm=nm, e=e, fg_=fg_: kb.dma(
                            "pool", tens[fg_][e * MSEG:(e + 1) * MSEG].rearrange("(r c) -> r c", c=256),
                            ext[nm][0, e][:, fg_ * 256:(fg_ + 1) * 256], [], [kb.wmoe]))
                for q in range(4):
                    for ft in range(7):
                        kb.defer("moe", lambda e=e, q=q, ft=ft: kb.dma(
                            "pool", md[q * 7 + ft][e * MSEG:(e + 1) * MSEG].rearrange("(r c) -> r c", c=512),
                            ext["moe_w_down"][0, e][ft * 1024:(ft + 1) * 1024, q * 512:(q + 1) * 512], [], [kb.wmoe]))
        cur = xin
        import os
        stages = os.environ.get("STAGES", "mix,op,ffn").split(",")
        for l in layers:
            if "mix" in stages:
                kb.flush_tag("win%d" % l)
                kb.build_rot(l, ext["w_in"], wrot)
                kb.inproj(l, cur, winb, wrot, seg, ext, rng, wdep=winL[l])
                for s in range(nseq):
                    kb.group_mla(l, s, seg, mixedT, ext, ext)
                    kb.group_ret(s, seg, mixedT)
                    kb.group_ssd(l, s, seg, mixedT, ext)
                kb.hyena_filter(l, ext, ext, Hs)
                kb.group_hyena(l, nseq, seg, mixedT, ext, ext, Hs, hyu, z1s)
            if "op" in stages:
                kb.flush_tag("wout%d" % l)
                kb.outproj_ln(l, mixedT, woutb, cur, xa, ext, rng, wdep=woutL[l])
            nxt = out if l == layers[-1] else xb
            if "ffn" not in stages:
                continue
            if l == 0:
                kb.flush_tag("ffn")
                kb.ffn(l, xa, nxt, [(fg.t, fu.t, fd.t)], DFF, ext, rng)
            else:
                if True:
                    kb.flush_tag("moe")
                    kb.moe_routed(l, xa, nxt, mg, mu, md, ext, rng[1], ext["moe_router"][0])
            cur = nxt
        kb.P.finish()
        print("instructions:", kb.P.n_inst, "sems:", kb.P.nsem)
    return nc


BIG = ("w_in", "w_out", "ffn_w_gate", "ffn_w_up", "ffn_w_down", "moe_router", "moe_w_gate", "moe_w_up", "moe_w_down",
       "ln1_g", "ln1_b", "ln2_g", "ln2_b")


def kernel(**inputs):
    common = {}
    for k in BIG:
        common[k] = np.ascontiguousarray(np.asarray(inputs[k], dtype=np.float32))
    common.update(host_consts())
    common.update(host_params(inputs))
    x = np.asarray(inputs["x"], dtype=np.float32)
    ncores = 8
    shapes = {k: (v.shape, BF16 if v.dtype == ml_dtypes.bfloat16 else F32) for k, v in common.items()}
    shapes["x"] = ((T, D), F32)
    nc = build_full(shapes)
    in_maps = []
    for c in range(ncores):
        m = dict(common)
        m["x"] = np.ascontiguousarray(x[c * NSEQ:(c + 1) * NSEQ].reshape(T, D))
        in_maps.append(m)
    res = run_bass_kernel_spmd(nc, in_maps, core_ids=list(range(ncores)))
    outs = [np.asarray(r["out"], dtype=np.float32).reshape(NSEQ, S, D) for r in res.results]
    return np.concatenate(outs, axis=0)


MSEG = 2048 * 256
NBLK = 24
I32 = mybir.dt.int32


def _moe_routed(self, l, xin, xout, mg, mu, md, prm, ntok, wr_ap):
    nc = self.nc
    NT = ntok // 128
    xslots = self.dram("xslots", [NBLK * 512, D], F32)
    yslots = self.dram("yslots", [NBLK * 512, D], F32)
    with Phase(self) as pr:
        M1 = self.sb(pr, [128, NT, 8], F32, "M1")
        M2 = self.sb(pr, [128, NT, 8], F32, "M2")
        LOC = self.sb(pr, [128, NT, 8], F32, "LOC")
        TMP = self.sb(pr, [128, NT, 8], F32, "TMPr")
        G12 = self.sb(pr, [128, NT, 2], F32, "G12")
        d1f = self.sb(pr, [128, NT], F32, "d1f")
        d2f = self.sb(pr, [128, NT], F32, "d2f")
        d1i = self.sb(pr, [128, NT], I32, "d1i")
        d2i = self.sb(pr, [128, NT], I32, "d2i")
        bei = self.sb(pr, [128, NBLK], I32, "bei")
        with Phase(self) as ph:
            xt = [self.sb(ph, [128, D], F32, "xt") for _ in range(2)]
            x32 = self.sb(ph, [128, 16, 128], F32, "x32")
            wr = self.sb(ph, [128, 16, 8], F32, "wr")
            self.dma("sp", wr.t[:], wr_ap.rearrange("(dc p) e -> p dc e", p=128), [], [wr])
            ltri = self.sb(ph, [128, 128], F32, "ltri")
            self.memset(ltri.t[:], 1.0, [ltri])
            self.P.op("pool", lambda: nc.gpsimd.affine_select(out=ltri.t[:], in_=ltri.t[:], pattern=[[1, 128]], compare_op=ALU.is_ge, fill=0.0, base=-1, channel_multiplier=-1), self._b([ltri]), self._b([ltri]))
            base = self.sb(ph, [128, 8], F32, "base")
            self.memset(base.t[:], 0.0, [base])
            lg = self.sb(ph, [128, 8], F32, "lg")
            srt = self.sb(ph, [128, 8], F32, "srt")
            gg = self.sb(ph, [128, 4], F32, "gg")
            ms = self.sb(ph, [128, 8], F32, "ms")
            for i in range(NT):
                x_ = xt[i % 2]
                self.dma("sp", x_.t[:], xin.t[i * 128:(i + 1) * 128, :], [xin], [x_])
                for j in range(4):
                    bk = self.banks[4 + j]
                    for k in range(4):
                        dc = 4 * j + k
                        self.tr(bk, bk.t[:, k * 128:(k + 1) * 128], x_.t[:, dc * 128:(dc + 1) * 128], [x_])
                    self.copy(x32.t[:, 4 * j:4 * j + 4, :], bk.t[:].rearrange("p (a b) -> p a b", a=4), [bk], [x32])
                bk = self.banks[0]
                for dc in range(16):
                    self.mm(bk, bk.t[:, 0:8], x32.t[:, dc, :], wr.t[:, dc, :], dc == 0, dc == 15, [x32, wr])
                self.copy(lg.t[:, :], bk.t[:, 0:8], [bk], [lg], eng="dve")
                self.P.op("dve", lambda: nc.vector.max(out=srt.t[:, :], in_=lg.t[:, :]), self._b([lg]), self._b([srt]))
                self.tt(gg.t[:, 0:1], srt.t[:, 1:2], srt.t[:, 0:1], ALU.subtract, [srt], [gg])
                self.act(G12.t[:, i, 1:2], gg.t[:, 0:1], AF.Sigmoid, [gg], [G12])
                self.ts(G12.t[:, i, 0:1], G12.t[:, i, 1:2], -1.0, 1.0, ALU.mult, ALU.add, [G12], [G12])
                self.ts(M1.t[:, i, :], lg.t[:, :], srt.t[:, 0:1], None, ALU.is_equal, None, [lg, srt], [M1])
                self.ts(M2.t[:, i, :], lg.t[:, :], srt.t[:, 1:2], None, ALU.is_equal, None, [lg, srt], [M2])
                self.tt(ms.t[:, :], M1.t[:, i, :], M2.t[:, i, :], ALU.add, [M1, M2], [ms])
                b1, b2 = self.banks[1], self.banks[2]
                self.mm(b1, b1.t[:, 0:8], ltri.t[:, :], ms.t[:, :], True, True, [ltri, ms])
                self.mm(b2, b2.t[:, 0:8], self.ones_f.t[:, :], ms.t[:, :], True, True, [self.ones_f, ms])
                self.tt(LOC.t[:, i, :], b1.t[:, 0:8], base.t[:, :], ALU.add, [b1, base], [LOC])
                self.tt(base.t[:, :], b2.t[:, 0:8], base.t[:, :], ALU.add, [b2, base], [base])
            pad = self.sb(ph, [128, 8], F32, "pad")
            pend = self.sb(ph, [128, 8], F32, "pend")
            pst = self.sb(ph, [128, 8], F32, "pst")
            one8 = self.sb(ph, [128, 8], F32, "one8")
            self.memset(one8.t[:], 1.0, [one8])
            self.ts(pad.t[:, :], base.t[:, :], 1.0 / 512, 0.4990234375, ALU.mult, ALU.add, [base], [pad])
            self.ts(pad.t[:, :], pad.t[:, :], MAGIC, None, ALU.add, None, [pad], [pad])
            self.ts(pad.t[:, :], pad.t[:, :], MAGIC, 512.0, ALU.subtract, ALU.mult, [pad], [pad])
            self.P.op("dve", lambda: nc.vector.tensor_tensor_scan(out=pend.t[:, :], data0=one8.t[:, :], data1=pad.t[:, :], initial=0.0, op0=ALU.mult, op1=ALU.add), self._b([one8, pad]), self._b([pend]))
            self.tt(pst.t[:, :], pend.t[:, :], pad.t[:, :], ALU.subtract, [pend, pad], [pst])
            for i in range(NT):
                self.tt(LOC.t[:, i, :], LOC.t[:, i, :], pst.t[:, :], ALU.add, [LOC, pst], [LOC])
            for Mx, df, di in ((M1, d1f, d1i), (M2, d2f, d2i)):
                self.tt(TMP.t[:], Mx.t[:], LOC.t[:], ALU.mult, [Mx, LOC], [TMP])
                self.P.op("dve", lambda df=df: nc.vector.tensor_reduce(out=df.t[:, :], in_=TMP.t[:], axis=mybir.AxisListType.X, op=ALU.add), self._b([TMP]), self._b([df]))
                self.copy(di.t[:, :], df.t[:, :], [df], [di], eng="dve")
            thr = self.sb(ph, [128, NBLK], F32, "thr")
            bef = self.sb(ph, [128, NBLK], F32, "bef")
            cmpt = self.sb(ph, [128, NBLK], F32, "cmpt")
            self.P.op("pool", lambda: nc.gpsimd.iota(thr.t[:], pattern=[[512, NBLK]], base=0, channel_multiplier=0, allow_small_or_imprecise_dtypes=True), [], [thr.b])
            self.memset(bef.t[:], 0.0, [bef])
            for e in range(8):
                self.ts(cmpt.t[:, :], thr.t[:, :], pend.t[:, e:e + 1], None, ALU.is_ge, None, [thr, pend], [cmpt])
                self.tt(bef.t[:, :], bef.t[:, :], cmpt.t[:, :], ALU.add, [bef, cmpt], [bef])
            self.ts(bef.t[:, :], bef.t[:, :], 7.0, float(MSEG), ALU.min, ALU.mult, [bef], [bef])
            self.copy(bei.t[:, :], bef.t[:, :], [bef], [bei], eng="dve")
            for i in range(NT):
                x_ = xt[i % 2]
                self.dma("sp", x_.t[:], xin.t[i * 128:(i + 1) * 128, :], [xin], [x_])
                for di in (d1i, d2i):
                    self.P.dma_raw("pool", lambda di=di, x_=x_, i=i: nc.gpsimd.indirect_dma_start(
                        out=xslots.t[:, :], out_offset=bass.IndirectOffsetOnAxis(ap=di.t[:, i:i + 1], axis=0), in_=x_.t[:, :], in_offset=None),
                        self._b([x_, di]), self._b([xslots]))
        self.P._deps("sp", self._b([bei]), [])
        cur_reg = [None]

        def experts(tok0):
            b = tok0 // 512
            if cur_reg[0] is not None:
                nc.sync.free_register(cur_reg[0])
            reg = nc.sync.alloc_register()
            nc.sync.reg_load(reg, bei.t[0:1, b:b + 1])
            r = nc.sync.snap(reg, donate=True, min_val=0, max_val=7 * MSEG)
            cur_reg[0] = reg
            return [(("dynflat", mg, r, "gu"), ("dynflat", mu, r, "gu"), ("dynflat", md, r, "d"))]

        self.ffn(l, xslots, yslots, experts, DFE, prm, (0, NBLK * 512), slot_mode=True, wdep=self.wmoe)
        if cur_reg[0] is not None:
            nc.sync.free_register(cur_reg[0])
        with Phase(self) as ph:
            gB = self.bcast_row(ph, prm["ln2_g"][l:l + 1, :], D, "gB2")
            bB = self.bcast_row(ph, prm["ln2_b"][l:l + 1, :], D, "bB2")
            st6 = self.sb(ph, [128, 4, 6], F32, "st6")
            mv = self.sb(ph, [128, 4], F32, "mv")
            xt = [self.sb(ph, [128, D], F32, "xt") for _ in range(2)]
            ya = [self.sb(ph, [128, D], F32, "ya") for _ in range(2)]
            yb = [self.sb(ph, [128, D], F32, "yb") for _ in range(2)]
            for i in range(NT):
                x_, a_, b_ = xt[i % 2], ya[i % 2], yb[i % 2]
                self.dma("sp", x_.t[:], xin.t[i * 128:(i + 1) * 128, :], [xin], [x_])
                for di, y_ in ((d1i, a_), (d2i, b_)):
                    self.P.dma_raw("pool", lambda di=di, y_=y_, i=i: nc.gpsimd.indirect_dma_start(
                        out=y_.t[:, :], out_offset=None, in_=yslots.t[:, :], in_offset=bass.IndirectOffsetOnAxis(ap=di.t[:, i:i + 1], axis=0)),
                        self._b([yslots, di]), self._b([y_]))
                self.ts(a_.t[:, :], a_.t[:, :], G12.t[:, i, 0:1], None, ALU.mult, None, [a_, G12], [a_])
                self.stt(a_.t[:, :], b_.t[:, :], G12.t[:, i, 1:2], a_.t[:, :], ALU.mult, ALU.add, [b_, G12, a_], [a_])
                self.stt(x_.t[:, :], x_.t[:, :], ALPHA, a_.t[:, :], ALU.mult, ALU.add, [x_, a_], [x_])
                _ln_rows(self, x_, gB, bB, st6, mv, 1e-5)
                self.dma("pool", xout.t[i * 128:(i + 1) * 128, :], x_.t[:, :], [x_], [xout])


KB.moe_routed = _moe_routed
```
